# Optimizing a Trainium2 kernel written in Bass

```python
import math
import jax, jax.numpy as jnp
from jax import lax
import numpy as np

D_MODEL = 2048
BATCH = 4
SEQ = 2048
DEPTH = 2
DEC_BATCH = 128
DEC_SEQ = 1
PAST_LEN = 16384
PAGE_SIZE = 128

N_RG_LAYERS = (DEPTH + 1) // 2
N_SSD_LAYERS = DEPTH // 2
CONV_W = 4
EPS = 1e-6
D_RNN = D_MODEL
RG_BLOCKS = 8
RG_BLOCK = D_RNN // RG_BLOCKS
RG_C = 8.0
SSD_EXPAND = 2
D_INNER = SSD_EXPAND * D_MODEL
SSD_HEAD_DIM = 64
SSD_HEADS = D_INNER // SSD_HEAD_DIM
SSD_GROUPS = 8
SSD_HPG = SSD_HEADS // SSD_GROUPS
SSD_STATE = 128
SSD_CONV_DIM = D_INNER + 2 * SSD_GROUPS * SSD_STATE
SSD_IN_DIM = D_INNER + SSD_CONV_DIM + SSD_HEADS
SSD_CHUNK = 128
PEER_HEADS = 8
PEER_NKEYS = 128
PEER_EXPERTS = PEER_NKEYS * PEER_NKEYS
PEER_DKEY = 256
PEER_HALF = PEER_DKEY // 2
PEER_TOPK = 16
PEER_TOKEN_BLOCK = 128

kernel_name = 'hybrid_rglru_ssd_peer_adaln_step'


def rmsnorm(x, g):
    xf = x.astype(jnp.float32)
    y = xf * lax.rsqrt(jnp.mean(xf * xf, axis=-1, keepdims=True) + EPS)
    return (y * g.astype(jnp.float32)).astype(x.dtype)


def causal_dwconv(x, buf, w, b):
    L = x.shape[1]
    xp = jnp.concatenate([buf.astype(x.dtype), x], axis=1)
    y = b + w[0] * xp[:, 0:L]
    for k in range(1, CONV_W):
        y = y + w[k] * xp[:, k:k + L]
    return y, xp[:, -(CONV_W - 1):]


def rglru_mixer(h, conv_buf, h0, w_in, b_in, conv_w, conv_b, w_a, b_a, w_i, b_i, lam, w_out, b_out):
    f32 = jnp.float32
    Bn, L, _ = h.shape
    proj = h @ w_in + b_in
    gate_branch, xr = jnp.split(proj, 2, axis=-1)
    xc, new_buf = causal_dwconv(xr, conv_buf, conv_w, conv_b)
    xb = xc.reshape(Bn, L, RG_BLOCKS, RG_BLOCK)
    r = jax.nn.sigmoid(jnp.einsum('blki,kij->blkj', xb, w_a).reshape(Bn, L, D_RNN) + b_a)
    i = jax.nn.sigmoid(jnp.einsum('blki,kij->blkj', xb, w_i).reshape(Bn, L, D_RNN) + b_i)
    log_a = (-RG_C * jax.nn.softplus(-lam.astype(f32))) * r.astype(f32)
    a = jnp.exp(log_a)
    mult = jnp.sqrt(jnp.maximum(-jnp.expm1(2.0 * log_a), 0.0))
    bterm = mult * (i * xc).astype(f32)
    bterm = bterm.at[:, 0].add(a[:, 0] * h0.astype(f32))

    def combine(lhs, rhs):
        a1, b1 = lhs
        a2, b2 = rhs
        return a1 * a2, a2 * b1 + b2

    _, hs = lax.associative_scan(combine, (a, bterm), axis=1)
    y = hs.astype(h.dtype) * jax.nn.gelu(gate_branch)
    return y @ w_out + b_out, new_buf, hs[:, -1].astype(h.dtype)


def ssd_scan(x, dt, A, bmat, cmat, s0):
    f32 = jnp.float32
    Bn, L = x.shape[:2]
    Q = min(SSD_CHUNK, L)
    nc = -(-L // Q)
    pad = nc * Q - L
    x, dt, bmat, cmat = [t.astype(f32) for t in (x, dt, bmat, cmat)]
    if pad:
        padf = lambda t: jnp.pad(t, [(0, 0), (0, pad)] + [(0, 0)] * (t.ndim - 2))
        x, dt, bmat, cmat = padf(x), padf(dt), padf(bmat), padf(cmat)

    def chunks(t):
        return jnp.moveaxis(t.reshape((Bn, nc, Q) + t.shape[2:]), 1, 0)

    tri = jnp.tril(jnp.ones((Q, Q), dtype=bool))

    def step(s, inp):
        xq, dq, bq, cq = inp
        cum = jnp.cumsum(dq * A, axis=1)
        seg = cum[:, :, None] - cum[:, None, :]
        decay = jnp.exp(jnp.where(tri[None, :, :, None, None], seg, -jnp.inf))
        cb = jnp.einsum('btgn,bsgn->btsg', cq, bq)
        w = decay * cb[..., None] * dq[:, None]
        y = jnp.einsum('btsge,bsgep->btgep', w, xq)
        y = y + jnp.einsum('btgn,bgepn->btgep', cq, s) * jnp.exp(cum)[..., None]
        last = cum[:, -1]
        wend = jnp.exp(last[:, None] - cum) * dq
        s = jnp.exp(last)[..., None, None] * s + jnp.einsum('bsge,bsgn,bsgep->bgepn', wend, bq, xq)
        return s, y

    s, ys = lax.scan(step, s0.astype(f32), (chunks(x), chunks(dt), chunks(bmat), chunks(cmat)))
    y = jnp.moveaxis(ys, 0, 1).reshape((Bn, nc * Q) + ys.shape[3:])[:, :L]
    return y, s


def ssd_mixer(h, conv_buf, s0, w_in, conv_w, conv_b, dt_bias, a_log, d_skip, norm_g, w_out):
    f32 = jnp.float32
    Bn, L, _ = h.shape
    proj = h @ w_in
    z, xbc, dt_raw = jnp.split(proj, [D_INNER, D_INNER + SSD_CONV_DIM], axis=-1)
    xbc, new_buf = causal_dwconv(xbc, conv_buf, conv_w, conv_b)
    xbc = jax.nn.silu(xbc)
    xs, bmat, cmat = jnp.split(xbc, [D_INNER, D_INNER + SSD_GROUPS * SSD_STATE], axis=-1)
    dt = jax.nn.softplus(dt_raw.astype(f32) + dt_bias.astype(f32))
    A = -jnp.exp(a_log.astype(f32))
    xs5 = xs.reshape(Bn, L, SSD_GROUPS, SSD_HPG, SSD_HEAD_DIM)
    y, s_new = ssd_scan(xs5, dt.reshape(Bn, L, SSD_GROUPS, SSD_HPG), A.reshape(SSD_GROUPS, SSD_HPG),
                        bmat.reshape(Bn, L, SSD_GROUPS, SSD_STATE), cmat.reshape(Bn, L, SSD_GROUPS, SSD_STATE), s0)
    y = y + d_skip.astype(f32).reshape(SSD_GROUPS, SSD_HPG, 1) * xs5.astype(f32)
    y = y.reshape(Bn, L, D_INNER).astype(h.dtype)
    y = rmsnorm(y * jax.nn.silu(z), norm_g)
    return y @ w_out, new_buf, s_new.astype(h.dtype)


def peer_mixer(h, w_q, k1, k2, u, v):
    f32 = jnp.float32
    Bn, L, D = h.shape
    T = Bn * L
    xt = h.reshape(T, D)
    q = (xt @ w_q).reshape(T, PEER_HEADS, 2, PEER_HALF).astype(f32)
    s1 = jnp.einsum('thd,kd->thk', q[:, :, 0], k1.astype(f32))
    s2 = jnp.einsum('thd,kd->thk', q[:, :, 1], k2.astype(f32))
    v1, i1 = lax.top_k(s1, PEER_TOPK)
    v2, i2 = lax.top_k(s2, PEER_TOPK)
    cand = (v1[..., :, None] + v2[..., None, :]).reshape(T, PEER_HEADS, PEER_TOPK * PEER_TOPK)
    cidx = (i1[..., :, None] * PEER_NKEYS + i2[..., None, :]).reshape(T, PEER_HEADS, PEER_TOPK * PEER_TOPK)
    sc, pos = lax.top_k(cand, PEER_TOPK)
    idx = jnp.take_along_axis(cidx, pos, axis=-1)
    g = jax.nn.softmax(sc, axis=-1).astype(h.dtype)
    tb = min(PEER_TOKEN_BLOCK, T)
    nb = -(-T // tb)
    pad = nb * tb - T
    xpad = jnp.pad(xt, [(0, pad), (0, 0)]).reshape(nb, tb, D)
    ipad = jnp.pad(idx, [(0, pad), (0, 0), (0, 0)]).reshape(nb, tb, PEER_HEADS, PEER_TOPK)
    gpad = jnp.pad(g, [(0, pad), (0, 0), (0, 0)]).reshape(nb, tb, PEER_HEADS, PEER_TOPK)

    def block(args):
        xb, ib, gb = args
        act = jax.nn.gelu(jnp.einsum('td,thkd->thk', xb, u[ib]))
        return jnp.einsum('thk,thkd->td', act * gb, v[ib])

    out = lax.map(block, (xpad, ipad, gpad)).reshape(nb * tb, D)[:T]
    return out.reshape(Bn, L, D)


def run_trunk(x, c, rg_conv, rg_h, ssd_conv, ssd_s, p):
    rg_conv_new, rg_h_new, ssd_conv_new, ssd_new = [], [], [], []
    cs = jax.nn.silu(c)
    for l in range(DEPTH):
        mod = cs @ p['w_mod'][l] + p['b_mod'][l]
        sh1, sc1, g1, sh2, sc2, g2 = [m[:, None, :] for m in jnp.split(mod, 6, axis=-1)]
        hm = rmsnorm(x, p['norm1_g'][l]) * (1 + sc1) + sh1
        j = l // 2
        if l % 2 == 0:
            out, cb, hs = rglru_mixer(hm, rg_conv[j], rg_h[j], p['rg_w_in'][j], p['rg_b_in'][j],
                                      p['rg_conv_w'][j], p['rg_conv_b'][j], p['rg_w_a'][j], p['rg_b_a'][j],
                                      p['rg_w_i'][j], p['rg_b_i'][j], p['rg_lambda'][j],
                                      p['rg_w_out'][j], p['rg_b_out'][j])
            rg_conv_new.append(cb)
            rg_h_new.append(hs)
        else:
            out, cb, ss = ssd_mixer(hm, ssd_conv[j], ssd_s[j], p['ssd_w_in'][j], p['ssd_conv_w'][j],
                                    p['ssd_conv_b'][j], p['ssd_dt_bias'][j], p['ssd_a_log'][j],
                                    p['ssd_d'][j], p['ssd_norm_g'][j], p['ssd_w_out'][j])
            ssd_conv_new.append(cb)
            ssd_new.append(ss)
        x = x + g1 * out
        hc = rmsnorm(x, p['norm2_g'][l]) * (1 + sc2) + sh2
        x = x + g2 * peer_mixer(hc, p['peer_w_q'][l], p['peer_k1'][l], p['peer_k2'][l],
                                p['peer_u'][l], p['peer_v'][l])
    y = rmsnorm(x, p['final_g'])
    return y, jnp.stack(rg_conv_new), jnp.stack(rg_h_new), jnp.stack(ssd_conv_new), jnp.stack(ssd_new)


def setup_inputs(seed: int = 0) -> dict:
    key = jax.random.key(seed)
    ks = jax.random.split(key, 64)
    cnt = iter(range(64))
    f32 = jnp.float32

    def nrm(shape, scale):
        return jax.random.normal(ks[next(cnt)], shape, f32) * scale

    def unif(shape, lo, hi):
        return jax.random.uniform(ks[next(cnt)], shape, f32, lo, hi)

    d = D_MODEL
    out = {}
    out['x_prompt'] = nrm((BATCH, SEQ, d), 1.0)
    out['x_sample'] = nrm((DEC_BATCH, DEC_SEQ, d), 1.0)
    out['state_rg_conv'] = nrm((N_RG_LAYERS, DEC_BATCH, CONV_W - 1, D_RNN), 1.0)
    out['state_rg_h'] = nrm((N_RG_LAYERS, DEC_BATCH, D_RNN), 0.5)
    out['state_ssd_conv'] = nrm((N_SSD_LAYERS, DEC_BATCH, CONV_W - 1, SSD_CONV_DIM), 1.0)
    out['state_ssd'] = nrm((N_SSD_LAYERS, DEC_BATCH, SSD_GROUPS, SSD_HPG, SSD_HEAD_DIM, SSD_STATE), 0.1)
    out['c_prompt'] = nrm((BATCH, d), 1.0)
    out['c_sample'] = nrm((DEC_BATCH, d), 1.0)
    out['norm1_g'] = 1.0 + nrm((DEPTH, d), 0.05)
    out['norm2_g'] = 1.0 + nrm((DEPTH, d), 0.05)
    out['w_mod'] = nrm((DEPTH, d, 6 * d), 0.5 * d ** -0.5)
    out['b_mod'] = nrm((DEPTH, 6 * d), 0.02)
    out['rg_w_in'] = nrm((N_RG_LAYERS, d, 2 * D_RNN), d ** -0.5)
    out['rg_b_in'] = nrm((N_RG_LAYERS, 2 * D_RNN), 0.02)
    out['rg_conv_w'] = nrm((N_RG_LAYERS, CONV_W, D_RNN), CONV_W ** -0.5)
    out['rg_conv_b'] = nrm((N_RG_LAYERS, D_RNN), 0.02)
    out['rg_w_a'] = nrm((N_RG_LAYERS, RG_BLOCKS, RG_BLOCK, RG_BLOCK), RG_BLOCK ** -0.5)
    out['rg_b_a'] = nrm((N_RG_LAYERS, D_RNN), 0.02)
    out['rg_w_i'] = nrm((N_RG_LAYERS, RG_BLOCKS, RG_BLOCK, RG_BLOCK), RG_BLOCK ** -0.5)
    out['rg_b_i'] = nrm((N_RG_LAYERS, D_RNN), 0.02)
    a_base = unif((N_RG_LAYERS, D_RNN), 0.9, 0.999) ** (1.0 / RG_C)
    out['rg_lambda'] = jnp.log(a_base) - jnp.log1p(-a_base)
    out['rg_w_out'] = nrm((N_RG_LAYERS, D_RNN, d), D_RNN ** -0.5)
    out['rg_b_out'] = nrm((N_RG_LAYERS, d), 0.02)
    out['ssd_w_in'] = nrm((N_SSD_LAYERS, d, SSD_IN_DIM), d ** -0.5)
    out['ssd_conv_w'] = nrm((N_SSD_LAYERS, CONV_W, SSD_CONV_DIM), CONV_W ** -0.5)
    out['ssd_conv_b'] = nrm((N_SSD_LAYERS, SSD_CONV_DIM), 0.02)
    dt0 = jnp.exp(unif((N_SSD_LAYERS, SSD_HEADS), math.log(1e-3), math.log(1e-1)))
    out['ssd_dt_bias'] = dt0 + jnp.log(-jnp.expm1(-dt0))
    out['ssd_a_log'] = jnp.log(unif((N_SSD_LAYERS, SSD_HEADS), 1.0, 16.0))
    out['ssd_d'] = 1.0 + nrm((N_SSD_LAYERS, SSD_HEADS), 0.1)
    out['ssd_norm_g'] = 1.0 + nrm((N_SSD_LAYERS, D_INNER), 0.05)
    out['ssd_w_out'] = nrm((N_SSD_LAYERS, D_INNER, d), D_INNER ** -0.5)
    out['peer_w_q'] = nrm((DEPTH, d, PEER_HEADS * PEER_DKEY), d ** -0.5)
    out['peer_k1'] = nrm((DEPTH, PEER_NKEYS, PEER_HALF), PEER_HALF ** -0.5)
    out['peer_k2'] = nrm((DEPTH, PEER_NKEYS, PEER_HALF), PEER_HALF ** -0.5)
    out['peer_u'] = nrm((DEPTH, PEER_EXPERTS, d), d ** -0.5)
    out['peer_v'] = nrm((DEPTH, PEER_EXPERTS, d), PEER_HEADS ** -0.5)
    out['final_g'] = 1.0 + nrm((d,), 0.05)
    return out


def reference(x_prompt, x_sample, state_rg_conv, state_rg_h, state_ssd_conv, state_ssd, c_prompt, c_sample,
              norm1_g, norm2_g, w_mod, b_mod,
              rg_w_in, rg_b_in, rg_conv_w, rg_conv_b, rg_w_a, rg_b_a, rg_w_i, rg_b_i, rg_lambda, rg_w_out, rg_b_out,
              ssd_w_in, ssd_conv_w, ssd_conv_b, ssd_dt_bias, ssd_a_log, ssd_d, ssd_norm_g, ssd_w_out,
              peer_w_q, peer_k1, peer_k2, peer_u, peer_v, final_g):
    p = dict(norm1_g=norm1_g, norm2_g=norm2_g, w_mod=w_mod, b_mod=b_mod,
             rg_w_in=rg_w_in, rg_b_in=rg_b_in, rg_conv_w=rg_conv_w, rg_conv_b=rg_conv_b,
             rg_w_a=rg_w_a, rg_b_a=rg_b_a, rg_w_i=rg_w_i, rg_b_i=rg_b_i, rg_lambda=rg_lambda,
             rg_w_out=rg_w_out, rg_b_out=rg_b_out,
             ssd_w_in=ssd_w_in, ssd_conv_w=ssd_conv_w, ssd_conv_b=ssd_conv_b, ssd_dt_bias=ssd_dt_bias,
             ssd_a_log=ssd_a_log, ssd_d=ssd_d, ssd_norm_g=ssd_norm_g, ssd_w_out=ssd_w_out,
             peer_w_q=peer_w_q, peer_k1=peer_k1, peer_k2=peer_k2, peer_u=peer_u, peer_v=peer_v,
             final_g=final_g)
    bp = x_prompt.shape[0]
    dtp = x_prompt.dtype
    z_rg_conv = jnp.zeros((N_RG_LAYERS, bp, CONV_W - 1, D_RNN), dtp)
    z_rg_h = jnp.zeros((N_RG_LAYERS, bp, D_RNN), dtp)
    z_ssd_conv = jnp.zeros((N_SSD_LAYERS, bp, CONV_W - 1, SSD_CONV_DIM), dtp)
    z_ssd = jnp.zeros((N_SSD_LAYERS, bp, SSD_GROUPS, SSD_HPG, SSD_HEAD_DIM, SSD_STATE), dtp)
    y_prompt, p_rg_conv, p_rg_h, p_ssd_conv, p_ssd = run_trunk(
        x_prompt, c_prompt, z_rg_conv, z_rg_h, z_ssd_conv, z_ssd, p)
    y_sample, s_rg_conv, s_rg_h, s_ssd_conv, s_ssd = run_trunk(
        x_sample, c_sample, state_rg_conv, state_rg_h, state_ssd_conv, state_ssd, p)
    return (y_prompt, y_sample, p_rg_conv, p_rg_h, p_ssd_conv, p_ssd, s_rg_conv, s_rg_h, s_ssd_conv, s_ssd)
```

```python
import contextlib
import numpy as np
import concourse.bass as bass
import concourse.mybir as mybir
from concourse.bass_utils import run_bass_kernel_spmd

F32 = mybir.dt.float32
BF16 = mybir.dt.bfloat16
AF = mybir.ActivationFunctionType
ALU = mybir.AluOpType

D = 2048
KC = 16
NS = 16
NB = 512
EPS = 1e-6
NDS = 12
GA = 4
NEG = -30000.0


class Trk:
    __slots__ = ("w", "r")

    def __init__(self):
        self.w = None
        self.r = {}


class Buf:
    def __init__(self, ap):
        self.ap = ap
        self.t = Trk()


class Prog:
    def __init__(self, nc, es):
        self.nc = nc
        self.eng = {"pe": nc.tensor, "dve": nc.vector, "act": nc.scalar, "pool": nc.gpsimd, "sp": nc.sync}
        self.sem = {k: es.enter_context(nc.semaphore("s_" + k)) for k in ("pe", "dve", "act", "pool")}
        self.cnt = {k: 0 for k in self.sem}
        self.seen = {e: {k: 0 for k in self.sem} for e in self.eng}
        self.dsem = [es.enter_context(nc.semaphore("d%d" % i)) for i in range(NDS)]
        self.dcnt = [0] * NDS
        self.dn = 0
        self.dseen = {e: [0] * NDS for e in self.eng}

    def _need(self, e, tok):
        if tok is None:
            return
        if tok[0] == "c":
            _, f, n = tok
            if f == e and e == "pe":
                return
            if self.seen[e][f] < n:
                self.eng[e].wait_ge(self.sem[f], n)
                self.seen[e][f] = n
        else:
            _, i, v = tok
            if self.dseen[e][i] < v:
                self.eng[e].wait_ge(self.dsem[i], v)
                self.dseen[e][i] = v

    def _sync(self, e, R, W):
        for b in R:
            self._need(e, b.t.w)
        for b in W:
            self._need(e, b.t.w)
            for tok in b.t.r.values():
                self._need(e, tok)

    def op(self, e, fn, R=(), W=()):
        self._sync(e, R, W)
        ins = fn(self.eng[e])
        self.cnt[e] += 1
        ins.then_inc(self.sem[e], 1)
        tok = ("c", e, self.cnt[e])
        for b in R:
            b.t.r[e] = tok
        for b in W:
            b.t.w = tok
            b.t.r = {}
        return ins

    def dma(self, out, in_, R=(), W=(), q="sp"):
        i = self.dn % NDS
        self.dn += 1
        self._need(q, ("d", i, self.dcnt[i]))
        self._sync(q, R, W)
        ins = self.eng[q].dma_start(out=out, in_=in_)
        self.dcnt[i] += 16
        ins.then_inc(self.dsem[i], 16)
        tok = ("d", i, self.dcnt[i])
        for b in R:
            b.t.r["dma%d" % i] = tok
        for b in W:
            b.t.w = tok
            b.t.r = {}

    def barrier(self):
        for e in self.eng:
            for f in self.sem:
                self._need(e, ("c", f, self.cnt[f]))
            for i in range(NDS):
                self._need(e, ("d", i, self.dcnt[i]))

    def finish(self):
        for i in range(NDS):
            self._need("sp", ("d", i, self.dcnt[i]))
        for f in self.sem:
            self._need("sp", ("c", f, self.cnt[f]))


class Arena:
    def __init__(self, ap, width):
        self.ap = ap
        self.width = width
        self.off = 0
        self.hw = 0

    def mark(self):
        return self.off

    def reset(self, m):
        self.off = m

    def f32(self, *shape):
        n = int(np.prod(shape))
        assert self.off + n <= self.width, ("arena overflow", self.off, n, self.width)
        v = self.ap[:, self.off:self.off + n]
        self.off += n
        self.hw = max(self.hw, self.off)
        return Buf(_shape(v, shape))

    def bf16(self, *shape):
        n = int(np.prod(shape))
        w = (n + 1) // 2
        assert self.off + w <= self.width, ("arena overflow", self.off, w, self.width)
        v = self.ap[:, self.off:self.off + w].bitcast(BF16)[:, 0:n]
        self.off += w
        self.hw = max(self.hw, self.off)
        return Buf(_shape(v, shape))


def _shape(v, shape):
    if len(shape) == 1:
        return v
    if len(shape) == 2:
        return v.rearrange("p (a b) -> p a b", b=shape[1])
    if len(shape) == 3:
        return v.rearrange("p (a b c) -> p a b c", b=shape[1], c=shape[2])
    raise ValueError(shape)


def bc(ap, axis, shape):
    return ap.unsqueeze(axis).to_broadcast(list(shape))


def build(TP):
    nblk = TP // NB
    nc = bass.Bass("TRN2", target_bir_lowering=False)
    es = contextlib.ExitStack()

    def din(name, shape):
        return nc.dram_tensor(name, list(shape), F32, kind="ExternalInput").ap()

    def dout(name, shape):
        return nc.dram_tensor(name, list(shape), F32, kind="ExternalOutput").ap()

    i_xT = din("xT", [D, TP])
    i_xsT = din("xsT", [D, NS])
    i_cT = din("cT", [D, 1 + NS])
    i_rgconv = din("rgconv", [D, 3, NS])
    i_rgh = din("rgh", [D, NS])
    i_ssdconv = din("ssdconv", [6144, 3, NS])
    i_ssds = din("ssds", [NS, 128, 64, 64])
    i_cst = din("cst", [128, 4 * 128 + 16])
    i_vec = din("vec", [128, 16 * 11 + 32 * 2 + 48 + 32])
    i_bmod = din("bmod", [128, 2, 96])
    i_rgcw = din("rgcw", [128, 16, 4])
    i_ssdcw = din("ssdcw", [128, 48, 4])
    i_ssdh = din("ssdh", [3, 64])
    i_wmod = din("wmod", [2, 96, 128, 16, 128])
    i_rgwin = din("rgwin", [32, 128, 16, 128])
    i_rgwa = din("rgwa", [16, 128, 2, 128])
    i_rgwi = din("rgwi", [16, 128, 2, 128])
    i_rgwout = din("rgwout", [16, 128, 16, 128])
    i_ssdwin = din("ssdwin", [80, 128, 16, 128])
    i_ssdwdt = din("ssdwdt", [128, 16, 64])
    i_ssdwout = din("ssdwout", [16, 128, 32, 128])
    i_wq = din("wq", [2, 16, 128, 16, 128])
    i_kT = din("kT", [2, 2, 128, 128])
    i_uT = din("uT", [2, 128, 128, 16, 128])
    i_v = din("v", [2, 128, 128, 2048])
    o_yT = dout("o_yT", [D, TP])
    o_ysT = dout("o_ysT", [D, NS])
    o_rgconv_p = dout("o_rgconv_p", [128, 16, 3])
    o_rgh_p = dout("o_rgh_p", [128, 16])
    o_ssdconv_p = dout("o_ssdconv_p", [128, 48, 3])
    o_ssd_p = dout("o_ssd_p", [128, 64, 64])
    o_rgconv_s = dout("o_rgconv_s", [128, 16, 3, NS])
    o_rgh_s = dout("o_rgh_s", [128, 16, NS])
    o_ssdconv_s = dout("o_ssdconv_s", [128, 48, 3, NS])
    o_ssd_s = dout("o_ssd_s", [NS, 128, 64, 64])

    AW = 51800
    arena_t = es.enter_context(nc.sbuf_tensor("arena", [128, AW], F32))
    A = Arena(arena_t[:, :], AW)
    banks = [Buf(es.enter_context(nc.psum_tensor("pb%d" % i, [128, 512], F32))[:, :]) for i in range(8)]
    P = Prog(nc, es)
    op, dma = P.op, P.dma

    cst = A.f32(4 * 128 + 16)
    ident = cst.ap[:, 0:128]
    ones = cst.ap[:, 128:256]
    tri = cst.ap[:, 256:384]
    mneg = cst.ap[:, 384:512]
    selm = cst.ap[:, 512:528]
    dma(cst.ap, i_cst, W=[cst])
    identb = A.bf16(128)
    op("dve", lambda e: e.tensor_copy(out=identb.ap, in_=ident), R=[cst], W=[identb])
    vec = A.f32(16 * 11 + 32 * 2 + 48 + 32)
    dma(vec.ap, i_vec, W=[vec])

    def vsl(o, n):
        return vec.ap[:, o:o + n]
    n1g = [vsl(0, 16), vsl(16, 16)]
    n2g = [vsl(32, 16), vsl(48, 16)]
    fing = vsl(64, 16)
    rg_bconv = vsl(80, 16)
    rg_ba = vsl(96, 16)
    rg_bi = vsl(112, 16)
    rg_lam = vsl(128, 16)
    rg_bout = vsl(144, 16)
    rg_bin = vsl(176, 32)
    ssd_ng = vsl(208, 32)
    ssd_bconv = vsl(240, 48)
    ssd_dexp = vsl(288, 32)
    rgcw = A.f32(16, 4)
    dma(rgcw.ap, i_rgcw, W=[rgcw])
    ssdcw = A.f32(48, 4)
    dma(ssdcw.ap, i_ssdcw, W=[ssdcw])
    ssdh = A.f32(3, 64)
    dma(ssdh.ap, i_ssdh.partition_broadcast(128), W=[ssdh])
    Abc = A.f32(64)
    op("act", lambda e: e.activation(out=Abc.ap, in_=ssdh.ap[:, 1, :], func=AF.Exp), R=[ssdh], W=[Abc])
    op("dve", lambda e: e.tensor_scalar(out=Abc.ap, in0=Abc.ap, scalar1=-1.0, scalar2=None, op0=ALU.mult),
       R=[Abc], W=[Abc])
    c8 = A.f32(16)
    op("act", lambda e: e.activation(out=c8.ap, in_=rg_lam, func=AF.Exp, scale=-1.0), R=[vec], W=[c8])
    op("act", lambda e: e.activation(out=c8.ap, in_=c8.ap, func=AF.Ln, bias=1.0), R=[c8], W=[c8])
    op("dve", lambda e: e.tensor_scalar(out=c8.ap, in0=c8.ap, scalar1=-8.0, scalar2=None, op0=ALU.mult),
       R=[c8], W=[c8])
    epsb = A.f32(1)
    op("pool", lambda e: e.memset(epsb.ap, EPS), W=[epsb])
    kT = A.f32(4, 128)
    for l_ in range(2):
        for h_ in range(2):
            dma(kT.ap[:, l_ * 2 + h_, :], i_kT[l_, h_], W=[kT])

    modT = A.f32(2, 96, 1 + NS)
    csT = A.f32(KC, 1 + NS)
    for kc_ in range(KC):
        dma(csT.ap[:, kc_, :], i_cT[kc_ * 128:(kc_ + 1) * 128, :], W=[csT])
    op("act", lambda e: e.activation(out=csT.ap, in_=csT.ap, func=AF.Silu), R=[csT], W=[csT])
    bmod = A.f32(2, 96)
    dma(bmod.ap, i_bmod, W=[bmod])
    m0 = A.mark()
    NST = 4
    stg = [A.f32(KC, 128) for _ in range(NST)]
    jobs = [(l, j) for l in range(2) for j in range(96)]
    for idx in range(min(NST - 1, len(jobs))):
        l, j = jobs[idx]
        dma(stg[idx % NST].ap, i_wmod[l, j], W=[stg[idx % NST]])
    for idx, (l, j) in enumerate(jobs):
        nx = idx + NST - 1
        if nx < len(jobs):
            dma(stg[nx % NST].ap, i_wmod[jobs[nx][0], jobs[nx][1]], W=[stg[nx % NST]])
        s = stg[idx % NST]
        pb = banks[idx % 2]
        for kc in range(KC):
            op("pe", lambda e, kc=kc: e.matmul(pb.ap[:, 0:1 + NS], s.ap[:, kc, :], csT.ap[:, kc, :],
                                                start=(kc == 0), stop=(kc == KC - 1)), R=[s, csT], W=[pb])
        op("dve", lambda e: e.tensor_scalar(out=modT.ap[:, l, j, :], in0=pb.ap[:, 0:1 + NS],
                                            scalar1=bmod.ap[:, l, j:j + 1], scalar2=None, op0=ALU.add),
           R=[pb, bmod], W=[modT])
    A.reset(m0)
    P.barrier()
    gmp = A.f32(2, 2, 16)
    gms = A.f32(4, 16, NS)
    for l in range(2):
        for k, (sc0, ng) in enumerate(((16, n1g[l]), (64, n2g[l]))):
            op("dve", lambda e: e.scalar_tensor_tensor(out=gmp.ap[:, l, k, :], in0=modT.ap[:, l, sc0:sc0 + 16, 0],
                                                       scalar=1.0, in1=ng, op0=ALU.add, op1=ALU.mult),
               R=[modT, vec], W=[gmp])
            op("dve", lambda e: e.scalar_tensor_tensor(out=gms.ap[:, l * 2 + k, :, :],
                                                       in0=modT.ap[:, l, sc0:sc0 + 16, 1:1 + NS], scalar=1.0,
                                                       in1=bc(ng, 2, [128, 16, NS]), op0=ALU.add, op1=ALU.mult),
               R=[modT, vec], W=[gms])

    def modv(l, k, c, samp):
        if samp:
            return modT.ap[:, l, 16 * k + c, 1:1 + NS]
        return modT.ap[:, l, 16 * k + c, 0:1]

    xT = A.f32(KC, NB)
    rg_tail = A.f32(16, 3)
    rg_carry = A.f32(16)
    ssd_tail = A.f32(48, 3)
    for b_ in (rg_tail, rg_carry, ssd_tail):
        op("pool", lambda e, b_=b_: e.memset(b_.ap, 0.0), W=[b_])
    sT_dram = Buf(o_ssd_p)
    pmark = A.mark()

    class WStream:
        def __init__(self, srcs, shape, ceng=("pool",), nstage=3, nbf=3):
            self.srcs = srcs
            self.shape = shape
            self.stage = [A.f32(*shape) for _ in range(nstage)]
            self.bfs = [A.bf16(*shape) for _ in range(nbf)]
            self.ceng = ceng
            self.nl = 0
            self.ncast = 0
            self.pf = nstage - 1
            for _ in range(min(self.pf, len(srcs))):
                self._load()

        def _load(self):
            if self.nl < len(self.srcs):
                s = self.stage[self.nl % len(self.stage)]
                dma(s.ap, self.srcs[self.nl], W=[s])
                self.nl += 1

        def get(self):
            i = self.ncast
            self._load()
            s = self.stage[i % len(self.stage)]
            b = self.bfs[i % len(self.bfs)]
            e_ = self.ceng[i % len(self.ceng)]
            if e_ == "act":
                op("act", lambda e: e.copy(out=b.ap, in_=s.ap), R=[s], W=[b])
            else:
                op(e_, lambda e: e.tensor_copy(out=b.ap, in_=s.ap), R=[s], W=[b])
            self.ncast += 1
            return b

    def rms_stats(src_chunks, srcbufs, N, nch, scratch, pb, rstd, eng_sq="act"):
        for c in range(nch):
            sq = scratch[c % len(scratch)]
            op("act", lambda e, c=c, sq=sq: e.activation(out=sq.ap[:, 0:N], in_=src_chunks(c), func=AF.Square),
               R=srcbufs, W=[sq])
            op("pe", lambda e, c=c, sq=sq: e.matmul(pb.ap[:, 0:N], ones, sq.ap[:, 0:N], start=(c == 0),
                                                    stop=(c == nch - 1)), R=[sq, cst], W=[pb])
        op("act", lambda e: e.activation(out=rstd.ap[:, 0:N], in_=pb.ap[:, 0:N], func=AF.Sqrt,
                                         scale=1.0 / (nch * 128), bias=epsb.ap[:, 0:1]), R=[pb, epsb], W=[rstd])
        op("dve", lambda e: e.reciprocal(out=rstd.ap[:, 0:N], in_=rstd.ap[:, 0:N]), R=[rstd], W=[rstd])

    def norm_mod(l, which, N, samp, dst):
        m = A.mark()
        scr = [A.f32(NB), A.f32(NB)]
        rstd = A.f32(NB)
        tmp = [A.f32(NB), A.f32(NB)]
        rms_stats(lambda c: xT.ap[:, c, 0:N], [xT], N, KC, scr, banks[7], rstd)
        for c in range(KC):
            t = tmp[c % 2]
            if samp:
                gm = gms.ap[:, l * 2 + which, c, :]
                sh = modv(l, 3 * which, c, True)
                op("dve", lambda e: e.tensor_tensor(out=t.ap[:, 0:N], in0=xT.ap[:, c, 0:N], in1=rstd.ap[:, 0:N],
                                                    op=ALU.mult), R=[xT, rstd], W=[t])
                op("dve", lambda e: e.tensor_tensor(out=t.ap[:, 0:N], in0=t.ap[:, 0:N], in1=gm, op=ALU.mult),
                   R=[t, gms], W=[t])
                op("dve", lambda e: e.tensor_tensor(out=dst.ap[:, c, 0:N], in0=t.ap[:, 0:N], in1=sh, op=ALU.add),
                   R=[t, modT], W=[dst])
            else:
                gm = gmp.ap[:, l, which, c:c + 1]
                sh = modv(l, 3 * which, c, False)
                op("dve", lambda e: e.scalar_tensor_tensor(out=t.ap[:, 0:N], in0=xT.ap[:, c, 0:N], scalar=gm,
                                                           in1=rstd.ap[:, 0:N], op0=ALU.mult, op1=ALU.mult),
                   R=[xT, rstd, gmp], W=[t])
                op("act", lambda e: e.activation(out=dst.ap[:, c, 0:N], in_=t.ap[:, 0:N], func=AF.Identity,
                                                 bias=sh, scale=1.0), R=[t, modT], W=[dst])
        A.reset(m)

    def resid_add(c, pb, N, bias, gate, samp, tmpb):
        if samp:
            if bias is not None:
                op("dve", lambda e: e.scalar_tensor_tensor(out=tmpb.ap[:, 0:N], in0=pb.ap[:, 0:N], scalar=bias,
                                                           in1=gate, op0=ALU.add, op1=ALU.mult),
                   R=[pb, vec, modT], W=[tmpb])
            else:
                op("dve", lambda e: e.tensor_tensor(out=tmpb.ap[:, 0:N], in0=pb.ap[:, 0:N], in1=gate, op=ALU.mult),
                   R=[pb, modT], W=[tmpb])
            op("dve", lambda e: e.tensor_tensor(out=xT.ap[:, c, 0:N], in0=xT.ap[:, c, 0:N], in1=tmpb.ap[:, 0:N],
                                                op=ALU.add), R=[tmpb, xT], W=[xT])
        else:
            if bias is not None:
                op("dve", lambda e: e.tensor_scalar(out=tmpb.ap[:, 0:N], in0=pb.ap[:, 0:N], scalar1=bias,
                                                    scalar2=gate, op0=ALU.add, op1=ALU.mult),
                   R=[pb, vec, modT], W=[tmpb])
                op("dve", lambda e: e.tensor_tensor(out=xT.ap[:, c, 0:N], in0=xT.ap[:, c, 0:N],
                                                    in1=tmpb.ap[:, 0:N], op=ALU.add), R=[tmpb, xT], W=[xT])
            else:
                op("dve", lambda e: e.scalar_tensor_tensor(out=xT.ap[:, c, 0:N], in0=pb.ap[:, 0:N], scalar=gate,
                                                           in1=xT.ap[:, c, 0:N], op0=ALU.mult, op1=ALU.add),
                   R=[pb, modT, xT], W=[xT])

    def conv4(dst, taps, wcol, bcol, N, Rb, Wb):
        op("dve", lambda e: e.tensor_scalar(out=dst, in0=taps[0], scalar1=wcol(0), scalar2=bcol, op0=ALU.mult,
                                            op1=ALU.add), R=Rb, W=Wb)
        for k in range(1, 4):
            op("dve", lambda e, k=k: e.scalar_tensor_tensor(out=dst, in0=taps[k], scalar=wcol(k), in1=dst,
                                                            op0=ALU.mult, op1=ALU.add), R=Rb + Wb, W=Wb)

    def rg_mixer(N, samp, last, hmT):
        m = A.mark()
        gT = A.bf16(KC, NB)
        yT = A.bf16(KC, NB)
        if samp:
            nbuf = A.f32(KC, 3, NS)
            hout = A.f32(KC, NS)
        m_in = A.mark()
        win_srcs = []
        for p in range(8):
            win_srcs += [i_rgwin[2 * p], i_rgwin[2 * p + 1], i_rgwin[16 + 2 * p], i_rgwin[16 + 2 * p + 1]]
        ws = WStream(win_srcs, (KC, 128), ceng=("pool", "act"), nstage=2, nbf=3)
        gsrcs = []
        for p in range(8):
            gsrcs += [i_rgwa[2 * p], i_rgwa[2 * p + 1], i_rgwi[2 * p], i_rgwi[2 * p + 1]]
        gs = WStream(gsrcs, (2, 128), ceng=("pool",), nstage=4, nbf=4)
        xe = [A.f32(2, NB + 3), A.f32(2, NB + 3)]
        xc = [A.f32(2, NB), A.f32(2, NB)]
        xcb = [A.bf16(2, NB), A.bf16(2, NB)]
        gate_r = [A.f32(NB), A.f32(NB)]
        gate_i = [A.f32(NB), A.f32(NB)]
        av = [A.f32(NB), A.f32(NB)]
        bv = [A.f32(NB), A.f32(NB)]
        hs = [A.f32(NB), A.f32(NB)]
        if samp:
            st_c = A.f32(KC, 3, NS)
            for c_ in range(KC):
                dma(st_c.ap[:, c_], i_rgconv[c_ * 128:(c_ + 1) * 128], W=[st_c])
            h0s = A.f32(KC, NS)
            for c_ in range(KC):
                dma(h0s.ap[:, c_, :], i_rgh[c_ * 128:(c_ + 1) * 128, :], W=[h0s])
        for p in range(8):
            xe_, xc_, xcb_ = xe[p % 2], xc[p % 2], xcb[p % 2]
            for q4 in range(4):
                wb = ws.get()
                pb = banks[q4 % 4]
                for kc in range(KC):
                    op("pe", lambda e, kc=kc: e.matmul(pb.ap[:, 0:N], wb.ap[:, kc, :], hmT.ap[:, kc, 0:N],
                                                        start=(kc == 0), stop=(kc == KC - 1)), R=[wb, hmT], W=[pb])
                if q4 < 2:
                    c = 2 * p + q4
                    op("act", lambda e: e.activation(out=gT.ap[:, c, 0:N], in_=pb.ap[:, 0:N],
                                                     func=AF.Gelu_apprx_tanh, bias=rg_bin[:, c:c + 1], scale=1.0),
                       R=[pb, vec], W=[gT])
                else:
                    q = q4 - 2
                    c = 2 * p + q
                    if not samp:
                        op("pool", lambda e: e.tensor_copy(out=xe_.ap[:, q, 0:3], in_=rg_tail.ap[:, c, :]),
                           R=[rg_tail], W=[xe_])
                    op("act", lambda e: e.activation(out=xe_.ap[:, q, 3:3 + N], in_=pb.ap[:, 0:N], func=AF.Identity,
                                                     bias=rg_bin[:, 16 + c:16 + c + 1], scale=1.0),
                       R=[pb, vec], W=[xe_])
            for q in range(2):
                c = 2 * p + q
                if samp:
                    taps = [st_c.ap[:, c, 0, :], st_c.ap[:, c, 1, :], st_c.ap[:, c, 2, :], xe_.ap[:, q, 3:3 + N]]
                    Rb = [xe_, st_c, rgcw, vec]
                else:
                    taps = [xe_.ap[:, q, k:k + N] for k in range(4)]
                    Rb = [xe_, rgcw, vec]
                conv4(xc_.ap[:, q, 0:N], taps, lambda k: rgcw.ap[:, c, k:k + 1], rg_bconv[:, c:c + 1], N, Rb, [xc_])
                op("act", lambda e: e.copy(out=xcb_.ap[:, q, 0:N], in_=xc_.ap[:, q, 0:N]), R=[xc_], W=[xcb_])
                if samp:
                    for k in range(2):
                        op("pool", lambda e, k=k: e.tensor_copy(out=nbuf.ap[:, c, k, :], in_=st_c.ap[:, c, k + 1, :]),
                           R=[st_c], W=[nbuf])
                    op("pool", lambda e: e.tensor_copy(out=nbuf.ap[:, c, 2, :], in_=xe_.ap[:, q, 3:3 + N]),
                       R=[xe_], W=[nbuf])
                else:
                    op("pool", lambda e: e.tensor_copy(out=rg_tail.ap[:, c, :], in_=xe_.ap[:, q, N:N + 3]),
                       R=[xe_], W=[rg_tail])
            gw = [gs.get() for _ in range(4)]
            for jh in range(2):
                c = 2 * p + jh
                r_, i_, a_, b_, h_ = gate_r[jh], gate_i[jh], av[jh], bv[jh], hs[jh]
                for gi_, (wt, dstb, bcol) in enumerate(((gw[jh], r_, rg_ba), (gw[2 + jh], i_, rg_bi))):
                    pb = banks[4 + (2 * jh + gi_) % 4]
                    for ih in range(2):
                        op("pe", lambda e, ih=ih: e.matmul(pb.ap[:, 0:N], wt.ap[:, ih, :], xcb_.ap[:, ih, 0:N],
                                                            start=(ih == 0), stop=(ih == 1)), R=[wt, xcb_], W=[pb])
                    op("act", lambda e: e.activation(out=dstb.ap[:, 0:N], in_=pb.ap[:, 0:N], func=AF.Sigmoid,
                                                     bias=bcol[:, c:c + 1], scale=1.0), R=[pb, vec], W=[dstb])
                op("act", lambda e: e.activation(out=a_.ap[:, 0:N], in_=r_.ap[:, 0:N], func=AF.Exp,
                                                 scale=c8.ap[:, c:c + 1]), R=[r_, c8], W=[a_])
                op("dve", lambda e: e.tensor_tensor(out=b_.ap[:, 0:N], in0=a_.ap[:, 0:N], in1=a_.ap[:, 0:N],
                                                    op=ALU.mult), R=[a_], W=[b_])
                op("dve", lambda e: e.tensor_scalar(out=b_.ap[:, 0:N], in0=b_.ap[:, 0:N], scalar1=-1.0, scalar2=1.0,
                                                    op0=ALU.mult, op1=ALU.add), R=[b_], W=[b_])
                op("dve", lambda e: e.tensor_scalar(out=b_.ap[:, 0:N], in0=b_.ap[:, 0:N], scalar1=1e-30,
                                                    scalar2=None, op0=ALU.max), R=[b_], W=[b_])
                op("act", lambda e: e.activation(out=b_.ap[:, 0:N], in_=b_.ap[:, 0:N], func=AF.Sqrt),
                   R=[b_], W=[b_])
                op("dve", lambda e: e.tensor_tensor(out=i_.ap[:, 0:N], in0=i_.ap[:, 0:N], in1=xc_.ap[:, jh, 0:N],
                                                    op=ALU.mult), R=[i_, xc_], W=[i_])
                op("dve", lambda e: e.tensor_tensor(out=b_.ap[:, 0:N], in0=b_.ap[:, 0:N], in1=i_.ap[:, 0:N],
                                                    op=ALU.mult), R=[b_, i_], W=[b_])
                if samp:
                    op("dve", lambda e: e.tensor_tensor(out=h_.ap[:, 0:N], in0=a_.ap[:, 0:N], in1=h0s.ap[:, c, :],
                                                        op=ALU.mult), R=[a_, h0s], W=[h_])
                    op("dve", lambda e: e.tensor_tensor(out=h_.ap[:, 0:N], in0=h_.ap[:, 0:N], in1=b_.ap[:, 0:N],
                                                        op=ALU.add), R=[h_, b_], W=[h_])
                    op("pool", lambda e: e.tensor_copy(out=hout.ap[:, c, :], in_=h_.ap[:, 0:N]), R=[h_], W=[hout])
                else:
                    op("dve", lambda e: e.tensor_tensor_scan(out=h_.ap[:, 0:N], data0=a_.ap[:, 0:N],
                                                             data1=b_.ap[:, 0:N], initial=rg_carry.ap[:, c:c + 1],
                                                             op0=ALU.mult, op1=ALU.add),
                       R=[a_, b_, rg_carry], W=[h_])
                    op("pool", lambda e: e.tensor_copy(out=rg_carry.ap[:, c:c + 1], in_=h_.ap[:, N - 1:N]),
                       R=[h_], W=[rg_carry])
                op("dve", lambda e: e.tensor_tensor(out=yT.ap[:, c, 0:N], in0=h_.ap[:, 0:N], in1=gT.ap[:, c, 0:N],
                                                    op=ALU.mult), R=[h_, gT], W=[yT])
        P.barrier()
        A.reset(m_in)
        wo = WStream([i_rgwout[j] for j in range(16)], (KC, 128), ceng=("pool", "act"))
        tmpb = [A.f32(NB), A.f32(NB)]
        for j in range(16):
            wb = wo.get()
            pb = banks[j % 4]
            for kc in range(KC):
                op("pe", lambda e, kc=kc: e.matmul(pb.ap[:, 0:N], wb.ap[:, kc, :], yT.ap[:, kc, 0:N],
                                                    start=(kc == 0), stop=(kc == KC - 1)), R=[wb, yT], W=[pb])
            resid_add(j, pb, N, rg_bout[:, j:j + 1], modv(0, 2, j, samp), samp, tmpb[j % 2])
        if samp:
            dma(o_rgconv_s, nbuf.ap, R=[nbuf])
            dma(o_rgh_s, hout.ap, R=[hout])
        elif last:
            dma(o_rgconv_p, rg_tail.ap, R=[rg_tail])
            dma(o_rgh_p, rg_carry.ap, R=[rg_carry])
        P.barrier()
        A.reset(m)

    def ssd_chunk(Q, t0, sT, hmT, xbcT, yT, dtw):
        m = A.mark()
        pdt = banks[0]
        for kc in range(KC):
            op("pe", lambda e, kc=kc: e.matmul(pdt.ap[0:Q, 0:64], hmT.ap[:, kc, t0:t0 + Q], dtw.ap[:, kc, :],
                                                start=(kc == 0), stop=(kc == KC - 1)), R=[hmT, dtw], W=[pdt])
        dt = A.f32(64)
        dtA = A.f32(64)
        op("dve", lambda e: e.tensor_tensor(out=dt.ap[0:Q, :], in0=pdt.ap[0:Q, 0:64], in1=ssdh.ap[0:Q, 0, :],
                                            op=ALU.add), R=[pdt, ssdh], W=[dt])
        op("act", lambda e: e.activation(out=dt.ap[0:Q, :], in_=dt.ap[0:Q, :], func=AF.Exp), R=[dt], W=[dt])
        op("act", lambda e: e.activation(out=dt.ap[0:Q, :], in_=dt.ap[0:Q, :], func=AF.Ln, bias=1.0),
           R=[dt], W=[dt])
        op("dve", lambda e: e.tensor_tensor(out=dtA.ap[0:Q, :], in0=dt.ap[0:Q, :], in1=Abc.ap[0:Q, :],
                                            op=ALU.mult), R=[dt, Abc], W=[dtA])
        pc = banks[1]
        op("pe", lambda e: e.matmul(pc.ap[0:Q, 0:64], tri[0:Q, 0:Q], dtA.ap[0:Q, :], start=True, stop=True),
           R=[dtA, cst], W=[pc])
        ncum = A.f32(64)
        op("dve", lambda e: e.tensor_scalar(out=ncum.ap[0:Q, :], in0=pc.ap[0:Q, 0:64], scalar1=-1.0, scalar2=None,
                                            op0=ALU.mult), R=[pc], W=[ncum])
        xtok = A.bf16(4096)
        btok = A.bf16(1024)
        for grp in range(5):
            pbt = banks[2 + grp % 2]
            pv = pbt.ap.bitcast(BF16)
            for k in range(8):
                j = grp * 8 + k
                op("pe", lambda e, j=j, k=k: e.transpose(pv[0:Q, k * 128:(k + 1) * 128], xbcT.ap[:, j, t0:t0 + Q],
                                                         identb.ap), R=[xbcT, identb], W=[pbt])
            if grp < 4:
                op("act", lambda e: e.copy(out=xtok.ap[0:Q, grp * 1024:(grp + 1) * 1024], in_=pv[0:Q, :]),
                   R=[pbt], W=[xtok])
            else:
                op("act", lambda e: e.copy(out=btok.ap[0:Q, :], in_=pv[0:Q, :]), R=[pbt], W=[btok])
        cbt = A.f32(8, 128)
        for g in range(8):
            pcb = banks[4 + g % 2]
            op("pe", lambda e: e.matmul(pcb.ap[0:Q, 0:Q], xbcT.ap[:, 32 + g, t0:t0 + Q], xbcT.ap[:, 40 + g, t0:t0 + Q],
                                        start=True, stop=True), R=[xbcT], W=[pcb])
            op("act", lambda e: e.copy(out=cbt.ap[0:Q, g, 0:Q], in_=pcb.ap[0:Q, 0:Q]), R=[pcb], W=[cbt])
        sTb = A.bf16(64, 64)
        op("pool", lambda e: e.tensor_copy(out=sTb.ap, in_=sT.ap), R=[sT], W=[sTb])
        rhs4 = [A.f32(4, 128), A.f32(4, 128)]
        arg4 = [A.f32(4, 128), A.f32(4, 128)]
        ET4 = [A.f32(4, 128), A.f32(4, 128)]
        ecr4 = [A.f32(4, 128), A.f32(4, 128)]
        WT = [A.bf16(128) for _ in range(4)]
        CpT = [A.bf16(128) for _ in range(4)]
        xw = [A.bf16(64) for _ in range(4)]
        for h4 in range(16):
            r4, a4, E4, c4 = rhs4[h4 % 2], arg4[h4 % 2], ET4[h4 % 2], ecr4[h4 % 2]
            g = h4 // 2
            op("pool", lambda e: e.tensor_tensor(out=r4.ap[0:Q, :, 0:Q], in0=bc(tri[0:Q, 0:Q], 1, [Q, 4, Q]),
                                                 in1=bc(dtA.ap[0:Q, h4 * 4:h4 * 4 + 4], 2, [Q, 4, Q]), op=ALU.mult),
               R=[dtA, cst], W=[r4])
            pcr = banks[6 + h4 % 2]
            pcr_v = pcr.ap.rearrange("p (a b) -> p a b", b=128)
            for hh in range(4):
                op("pe", lambda e, hh=hh: e.matmul(pcr_v[:, hh, 0:Q], ones[0:Q, :], r4.ap[0:Q, hh, 0:Q],
                                                    start=True, stop=True), R=[r4, cst], W=[pcr])
            for hh in range(4):
                h = h4 * 4 + hh
                op("dve", lambda e, hh=hh, h=h: e.scalar_tensor_tensor(out=a4.ap[0:Q, hh, 0:Q], in0=pcr_v[0:Q, hh, 0:Q],
                                                                       scalar=ncum.ap[0:Q, h:h + 1], in1=mneg[0:Q, 0:Q],
                                                                       op0=ALU.add, op1=ALU.min),
                   R=[pcr, ncum, cst], W=[a4])
            op("act", lambda e: e.activation(out=E4.ap[0:Q, :, 0:Q], in_=a4.ap[0:Q, :, 0:Q], func=AF.Exp),
               R=[a4], W=[E4])
            op("act", lambda e: e.activation(out=c4.ap[:, :, 0:Q], in_=pcr_v[:, :, 0:Q], func=AF.Exp),
               R=[pcr], W=[c4])
            for hh in range(4):
                h = h4 * 4 + hh
                w_, cp_, xw_ = WT[hh], CpT[hh], xw[hh]
                op("dve", lambda e, hh=hh, h=h, w_=w_: e.scalar_tensor_tensor(
                    out=w_.ap[0:Q, 0:Q], in0=E4.ap[0:Q, hh, 0:Q], scalar=dt.ap[0:Q, h:h + 1], in1=cbt.ap[0:Q, g, 0:Q],
                    op0=ALU.mult, op1=ALU.mult), R=[E4, dt, cbt], W=[w_])
                op("pool", lambda e, hh=hh, cp_=cp_: e.tensor_tensor(
                    out=cp_.ap[:, 0:Q], in0=xbcT.ap[:, 40 + g, t0:t0 + Q], in1=c4.ap[:, hh, 0:Q], op=ALU.mult),
                   R=[xbcT, c4], W=[cp_])
                c = h // 2
                pby = banks[0 + (c // 4) % 2]
                po = (h % 2) * 64
                col = (c % 4) * 128
                op("pe", lambda e, h=h, w_=w_, pby=pby, po=po, col=col: e.matmul(
                    pby.ap[po:po + 64, col:col + Q], xtok.ap[0:Q, h * 64:(h + 1) * 64], w_.ap[0:Q, 0:Q],
                    start=True, stop=False), R=[xtok, w_], W=[pby])
                op("pe", lambda e, h=h, cp_=cp_, pby=pby, po=po, col=col: e.matmul(
                    pby.ap[po:po + 64, col:col + Q], sTb.ap[:, h, :], cp_.ap[:, 0:Q],
                    start=False, stop=True), R=[sTb, cp_], W=[pby])
                if h % 2 == 1:
                    op("dve", lambda e, c=c, pby=pby, col=col: e.scalar_tensor_tensor(
                        out=yT.ap[:, c, t0:t0 + Q], in0=xbcT.ap[:, c, t0:t0 + Q], scalar=ssd_dexp[:, c:c + 1],
                        in1=pby.ap[:, col:col + Q], op0=ALU.mult, op1=ALU.add), R=[xbcT, vec, pby], W=[yT])
                op("dve", lambda e, hh=hh, h=h, xw_=xw_: e.tensor_scalar(
                    out=xw_.ap[0:Q, :], in0=xtok.ap[0:Q, h * 64:(h + 1) * 64], scalar1=E4.ap[0:Q, hh, Q - 1:Q],
                    scalar2=dt.ap[0:Q, h:h + 1], op0=ALU.mult, op1=ALU.mult), R=[xtok, E4, dt], W=[xw_])
                pst = banks[2 + (h // 8) % 2]
                scol = (h % 8) * 64
                op("pe", lambda e, xw_=xw_, pst=pst, scol=scol: e.matmul(
                    pst.ap[:, scol:scol + 64], btok.ap[0:Q, g * 128:(g + 1) * 128], xw_.ap[0:Q, :],
                    start=True, stop=True), R=[btok, xw_], W=[pst])
                op("dve", lambda e, hh=hh, h=h, pst=pst, scol=scol: e.scalar_tensor_tensor(
                    out=sT.ap[:, h, :], in0=sT.ap[:, h, :], scalar=c4.ap[:, hh, Q - 1:Q], in1=pst.ap[:, scol:scol + 64],
                    op0=ALU.mult, op1=ALU.add), R=[sT, c4, pst], W=[sT])
        P.barrier()
        A.reset(m)

    def ssd_mixer(N, samp, first, last, hmT):
        m = A.mark()
        xbcT = A.bf16(48, NB)
        yT = xbcT
        dtw = A.bf16(KC, 64)
        m1 = A.mark()
        dst_ = A.f32(KC, 64)
        dma(dst_.ap, i_ssdwdt, W=[dst_])
        op("pool", lambda e: e.tensor_copy(out=dtw.ap, in_=dst_.ap), R=[dst_], W=[dtw])
        ws = WStream([i_ssdwin[32 + j] for j in range(48)], (KC, 128), ceng=("pool", "act"))
        xe = [A.f32(NB + 3), A.f32(NB + 3)]
        xcv = [A.f32(NB), A.f32(NB)]
        if samp:
            st_c = A.f32(48, 3, NS)
            for j in range(48):
                dma(st_c.ap[:, j], i_ssdconv[j * 128:(j + 1) * 128], W=[st_c])
            nbuf = A.f32(48, 3, NS)
        for j in range(48):
            wb = ws.get()
            pb = banks[j % 4]
            xe_, xc_ = xe[j % 2], xcv[j % 2]
            for kc in range(KC):
                op("pe", lambda e, kc=kc: e.matmul(pb.ap[:, 0:N], wb.ap[:, kc, :], hmT.ap[:, kc, 0:N],
                                                    start=(kc == 0), stop=(kc == KC - 1)), R=[wb, hmT], W=[pb])
            if not samp:
                op("pool", lambda e: e.tensor_copy(out=xe_.ap[:, 0:3], in_=ssd_tail.ap[:, j, :]),
                   R=[ssd_tail], W=[xe_])
            op("act", lambda e: e.copy(out=xe_.ap[:, 3:3 + N], in_=pb.ap[:, 0:N]), R=[pb], W=[xe_])
            if samp:
                taps = [st_c.ap[:, j, 0, :], st_c.ap[:, j, 1, :], st_c.ap[:, j, 2, :], xe_.ap[:, 3:3 + N]]
                Rb = [xe_, st_c, ssdcw, vec]
            else:
                taps = [xe_.ap[:, k:k + N] for k in range(4)]
                Rb = [xe_, ssdcw, vec]
            conv4(xc_.ap[:, 0:N], taps, lambda k: ssdcw.ap[:, j, k:k + 1], ssd_bconv[:, j:j + 1], N, Rb, [xc_])
            op("act", lambda e: e.activation(out=xbcT.ap[:, j, 0:N], in_=xc_.ap[:, 0:N], func=AF.Silu),
               R=[xc_], W=[xbcT])
            if samp:
                for k in range(2):
                    op("pool", lambda e, k=k: e.tensor_copy(out=nbuf.ap[:, j, k, :], in_=st_c.ap[:, j, k + 1, :]),
                       R=[st_c], W=[nbuf])
                op("pool", lambda e: e.tensor_copy(out=nbuf.ap[:, j, 2, :], in_=xe_.ap[:, 3:3 + N]),
                   R=[xe_], W=[nbuf])
            else:
                op("pool", lambda e: e.tensor_copy(out=ssd_tail.ap[:, j, :], in_=xe_.ap[:, N:N + 3]),
                   R=[xe_], W=[ssd_tail])
        if samp:
            dma(o_ssdconv_s, nbuf.ap, R=[nbuf])
        elif last:
            dma(o_ssdconv_p, ssd_tail.ap, R=[ssd_tail])
        P.barrier()
        A.reset(m1)
        sT = A.f32(64, 64)
        if samp:
            for b in range(NS):
                dma(sT.ap, i_ssds[b], W=[sT])
                ssd_chunk(1, b, sT, hmT, xbcT, yT, dtw)
                dma(o_ssd_s[b], sT.ap, R=[sT])
        else:
            if first:
                op("pool", lambda e: e.memset(sT.ap, 0.0), W=[sT])
            else:
                dma(sT.ap, o_ssd_p, R=[sT_dram], W=[sT])
            for q in range(N // 128):
                ssd_chunk(128, q * 128, sT, hmT, xbcT, yT, dtw)
            dma(o_ssd_p, sT.ap, R=[sT], W=[sT_dram])
        P.barrier()
        A.reset(m1)
        wz = WStream([i_ssdwin[j] for j in range(32)], (KC, 128), ceng=("pool", "act"))
        sz = [A.f32(NB), A.f32(NB)]
        sq = [A.f32(NB), A.f32(NB)]
        pst = banks[7]
        for j in range(32):
            wb = wz.get()
            pb = banks[j % 4]
            for kc in range(KC):
                op("pe", lambda e, kc=kc: e.matmul(pb.ap[:, 0:N], wb.ap[:, kc, :], hmT.ap[:, kc, 0:N],
                                                    start=(kc == 0), stop=(kc == KC - 1)), R=[wb, hmT], W=[pb])
            s_, q_ = sz[j % 2], sq[j % 2]
            op("act", lambda e: e.activation(out=s_.ap[:, 0:N], in_=pb.ap[:, 0:N], func=AF.Silu), R=[pb], W=[s_])
            op("dve", lambda e: e.tensor_tensor(out=yT.ap[:, j, 0:N], in0=yT.ap[:, j, 0:N], in1=s_.ap[:, 0:N],
                                                op=ALU.mult), R=[yT, s_], W=[yT])
            op("act", lambda e: e.activation(out=q_.ap[:, 0:N], in_=yT.ap[:, j, 0:N], func=AF.Square),
               R=[yT], W=[q_])
            op("pe", lambda e: e.matmul(pst.ap[:, 0:N], ones, q_.ap[:, 0:N], start=(j == 0), stop=(j == 31)),
               R=[q_, cst], W=[pst])
        P.barrier()
        A.reset(m1)
        rstd = A.f32(NB)
        op("act", lambda e: e.activation(out=rstd.ap[:, 0:N], in_=pst.ap[:, 0:N], func=AF.Sqrt, scale=1.0 / 4096,
                                         bias=epsb.ap[:, 0:1]), R=[pst, epsb], W=[rstd])
        op("dve", lambda e: e.reciprocal(out=rstd.ap[:, 0:N], in_=rstd.ap[:, 0:N]), R=[rstd], W=[rstd])
        for j in range(32):
            op("dve", lambda e: e.scalar_tensor_tensor(out=yT.ap[:, j, 0:N], in0=yT.ap[:, j, 0:N],
                                                       scalar=ssd_ng[:, j:j + 1], in1=rstd.ap[:, 0:N],
                                                       op0=ALU.mult, op1=ALU.mult), R=[yT, vec, rstd], W=[yT])
        wo = WStream([i_ssdwout[j] for j in range(16)], (32, 128), ceng=("pool", "act"), nstage=2, nbf=2)
        tmpb = [A.f32(NB), A.f32(NB)]
        for j in range(16):
            wb = wo.get()
            pb = banks[j % 4]
            for kc in range(32):
                op("pe", lambda e, kc=kc: e.matmul(pb.ap[:, 0:N], wb.ap[:, kc, :], yT.ap[:, kc, 0:N],
                                                    start=(kc == 0), stop=(kc == 31)), R=[wb, yT], W=[pb])
            resid_add(j, pb, N, None, modv(1, 2, j, samp), samp, tmpb[j % 2])
        P.barrier()
        A.reset(m)

    def peer(l, N, samp, hcT):
        m = A.mark()
        NG = N // 16
        s12 = A.f32(NB // 16, 256)
        thr = A.f32(NB // 16)
        negm = A.f32(NB // 16)
        Sel = A.bf16(NB // 16, 16)
        m1 = A.mark()
        qT = A.f32(2, NB, 8)
        wq = WStream([i_wq[l, j] for j in range(16)], (KC, 128), ceng=("pool", "act"))
        for j in range(16):
            wb = wq.get()
            pb = banks[j % 4]
            for kc in range(KC):
                op("pe", lambda e, kc=kc: e.matmul(pb.ap[:, 0:N], wb.ap[:, kc, :], hcT.ap[:, kc, 0:N],
                                                    start=(kc == 0), stop=(kc == KC - 1)), R=[wb, hcT], W=[pb])
            op("act", lambda e: e.copy(out=qT.ap[:, j % 2, 0:N, j // 2], in_=pb.ap[:, 0:N]), R=[pb], W=[qT])
        vv = [A.f32(2, 16) for _ in range(2)]
        tmp1 = [A.f32(128) for _ in range(2)]
        cand = [A.f32(256) for _ in range(2)]
        cand2 = [A.f32(256) for _ in range(2)]
        c24 = [A.f32(24) for _ in range(2)]
        ez = [A.f32(16) for _ in range(2)]
        zz = [A.f32(2) for _ in range(2)]
        for gi in range(NG):
            pb = banks[4 + gi % 2]
            for half in range(2):
                lhsT = qT.ap[:, half, gi * 16:(gi + 1) * 16, :].rearrange("p t h -> p (t h)")
                op("pe", lambda e, half=half, lhsT=lhsT: e.matmul(pb.ap[:, half * 128:(half + 1) * 128], lhsT,
                                                                  kT.ap[:, l * 2 + half, :], start=True, stop=True),
                   R=[qT, kT], W=[pb])
            op("act", lambda e: e.copy(out=s12.ap[:, gi, :], in_=pb.ap[:, 0:256]), R=[pb], W=[s12])
            v_, t1, cd, cd2, c_, ez_, z_ = (vv[gi % 2], tmp1[gi % 2], cand[gi % 2], cand2[gi % 2], c24[gi % 2],
                                            ez[gi % 2], zz[gi % 2])
            for half in range(2):
                w = s12.ap[:, gi, half * 128:(half + 1) * 128]
                op("dve", lambda e: e.max(out=v_.ap[:, half, 0:8], in_=w), R=[s12], W=[v_])
                op("dve", lambda e: e.match_replace(out=t1.ap, in_to_replace=v_.ap[:, half, 0:8], in_values=w,
                                                    imm_value=-1e30), R=[s12, v_], W=[t1])
                op("dve", lambda e: e.max(out=v_.ap[:, half, 8:16], in_=t1.ap), R=[t1], W=[v_])
            cd3 = cd.ap.rearrange("p (a b) -> p a b", b=16)
            op("dve", lambda e: e.tensor_tensor(out=cd3, in0=bc(v_.ap[:, 0, :], 2, [128, 16, 16]),
                                                in1=bc(v_.ap[:, 1, :], 1, [128, 16, 16]), op=ALU.add),
               R=[v_], W=[cd])
            op("dve", lambda e: e.max(out=c_.ap[:, 0:8], in_=cd.ap), R=[cd], W=[c_])
            op("dve", lambda e: e.match_replace(out=cd2.ap, in_to_replace=c_.ap[:, 0:8], in_values=cd.ap,
                                                imm_value=-1e30), R=[cd, c_], W=[cd2])
            op("dve", lambda e: e.max(out=c_.ap[:, 8:16], in_=cd2.ap), R=[cd2], W=[c_])
            op("dve", lambda e: e.match_replace(out=cd.ap, in_to_replace=c_.ap[:, 8:16], in_values=cd2.ap,
                                                imm_value=-1e30), R=[cd2, c_], W=[cd])
            op("dve", lambda e: e.max(out=c_.ap[:, 16:24], in_=cd.ap), R=[cd], W=[c_])
            op("dve", lambda e: e.tensor_scalar(out=thr.ap[:, gi:gi + 1], in0=c_.ap[:, 15:16],
                                                scalar1=c_.ap[:, 16:17], scalar2=0.5, op0=ALU.add, op1=ALU.mult),
               R=[c_], W=[thr])
            op("dve", lambda e: e.tensor_scalar(out=negm.ap[:, gi:gi + 1], in0=c_.ap[:, 0:1], scalar1=-1.0,
                                                scalar2=None, op0=ALU.mult), R=[c_], W=[negm])
            op("act", lambda e: e.activation(out=ez_.ap, in_=c_.ap[:, 0:16], func=AF.Exp,
                                             bias=negm.ap[:, gi:gi + 1], scale=1.0, accum_out=z_.ap[:, 0:1]),
               R=[c_, negm], W=[ez_, z_])
            op("dve", lambda e: e.reciprocal(out=z_.ap[:, 1:2], in_=z_.ap[:, 0:1]), R=[z_], W=[z_])
            op("dve", lambda e: e.tensor_scalar(out=Sel.ap[:, gi, :], in0=selm, scalar1=z_.ap[:, 1:2], scalar2=None,
                                                op0=ALU.mult), R=[z_, cst], W=[Sel])
        P.barrier()
        A.reset(m1)
        usrc, vsrc = [], []
        for a in range(128):
            usrc += [i_uT[l, a][:, 0:8, :], i_uT[l, a][:, 8:16, :]]
            vsrc += [i_v[l, a][:, 0:1024], i_v[l, a][:, 1024:2048]]
        us = WStream(usrc, (8, 128), ceng=("act",), nstage=3, nbf=2 * GA + 1)
        vs = WStream(vsrc, (1024,), ceng=("act",), nstage=3, nbf=2 * GA + 1)
        FR = 16
        LAG = 3
        NGA = 128 // GA
        NGh = min(NG, 16)
        Dd = [A.f32(GA, 128) for _ in range(3)]
        ee = [A.bf16(GA, 128) for _ in range(3)]
        Fr = [A.bf16(GA, 128) for _ in range(FR)]
        gel = [A.bf16(NB) for _ in range(GA)]
        AT = [A.bf16(NB) for _ in range(GA)]
        st = {"f": 0, "pend": [], "F": {}, "u": {}, "v": {}}

        def fgen(ag, gi):
            k = st["f"]
            st["f"] += 1
            d_, e_, f_ = Dd[k % 3], ee[k % 3], Fr[k % FR]
            a0 = ag * GA
            op("dve", lambda e: e.tensor_tensor(out=d_.ap, in0=bc(s12.ap[:, gi, 128:256], 1, [128, GA, 128]),
                                                in1=bc(s12.ap[:, gi, a0:a0 + GA], 2, [128, GA, 128]),
                                                op=ALU.add), R=[s12], W=[d_])
            op("act", lambda e: e.activation(out=e_.ap, in_=d_.ap, func=AF.Exp, bias=negm.ap[:, gi:gi + 1],
                                             scale=1.0), R=[d_, negm], W=[e_])
            op("dve", lambda e: e.scalar_tensor_tensor(out=f_.ap, in0=d_.ap, scalar=thr.ap[:, gi:gi + 1],
                                                       in1=e_.ap, op0=ALU.is_ge, op1=ALU.mult),
               R=[d_, e_, thr], W=[f_])
            st["pend"].append((gi, f_))

        def gmm_emit(lag):
            while len(st["pend"]) > lag:
                gi, f_ = st["pend"].pop(0)
                for ai in range(GA):
                    pG = banks[2 + ai]
                    op("pe", lambda e, ai=ai, pG=pG: e.matmul(pG.ap[:, gi * 16:(gi + 1) * 16], f_.ap[:, ai, :],
                                                              Sel.ap[:, gi, :], start=True, stop=True),
                       R=[f_, Sel], W=[pG])

        def ucast(ag, k):
            st["u"][(ag, k)] = us.get()

        def vcast(ag, k):
            st["v"][(ag, k)] = vs.get()

        def sTmm(ag, ai):
            pS = banks[ai % 2]
            for kc in range(KC):
                ub = st["u"][(ag, 2 * ai + kc // 8)]
                op("pe", lambda e, kc=kc, ub=ub: e.matmul(pS.ap[:, 0:N], ub.ap[:, kc % 8, :], hcT.ap[:, kc, 0:N],
                                                          start=(kc == 0), stop=(kc == KC - 1)),
                   R=[ub, hcT], W=[pS])
            g_ = gel[ai]
            op("act", lambda e: e.activation(out=g_.ap[:, 0:N], in_=pS.ap[:, 0:N], func=AF.Gelu_apprx_tanh),
               R=[pS], W=[g_])

        def aTm():
            for ai in range(GA):
                pG = banks[2 + ai]
                g_, at_ = gel[ai], AT[ai]
                op("dve", lambda e: e.tensor_tensor(out=at_.ap[:, 0:N], in0=g_.ap[:, 0:N], in1=pG.ap[:, 0:N],
                                                    op=ALU.mult), R=[g_, pG], W=[at_])

        def outp(ag, dc):
            pO = banks[6 + dc % 2]
            for ai in range(GA):
                at_ = AT[ai]
                vb = st["v"][(ag, 2 * ai + dc // 8)]
                op("pe", lambda e, ai=ai, at_=at_, vb=vb: e.matmul(
                    pO.ap[:, 0:N], vb.ap[:, (dc % 8) * 128:(dc % 8 + 1) * 128], at_.ap[:, 0:N],
                    start=(ai == 0), stop=(ai == GA - 1)), R=[vb, at_], W=[pO])
            if samp:
                t_ = Dd[dc % 2]
                tv = t_.ap.rearrange("p a b -> p (a b)")
                op("dve", lambda e: e.tensor_tensor(out=tv[:, 0:N], in0=pO.ap[:, 0:N], in1=modv(l, 5, dc, True),
                                                    op=ALU.mult), R=[pO, modT], W=[t_])
                op("dve", lambda e: e.tensor_tensor(out=xT.ap[:, dc, 0:N], in0=xT.ap[:, dc, 0:N], in1=tv[:, 0:N],
                                                    op=ALU.add), R=[t_, xT], W=[xT])
            else:
                op("dve", lambda e: e.scalar_tensor_tensor(out=xT.ap[:, dc, 0:N], in0=pO.ap[:, 0:N],
                                                           scalar=modv(l, 5, dc, False), in1=xT.ap[:, dc, 0:N],
                                                           op0=ALU.mult, op1=ALU.add), R=[pO, modT, xT], W=[xT])

        def phaseY(ag):
            for ai in range(GA):
                sTmm(ag, ai)
                vcast(ag, 2 * ai)
                vcast(ag, 2 * ai + 1)
                for k in range(4):
                    gi = NGh + 4 * ai + k
                    if gi < NG:
                        fgen(ag, gi)
                        gmm_emit(LAG)
            gmm_emit(0)
            aTm()

        for k in range(2 * GA):
            ucast(0, k)
        for gi in range(NGh):
            fgen(0, gi)
            gmm_emit(LAG)
        gmm_emit(0)
        phaseY(0)
        for ag in range(NGA):
            nxt = ag + 1 < NGA
            for dc in range(16):
                outp(ag, dc)
                if nxt:
                    if dc % 2 == 0:
                        ucast(ag + 1, dc // 2)
                    if dc < NGh:
                        fgen(ag + 1, dc)
                    gmm_emit(LAG)
            if nxt:
                gmm_emit(0)
                phaseY(ag + 1)
        P.barrier()
        A.reset(m)

    def run_block(N, samp, first, last, src, dst):
        dma(xT.ap[:, :, 0:N], src, W=[xT])
        for l in range(2):
            m = A.mark()
            hmT = A.bf16(KC, NB)
            norm_mod(l, 0, N, samp, hmT)
            if l == 0:
                rg_mixer(N, samp, last, hmT)
            else:
                ssd_mixer(N, samp, first, last, hmT)
            norm_mod(l, 1, N, samp, hmT)
            peer(l, N, samp, hmT)
            A.reset(m)
        m = A.mark()
        scr = [A.f32(NB), A.f32(NB)]
        rstd = A.f32(NB)
        yo = A.f32(KC, NB)
        rms_stats(lambda c: xT.ap[:, c, 0:N], [xT], N, KC, scr, banks[7], rstd)
        for c in range(KC):
            op("dve", lambda e: e.scalar_tensor_tensor(out=yo.ap[:, c, 0:N], in0=xT.ap[:, c, 0:N],
                                                       scalar=fing[:, c:c + 1], in1=rstd.ap[:, 0:N], op0=ALU.mult,
                                                       op1=ALU.mult), R=[xT, vec, rstd], W=[yo])
        dma(dst, yo.ap[:, :, 0:N], R=[yo])
        P.barrier()
        A.reset(m)

    for blk in range(nblk):
        run_block(NB, False, blk == 0, blk == nblk - 1,
                  i_xT[:, blk * NB:(blk + 1) * NB].rearrange("(c p) t -> p c t", p=128),
                  o_yT[:, blk * NB:(blk + 1) * NB].rearrange("(c p) t -> p c t", p=128))
    run_block(NS, True, True, False, i_xsT.rearrange("(c p) t -> p c t", p=128),
              o_ysT.rearrange("(c p) t -> p c t", p=128))
    P.finish()
    es.close()
    nc._arena_hw = A.hw
    return nc


def _wlay(w):
    K, N = w.shape
    return np.ascontiguousarray(w.reshape(K // 128, 128, N // 128, 128).transpose(2, 1, 0, 3))


def _vlay(v):
    return v.reshape(-1, 128).T


_CACHE = {}


def _prep_shared(inp):
    f = np.float32
    sh = {}
    cst = np.zeros((128, 4 * 128 + 16), f)
    cst[:, 0:128] = np.eye(128)
    cst[:, 128:256] = 1.0
    k = np.arange(128)
    cst[:, 256:384] = (k[:, None] <= k[None, :])
    cst[:, 384:512] = np.where(k[:, None] <= k[None, :], 0.0, NEG)
    cst[:, 512:528] = (k[:, None] // 8 == np.arange(16)[None, :])
    sh["cst"] = cst
    vec = np.zeros((128, 16 * 11 + 32 * 2 + 48 + 32), f)
    vec[:, 0:16] = _vlay(inp["norm1_g"][0]); vec[:, 16:32] = _vlay(inp["norm1_g"][1])
    vec[:, 32:48] = _vlay(inp["norm2_g"][0]); vec[:, 48:64] = _vlay(inp["norm2_g"][1])
    vec[:, 64:80] = _vlay(inp["final_g"])
    vec[:, 80:96] = _vlay(inp["rg_conv_b"][0])
    vec[:, 96:112] = _vlay(inp["rg_b_a"][0])
    vec[:, 112:128] = _vlay(inp["rg_b_i"][0])
    vec[:, 128:144] = _vlay(inp["rg_lambda"][0])
    vec[:, 144:160] = _vlay(inp["rg_b_out"][0])
    vec[:, 176:208] = _vlay(inp["rg_b_in"][0])
    vec[:, 208:240] = _vlay(inp["ssd_norm_g"][0])
    vec[:, 240:288] = _vlay(inp["ssd_conv_b"][0])
    vec[:, 288:320] = _vlay(np.repeat(inp["ssd_d"][0], 64))
    sh["vec"] = vec
    sh["bmod"] = np.ascontiguousarray(np.stack([_vlay(inp["b_mod"][l]) for l in range(2)], axis=1))
    sh["rgcw"] = np.ascontiguousarray(inp["rg_conv_w"][0].T.reshape(16, 128, 4).transpose(1, 0, 2))
    sh["ssdcw"] = np.ascontiguousarray(inp["ssd_conv_w"][0].T.reshape(48, 128, 4).transpose(1, 0, 2))
    sh["ssdh"] = np.ascontiguousarray(np.stack([inp["ssd_dt_bias"][0], inp["ssd_a_log"][0],
                                                inp["ssd_d"][0]]).astype(f))
    sh["wmod"] = np.stack([_wlay(inp["w_mod"][l]) for l in range(2)])
    sh["rgwin"] = _wlay(inp["rg_w_in"][0])

    def glay(w):
        return np.ascontiguousarray(w.reshape(8, 2, 128, 2, 128).transpose(0, 3, 2, 1, 4).reshape(16, 128, 2, 128))
    sh["rgwa"] = glay(inp["rg_w_a"][0])
    sh["rgwi"] = glay(inp["rg_w_i"][0])
    sh["rgwout"] = _wlay(inp["rg_w_out"][0])
    w = inp["ssd_w_in"][0]
    sh["ssdwin"] = _wlay(np.ascontiguousarray(w[:, :10240]))
    sh["ssdwdt"] = np.ascontiguousarray(w[:, 10240:].reshape(16, 128, 64).transpose(1, 0, 2))
    sh["ssdwout"] = _wlay(inp["ssd_w_out"][0])
    sh["wq"] = np.stack([_wlay(inp["peer_w_q"][l]) for l in range(2)])
    sh["kT"] = np.ascontiguousarray(np.stack([np.stack([inp["peer_k1"][l].T, inp["peer_k2"][l].T])
                                              for l in range(2)]))
    sh["uT"] = np.stack([np.ascontiguousarray(inp["peer_u"][l].reshape(128, 128, 16, 128).transpose(0, 3, 2, 1))
                         for l in range(2)])
    sh["v"] = np.ascontiguousarray(inp["peer_v"].reshape(2, 128, 128, 2048))
    return sh


def kernel(**inp):
    inp = {k: np.asarray(v) for k, v in inp.items()}
    TP = inp["x_prompt"].shape[1]
    nc = build(TP)
    sh = _prep_shared(inp)
    in_maps = []
    for c in range(8):
        s = c % 4
        sl = slice(c * NS, (c + 1) * NS)
        d = dict(sh)
        d["xT"] = np.ascontiguousarray(inp["x_prompt"][s].T)
        d["xsT"] = np.ascontiguousarray(inp["x_sample"][sl, 0, :].T)
        d["cT"] = np.ascontiguousarray(np.concatenate([inp["c_prompt"][s][None], inp["c_sample"][sl]], 0).T)
        d["rgconv"] = np.ascontiguousarray(inp["state_rg_conv"][0, sl].transpose(2, 1, 0))
        d["rgh"] = np.ascontiguousarray(inp["state_rg_h"][0, sl].T)
        d["ssdconv"] = np.ascontiguousarray(inp["state_ssd_conv"][0, sl].transpose(2, 1, 0))
        d["ssds"] = np.ascontiguousarray(inp["state_ssd"][0, sl].reshape(NS, 64, 64, 128).transpose(0, 3, 1, 2))
        in_maps.append(d)
    res = run_bass_kernel_spmd(nc, in_maps, core_ids=list(range(8))).results
    f = np.float32
    y_p = np.stack([res[s]["o_yT"].T for s in range(4)]).astype(f)
    y_s = np.concatenate([res[c]["o_ysT"].T for c in range(8)], 0)[:, None, :].astype(f)

    def unv(a):
        return a.transpose(1, 0, *range(2, a.ndim)).reshape(-1, *a.shape[2:])
    rg_conv_p = np.stack([unv(res[s]["o_rgconv_p"]).T for s in range(4)])[None].astype(f)
    rg_h_p = np.stack([unv(res[s]["o_rgh_p"]) for s in range(4)])[None].astype(f)
    ssd_conv_p = np.stack([unv(res[s]["o_ssdconv_p"]).T for s in range(4)])[None].astype(f)
    ssd_p = np.stack([res[s]["o_ssd_p"].transpose(1, 2, 0).reshape(8, 8, 64, 128) for s in range(4)])[None].astype(f)
    rg_conv_s = np.concatenate([unv(res[c]["o_rgconv_s"]).transpose(2, 1, 0) for c in range(8)], 0)[None].astype(f)
    rg_h_s = np.concatenate([unv(res[c]["o_rgh_s"]).T for c in range(8)], 0)[None].astype(f)
    ssd_conv_s = np.concatenate([unv(res[c]["o_ssdconv_s"]).transpose(2, 1, 0) for c in range(8)], 0)[None].astype(f)
    ssd_s = np.concatenate([res[c]["o_ssd_s"].transpose(0, 2, 3, 1).reshape(NS, 8, 8, 64, 128)
                            for c in range(8)], 0)[None].astype(f)
    return (y_p, y_s, rg_conv_p, rg_h_p, ssd_conv_p, ssd_p, rg_conv_s, rg_h_s, ssd_conv_s, ssd_s)
```

```python
import contextlib
import numpy as np
import concourse.bass as bass
import concourse.mybir as mybir
from concourse.bass_utils import run_bass_kernel_spmd

F32 = mybir.dt.float32
BF16 = mybir.dt.bfloat16
AF = mybir.ActivationFunctionType
ALU = mybir.AluOpType

D = 2048
KC = 16
NS = 16
NB = 512
EPS = 1e-6
NDS = 12
GA = 4
NEG = -30000.0


class Trk:
    __slots__ = ("w", "r")

    def __init__(self):
        self.w = None
        self.r = {}


class Buf:
    def __init__(self, ap):
        self.ap = ap
        self.t = Trk()


class Prog:
    def __init__(self, nc, es):
        self.nc = nc
        self.eng = {"pe": nc.tensor, "dve": nc.vector, "act": nc.scalar, "pool": nc.gpsimd, "sp": nc.sync}
        self.sem = {k: es.enter_context(nc.semaphore("s_" + k)) for k in ("pe", "dve", "act", "pool")}
        self.cnt = {k: 0 for k in self.sem}
        self.seen = {e: {k: 0 for k in self.sem} for e in self.eng}
        self.dsem = [es.enter_context(nc.semaphore("d%d" % i)) for i in range(NDS)]
        self.dcnt = [0] * NDS
        self.dn = 0
        self.dseen = {e: [0] * NDS for e in self.eng}

    def _need(self, e, tok):
        if tok is None:
            return
        if tok[0] == "c":
            _, f, n = tok
            if f == e and e == "pe":
                return
            if self.seen[e][f] < n:
                self.eng[e].wait_ge(self.sem[f], n)
                self.seen[e][f] = n
        else:
            _, i, v = tok
            if self.dseen[e][i] < v:
                self.eng[e].wait_ge(self.dsem[i], v)
                self.dseen[e][i] = v

    def _sync(self, e, R, W):
        for b in R:
            self._need(e, b.t.w)
        for b in W:
            self._need(e, b.t.w)
            for tok in b.t.r.values():
                self._need(e, tok)

    def op(self, e, fn, R=(), W=()):
        self._sync(e, R, W)
        ins = fn(self.eng[e])
        self.cnt[e] += 1
        ins.then_inc(self.sem[e], 1)
        tok = ("c", e, self.cnt[e])
        for b in R:
            b.t.r[e] = tok
        for b in W:
            b.t.w = tok
            b.t.r = {}
        return ins

    def dma(self, out, in_, R=(), W=(), q="sp"):
        i = self.dn % NDS
        self.dn += 1
        self._need(q, ("d", i, self.dcnt[i]))
        self._sync(q, R, W)
        ins = self.eng[q].dma_start(out=out, in_=in_)
        self.dcnt[i] += 16
        ins.then_inc(self.dsem[i], 16)
        tok = ("d", i, self.dcnt[i])
        for b in R:
            b.t.r["dma%d" % i] = tok
        for b in W:
            b.t.w = tok
            b.t.r = {}

    def barrier(self):
        for e in self.eng:
            for f in self.sem:
                self._need(e, ("c", f, self.cnt[f]))
            for i in range(NDS):
                self._need(e, ("d", i, self.dcnt[i]))

    def finish(self):
        for i in range(NDS):
            self._need("sp", ("d", i, self.dcnt[i]))
        for f in self.sem:
            self._need("sp", ("c", f, self.cnt[f]))


class Arena:
    def __init__(self, ap, width):
        self.ap = ap
        self.width = width
        self.off = 0
        self.hw = 0

    def mark(self):
        return self.off

    def reset(self, m):
        self.off = m

    def f32(self, *shape):
        n = int(np.prod(shape))
        assert self.off + n <= self.width, ("arena overflow", self.off, n, self.width)
        v = self.ap[:, self.off:self.off + n]
        self.off += n
        self.hw = max(self.hw, self.off)
        return Buf(_shape(v, shape))

    def bf16(self, *shape):
        n = int(np.prod(shape))
        w = (n + 1) // 2
        assert self.off + w <= self.width, ("arena overflow", self.off, w, self.width)
        v = self.ap[:, self.off:self.off + w].bitcast(BF16)[:, 0:n]
        self.off += w
        self.hw = max(self.hw, self.off)
        return Buf(_shape(v, shape))


def _shape(v, shape):
    if len(shape) == 1:
        return v
    if len(shape) == 2:
        return v.rearrange("p (a b) -> p a b", b=shape[1])
    if len(shape) == 3:
        return v.rearrange("p (a b c) -> p a b c", b=shape[1], c=shape[2])
    raise ValueError(shape)


def bc(ap, axis, shape):
    return ap.unsqueeze(axis).to_broadcast(list(shape))


def build(TP):
    nblk = TP // NB
    nc = bass.Bass("TRN2", target_bir_lowering=False)
    es = contextlib.ExitStack()

    def din(name, shape):
        return nc.dram_tensor(name, list(shape), F32, kind="ExternalInput").ap()

    def dout(name, shape):
        return nc.dram_tensor(name, list(shape), F32, kind="ExternalOutput").ap()

    i_xT = din("xT", [D, TP])
    i_xsT = din("xsT", [D, NS])
    i_cT = din("cT", [D, 1 + NS])
    i_rgconv = din("rgconv", [D, 3, NS])
    i_rgh = din("rgh", [D, NS])
    i_ssdconv = din("ssdconv", [6144, 3, NS])
    i_ssds = din("ssds", [NS, 128, 64, 64])
    i_cst = din("cst", [128, 4 * 128 + 16])
    i_vec = din("vec", [128, 16 * 11 + 32 * 2 + 48 + 32])
    i_bmod = din("bmod", [128, 2, 96])
    i_rgcw = din("rgcw", [128, 16, 4])
    i_ssdcw = din("ssdcw", [128, 48, 4])
    i_ssdh = din("ssdh", [3, 64])
    i_wmod = din("wmod", [2, 96, 128, 16, 128])
    i_rgwin = din("rgwin", [32, 128, 16, 128])
    i_rgwa = din("rgwa", [16, 128, 2, 128])
    i_rgwi = din("rgwi", [16, 128, 2, 128])
    i_rgwout = din("rgwout", [16, 128, 16, 128])
    i_ssdwin = din("ssdwin", [80, 128, 16, 128])
    i_ssdwdt = din("ssdwdt", [128, 16, 64])
    i_ssdwout = din("ssdwout", [16, 128, 32, 128])
    i_wq = din("wq", [2, 16, 128, 16, 128])
    i_kT = din("kT", [2, 2, 128, 128])
    i_uT = din("uT", [2, 128, 128, 16, 128])
    i_v = din("v", [2, 128, 128, 2048])
    o_yT = dout("o_yT", [D, TP])
    o_ysT = dout("o_ysT", [D, NS])
    o_rgconv_p = dout("o_rgconv_p", [128, 16, 3])
    o_rgh_p = dout("o_rgh_p", [128, 16])
    o_ssdconv_p = dout("o_ssdconv_p", [128, 48, 3])
    o_ssd_p = dout("o_ssd_p", [128, 64, 64])
    o_rgconv_s = dout("o_rgconv_s", [128, 16, 3, NS])
    o_rgh_s = dout("o_rgh_s", [128, 16, NS])
    o_ssdconv_s = dout("o_ssdconv_s", [128, 48, 3, NS])
    o_ssd_s = dout("o_ssd_s", [NS, 128, 64, 64])

    AW = 51800
    arena_t = es.enter_context(nc.sbuf_tensor("arena", [128, AW], F32))
    A = Arena(arena_t[:, :], AW)
    banks = [Buf(es.enter_context(nc.psum_tensor("pb%d" % i, [128, 512], F32))[:, :]) for i in range(8)]
    P = Prog(nc, es)
    op, dma = P.op, P.dma

    cst = A.f32(4 * 128 + 16)
    ident = cst.ap[:, 0:128]
    ones = cst.ap[:, 128:256]
    tri = cst.ap[:, 256:384]
    mneg = cst.ap[:, 384:512]
    selm = cst.ap[:, 512:528]
    dma(cst.ap, i_cst, W=[cst])
    identb = A.bf16(128)
    op("dve", lambda e: e.tensor_copy(out=identb.ap, in_=ident), R=[cst], W=[identb])
    vec = A.f32(16 * 11 + 32 * 2 + 48 + 32)
    dma(vec.ap, i_vec, W=[vec])

    def vsl(o, n):
        return vec.ap[:, o:o + n]
    n1g = [vsl(0, 16), vsl(16, 16)]
    n2g = [vsl(32, 16), vsl(48, 16)]
    fing = vsl(64, 16)
    rg_bconv = vsl(80, 16)
    rg_ba = vsl(96, 16)
    rg_bi = vsl(112, 16)
    rg_lam = vsl(128, 16)
    rg_bout = vsl(144, 16)
    rg_bin = vsl(176, 32)
    ssd_ng = vsl(208, 32)
    ssd_bconv = vsl(240, 48)
    ssd_dexp = vsl(288, 32)
    rgcw = A.f32(16, 4)
    dma(rgcw.ap, i_rgcw, W=[rgcw])
    ssdcw = A.f32(48, 4)
    dma(ssdcw.ap, i_ssdcw, W=[ssdcw])
    ssdh = A.f32(3, 64)
    dma(ssdh.ap, i_ssdh.partition_broadcast(128), W=[ssdh])
    Abc = A.f32(64)
    op("act", lambda e: e.activation(out=Abc.ap, in_=ssdh.ap[:, 1, :], func=AF.Exp), R=[ssdh], W=[Abc])
    op("dve", lambda e: e.tensor_scalar(out=Abc.ap, in0=Abc.ap, scalar1=-1.0, scalar2=None, op0=ALU.mult),
       R=[Abc], W=[Abc])
    c8 = A.f32(16)
    op("act", lambda e: e.activation(out=c8.ap, in_=rg_lam, func=AF.Exp, scale=-1.0), R=[vec], W=[c8])
    op("act", lambda e: e.activation(out=c8.ap, in_=c8.ap, func=AF.Ln, bias=1.0), R=[c8], W=[c8])
    op("dve", lambda e: e.tensor_scalar(out=c8.ap, in0=c8.ap, scalar1=-8.0, scalar2=None, op0=ALU.mult),
       R=[c8], W=[c8])
    epsb = A.f32(1)
    op("pool", lambda e: e.memset(epsb.ap, EPS), W=[epsb])
    kT = A.f32(4, 128)
    for l_ in range(2):
        for h_ in range(2):
            dma(kT.ap[:, l_ * 2 + h_, :], i_kT[l_, h_], W=[kT])

    modT = A.f32(2, 96, 1 + NS)
    csT = A.f32(KC, 1 + NS)
    for kc_ in range(KC):
        dma(csT.ap[:, kc_, :], i_cT[kc_ * 128:(kc_ + 1) * 128, :], W=[csT])
    op("act", lambda e: e.activation(out=csT.ap, in_=csT.ap, func=AF.Silu), R=[csT], W=[csT])
    bmod = A.f32(2, 96)
    dma(bmod.ap, i_bmod, W=[bmod])
    m0 = A.mark()
    NST = 4
    stg = [A.f32(KC, 128) for _ in range(NST)]
    jobs = [(l, j) for l in range(2) for j in range(96)]
    for idx in range(min(NST - 1, len(jobs))):
        l, j = jobs[idx]
        dma(stg[idx % NST].ap, i_wmod[l, j], W=[stg[idx % NST]])
    for idx, (l, j) in enumerate(jobs):
        nx = idx + NST - 1
        if nx < len(jobs):
            dma(stg[nx % NST].ap, i_wmod[jobs[nx][0], jobs[nx][1]], W=[stg[nx % NST]])
        s = stg[idx % NST]
        pb = banks[idx % 2]
        for kc in range(KC):
            op("pe", lambda e, kc=kc: e.matmul(pb.ap[:, 0:1 + NS], s.ap[:, kc, :], csT.ap[:, kc, :],
                                                start=(kc == 0), stop=(kc == KC - 1)), R=[s, csT], W=[pb])
        op("dve", lambda e: e.tensor_scalar(out=modT.ap[:, l, j, :], in0=pb.ap[:, 0:1 + NS],
                                            scalar1=bmod.ap[:, l, j:j + 1], scalar2=None, op0=ALU.add),
           R=[pb, bmod], W=[modT])
    A.reset(m0)
    P.barrier()
    gmp = A.f32(2, 2, 16)
    gms = A.f32(4, 16, NS)
    for l in range(2):
        for k, (sc0, ng) in enumerate(((16, n1g[l]), (64, n2g[l]))):
            op("dve", lambda e: e.scalar_tensor_tensor(out=gmp.ap[:, l, k, :], in0=modT.ap[:, l, sc0:sc0 + 16, 0],
                                                       scalar=1.0, in1=ng, op0=ALU.add, op1=ALU.mult),
               R=[modT, vec], W=[gmp])
            op("dve", lambda e: e.scalar_tensor_tensor(out=gms.ap[:, l * 2 + k, :, :],
                                                       in0=modT.ap[:, l, sc0:sc0 + 16, 1:1 + NS], scalar=1.0,
                                                       in1=bc(ng, 2, [128, 16, NS]), op0=ALU.add, op1=ALU.mult),
               R=[modT, vec], W=[gms])

    def modv(l, k, c, samp):
        if samp:
            return modT.ap[:, l, 16 * k + c, 1:1 + NS]
        return modT.ap[:, l, 16 * k + c, 0:1]

    xT = A.f32(KC, NB)
    rg_tail = A.f32(16, 3)
    rg_carry = A.f32(16)
    ssd_tail = A.f32(48, 3)
    for b_ in (rg_tail, rg_carry, ssd_tail):
        op("pool", lambda e, b_=b_: e.memset(b_.ap, 0.0), W=[b_])
    sT_dram = Buf(o_ssd_p)
    pmark = A.mark()

    class WStream:
        def __init__(self, srcs, shape, ceng=("pool",), nstage=3, nbf=3):
            self.srcs = srcs
            self.shape = shape
            self.stage = [A.f32(*shape) for _ in range(nstage)]
            self.bfs = [A.bf16(*shape) for _ in range(nbf)]
            self.ceng = ceng
            self.nl = 0
            self.ncast = 0
            self.pf = nstage - 1
            for _ in range(min(self.pf, len(srcs))):
                self._load()

        def _load(self):
            if self.nl < len(self.srcs):
                s = self.stage[self.nl % len(self.stage)]
                dma(s.ap, self.srcs[self.nl], W=[s])
                self.nl += 1

        def get(self):
            i = self.ncast
            self._load()
            s = self.stage[i % len(self.stage)]
            b = self.bfs[i % len(self.bfs)]
            e_ = self.ceng[i % len(self.ceng)]
            if e_ == "act":
                op("act", lambda e: e.copy(out=b.ap, in_=s.ap), R=[s], W=[b])
            else:
                op(e_, lambda e: e.tensor_copy(out=b.ap, in_=s.ap), R=[s], W=[b])
            self.ncast += 1
            return b

    def rms_stats(src_chunks, srcbufs, N, nch, scratch, pb, rstd, eng_sq="act"):
        for c in range(nch):
            sq = scratch[c % len(scratch)]
            op("act", lambda e, c=c, sq=sq: e.activation(out=sq.ap[:, 0:N], in_=src_chunks(c), func=AF.Square),
               R=srcbufs, W=[sq])
            op("pe", lambda e, c=c, sq=sq: e.matmul(pb.ap[:, 0:N], ones, sq.ap[:, 0:N], start=(c == 0),
                                                    stop=(c == nch - 1)), R=[sq, cst], W=[pb])
        op("act", lambda e: e.activation(out=rstd.ap[:, 0:N], in_=pb.ap[:, 0:N], func=AF.Sqrt,
                                         scale=1.0 / (nch * 128), bias=epsb.ap[:, 0:1]), R=[pb, epsb], W=[rstd])
        op("dve", lambda e: e.reciprocal(out=rstd.ap[:, 0:N], in_=rstd.ap[:, 0:N]), R=[rstd], W=[rstd])

    def norm_mod(l, which, N, samp, dst):
        m = A.mark()
        scr = [A.f32(NB), A.f32(NB)]
        rstd = A.f32(NB)
        tmp = [A.f32(NB), A.f32(NB)]
        rms_stats(lambda c: xT.ap[:, c, 0:N], [xT], N, KC, scr, banks[7], rstd)
        for c in range(KC):
            t = tmp[c % 2]
            if samp:
                gm = gms.ap[:, l * 2 + which, c, :]
                sh = modv(l, 3 * which, c, True)
                op("dve", lambda e: e.tensor_tensor(out=t.ap[:, 0:N], in0=xT.ap[:, c, 0:N], in1=rstd.ap[:, 0:N],
                                                    op=ALU.mult), R=[xT, rstd], W=[t])
                op("dve", lambda e: e.tensor_tensor(out=t.ap[:, 0:N], in0=t.ap[:, 0:N], in1=gm, op=ALU.mult),
                   R=[t, gms], W=[t])
                op("dve", lambda e: e.tensor_tensor(out=dst.ap[:, c, 0:N], in0=t.ap[:, 0:N], in1=sh, op=ALU.add),
                   R=[t, modT], W=[dst])
            else:
                gm = gmp.ap[:, l, which, c:c + 1]
                sh = modv(l, 3 * which, c, False)
                op("dve", lambda e: e.scalar_tensor_tensor(out=t.ap[:, 0:N], in0=xT.ap[:, c, 0:N], scalar=gm,
                                                           in1=rstd.ap[:, 0:N], op0=ALU.mult, op1=ALU.mult),
                   R=[xT, rstd, gmp], W=[t])
                op("act", lambda e: e.activation(out=dst.ap[:, c, 0:N], in_=t.ap[:, 0:N], func=AF.Identity,
                                                 bias=sh, scale=1.0), R=[t, modT], W=[dst])
        A.reset(m)

    def resid_add(c, pb, N, bias, gate, samp, tmpb):
        if samp:
            if bias is not None:
                op("dve", lambda e: e.scalar_tensor_tensor(out=tmpb.ap[:, 0:N], in0=pb.ap[:, 0:N], scalar=bias,
                                                           in1=gate, op0=ALU.add, op1=ALU.mult),
                   R=[pb, vec, modT], W=[tmpb])
            else:
                op("dve", lambda e: e.tensor_tensor(out=tmpb.ap[:, 0:N], in0=pb.ap[:, 0:N], in1=gate, op=ALU.mult),
                   R=[pb, modT], W=[tmpb])
            op("dve", lambda e: e.tensor_tensor(out=xT.ap[:, c, 0:N], in0=xT.ap[:, c, 0:N], in1=tmpb.ap[:, 0:N],
                                                op=ALU.add), R=[tmpb, xT], W=[xT])
        else:
            if bias is not None:
                op("dve", lambda e: e.tensor_scalar(out=tmpb.ap[:, 0:N], in0=pb.ap[:, 0:N], scalar1=bias,
                                                    scalar2=gate, op0=ALU.add, op1=ALU.mult),
                   R=[pb, vec, modT], W=[tmpb])
                op("dve", lambda e: e.tensor_tensor(out=xT.ap[:, c, 0:N], in0=xT.ap[:, c, 0:N],
                                                    in1=tmpb.ap[:, 0:N], op=ALU.add), R=[tmpb, xT], W=[xT])
            else:
                op("dve", lambda e: e.scalar_tensor_tensor(out=xT.ap[:, c, 0:N], in0=pb.ap[:, 0:N], scalar=gate,
                                                           in1=xT.ap[:, c, 0:N], op0=ALU.mult, op1=ALU.add),
                   R=[pb, modT, xT], W=[xT])

    def conv4(dst, taps, wcol, bcol, N, Rb, Wb):
        op("dve", lambda e: e.tensor_scalar(out=dst, in0=taps[0], scalar1=wcol(0), scalar2=bcol, op0=ALU.mult,
                                            op1=ALU.add), R=Rb, W=Wb)
        for k in range(1, 4):
            op("dve", lambda e, k=k: e.scalar_tensor_tensor(out=dst, in0=taps[k], scalar=wcol(k), in1=dst,
                                                            op0=ALU.mult, op1=ALU.add), R=Rb + Wb, W=Wb)

    def rg_mixer(N, samp, last, hmT):
        m = A.mark()
        gT = A.bf16(KC, NB)
        yT = A.bf16(KC, NB)
        if samp:
            nbuf = A.f32(KC, 3, NS)
            hout = A.f32(KC, NS)
        m_in = A.mark()
        win_srcs = []
        for p in range(8):
            win_srcs += [i_rgwin[2 * p], i_rgwin[2 * p + 1], i_rgwin[16 + 2 * p], i_rgwin[16 + 2 * p + 1]]
        ws = WStream(win_srcs, (KC, 128), ceng=("pool", "act"), nstage=2, nbf=3)
        gsrcs = []
        for p in range(8):
            gsrcs += [i_rgwa[2 * p], i_rgwa[2 * p + 1], i_rgwi[2 * p], i_rgwi[2 * p + 1]]
        gs = WStream(gsrcs, (2, 128), ceng=("pool",), nstage=4, nbf=4)
        xe = [A.f32(2, NB + 3), A.f32(2, NB + 3)]
        xc = [A.f32(2, NB), A.f32(2, NB)]
        xcb = [A.bf16(2, NB), A.bf16(2, NB)]
        gate_r = [A.f32(NB), A.f32(NB)]
        gate_i = [A.f32(NB), A.f32(NB)]
        av = [A.f32(NB), A.f32(NB)]
        bv = [A.f32(NB), A.f32(NB)]
        hs = [A.f32(NB), A.f32(NB)]
        if samp:
            st_c = A.f32(KC, 3, NS)
            for c_ in range(KC):
                dma(st_c.ap[:, c_], i_rgconv[c_ * 128:(c_ + 1) * 128], W=[st_c])
            h0s = A.f32(KC, NS)
            for c_ in range(KC):
                dma(h0s.ap[:, c_, :], i_rgh[c_ * 128:(c_ + 1) * 128, :], W=[h0s])
        for p in range(8):
            xe_, xc_, xcb_ = xe[p % 2], xc[p % 2], xcb[p % 2]
            for q4 in range(4):
                wb = ws.get()
                pb = banks[q4 % 4]
                for kc in range(KC):
                    op("pe", lambda e, kc=kc: e.matmul(pb.ap[:, 0:N], wb.ap[:, kc, :], hmT.ap[:, kc, 0:N],
                                                        start=(kc == 0), stop=(kc == KC - 1)), R=[wb, hmT], W=[pb])
                if q4 < 2:
                    c = 2 * p + q4
                    op("act", lambda e: e.activation(out=gT.ap[:, c, 0:N], in_=pb.ap[:, 0:N],
                                                     func=AF.Gelu_apprx_tanh, bias=rg_bin[:, c:c + 1], scale=1.0),
                       R=[pb, vec], W=[gT])
                else:
                    q = q4 - 2
                    c = 2 * p + q
                    if not samp:
                        op("pool", lambda e: e.tensor_copy(out=xe_.ap[:, q, 0:3], in_=rg_tail.ap[:, c, :]),
                           R=[rg_tail], W=[xe_])
                    op("act", lambda e: e.activation(out=xe_.ap[:, q, 3:3 + N], in_=pb.ap[:, 0:N], func=AF.Identity,
                                                     bias=rg_bin[:, 16 + c:16 + c + 1], scale=1.0),
                       R=[pb, vec], W=[xe_])
            for q in range(2):
                c = 2 * p + q
                if samp:
                    taps = [st_c.ap[:, c, 0, :], st_c.ap[:, c, 1, :], st_c.ap[:, c, 2, :], xe_.ap[:, q, 3:3 + N]]
                    Rb = [xe_, st_c, rgcw, vec]
                else:
                    taps = [xe_.ap[:, q, k:k + N] for k in range(4)]
                    Rb = [xe_, rgcw, vec]
                conv4(xc_.ap[:, q, 0:N], taps, lambda k: rgcw.ap[:, c, k:k + 1], rg_bconv[:, c:c + 1], N, Rb, [xc_])
                op("act", lambda e: e.copy(out=xcb_.ap[:, q, 0:N], in_=xc_.ap[:, q, 0:N]), R=[xc_], W=[xcb_])
                if samp:
                    for k in range(2):
                        op("pool", lambda e, k=k: e.tensor_copy(out=nbuf.ap[:, c, k, :], in_=st_c.ap[:, c, k + 1, :]),
                           R=[st_c], W=[nbuf])
                    op("pool", lambda e: e.tensor_copy(out=nbuf.ap[:, c, 2, :], in_=xe_.ap[:, q, 3:3 + N]),
                       R=[xe_], W=[nbuf])
                else:
                    op("pool", lambda e: e.tensor_copy(out=rg_tail.ap[:, c, :], in_=xe_.ap[:, q, N:N + 3]),
                       R=[xe_], W=[rg_tail])
            gw = [gs.get() for _ in range(4)]
            for jh in range(2):
                c = 2 * p + jh
                r_, i_, a_, b_, h_ = gate_r[jh], gate_i[jh], av[jh], bv[jh], hs[jh]
                for gi_, (wt, dstb, bcol) in enumerate(((gw[jh], r_, rg_ba), (gw[2 + jh], i_, rg_bi))):
                    pb = banks[4 + (2 * jh + gi_) % 4]
                    for ih in range(2):
                        op("pe", lambda e, ih=ih: e.matmul(pb.ap[:, 0:N], wt.ap[:, ih, :], xcb_.ap[:, ih, 0:N],
                                                            start=(ih == 0), stop=(ih == 1)), R=[wt, xcb_], W=[pb])
                    op("act", lambda e: e.activation(out=dstb.ap[:, 0:N], in_=pb.ap[:, 0:N], func=AF.Sigmoid,
                                                     bias=bcol[:, c:c + 1], scale=1.0), R=[pb, vec], W=[dstb])
                op("act", lambda e: e.activation(out=a_.ap[:, 0:N], in_=r_.ap[:, 0:N], func=AF.Exp,
                                                 scale=c8.ap[:, c:c + 1]), R=[r_, c8], W=[a_])
                op("dve", lambda e: e.tensor_tensor(out=b_.ap[:, 0:N], in0=a_.ap[:, 0:N], in1=a_.ap[:, 0:N],
                                                    op=ALU.mult), R=[a_], W=[b_])
                op("dve", lambda e: e.tensor_scalar(out=b_.ap[:, 0:N], in0=b_.ap[:, 0:N], scalar1=-1.0, scalar2=1.0,
                                                    op0=ALU.mult, op1=ALU.add), R=[b_], W=[b_])
                op("dve", lambda e: e.tensor_scalar(out=b_.ap[:, 0:N], in0=b_.ap[:, 0:N], scalar1=1e-30,
                                                    scalar2=None, op0=ALU.max), R=[b_], W=[b_])
                op("act", lambda e: e.activation(out=b_.ap[:, 0:N], in_=b_.ap[:, 0:N], func=AF.Sqrt),
                   R=[b_], W=[b_])
                op("dve", lambda e: e.tensor_tensor(out=i_.ap[:, 0:N], in0=i_.ap[:, 0:N], in1=xc_.ap[:, jh, 0:N],
                                                    op=ALU.mult), R=[i_, xc_], W=[i_])
                op("dve", lambda e: e.tensor_tensor(out=b_.ap[:, 0:N], in0=b_.ap[:, 0:N], in1=i_.ap[:, 0:N],
                                                    op=ALU.mult), R=[b_, i_], W=[b_])
                if samp:
                    op("dve", lambda e: e.tensor_tensor(out=h_.ap[:, 0:N], in0=a_.ap[:, 0:N], in1=h0s.ap[:, c, :],
                                                        op=ALU.mult), R=[a_, h0s], W=[h_])
                    op("dve", lambda e: e.tensor_tensor(out=h_.ap[:, 0:N], in0=h_.ap[:, 0:N], in1=b_.ap[:, 0:N],
                                                        op=ALU.add), R=[h_, b_], W=[h_])
                    op("pool", lambda e: e.tensor_copy(out=hout.ap[:, c, :], in_=h_.ap[:, 0:N]), R=[h_], W=[hout])
                else:
                    op("dve", lambda e: e.tensor_tensor_scan(out=h_.ap[:, 0:N], data0=a_.ap[:, 0:N],
                                                             data1=b_.ap[:, 0:N], initial=rg_carry.ap[:, c:c + 1],
                                                             op0=ALU.mult, op1=ALU.add),
                       R=[a_, b_, rg_carry], W=[h_])
                    op("pool", lambda e: e.tensor_copy(out=rg_carry.ap[:, c:c + 1], in_=h_.ap[:, N - 1:N]),
                       R=[h_], W=[rg_carry])
                op("dve", lambda e: e.tensor_tensor(out=yT.ap[:, c, 0:N], in0=h_.ap[:, 0:N], in1=gT.ap[:, c, 0:N],
                                                    op=ALU.mult), R=[h_, gT], W=[yT])
        P.barrier()
        A.reset(m_in)
        wo = WStream([i_rgwout[j] for j in range(16)], (KC, 128), ceng=("pool", "act"))
        tmpb = [A.f32(NB), A.f32(NB)]
        for j in range(16):
            wb = wo.get()
            pb = banks[j % 4]
            for kc in range(KC):
                op("pe", lambda e, kc=kc: e.matmul(pb.ap[:, 0:N], wb.ap[:, kc, :], yT.ap[:, kc, 0:N],
                                                    start=(kc == 0), stop=(kc == KC - 1)), R=[wb, yT], W=[pb])
            resid_add(j, pb, N, rg_bout[:, j:j + 1], modv(0, 2, j, samp), samp, tmpb[j % 2])
        if samp:
            dma(o_rgconv_s, nbuf.ap, R=[nbuf])
            dma(o_rgh_s, hout.ap, R=[hout])
        elif last:
            dma(o_rgconv_p, rg_tail.ap, R=[rg_tail])
            dma(o_rgh_p, rg_carry.ap, R=[rg_carry])
        P.barrier()
        A.reset(m)

    def ssd_chunk(Q, t0, sT, hmT, xbcT, yT, dtw):
        m = A.mark()
        pdt = banks[0]
        for kc in range(KC):
            op("pe", lambda e, kc=kc: e.matmul(pdt.ap[0:Q, 0:64], hmT.ap[:, kc, t0:t0 + Q], dtw.ap[:, kc, :],
                                                start=(kc == 0), stop=(kc == KC - 1)), R=[hmT, dtw], W=[pdt])
        dt = A.f32(64)
        dtA = A.f32(64)
        op("dve", lambda e: e.tensor_tensor(out=dt.ap[0:Q, :], in0=pdt.ap[0:Q, 0:64], in1=ssdh.ap[0:Q, 0, :],
                                            op=ALU.add), R=[pdt, ssdh], W=[dt])
        op("act", lambda e: e.activation(out=dt.ap[0:Q, :], in_=dt.ap[0:Q, :], func=AF.Exp), R=[dt], W=[dt])
        op("act", lambda e: e.activation(out=dt.ap[0:Q, :], in_=dt.ap[0:Q, :], func=AF.Ln, bias=1.0),
           R=[dt], W=[dt])
        op("dve", lambda e: e.tensor_tensor(out=dtA.ap[0:Q, :], in0=dt.ap[0:Q, :], in1=Abc.ap[0:Q, :],
                                            op=ALU.mult), R=[dt, Abc], W=[dtA])
        pc = banks[1]
        op("pe", lambda e: e.matmul(pc.ap[0:Q, 0:64], tri[0:Q, 0:Q], dtA.ap[0:Q, :], start=True, stop=True),
           R=[dtA, cst], W=[pc])
        ncum = A.f32(64)
        op("dve", lambda e: e.tensor_scalar(out=ncum.ap[0:Q, :], in0=pc.ap[0:Q, 0:64], scalar1=-1.0, scalar2=None,
                                            op0=ALU.mult), R=[pc], W=[ncum])
        xtok = A.bf16(4096)
        btok = A.bf16(1024)
        for grp in range(5):
            pbt = banks[2 + grp % 2]
            pv = pbt.ap.bitcast(BF16)
            for k in range(8):
                j = grp * 8 + k
                op("pe", lambda e, j=j, k=k: e.transpose(pv[0:Q, k * 128:(k + 1) * 128], xbcT.ap[:, j, t0:t0 + Q],
                                                         identb.ap), R=[xbcT, identb], W=[pbt])
            if grp < 4:
                op("act", lambda e: e.copy(out=xtok.ap[0:Q, grp * 1024:(grp + 1) * 1024], in_=pv[0:Q, :]),
                   R=[pbt], W=[xtok])
            else:
                op("act", lambda e: e.copy(out=btok.ap[0:Q, :], in_=pv[0:Q, :]), R=[pbt], W=[btok])
        cbt = A.f32(8, 128)
        for g in range(8):
            pcb = banks[4 + g % 2]
            op("pe", lambda e: e.matmul(pcb.ap[0:Q, 0:Q], xbcT.ap[:, 32 + g, t0:t0 + Q], xbcT.ap[:, 40 + g, t0:t0 + Q],
                                        start=True, stop=True), R=[xbcT], W=[pcb])
            op("act", lambda e: e.copy(out=cbt.ap[0:Q, g, 0:Q], in_=pcb.ap[0:Q, 0:Q]), R=[pcb], W=[cbt])
        sTb = A.bf16(64, 64)
        op("pool", lambda e: e.tensor_copy(out=sTb.ap, in_=sT.ap), R=[sT], W=[sTb])
        rhs4 = [A.f32(4, 128), A.f32(4, 128)]
        arg4 = [A.f32(4, 128), A.f32(4, 128)]
        ET4 = [A.f32(4, 128), A.f32(4, 128)]
        ecr4 = [A.f32(4, 128), A.f32(4, 128)]
        WT = [A.bf16(128) for _ in range(4)]
        CpT = [A.bf16(128) for _ in range(4)]
        xw = [A.bf16(64) for _ in range(4)]
        for h4 in range(16):
            r4, a4, E4, c4 = rhs4[h4 % 2], arg4[h4 % 2], ET4[h4 % 2], ecr4[h4 % 2]
            g = h4 // 2
            op("pool", lambda e: e.tensor_tensor(out=r4.ap[0:Q, :, 0:Q], in0=bc(tri[0:Q, 0:Q], 1, [Q, 4, Q]),
                                                 in1=bc(dtA.ap[0:Q, h4 * 4:h4 * 4 + 4], 2, [Q, 4, Q]), op=ALU.mult),
               R=[dtA, cst], W=[r4])
            pcr = banks[6 + h4 % 2]
            pcr_v = pcr.ap.rearrange("p (a b) -> p a b", b=128)
            for hh in range(4):
                op("pe", lambda e, hh=hh: e.matmul(pcr_v[:, hh, 0:Q], ones[0:Q, :], r4.ap[0:Q, hh, 0:Q],
                                                    start=True, stop=True), R=[r4, cst], W=[pcr])
            for hh in range(4):
                h = h4 * 4 + hh
                op("dve", lambda e, hh=hh, h=h: e.scalar_tensor_tensor(out=a4.ap[0:Q, hh, 0:Q], in0=pcr_v[0:Q, hh, 0:Q],
                                                                       scalar=ncum.ap[0:Q, h:h + 1], in1=mneg[0:Q, 0:Q],
                                                                       op0=ALU.add, op1=ALU.min),
                   R=[pcr, ncum, cst], W=[a4])
            op("act", lambda e: e.activation(out=E4.ap[0:Q, :, 0:Q], in_=a4.ap[0:Q, :, 0:Q], func=AF.Exp),
               R=[a4], W=[E4])
            op("act", lambda e: e.activation(out=c4.ap[:, :, 0:Q], in_=pcr_v[:, :, 0:Q], func=AF.Exp),
               R=[pcr], W=[c4])
            for hh in range(4):
                h = h4 * 4 + hh
                w_, cp_, xw_ = WT[hh], CpT[hh], xw[hh]
                op("dve", lambda e, hh=hh, h=h, w_=w_: e.scalar_tensor_tensor(
                    out=w_.ap[0:Q, 0:Q], in0=E4.ap[0:Q, hh, 0:Q], scalar=dt.ap[0:Q, h:h + 1], in1=cbt.ap[0:Q, g, 0:Q],
                    op0=ALU.mult, op1=ALU.mult), R=[E4, dt, cbt], W=[w_])
                op("pool", lambda e, hh=hh, cp_=cp_: e.tensor_tensor(
                    out=cp_.ap[:, 0:Q], in0=xbcT.ap[:, 40 + g, t0:t0 + Q], in1=c4.ap[:, hh, 0:Q], op=ALU.mult),
                   R=[xbcT, c4], W=[cp_])
                c = h // 2
                pby = banks[0 + (c // 4) % 2]
                po = (h % 2) * 64
                col = (c % 4) * 128
                op("pe", lambda e, h=h, w_=w_, pby=pby, po=po, col=col: e.matmul(
                    pby.ap[po:po + 64, col:col + Q], xtok.ap[0:Q, h * 64:(h + 1) * 64], w_.ap[0:Q, 0:Q],
                    start=True, stop=False), R=[xtok, w_], W=[pby])
                op("pe", lambda e, h=h, cp_=cp_, pby=pby, po=po, col=col: e.matmul(
                    pby.ap[po:po + 64, col:col + Q], sTb.ap[:, h, :], cp_.ap[:, 0:Q],
                    start=False, stop=True), R=[sTb, cp_], W=[pby])
                if h % 2 == 1:
                    op("dve", lambda e, c=c, pby=pby, col=col: e.scalar_tensor_tensor(
                        out=yT.ap[:, c, t0:t0 + Q], in0=xbcT.ap[:, c, t0:t0 + Q], scalar=ssd_dexp[:, c:c + 1],
                        in1=pby.ap[:, col:col + Q], op0=ALU.mult, op1=ALU.add), R=[xbcT, vec, pby], W=[yT])
                op("dve", lambda e, hh=hh, h=h, xw_=xw_: e.tensor_scalar(
                    out=xw_.ap[0:Q, :], in0=xtok.ap[0:Q, h * 64:(h + 1) * 64], scalar1=E4.ap[0:Q, hh, Q - 1:Q],
                    scalar2=dt.ap[0:Q, h:h + 1], op0=ALU.mult, op1=ALU.mult), R=[xtok, E4, dt], W=[xw_])
                pst = banks[2 + (h // 8) % 2]
                scol = (h % 8) * 64
                op("pe", lambda e, xw_=xw_, pst=pst, scol=scol: e.matmul(
                    pst.ap[:, scol:scol + 64], btok.ap[0:Q, g * 128:(g + 1) * 128], xw_.ap[0:Q, :],
                    start=True, stop=True), R=[btok, xw_], W=[pst])
                op("dve", lambda e, hh=hh, h=h, pst=pst, scol=scol: e.scalar_tensor_tensor(
                    out=sT.ap[:, h, :], in0=sT.ap[:, h, :], scalar=c4.ap[:, hh, Q - 1:Q], in1=pst.ap[:, scol:scol + 64],
                    op0=ALU.mult, op1=ALU.add), R=[sT, c4, pst], W=[sT])
        P.barrier()
        A.reset(m)

    def ssd_mixer(N, samp, first, last, hmT):
        m = A.mark()
        xbcT = A.bf16(48, NB)
        yT = xbcT
        dtw = A.bf16(KC, 64)
        m1 = A.mark()
        dst_ = A.f32(KC, 64)
        dma(dst_.ap, i_ssdwdt, W=[dst_])
        op("pool", lambda e: e.tensor_copy(out=dtw.ap, in_=dst_.ap), R=[dst_], W=[dtw])
        ws = WStream([i_ssdwin[32 + j] for j in range(48)], (KC, 128), ceng=("pool", "act"))
        xe = [A.f32(NB + 3), A.f32(NB + 3)]
        xcv = [A.f32(NB), A.f32(NB)]
        if samp:
            st_c = A.f32(48, 3, NS)
            for j in range(48):
                dma(st_c.ap[:, j], i_ssdconv[j * 128:(j + 1) * 128], W=[st_c])
            nbuf = A.f32(48, 3, NS)
        for j in range(48):
            wb = ws.get()
            pb = banks[j % 4]
            xe_, xc_ = xe[j % 2], xcv[j % 2]
            for kc in range(KC):
                op("pe", lambda e, kc=kc: e.matmul(pb.ap[:, 0:N], wb.ap[:, kc, :], hmT.ap[:, kc, 0:N],
                                                    start=(kc == 0), stop=(kc == KC - 1)), R=[wb, hmT], W=[pb])
            if not samp:
                op("pool", lambda e: e.tensor_copy(out=xe_.ap[:, 0:3], in_=ssd_tail.ap[:, j, :]),
                   R=[ssd_tail], W=[xe_])
            op("act", lambda e: e.copy(out=xe_.ap[:, 3:3 + N], in_=pb.ap[:, 0:N]), R=[pb], W=[xe_])
            if samp:
                taps = [st_c.ap[:, j, 0, :], st_c.ap[:, j, 1, :], st_c.ap[:, j, 2, :], xe_.ap[:, 3:3 + N]]
                Rb = [xe_, st_c, ssdcw, vec]
            else:
                taps = [xe_.ap[:, k:k + N] for k in range(4)]
                Rb = [xe_, ssdcw, vec]
            conv4(xc_.ap[:, 0:N], taps, lambda k: ssdcw.ap[:, j, k:k + 1], ssd_bconv[:, j:j + 1], N, Rb, [xc_])
            op("act", lambda e: e.activation(out=xbcT.ap[:, j, 0:N], in_=xc_.ap[:, 0:N], func=AF.Silu),
               R=[xc_], W=[xbcT])
            if samp:
                for k in range(2):
                    op("pool", lambda e, k=k: e.tensor_copy(out=nbuf.ap[:, j, k, :], in_=st_c.ap[:, j, k + 1, :]),
                       R=[st_c], W=[nbuf])
                op("pool", lambda e: e.tensor_copy(out=nbuf.ap[:, j, 2, :], in_=xe_.ap[:, 3:3 + N]),
                   R=[xe_], W=[nbuf])
            else:
                op("pool", lambda e: e.tensor_copy(out=ssd_tail.ap[:, j, :], in_=xe_.ap[:, N:N + 3]),
                   R=[xe_], W=[ssd_tail])
        if samp:
            dma(o_ssdconv_s, nbuf.ap, R=[nbuf])
        elif last:
            dma(o_ssdconv_p, ssd_tail.ap, R=[ssd_tail])
        P.barrier()
        A.reset(m1)
        sT = A.f32(64, 64)
        if samp:
            for b in range(NS):
                dma(sT.ap, i_ssds[b], W=[sT])
                ssd_chunk(1, b, sT, hmT, xbcT, yT, dtw)
                dma(o_ssd_s[b], sT.ap, R=[sT])
        else:
            if first:
                op("pool", lambda e: e.memset(sT.ap, 0.0), W=[sT])
            else:
                dma(sT.ap, o_ssd_p, R=[sT_dram], W=[sT])
            for q in range(N // 128):
                ssd_chunk(128, q * 128, sT, hmT, xbcT, yT, dtw)
            dma(o_ssd_p, sT.ap, R=[sT], W=[sT_dram])
        P.barrier()
        A.reset(m1)
        wz = WStream([i_ssdwin[j] for j in range(32)], (KC, 128), ceng=("pool", "act"))
        sz = [A.f32(NB), A.f32(NB)]
        sq = [A.f32(NB), A.f32(NB)]
        pst = banks[7]
        for j in range(32):
            wb = wz.get()
            pb = banks[j % 4]
            for kc in range(KC):
                op("pe", lambda e, kc=kc: e.matmul(pb.ap[:, 0:N], wb.ap[:, kc, :], hmT.ap[:, kc, 0:N],
                                                    start=(kc == 0), stop=(kc == KC - 1)), R=[wb, hmT], W=[pb])
            s_, q_ = sz[j % 2], sq[j % 2]
            op("act", lambda e: e.activation(out=s_.ap[:, 0:N], in_=pb.ap[:, 0:N], func=AF.Silu), R=[pb], W=[s_])
            op("dve", lambda e: e.tensor_tensor(out=yT.ap[:, j, 0:N], in0=yT.ap[:, j, 0:N], in1=s_.ap[:, 0:N],
                                                op=ALU.mult), R=[yT, s_], W=[yT])
            op("act", lambda e: e.activation(out=q_.ap[:, 0:N], in_=yT.ap[:, j, 0:N], func=AF.Square),
               R=[yT], W=[q_])
            op("pe", lambda e: e.matmul(pst.ap[:, 0:N], ones, q_.ap[:, 0:N], start=(j == 0), stop=(j == 31)),
               R=[q_, cst], W=[pst])
        P.barrier()
        A.reset(m1)
        rstd = A.f32(NB)
        op("act", lambda e: e.activation(out=rstd.ap[:, 0:N], in_=pst.ap[:, 0:N], func=AF.Sqrt, scale=1.0 / 4096,
                                         bias=epsb.ap[:, 0:1]), R=[pst, epsb], W=[rstd])
        op("dve", lambda e: e.reciprocal(out=rstd.ap[:, 0:N], in_=rstd.ap[:, 0:N]), R=[rstd], W=[rstd])
        for j in range(32):
            op("dve", lambda e: e.scalar_tensor_tensor(out=yT.ap[:, j, 0:N], in0=yT.ap[:, j, 0:N],
                                                       scalar=ssd_ng[:, j:j + 1], in1=rstd.ap[:, 0:N],
                                                       op0=ALU.mult, op1=ALU.mult), R=[yT, vec, rstd], W=[yT])
        wo = WStream([i_ssdwout[j] for j in range(16)], (32, 128), ceng=("pool", "act"), nstage=2, nbf=2)
        tmpb = [A.f32(NB), A.f32(NB)]
        for j in range(16):
            wb = wo.get()
            pb = banks[j % 4]
            for kc in range(32):
                op("pe", lambda e, kc=kc: e.matmul(pb.ap[:, 0:N], wb.ap[:, kc, :], yT.ap[:, kc, 0:N],
                                                    start=(kc == 0), stop=(kc == 31)), R=[wb, yT], W=[pb])
            resid_add(j, pb, N, None, modv(1, 2, j, samp), samp, tmpb[j % 2])
        P.barrier()
        A.reset(m)

    def peer(l, N, samp, hcT):
        m = A.mark()
        NG = N // 16
        s12 = A.f32(NB // 16, 256)
        thr = A.f32(NB // 16)
        negm = A.f32(NB // 16)
        Sel = A.bf16(NB // 16, 16)
        m1 = A.mark()
        qT = A.f32(2, NB, 8)
        wq = WStream([i_wq[l, j] for j in range(16)], (KC, 128), ceng=("pool", "act"))
        for j in range(16):
            wb = wq.get()
            pb = banks[j % 4]
            for kc in range(KC):
                op("pe", lambda e, kc=kc: e.matmul(pb.ap[:, 0:N], wb.ap[:, kc, :], hcT.ap[:, kc, 0:N],
                                                    start=(kc == 0), stop=(kc == KC - 1)), R=[wb, hcT], W=[pb])
            op("act", lambda e: e.copy(out=qT.ap[:, j % 2, 0:N, j // 2], in_=pb.ap[:, 0:N]), R=[pb], W=[qT])
        vv = [A.f32(2, 16) for _ in range(2)]
        tmp1 = [A.f32(128) for _ in range(2)]
        cand = [A.f32(256) for _ in range(2)]
        cand2 = [A.f32(256) for _ in range(2)]
        c24 = [A.f32(24) for _ in range(2)]
        ez = [A.f32(16) for _ in range(2)]
        zz = [A.f32(2) for _ in range(2)]
        for gi in range(NG):
            pb = banks[4 + gi % 2]
            for half in range(2):
                lhsT = qT.ap[:, half, gi * 16:(gi + 1) * 16, :].rearrange("p t h -> p (t h)")
                op("pe", lambda e, half=half, lhsT=lhsT: e.matmul(pb.ap[:, half * 128:(half + 1) * 128], lhsT,
                                                                  kT.ap[:, l * 2 + half, :], start=True, stop=True),
                   R=[qT, kT], W=[pb])
            op("act", lambda e: e.copy(out=s12.ap[:, gi, :], in_=pb.ap[:, 0:256]), R=[pb], W=[s12])
            v_, t1, cd, cd2, c_, ez_, z_ = (vv[gi % 2], tmp1[gi % 2], cand[gi % 2], cand2[gi % 2], c24[gi % 2],
                                            ez[gi % 2], zz[gi % 2])
            for half in range(2):
                w = s12.ap[:, gi, half * 128:(half + 1) * 128]
                op("dve", lambda e: e.max(out=v_.ap[:, half, 0:8], in_=w), R=[s12], W=[v_])
                op("dve", lambda e: e.match_replace(out=t1.ap, in_to_replace=v_.ap[:, half, 0:8], in_values=w,
                                                    imm_value=-1e30), R=[s12, v_], W=[t1])
                op("dve", lambda e: e.max(out=v_.ap[:, half, 8:16], in_=t1.ap), R=[t1], W=[v_])
            cd3 = cd.ap.rearrange("p (a b) -> p a b", b=16)
            op("dve", lambda e: e.tensor_tensor(out=cd3, in0=bc(v_.ap[:, 0, :], 2, [128, 16, 16]),
                                                in1=bc(v_.ap[:, 1, :], 1, [128, 16, 16]), op=ALU.add),
               R=[v_], W=[cd])
            op("dve", lambda e: e.max(out=c_.ap[:, 0:8], in_=cd.ap), R=[cd], W=[c_])
            op("dve", lambda e: e.match_replace(out=cd2.ap, in_to_replace=c_.ap[:, 0:8], in_values=cd.ap,
                                                imm_value=-1e30), R=[cd, c_], W=[cd2])
            op("dve", lambda e: e.max(out=c_.ap[:, 8:16], in_=cd2.ap), R=[cd2], W=[c_])
            op("dve", lambda e: e.match_replace(out=cd.ap, in_to_replace=c_.ap[:, 8:16], in_values=cd2.ap,
                                                imm_value=-1e30), R=[cd2, c_], W=[cd])
            op("dve", lambda e: e.max(out=c_.ap[:, 16:24], in_=cd.ap), R=[cd], W=[c_])
            op("dve", lambda e: e.tensor_scalar(out=thr.ap[:, gi:gi + 1], in0=c_.ap[:, 15:16],
                                                scalar1=c_.ap[:, 16:17], scalar2=0.5, op0=ALU.add, op1=ALU.mult),
               R=[c_], W=[thr])
            op("dve", lambda e: e.tensor_scalar(out=negm.ap[:, gi:gi + 1], in0=c_.ap[:, 0:1], scalar1=-1.0,
                                                scalar2=None, op0=ALU.mult), R=[c_], W=[negm])
            op("act", lambda e: e.activation(out=ez_.ap, in_=c_.ap[:, 0:16], func=AF.Exp,
                                             bias=negm.ap[:, gi:gi + 1], scale=1.0, accum_out=z_.ap[:, 0:1]),
               R=[c_, negm], W=[ez_, z_])
            op("dve", lambda e: e.reciprocal(out=z_.ap[:, 1:2], in_=z_.ap[:, 0:1]), R=[z_], W=[z_])
            op("dve", lambda e: e.tensor_scalar(out=Sel.ap[:, gi, :], in0=selm, scalar1=z_.ap[:, 1:2], scalar2=None,
                                                op0=ALU.mult), R=[z_, cst], W=[Sel])
        P.barrier()
        A.reset(m1)
        usrc, vsrc = [], []
        for a in range(128):
            usrc += [i_uT[l, a][:, 0:8, :], i_uT[l, a][:, 8:16, :]]
            vsrc += [i_v[l, a][:, 0:1024], i_v[l, a][:, 1024:2048]]
        us = WStream(usrc, (8, 128), ceng=("act",), nstage=3, nbf=2 * GA + 1)
        vs = WStream(vsrc, (1024,), ceng=("act",), nstage=3, nbf=2 * GA + 1)
        FR = 16
        LAG = 3
        NGA = 128 // GA
        NGh = min(NG, 16)
        Dd = [A.f32(GA, 128) for _ in range(3)]
        ee = [A.bf16(GA, 128) for _ in range(3)]
        Fr = [A.bf16(GA, 128) for _ in range(FR)]
        gel = [A.bf16(NB) for _ in range(GA)]
        AT = [A.bf16(NB) for _ in range(GA)]
        st = {"f": 0, "pend": [], "fb": [], "u": {}, "v": {}}

        def fgen(ag, gi):
            k = st["f"]
            st["f"] += 1
            d_, e_, f_ = Dd[k % 3], ee[k % 3], Fr[k % FR]
            a0 = ag * GA
            op("dve", lambda e: e.tensor_tensor(out=d_.ap, in0=bc(s12.ap[:, gi, 128:256], 1, [128, GA, 128]),
                                                in1=bc(s12.ap[:, gi, a0:a0 + GA], 2, [128, GA, 128]),
                                                op=ALU.add), R=[s12], W=[d_])
            op("act", lambda e: e.activation(out=e_.ap, in_=d_.ap, func=AF.Exp, bias=negm.ap[:, gi:gi + 1],
                                             scale=1.0), R=[d_, negm], W=[e_])
            st["fb"].append((gi, d_, e_, f_))
            fgen_b(1)

        def fgen_b(lag):
            while len(st["fb"]) > lag:
                gi, d_, e_, f_ = st["fb"].pop(0)
                op("dve", lambda e: e.scalar_tensor_tensor(out=f_.ap, in0=d_.ap, scalar=thr.ap[:, gi:gi + 1],
                                                           in1=e_.ap, op0=ALU.is_ge, op1=ALU.mult),
                   R=[d_, e_, thr], W=[f_])
                st["pend"].append((gi, f_))

        def gmm_emit(lag):
            if lag == 0:
                fgen_b(0)
            while len(st["pend"]) > lag:
                gi, f_ = st["pend"].pop(0)
                for ai in range(GA):
                    pG = banks[2 + ai]
                    op("pe", lambda e, ai=ai, pG=pG: e.matmul(pG.ap[:, gi * 16:(gi + 1) * 16], f_.ap[:, ai, :],
                                                              Sel.ap[:, gi, :], start=True, stop=True),
                       R=[f_, Sel], W=[pG])

        def ucast(ag, k):
            st["u"][(ag, k)] = us.get()

        def vcast(ag, k):
            st["v"][(ag, k)] = vs.get()

        def sT_pe(ag, ai):
            pS = banks[ai % 2]
            for kc in range(KC):
                ub = st["u"][(ag, 2 * ai + kc // 8)]
                op("pe", lambda e, kc=kc, ub=ub: e.matmul(pS.ap[:, 0:N], ub.ap[:, kc % 8, :], hcT.ap[:, kc, 0:N],
                                                          start=(kc == 0), stop=(kc == KC - 1)),
                   R=[ub, hcT], W=[pS])

        def sT_gelu(ai):
            pS = banks[ai % 2]
            g_ = gel[ai]
            op("act", lambda e: e.activation(out=g_.ap[:, 0:N], in_=pS.ap[:, 0:N], func=AF.Gelu_apprx_tanh),
               R=[pS], W=[g_])

        def aTm():
            for ai in range(GA):
                pG = banks[2 + ai]
                g_, at_ = gel[ai], AT[ai]
                op("dve", lambda e: e.tensor_tensor(out=at_.ap[:, 0:N], in0=g_.ap[:, 0:N], in1=pG.ap[:, 0:N],
                                                    op=ALU.mult), R=[g_, pG], W=[at_])

        def outp(ag, dc):
            pO = banks[6 + dc % 2]
            for ai in range(GA):
                at_ = AT[ai]
                vb = st["v"][(ag, 2 * ai + dc // 8)]
                op("pe", lambda e, ai=ai, at_=at_, vb=vb: e.matmul(
                    pO.ap[:, 0:N], vb.ap[:, (dc % 8) * 128:(dc % 8 + 1) * 128], at_.ap[:, 0:N],
                    start=(ai == 0), stop=(ai == GA - 1)), R=[vb, at_], W=[pO])
            if samp:
                t_ = Dd[dc % 2]
                tv = t_.ap.rearrange("p a b -> p (a b)")
                op("dve", lambda e: e.tensor_tensor(out=tv[:, 0:N], in0=pO.ap[:, 0:N], in1=modv(l, 5, dc, True),
                                                    op=ALU.mult), R=[pO, modT], W=[t_])
                op("dve", lambda e: e.tensor_tensor(out=xT.ap[:, dc, 0:N], in0=xT.ap[:, dc, 0:N], in1=tv[:, 0:N],
                                                    op=ALU.add), R=[t_, xT], W=[xT])
            else:
                op("dve", lambda e: e.scalar_tensor_tensor(out=xT.ap[:, dc, 0:N], in0=pO.ap[:, 0:N],
                                                           scalar=modv(l, 5, dc, False), in1=xT.ap[:, dc, 0:N],
                                                           op0=ALU.mult, op1=ALU.add), R=[pO, modT, xT], W=[xT])

        def phaseY(ag):
            for pr in range(GA // 2):
                sT_pe(ag, 2 * pr)
                sT_pe(ag, 2 * pr + 1)
                sT_gelu(2 * pr)
                sT_gelu(2 * pr + 1)
                for k in range(4):
                    vcast(ag, 4 * pr + k)
                for k in range(8):
                    gi = NGh + 8 * pr + k
                    if gi < NG:
                        fgen(ag, gi)
                        gmm_emit(LAG)
            gmm_emit(0)
            aTm()

        for k in range(2 * GA):
            ucast(0, k)
        for gi in range(NGh):
            fgen(0, gi)
            gmm_emit(LAG)
        gmm_emit(0)
        phaseY(0)
        for ag in range(NGA):
            nxt = ag + 1 < NGA
            for dc in range(16):
                outp(ag, dc)
                if nxt:
                    if dc % 2 == 0:
                        ucast(ag + 1, dc // 2)
                    if dc < NGh:
                        fgen(ag + 1, dc)
                    gmm_emit(LAG)
            if nxt:
                gmm_emit(0)
                phaseY(ag + 1)
        P.barrier()
        A.reset(m)

    def run_block(N, samp, first, last, src, dst):
        dma(xT.ap[:, :, 0:N], src, W=[xT])
        for l in range(2):
            m = A.mark()
            hmT = A.bf16(KC, NB)
            norm_mod(l, 0, N, samp, hmT)
            if l == 0:
                rg_mixer(N, samp, last, hmT)
            else:
                ssd_mixer(N, samp, first, last, hmT)
            norm_mod(l, 1, N, samp, hmT)
            peer(l, N, samp, hmT)
            A.reset(m)
        m = A.mark()
        scr = [A.f32(NB), A.f32(NB)]
        rstd = A.f32(NB)
        yo = A.f32(KC, NB)
        rms_stats(lambda c: xT.ap[:, c, 0:N], [xT], N, KC, scr, banks[7], rstd)
        for c in range(KC):
            op("dve", lambda e: e.scalar_tensor_tensor(out=yo.ap[:, c, 0:N], in0=xT.ap[:, c, 0:N],
                                                       scalar=fing[:, c:c + 1], in1=rstd.ap[:, 0:N], op0=ALU.mult,
                                                       op1=ALU.mult), R=[xT, vec, rstd], W=[yo])
        dma(dst, yo.ap[:, :, 0:N], R=[yo])
        P.barrier()
        A.reset(m)

    for blk in range(nblk):
        run_block(NB, False, blk == 0, blk == nblk - 1,
                  i_xT[:, blk * NB:(blk + 1) * NB].rearrange("(c p) t -> p c t", p=128),
                  o_yT[:, blk * NB:(blk + 1) * NB].rearrange("(c p) t -> p c t", p=128))
    run_block(NS, True, True, False, i_xsT.rearrange("(c p) t -> p c t", p=128),
              o_ysT.rearrange("(c p) t -> p c t", p=128))
    P.finish()
    es.close()
    nc._arena_hw = A.hw
    return nc


def _wlay(w):
    K, N = w.shape
    return np.ascontiguousarray(w.reshape(K // 128, 128, N // 128, 128).transpose(2, 1, 0, 3))


def _vlay(v):
    return v.reshape(-1, 128).T


_CACHE = {}


def _prep_shared(inp):
    f = np.float32
    sh = {}
    cst = np.zeros((128, 4 * 128 + 16), f)
    cst[:, 0:128] = np.eye(128)
    cst[:, 128:256] = 1.0
    k = np.arange(128)
    cst[:, 256:384] = (k[:, None] <= k[None, :])
    cst[:, 384:512] = np.where(k[:, None] <= k[None, :], 0.0, NEG)
    cst[:, 512:528] = (k[:, None] // 8 == np.arange(16)[None, :])
    sh["cst"] = cst
    vec = np.zeros((128, 16 * 11 + 32 * 2 + 48 + 32), f)
    vec[:, 0:16] = _vlay(inp["norm1_g"][0]); vec[:, 16:32] = _vlay(inp["norm1_g"][1])
    vec[:, 32:48] = _vlay(inp["norm2_g"][0]); vec[:, 48:64] = _vlay(inp["norm2_g"][1])
    vec[:, 64:80] = _vlay(inp["final_g"])
    vec[:, 80:96] = _vlay(inp["rg_conv_b"][0])
    vec[:, 96:112] = _vlay(inp["rg_b_a"][0])
    vec[:, 112:128] = _vlay(inp["rg_b_i"][0])
    vec[:, 128:144] = _vlay(inp["rg_lambda"][0])
    vec[:, 144:160] = _vlay(inp["rg_b_out"][0])
    vec[:, 176:208] = _vlay(inp["rg_b_in"][0])
    vec[:, 208:240] = _vlay(inp["ssd_norm_g"][0])
    vec[:, 240:288] = _vlay(inp["ssd_conv_b"][0])
    vec[:, 288:320] = _vlay(np.repeat(inp["ssd_d"][0], 64))
    sh["vec"] = vec
    sh["bmod"] = np.ascontiguousarray(np.stack([_vlay(inp["b_mod"][l]) for l in range(2)], axis=1))
    sh["rgcw"] = np.ascontiguousarray(inp["rg_conv_w"][0].T.reshape(16, 128, 4).transpose(1, 0, 2))
    sh["ssdcw"] = np.ascontiguousarray(inp["ssd_conv_w"][0].T.reshape(48, 128, 4).transpose(1, 0, 2))
    sh["ssdh"] = np.ascontiguousarray(np.stack([inp["ssd_dt_bias"][0], inp["ssd_a_log"][0],
                                                inp["ssd_d"][0]]).astype(f))
    sh["wmod"] = np.stack([_wlay(inp["w_mod"][l]) for l in range(2)])
    sh["rgwin"] = _wlay(inp["rg_w_in"][0])

    def glay(w):
        return np.ascontiguousarray(w.reshape(8, 2, 128, 2, 128).transpose(0, 3, 2, 1, 4).reshape(16, 128, 2, 128))
    sh["rgwa"] = glay(inp["rg_w_a"][0])
    sh["rgwi"] = glay(inp["rg_w_i"][0])
    sh["rgwout"] = _wlay(inp["rg_w_out"][0])
    w = inp["ssd_w_in"][0]
    sh["ssdwin"] = _wlay(np.ascontiguousarray(w[:, :10240]))
    sh["ssdwdt"] = np.ascontiguousarray(w[:, 10240:].reshape(16, 128, 64).transpose(1, 0, 2))
    sh["ssdwout"] = _wlay(inp["ssd_w_out"][0])
    sh["wq"] = np.stack([_wlay(inp["peer_w_q"][l]) for l in range(2)])
    sh["kT"] = np.ascontiguousarray(np.stack([np.stack([inp["peer_k1"][l].T, inp["peer_k2"][l].T])
                                              for l in range(2)]))
    sh["uT"] = np.stack([np.ascontiguousarray(inp["peer_u"][l].reshape(128, 128, 16, 128).transpose(0, 3, 2, 1))
                         for l in range(2)])
    sh["v"] = np.ascontiguousarray(inp["peer_v"].reshape(2, 128, 128, 2048))
    return sh


def kernel(**inp):
    inp = {k: np.asarray(v) for k, v in inp.items()}
    TP = inp["x_prompt"].shape[1]
    nc = build(TP)
    sh = _prep_shared(inp)
    in_maps = []
    for c in range(8):
        s = c % 4
        sl = slice(c * NS, (c + 1) * NS)
        d = dict(sh)
        d["xT"] = np.ascontiguousarray(inp["x_prompt"][s].T)
        d["xsT"] = np.ascontiguousarray(inp["x_sample"][sl, 0, :].T)
        d["cT"] = np.ascontiguousarray(np.concatenate([inp["c_prompt"][s][None], inp["c_sample"][sl]], 0).T)
        d["rgconv"] = np.ascontiguousarray(inp["state_rg_conv"][0, sl].transpose(2, 1, 0))
        d["rgh"] = np.ascontiguousarray(inp["state_rg_h"][0, sl].T)
        d["ssdconv"] = np.ascontiguousarray(inp["state_ssd_conv"][0, sl].transpose(2, 1, 0))
        d["ssds"] = np.ascontiguousarray(inp["state_ssd"][0, sl].reshape(NS, 64, 64, 128).transpose(0, 3, 1, 2))
        in_maps.append(d)
    res = run_bass_kernel_spmd(nc, in_maps, core_ids=list(range(8))).results
    f = np.float32
    y_p = np.stack([res[s]["o_yT"].T for s in range(4)]).astype(f)
    y_s = np.concatenate([res[c]["o_ysT"].T for c in range(8)], 0)[:, None, :].astype(f)

    def unv(a):
        return a.transpose(1, 0, *range(2, a.ndim)).reshape(-1, *a.shape[2:])
    rg_conv_p = np.stack([unv(res[s]["o_rgconv_p"]).T for s in range(4)])[None].astype(f)
    rg_h_p = np.stack([unv(res[s]["o_rgh_p"]) for s in range(4)])[None].astype(f)
    ssd_conv_p = np.stack([unv(res[s]["o_ssdconv_p"]).T for s in range(4)])[None].astype(f)
    ssd_p = np.stack([res[s]["o_ssd_p"].transpose(1, 2, 0).reshape(8, 8, 64, 128) for s in range(4)])[None].astype(f)
    rg_conv_s = np.concatenate([unv(res[c]["o_rgconv_s"]).transpose(2, 1, 0) for c in range(8)], 0)[None].astype(f)
    rg_h_s = np.concatenate([unv(res[c]["o_rgh_s"]).T for c in range(8)], 0)[None].astype(f)
    ssd_conv_s = np.concatenate([unv(res[c]["o_ssdconv_s"]).transpose(2, 1, 0) for c in range(8)], 0)[None].astype(f)
    ssd_s = np.concatenate([res[c]["o_ssd_s"].transpose(0, 2, 3, 1).reshape(NS, 8, 8, 64, 128)
                            for c in range(8)], 0)[None].astype(f)
    return (y_p, y_s, rg_conv_p, rg_h_p, ssd_conv_p, ssd_p, rg_conv_s, rg_h_s, ssd_conv_s, ssd_s)
```

```python
import contextlib
import numpy as np
import concourse.bass as bass
import concourse.mybir as mybir
from concourse.bass_utils import run_bass_kernel_spmd

F32 = mybir.dt.float32
BF16 = mybir.dt.bfloat16
AF = mybir.ActivationFunctionType
ALU = mybir.AluOpType

D = 2048
KC = 16
NS = 16
NB = 512
EPS = 1e-6
NDS = 12
GA = 4
NEG = -30000.0


class Trk:
    __slots__ = ("w", "r")

    def __init__(self):
        self.w = None
        self.r = {}


class Buf:
    def __init__(self, ap):
        self.ap = ap
        self.t = Trk()


class Prog:
    def __init__(self, nc, es):
        self.nc = nc
        self.eng = {"pe": nc.tensor, "dve": nc.vector, "act": nc.scalar, "pool": nc.gpsimd, "sp": nc.sync}
        self.sem = {k: es.enter_context(nc.semaphore("s_" + k)) for k in ("pe", "dve", "act", "pool")}
        self.cnt = {k: 0 for k in self.sem}
        self.seen = {e: {k: 0 for k in self.sem} for e in self.eng}
        self.dsem = [es.enter_context(nc.semaphore("d%d" % i)) for i in range(NDS)]
        self.dcnt = [0] * NDS
        self.dn = 0
        self.dseen = {e: [0] * NDS for e in self.eng}

    def _need(self, e, tok):
        if tok is None:
            return
        if tok[0] == "c":
            _, f, n = tok
            if f == e and e == "pe":
                return
            if self.seen[e][f] < n:
                self.eng[e].wait_ge(self.sem[f], n)
                self.seen[e][f] = n
        else:
            _, i, v = tok
            if self.dseen[e][i] < v:
                self.eng[e].wait_ge(self.dsem[i], v)
                self.dseen[e][i] = v

    def _sync(self, e, R, W):
        for b in R:
            self._need(e, b.t.w)
        for b in W:
            self._need(e, b.t.w)
            for tok in b.t.r.values():
                self._need(e, tok)

    def op(self, e, fn, R=(), W=()):
        self._sync(e, R, W)
        ins = fn(self.eng[e])
        self.cnt[e] += 1
        ins.then_inc(self.sem[e], 1)
        tok = ("c", e, self.cnt[e])
        for b in R:
            b.t.r[e] = tok
        for b in W:
            b.t.w = tok
            b.t.r = {}
        return ins

    def dma(self, out, in_, R=(), W=(), q="sp"):
        i = self.dn % NDS
        self.dn += 1
        self._need(q, ("d", i, self.dcnt[i]))
        self._sync(q, R, W)
        ins = self.eng[q].dma_start(out=out, in_=in_)
        self.dcnt[i] += 16
        ins.then_inc(self.dsem[i], 16)
        tok = ("d", i, self.dcnt[i])
        for b in R:
            b.t.r["dma%d" % i] = tok
        for b in W:
            b.t.w = tok
            b.t.r = {}

    def barrier(self):
        for e in self.eng:
            for f in self.sem:
                self._need(e, ("c", f, self.cnt[f]))
            for i in range(NDS):
                self._need(e, ("d", i, self.dcnt[i]))

    def finish(self):
        for i in range(NDS):
            self._need("sp", ("d", i, self.dcnt[i]))
        for f in self.sem:
            self._need("sp", ("c", f, self.cnt[f]))


class Arena:
    def __init__(self, ap, width):
        self.ap = ap
        self.width = width
        self.off = 0
        self.hw = 0

    def mark(self):
        return self.off

    def reset(self, m):
        self.off = m

    def f32(self, *shape):
        n = int(np.prod(shape))
        assert self.off + n <= self.width, ("arena overflow", self.off, n, self.width)
        v = self.ap[:, self.off:self.off + n]
        self.off += n
        self.hw = max(self.hw, self.off)
        return Buf(_shape(v, shape))

    def bf16(self, *shape):
        n = int(np.prod(shape))
        w = (n + 1) // 2
        assert self.off + w <= self.width, ("arena overflow", self.off, w, self.width)
        v = self.ap[:, self.off:self.off + w].bitcast(BF16)[:, 0:n]
        self.off += w
        self.hw = max(self.hw, self.off)
        return Buf(_shape(v, shape))


def _shape(v, shape):
    if len(shape) == 1:
        return v
    if len(shape) == 2:
        return v.rearrange("p (a b) -> p a b", b=shape[1])
    if len(shape) == 3:
        return v.rearrange("p (a b c) -> p a b c", b=shape[1], c=shape[2])
    raise ValueError(shape)


def bc(ap, axis, shape):
    return ap.unsqueeze(axis).to_broadcast(list(shape))


def build(TP):
    nblk = TP // NB
    nc = bass.Bass("TRN2", target_bir_lowering=False)
    es = contextlib.ExitStack()

    def din(name, shape):
        return nc.dram_tensor(name, list(shape), F32, kind="ExternalInput").ap()

    def dout(name, shape):
        return nc.dram_tensor(name, list(shape), F32, kind="ExternalOutput").ap()

    i_xT = din("xT", [D, TP])
    i_xsT = din("xsT", [D, NS])
    i_cT = din("cT", [D, 1 + NS])
    i_rgconv = din("rgconv", [D, 3, NS])
    i_rgh = din("rgh", [D, NS])
    i_ssdconv = din("ssdconv", [6144, 3, NS])
    i_ssds = din("ssds", [NS, 128, 64, 64])
    i_cst = din("cst", [128, 4 * 128 + 16])
    i_vec = din("vec", [128, 16 * 11 + 32 * 2 + 48 + 32])
    i_bmod = din("bmod", [128, 2, 96])
    i_rgcw = din("rgcw", [128, 16, 4])
    i_ssdcw = din("ssdcw", [128, 48, 4])
    i_ssdh = din("ssdh", [3, 64])
    i_wmod = din("wmod", [2, 96, 128, 16, 128])
    i_rgwin = din("rgwin", [32, 128, 16, 128])
    i_rgwa = din("rgwa", [16, 128, 2, 128])
    i_rgwi = din("rgwi", [16, 128, 2, 128])
    i_rgwout = din("rgwout", [16, 128, 16, 128])
    i_ssdwin = din("ssdwin", [80, 128, 16, 128])
    i_ssdwdt = din("ssdwdt", [128, 16, 64])
    i_ssdwout = din("ssdwout", [16, 128, 32, 128])
    i_wq = din("wq", [2, 16, 128, 16, 128])
    i_kT = din("kT", [2, 2, 128, 128])
    i_uT = din("uT", [2, 128, 128, 16, 128])
    i_v = din("v", [2, 128, 128, 2048])
    o_yT = dout("o_yT", [D, TP])
    o_ysT = dout("o_ysT", [D, NS])
    o_rgconv_p = dout("o_rgconv_p", [128, 16, 3])
    o_rgh_p = dout("o_rgh_p", [128, 16])
    o_ssdconv_p = dout("o_ssdconv_p", [128, 48, 3])
    o_ssd_p = dout("o_ssd_p", [128, 64, 64])
    o_rgconv_s = dout("o_rgconv_s", [128, 16, 3, NS])
    o_rgh_s = dout("o_rgh_s", [128, 16, NS])
    o_ssdconv_s = dout("o_ssdconv_s", [128, 48, 3, NS])
    o_ssd_s = dout("o_ssd_s", [NS, 128, 64, 64])

    AW = 51800
    arena_t = es.enter_context(nc.sbuf_tensor("arena", [128, AW], F32))
    A = Arena(arena_t[:, :], AW)
    banks = [Buf(es.enter_context(nc.psum_tensor("pb%d" % i, [128, 512], F32))[:, :]) for i in range(8)]
    P = Prog(nc, es)
    op, dma = P.op, P.dma

    cst = A.f32(4 * 128 + 16)
    ident = cst.ap[:, 0:128]
    ones = cst.ap[:, 128:256]
    tri = cst.ap[:, 256:384]
    mneg = cst.ap[:, 384:512]
    selm = cst.ap[:, 512:528]
    dma(cst.ap, i_cst, W=[cst])
    identb = A.bf16(128)
    op("dve", lambda e: e.tensor_copy(out=identb.ap, in_=ident), R=[cst], W=[identb])
    vec = A.f32(16 * 11 + 32 * 2 + 48 + 32)
    dma(vec.ap, i_vec, W=[vec])

    def vsl(o, n):
        return vec.ap[:, o:o + n]
    n1g = [vsl(0, 16), vsl(16, 16)]
    n2g = [vsl(32, 16), vsl(48, 16)]
    fing = vsl(64, 16)
    rg_bconv = vsl(80, 16)
    rg_ba = vsl(96, 16)
    rg_bi = vsl(112, 16)
    rg_lam = vsl(128, 16)
    rg_bout = vsl(144, 16)
    rg_bin = vsl(176, 32)
    ssd_ng = vsl(208, 32)
    ssd_bconv = vsl(240, 48)
    ssd_dexp = vsl(288, 32)
    rgcw = A.f32(16, 4)
    dma(rgcw.ap, i_rgcw, W=[rgcw])
    ssdcw = A.f32(48, 4)
    dma(ssdcw.ap, i_ssdcw, W=[ssdcw])
    ssdh = A.f32(3, 64)
    dma(ssdh.ap, i_ssdh.partition_broadcast(128), W=[ssdh])
    Abc = A.f32(64)
    op("act", lambda e: e.activation(out=Abc.ap, in_=ssdh.ap[:, 1, :], func=AF.Exp), R=[ssdh], W=[Abc])
    op("dve", lambda e: e.tensor_scalar(out=Abc.ap, in0=Abc.ap, scalar1=-1.0, scalar2=None, op0=ALU.mult),
       R=[Abc], W=[Abc])
    c8 = A.f32(16)
    op("act", lambda e: e.activation(out=c8.ap, in_=rg_lam, func=AF.Exp, scale=-1.0), R=[vec], W=[c8])
    op("act", lambda e: e.activation(out=c8.ap, in_=c8.ap, func=AF.Ln, bias=1.0), R=[c8], W=[c8])
    op("dve", lambda e: e.tensor_scalar(out=c8.ap, in0=c8.ap, scalar1=-8.0, scalar2=None, op0=ALU.mult),
       R=[c8], W=[c8])
    epsb = A.f32(1)
    op("pool", lambda e: e.memset(epsb.ap, EPS), W=[epsb])
    kT = A.f32(4, 128)
    for l_ in range(2):
        for h_ in range(2):
            dma(kT.ap[:, l_ * 2 + h_, :], i_kT[l_, h_], W=[kT])

    modT = A.f32(2, 96, 1 + NS)
    csT = A.f32(KC, 1 + NS)
    for kc_ in range(KC):
        dma(csT.ap[:, kc_, :], i_cT[kc_ * 128:(kc_ + 1) * 128, :], W=[csT])
    op("act", lambda e: e.activation(out=csT.ap, in_=csT.ap, func=AF.Silu), R=[csT], W=[csT])
    bmod = A.f32(2, 96)
    dma(bmod.ap, i_bmod, W=[bmod])
    m0 = A.mark()
    NST = 4
    stg = [A.f32(KC, 128) for _ in range(NST)]
    jobs = [(l, j) for l in range(2) for j in range(96)]
    for idx in range(min(NST - 1, len(jobs))):
        l, j = jobs[idx]
        dma(stg[idx % NST].ap, i_wmod[l, j], W=[stg[idx % NST]])
    for idx, (l, j) in enumerate(jobs):
        nx = idx + NST - 1
        if nx < len(jobs):
            dma(stg[nx % NST].ap, i_wmod[jobs[nx][0], jobs[nx][1]], W=[stg[nx % NST]])
        s = stg[idx % NST]
        pb = banks[idx % 2]
        for kc in range(KC):
            op("pe", lambda e, kc=kc: e.matmul(pb.ap[:, 0:1 + NS], s.ap[:, kc, :], csT.ap[:, kc, :],
                                                start=(kc == 0), stop=(kc == KC - 1)), R=[s, csT], W=[pb])
        op("dve", lambda e: e.tensor_scalar(out=modT.ap[:, l, j, :], in0=pb.ap[:, 0:1 + NS],
                                            scalar1=bmod.ap[:, l, j:j + 1], scalar2=None, op0=ALU.add),
           R=[pb, bmod], W=[modT])
    A.reset(m0)
    P.barrier()
    gmp = A.f32(2, 2, 16)
    gms = A.f32(4, 16, NS)
    for l in range(2):
        for k, (sc0, ng) in enumerate(((16, n1g[l]), (64, n2g[l]))):
            op("dve", lambda e: e.scalar_tensor_tensor(out=gmp.ap[:, l, k, :], in0=modT.ap[:, l, sc0:sc0 + 16, 0],
                                                       scalar=1.0, in1=ng, op0=ALU.add, op1=ALU.mult),
               R=[modT, vec], W=[gmp])
            op("dve", lambda e: e.scalar_tensor_tensor(out=gms.ap[:, l * 2 + k, :, :],
                                                       in0=modT.ap[:, l, sc0:sc0 + 16, 1:1 + NS], scalar=1.0,
                                                       in1=bc(ng, 2, [128, 16, NS]), op0=ALU.add, op1=ALU.mult),
               R=[modT, vec], W=[gms])

    def modv(l, k, c, samp):
        if samp:
            return modT.ap[:, l, 16 * k + c, 1:1 + NS]
        return modT.ap[:, l, 16 * k + c, 0:1]

    xT = A.f32(KC, NB)
    rg_tail = A.f32(16, 3)
    rg_carry = A.f32(16)
    ssd_tail = A.f32(48, 3)
    for b_ in (rg_tail, rg_carry, ssd_tail):
        op("pool", lambda e, b_=b_: e.memset(b_.ap, 0.0), W=[b_])
    sT_dram = Buf(o_ssd_p)
    pmark = A.mark()

    class WStream:
        def __init__(self, srcs, shape, ceng=("pool",), nstage=3, nbf=3):
            self.srcs = srcs
            self.shape = shape
            self.stage = [A.f32(*shape) for _ in range(nstage)]
            self.bfs = [A.bf16(*shape) for _ in range(nbf)]
            self.ceng = ceng
            self.nl = 0
            self.ncast = 0
            self.pf = nstage - 1
            for _ in range(min(self.pf, len(srcs))):
                self._load()

        def _load(self):
            if self.nl < len(self.srcs):
                s = self.stage[self.nl % len(self.stage)]
                dma(s.ap, self.srcs[self.nl], W=[s])
                self.nl += 1

        def get(self):
            i = self.ncast
            self._load()
            s = self.stage[i % len(self.stage)]
            b = self.bfs[i % len(self.bfs)]
            e_ = self.ceng[i % len(self.ceng)]
            if e_ == "act":
                op("act", lambda e: e.copy(out=b.ap, in_=s.ap), R=[s], W=[b])
            else:
                op(e_, lambda e: e.tensor_copy(out=b.ap, in_=s.ap), R=[s], W=[b])
            self.ncast += 1
            return b

    def rms_stats(src_chunks, srcbufs, N, nch, scratch, pb, rstd, eng_sq="act"):
        for c in range(nch):
            sq = scratch[c % len(scratch)]
            op("act", lambda e, c=c, sq=sq: e.activation(out=sq.ap[:, 0:N], in_=src_chunks(c), func=AF.Square),
               R=srcbufs, W=[sq])
            op("pe", lambda e, c=c, sq=sq: e.matmul(pb.ap[:, 0:N], ones, sq.ap[:, 0:N], start=(c == 0),
                                                    stop=(c == nch - 1)), R=[sq, cst], W=[pb])
        op("act", lambda e: e.activation(out=rstd.ap[:, 0:N], in_=pb.ap[:, 0:N], func=AF.Sqrt,
                                         scale=1.0 / (nch * 128), bias=epsb.ap[:, 0:1]), R=[pb, epsb], W=[rstd])
        op("dve", lambda e: e.reciprocal(out=rstd.ap[:, 0:N], in_=rstd.ap[:, 0:N]), R=[rstd], W=[rstd])

    def norm_mod(l, which, N, samp, dst):
        m = A.mark()
        scr = [A.f32(NB), A.f32(NB)]
        rstd = A.f32(NB)
        tmp = [A.f32(NB), A.f32(NB)]
        rms_stats(lambda c: xT.ap[:, c, 0:N], [xT], N, KC, scr, banks[7], rstd)
        for c in range(KC):
            t = tmp[c % 2]
            if samp:
                gm = gms.ap[:, l * 2 + which, c, :]
                sh = modv(l, 3 * which, c, True)
                op("dve", lambda e: e.tensor_tensor(out=t.ap[:, 0:N], in0=xT.ap[:, c, 0:N], in1=rstd.ap[:, 0:N],
                                                    op=ALU.mult), R=[xT, rstd], W=[t])
                op("dve", lambda e: e.tensor_tensor(out=t.ap[:, 0:N], in0=t.ap[:, 0:N], in1=gm, op=ALU.mult),
                   R=[t, gms], W=[t])
                op("dve", lambda e: e.tensor_tensor(out=dst.ap[:, c, 0:N], in0=t.ap[:, 0:N], in1=sh, op=ALU.add),
                   R=[t, modT], W=[dst])
            else:
                gm = gmp.ap[:, l, which, c:c + 1]
                sh = modv(l, 3 * which, c, False)
                op("dve", lambda e: e.scalar_tensor_tensor(out=t.ap[:, 0:N], in0=xT.ap[:, c, 0:N], scalar=gm,
                                                           in1=rstd.ap[:, 0:N], op0=ALU.mult, op1=ALU.mult),
                   R=[xT, rstd, gmp], W=[t])
                op("act", lambda e: e.activation(out=dst.ap[:, c, 0:N], in_=t.ap[:, 0:N], func=AF.Identity,
                                                 bias=sh, scale=1.0), R=[t, modT], W=[dst])
        A.reset(m)

    def resid_add(c, pb, N, bias, gate, samp, tmpb):
        if samp:
            if bias is not None:
                op("dve", lambda e: e.scalar_tensor_tensor(out=tmpb.ap[:, 0:N], in0=pb.ap[:, 0:N], scalar=bias,
                                                           in1=gate, op0=ALU.add, op1=ALU.mult),
                   R=[pb, vec, modT], W=[tmpb])
            else:
                op("dve", lambda e: e.tensor_tensor(out=tmpb.ap[:, 0:N], in0=pb.ap[:, 0:N], in1=gate, op=ALU.mult),
                   R=[pb, modT], W=[tmpb])
            op("dve", lambda e: e.tensor_tensor(out=xT.ap[:, c, 0:N], in0=xT.ap[:, c, 0:N], in1=tmpb.ap[:, 0:N],
                                                op=ALU.add), R=[tmpb, xT], W=[xT])
        else:
            if bias is not None:
                op("dve", lambda e: e.tensor_scalar(out=tmpb.ap[:, 0:N], in0=pb.ap[:, 0:N], scalar1=bias,
                                                    scalar2=gate, op0=ALU.add, op1=ALU.mult),
                   R=[pb, vec, modT], W=[tmpb])
                op("dve", lambda e: e.tensor_tensor(out=xT.ap[:, c, 0:N], in0=xT.ap[:, c, 0:N],
                                                    in1=tmpb.ap[:, 0:N], op=ALU.add), R=[tmpb, xT], W=[xT])
            else:
                op("dve", lambda e: e.scalar_tensor_tensor(out=xT.ap[:, c, 0:N], in0=pb.ap[:, 0:N], scalar=gate,
                                                           in1=xT.ap[:, c, 0:N], op0=ALU.mult, op1=ALU.add),
                   R=[pb, modT, xT], W=[xT])

    def conv4(dst, taps, wcol, bcol, N, Rb, Wb):
        op("dve", lambda e: e.tensor_scalar(out=dst, in0=taps[0], scalar1=wcol(0), scalar2=bcol, op0=ALU.mult,
                                            op1=ALU.add), R=Rb, W=Wb)
        for k in range(1, 4):
            op("dve", lambda e, k=k: e.scalar_tensor_tensor(out=dst, in0=taps[k], scalar=wcol(k), in1=dst,
                                                            op0=ALU.mult, op1=ALU.add), R=Rb + Wb, W=Wb)

    def rg_mixer(N, samp, last, hmT):
        m = A.mark()
        gT = A.bf16(KC, NB)
        yT = A.bf16(KC, NB)
        if samp:
            nbuf = A.f32(KC, 3, NS)
            hout = A.f32(KC, NS)
        m_in = A.mark()
        win_srcs = []
        for p in range(8):
            win_srcs += [i_rgwin[2 * p], i_rgwin[2 * p + 1], i_rgwin[16 + 2 * p], i_rgwin[16 + 2 * p + 1]]
        ws = WStream(win_srcs, (KC, 128), ceng=("act", "dve", "pool", "act", "dve"), nstage=2, nbf=3)
        gsrcs = []
        for p in range(8):
            gsrcs += [i_rgwa[2 * p], i_rgwa[2 * p + 1], i_rgwi[2 * p], i_rgwi[2 * p + 1]]
        gs = WStream(gsrcs, (2, 128), ceng=("pool",), nstage=4, nbf=4)
        xe = [A.f32(2, NB + 3), A.f32(2, NB + 3)]
        xc = [A.f32(2, NB), A.f32(2, NB)]
        xcb = [A.bf16(2, NB), A.bf16(2, NB)]
        gate_r = [A.f32(NB), A.f32(NB)]
        gate_i = [A.f32(NB), A.f32(NB)]
        av = [A.f32(NB), A.f32(NB)]
        bv = [A.f32(NB), A.f32(NB)]
        hs = [A.f32(NB), A.f32(NB)]
        if samp:
            st_c = A.f32(KC, 3, NS)
            for c_ in range(KC):
                dma(st_c.ap[:, c_], i_rgconv[c_ * 128:(c_ + 1) * 128], W=[st_c])
            h0s = A.f32(KC, NS)
            for c_ in range(KC):
                dma(h0s.ap[:, c_, :], i_rgh[c_ * 128:(c_ + 1) * 128, :], W=[h0s])
        for p in range(8):
            xe_, xc_, xcb_ = xe[p % 2], xc[p % 2], xcb[p % 2]
            for q4 in range(4):
                wb = ws.get()
                pb = banks[q4 % 4]
                for kc in range(KC):
                    op("pe", lambda e, kc=kc: e.matmul(pb.ap[:, 0:N], wb.ap[:, kc, :], hmT.ap[:, kc, 0:N],
                                                        start=(kc == 0), stop=(kc == KC - 1)), R=[wb, hmT], W=[pb])
                if q4 < 2:
                    c = 2 * p + q4
                    op("act", lambda e: e.activation(out=gT.ap[:, c, 0:N], in_=pb.ap[:, 0:N],
                                                     func=AF.Gelu_apprx_tanh, bias=rg_bin[:, c:c + 1], scale=1.0),
                       R=[pb, vec], W=[gT])
                else:
                    q = q4 - 2
                    c = 2 * p + q
                    if not samp:
                        op("pool", lambda e: e.tensor_copy(out=xe_.ap[:, q, 0:3], in_=rg_tail.ap[:, c, :]),
                           R=[rg_tail], W=[xe_])
                    op("act", lambda e: e.activation(out=xe_.ap[:, q, 3:3 + N], in_=pb.ap[:, 0:N], func=AF.Identity,
                                                     bias=rg_bin[:, 16 + c:16 + c + 1], scale=1.0),
                       R=[pb, vec], W=[xe_])
            for q in range(2):
                c = 2 * p + q
                if samp:
                    taps = [st_c.ap[:, c, 0, :], st_c.ap[:, c, 1, :], st_c.ap[:, c, 2, :], xe_.ap[:, q, 3:3 + N]]
                    Rb = [xe_, st_c, rgcw, vec]
                else:
                    taps = [xe_.ap[:, q, k:k + N] for k in range(4)]
                    Rb = [xe_, rgcw, vec]
                conv4(xc_.ap[:, q, 0:N], taps, lambda k: rgcw.ap[:, c, k:k + 1], rg_bconv[:, c:c + 1], N, Rb, [xc_])
                op("act", lambda e: e.copy(out=xcb_.ap[:, q, 0:N], in_=xc_.ap[:, q, 0:N]), R=[xc_], W=[xcb_])
                if samp:
                    for k in range(2):
                        op("pool", lambda e, k=k: e.tensor_copy(out=nbuf.ap[:, c, k, :], in_=st_c.ap[:, c, k + 1, :]),
                           R=[st_c], W=[nbuf])
                    op("pool", lambda e: e.tensor_copy(out=nbuf.ap[:, c, 2, :], in_=xe_.ap[:, q, 3:3 + N]),
                       R=[xe_], W=[nbuf])
                else:
                    op("pool", lambda e: e.tensor_copy(out=rg_tail.ap[:, c, :], in_=xe_.ap[:, q, N:N + 3]),
                       R=[xe_], W=[rg_tail])
            gw = [gs.get() for _ in range(4)]
            for jh in range(2):
                c = 2 * p + jh
                r_, i_, a_, b_, h_ = gate_r[jh], gate_i[jh], av[jh], bv[jh], hs[jh]
                for gi_, (wt, dstb, bcol) in enumerate(((gw[jh], r_, rg_ba), (gw[2 + jh], i_, rg_bi))):
                    pb = banks[4 + (2 * jh + gi_) % 4]
                    for ih in range(2):
                        op("pe", lambda e, ih=ih: e.matmul(pb.ap[:, 0:N], wt.ap[:, ih, :], xcb_.ap[:, ih, 0:N],
                                                            start=(ih == 0), stop=(ih == 1)), R=[wt, xcb_], W=[pb])
                    op("act", lambda e: e.activation(out=dstb.ap[:, 0:N], in_=pb.ap[:, 0:N], func=AF.Sigmoid,
                                                     bias=bcol[:, c:c + 1], scale=1.0), R=[pb, vec], W=[dstb])
                op("act", lambda e: e.activation(out=a_.ap[:, 0:N], in_=r_.ap[:, 0:N], func=AF.Exp,
                                                 scale=c8.ap[:, c:c + 1]), R=[r_, c8], W=[a_])
                op("dve", lambda e: e.tensor_tensor(out=b_.ap[:, 0:N], in0=a_.ap[:, 0:N], in1=a_.ap[:, 0:N],
                                                    op=ALU.mult), R=[a_], W=[b_])
                op("dve", lambda e: e.tensor_scalar(out=b_.ap[:, 0:N], in0=b_.ap[:, 0:N], scalar1=-1.0, scalar2=1.0,
                                                    op0=ALU.mult, op1=ALU.add), R=[b_], W=[b_])
                op("dve", lambda e: e.tensor_scalar(out=b_.ap[:, 0:N], in0=b_.ap[:, 0:N], scalar1=1e-30,
                                                    scalar2=None, op0=ALU.max), R=[b_], W=[b_])
                op("act", lambda e: e.activation(out=b_.ap[:, 0:N], in_=b_.ap[:, 0:N], func=AF.Sqrt),
                   R=[b_], W=[b_])
                op("dve", lambda e: e.tensor_tensor(out=i_.ap[:, 0:N], in0=i_.ap[:, 0:N], in1=xc_.ap[:, jh, 0:N],
                                                    op=ALU.mult), R=[i_, xc_], W=[i_])
                op("dve", lambda e: e.tensor_tensor(out=b_.ap[:, 0:N], in0=b_.ap[:, 0:N], in1=i_.ap[:, 0:N],
                                                    op=ALU.mult), R=[b_, i_], W=[b_])
                if samp:
                    op("dve", lambda e: e.tensor_tensor(out=h_.ap[:, 0:N], in0=a_.ap[:, 0:N], in1=h0s.ap[:, c, :],
                                                        op=ALU.mult), R=[a_, h0s], W=[h_])
                    op("dve", lambda e: e.tensor_tensor(out=h_.ap[:, 0:N], in0=h_.ap[:, 0:N], in1=b_.ap[:, 0:N],
                                                        op=ALU.add), R=[h_, b_], W=[h_])
                    op("pool", lambda e: e.tensor_copy(out=hout.ap[:, c, :], in_=h_.ap[:, 0:N]), R=[h_], W=[hout])
                else:
                    op("dve", lambda e: e.tensor_tensor_scan(out=h_.ap[:, 0:N], data0=a_.ap[:, 0:N],
                                                             data1=b_.ap[:, 0:N], initial=rg_carry.ap[:, c:c + 1],
                                                             op0=ALU.mult, op1=ALU.add),
                       R=[a_, b_, rg_carry], W=[h_])
                    op("pool", lambda e: e.tensor_copy(out=rg_carry.ap[:, c:c + 1], in_=h_.ap[:, N - 1:N]),
                       R=[h_], W=[rg_carry])
                op("dve", lambda e: e.tensor_tensor(out=yT.ap[:, c, 0:N], in0=h_.ap[:, 0:N], in1=gT.ap[:, c, 0:N],
                                                    op=ALU.mult), R=[h_, gT], W=[yT])
        P.barrier()
        A.reset(m_in)
        wo = WStream([i_rgwout[j] for j in range(16)], (KC, 128), ceng=("act", "dve", "pool", "act", "dve"))
        tmpb = [A.f32(NB), A.f32(NB)]
        for j in range(16):
            wb = wo.get()
            pb = banks[j % 4]
            for kc in range(KC):
                op("pe", lambda e, kc=kc: e.matmul(pb.ap[:, 0:N], wb.ap[:, kc, :], yT.ap[:, kc, 0:N],
                                                    start=(kc == 0), stop=(kc == KC - 1)), R=[wb, yT], W=[pb])
            resid_add(j, pb, N, rg_bout[:, j:j + 1], modv(0, 2, j, samp), samp, tmpb[j % 2])
        if samp:
            dma(o_rgconv_s, nbuf.ap, R=[nbuf])
            dma(o_rgh_s, hout.ap, R=[hout])
        elif last:
            dma(o_rgconv_p, rg_tail.ap, R=[rg_tail])
            dma(o_rgh_p, rg_carry.ap, R=[rg_carry])
        P.barrier()
        A.reset(m)

    def ssd_chunk(Q, t0, sT, hmT, xbcT, yT, dtw):
        m = A.mark()
        pdt = banks[0]
        for kc in range(KC):
            op("pe", lambda e, kc=kc: e.matmul(pdt.ap[0:Q, 0:64], hmT.ap[:, kc, t0:t0 + Q], dtw.ap[:, kc, :],
                                                start=(kc == 0), stop=(kc == KC - 1)), R=[hmT, dtw], W=[pdt])
        dt = A.f32(64)
        dtA = A.f32(64)
        op("dve", lambda e: e.tensor_tensor(out=dt.ap[0:Q, :], in0=pdt.ap[0:Q, 0:64], in1=ssdh.ap[0:Q, 0, :],
                                            op=ALU.add), R=[pdt, ssdh], W=[dt])
        op("act", lambda e: e.activation(out=dt.ap[0:Q, :], in_=dt.ap[0:Q, :], func=AF.Exp), R=[dt], W=[dt])
        op("act", lambda e: e.activation(out=dt.ap[0:Q, :], in_=dt.ap[0:Q, :], func=AF.Ln, bias=1.0),
           R=[dt], W=[dt])
        op("dve", lambda e: e.tensor_tensor(out=dtA.ap[0:Q, :], in0=dt.ap[0:Q, :], in1=Abc.ap[0:Q, :],
                                            op=ALU.mult), R=[dt, Abc], W=[dtA])
        pc = banks[1]
        op("pe", lambda e: e.matmul(pc.ap[0:Q, 0:64], tri[0:Q, 0:Q], dtA.ap[0:Q, :], start=True, stop=True),
           R=[dtA, cst], W=[pc])
        ncum = A.f32(64)
        op("dve", lambda e: e.tensor_scalar(out=ncum.ap[0:Q, :], in0=pc.ap[0:Q, 0:64], scalar1=-1.0, scalar2=None,
                                            op0=ALU.mult), R=[pc], W=[ncum])
        xtok = A.bf16(4096)
        btok = A.bf16(1024)
        for grp in range(5):
            pbt = banks[2 + grp % 2]
            pv = pbt.ap.bitcast(BF16)
            for k in range(8):
                j = grp * 8 + k
                op("pe", lambda e, j=j, k=k: e.transpose(pv[0:Q, k * 128:(k + 1) * 128], xbcT.ap[:, j, t0:t0 + Q],
                                                         identb.ap), R=[xbcT, identb], W=[pbt])
            if grp < 4:
                op("act", lambda e: e.copy(out=xtok.ap[0:Q, grp * 1024:(grp + 1) * 1024], in_=pv[0:Q, :]),
                   R=[pbt], W=[xtok])
            else:
                op("act", lambda e: e.copy(out=btok.ap[0:Q, :], in_=pv[0:Q, :]), R=[pbt], W=[btok])
        cbt = A.f32(8, 128)
        for g in range(8):
            pcb = banks[4 + g % 2]
            op("pe", lambda e: e.matmul(pcb.ap[0:Q, 0:Q], xbcT.ap[:, 32 + g, t0:t0 + Q], xbcT.ap[:, 40 + g, t0:t0 + Q],
                                        start=True, stop=True), R=[xbcT], W=[pcb])
            op("act", lambda e: e.copy(out=cbt.ap[0:Q, g, 0:Q], in_=pcb.ap[0:Q, 0:Q]), R=[pcb], W=[cbt])
        sTb = A.bf16(64, 64)
        op("pool", lambda e: e.tensor_copy(out=sTb.ap, in_=sT.ap), R=[sT], W=[sTb])
        rhs4 = [A.f32(4, 128), A.f32(4, 128)]
        arg4 = [A.f32(4, 128), A.f32(4, 128)]
        ET4 = [A.f32(4, 128), A.f32(4, 128)]
        ecr4 = [A.f32(4, 128), A.f32(4, 128)]
        WT = [A.bf16(128) for _ in range(4)]
        CpT = [A.bf16(128) for _ in range(4)]
        xw = [A.bf16(64) for _ in range(4)]
        for h4 in range(16):
            r4, a4, E4, c4 = rhs4[h4 % 2], arg4[h4 % 2], ET4[h4 % 2], ecr4[h4 % 2]
            g = h4 // 2
            op("pool", lambda e: e.tensor_tensor(out=r4.ap[0:Q, :, 0:Q], in0=bc(tri[0:Q, 0:Q], 1, [Q, 4, Q]),
                                                 in1=bc(dtA.ap[0:Q, h4 * 4:h4 * 4 + 4], 2, [Q, 4, Q]), op=ALU.mult),
               R=[dtA, cst], W=[r4])
            pcr = banks[6 + h4 % 2]
            pcr_v = pcr.ap.rearrange("p (a b) -> p a b", b=128)
            for hh in range(4):
                op("pe", lambda e, hh=hh: e.matmul(pcr_v[:, hh, 0:Q], ones[0:Q, :], r4.ap[0:Q, hh, 0:Q],
                                                    start=True, stop=True), R=[r4, cst], W=[pcr])
            for hh in range(4):
                h = h4 * 4 + hh
                op("dve", lambda e, hh=hh, h=h: e.scalar_tensor_tensor(out=a4.ap[0:Q, hh, 0:Q], in0=pcr_v[0:Q, hh, 0:Q],
                                                                       scalar=ncum.ap[0:Q, h:h + 1], in1=mneg[0:Q, 0:Q],
                                                                       op0=ALU.add, op1=ALU.min),
                   R=[pcr, ncum, cst], W=[a4])
            op("act", lambda e: e.activation(out=E4.ap[0:Q, :, 0:Q], in_=a4.ap[0:Q, :, 0:Q], func=AF.Exp),
               R=[a4], W=[E4])
            op("act", lambda e: e.activation(out=c4.ap[:, :, 0:Q], in_=pcr_v[:, :, 0:Q], func=AF.Exp),
               R=[pcr], W=[c4])
            for hh in range(4):
                h = h4 * 4 + hh
                w_, cp_, xw_ = WT[hh], CpT[hh], xw[hh]
                op("dve", lambda e, hh=hh, h=h, w_=w_: e.scalar_tensor_tensor(
                    out=w_.ap[0:Q, 0:Q], in0=E4.ap[0:Q, hh, 0:Q], scalar=dt.ap[0:Q, h:h + 1], in1=cbt.ap[0:Q, g, 0:Q],
                    op0=ALU.mult, op1=ALU.mult), R=[E4, dt, cbt], W=[w_])
                op("pool", lambda e, hh=hh, cp_=cp_: e.tensor_tensor(
                    out=cp_.ap[:, 0:Q], in0=xbcT.ap[:, 40 + g, t0:t0 + Q], in1=c4.ap[:, hh, 0:Q], op=ALU.mult),
                   R=[xbcT, c4], W=[cp_])
                c = h // 2
                pby = banks[0 + (c // 4) % 2]
                po = (h % 2) * 64
                col = (c % 4) * 128
                op("pe", lambda e, h=h, w_=w_, pby=pby, po=po, col=col: e.matmul(
                    pby.ap[po:po + 64, col:col + Q], xtok.ap[0:Q, h * 64:(h + 1) * 64], w_.ap[0:Q, 0:Q],
                    start=True, stop=False), R=[xtok, w_], W=[pby])
                op("pe", lambda e, h=h, cp_=cp_, pby=pby, po=po, col=col: e.matmul(
                    pby.ap[po:po + 64, col:col + Q], sTb.ap[:, h, :], cp_.ap[:, 0:Q],
                    start=False, stop=True), R=[sTb, cp_], W=[pby])
                if h % 2 == 1:
                    op("dve", lambda e, c=c, pby=pby, col=col: e.scalar_tensor_tensor(
                        out=yT.ap[:, c, t0:t0 + Q], in0=xbcT.ap[:, c, t0:t0 + Q], scalar=ssd_dexp[:, c:c + 1],
                        in1=pby.ap[:, col:col + Q], op0=ALU.mult, op1=ALU.add), R=[xbcT, vec, pby], W=[yT])
                op("dve", lambda e, hh=hh, h=h, xw_=xw_: e.tensor_scalar(
                    out=xw_.ap[0:Q, :], in0=xtok.ap[0:Q, h * 64:(h + 1) * 64], scalar1=E4.ap[0:Q, hh, Q - 1:Q],
                    scalar2=dt.ap[0:Q, h:h + 1], op0=ALU.mult, op1=ALU.mult), R=[xtok, E4, dt], W=[xw_])
                pst = banks[2 + (h // 8) % 2]
                scol = (h % 8) * 64
                op("pe", lambda e, xw_=xw_, pst=pst, scol=scol: e.matmul(
                    pst.ap[:, scol:scol + 64], btok.ap[0:Q, g * 128:(g + 1) * 128], xw_.ap[0:Q, :],
                    start=True, stop=True), R=[btok, xw_], W=[pst])
                op("dve", lambda e, hh=hh, h=h, pst=pst, scol=scol: e.scalar_tensor_tensor(
                    out=sT.ap[:, h, :], in0=sT.ap[:, h, :], scalar=c4.ap[:, hh, Q - 1:Q], in1=pst.ap[:, scol:scol + 64],
                    op0=ALU.mult, op1=ALU.add), R=[sT, c4, pst], W=[sT])
        P.barrier()
        A.reset(m)

    def ssd_mixer(N, samp, first, last, hmT):
        m = A.mark()
        xbcT = A.bf16(48, NB)
        yT = xbcT
        dtw = A.bf16(KC, 64)
        m1 = A.mark()
        dst_ = A.f32(KC, 64)
        dma(dst_.ap, i_ssdwdt, W=[dst_])
        op("pool", lambda e: e.tensor_copy(out=dtw.ap, in_=dst_.ap), R=[dst_], W=[dtw])
        ws = WStream([i_ssdwin[32 + j] for j in range(48)], (KC, 128), ceng=("act", "dve", "pool", "act", "dve"))
        xe = [A.f32(NB + 3), A.f32(NB + 3)]
        xcv = [A.f32(NB), A.f32(NB)]
        if samp:
            st_c = A.f32(48, 3, NS)
            for j in range(48):
                dma(st_c.ap[:, j], i_ssdconv[j * 128:(j + 1) * 128], W=[st_c])
            nbuf = A.f32(48, 3, NS)
        for j in range(48):
            wb = ws.get()
            pb = banks[j % 4]
            xe_, xc_ = xe[j % 2], xcv[j % 2]
            for kc in range(KC):
                op("pe", lambda e, kc=kc: e.matmul(pb.ap[:, 0:N], wb.ap[:, kc, :], hmT.ap[:, kc, 0:N],
                                                    start=(kc == 0), stop=(kc == KC - 1)), R=[wb, hmT], W=[pb])
            if not samp:
                op("pool", lambda e: e.tensor_copy(out=xe_.ap[:, 0:3], in_=ssd_tail.ap[:, j, :]),
                   R=[ssd_tail], W=[xe_])
            op("act", lambda e: e.copy(out=xe_.ap[:, 3:3 + N], in_=pb.ap[:, 0:N]), R=[pb], W=[xe_])
            if samp:
                taps = [st_c.ap[:, j, 0, :], st_c.ap[:, j, 1, :], st_c.ap[:, j, 2, :], xe_.ap[:, 3:3 + N]]
                Rb = [xe_, st_c, ssdcw, vec]
            else:
                taps = [xe_.ap[:, k:k + N] for k in range(4)]
                Rb = [xe_, ssdcw, vec]
            conv4(xc_.ap[:, 0:N], taps, lambda k: ssdcw.ap[:, j, k:k + 1], ssd_bconv[:, j:j + 1], N, Rb, [xc_])
            op("act", lambda e: e.activation(out=xbcT.ap[:, j, 0:N], in_=xc_.ap[:, 0:N], func=AF.Silu),
               R=[xc_], W=[xbcT])
            if samp:
                for k in range(2):
                    op("pool", lambda e, k=k: e.tensor_copy(out=nbuf.ap[:, j, k, :], in_=st_c.ap[:, j, k + 1, :]),
                       R=[st_c], W=[nbuf])
                op("pool", lambda e: e.tensor_copy(out=nbuf.ap[:, j, 2, :], in_=xe_.ap[:, 3:3 + N]),
                   R=[xe_], W=[nbuf])
            else:
                op("pool", lambda e: e.tensor_copy(out=ssd_tail.ap[:, j, :], in_=xe_.ap[:, N:N + 3]),
                   R=[xe_], W=[ssd_tail])
        if samp:
            dma(o_ssdconv_s, nbuf.ap, R=[nbuf])
        elif last:
            dma(o_ssdconv_p, ssd_tail.ap, R=[ssd_tail])
        P.barrier()
        A.reset(m1)
        sT = A.f32(64, 64)
        if samp:
            for b in range(NS):
                dma(sT.ap, i_ssds[b], W=[sT])
                ssd_chunk(1, b, sT, hmT, xbcT, yT, dtw)
                dma(o_ssd_s[b], sT.ap, R=[sT])
        else:
            if first:
                op("pool", lambda e: e.memset(sT.ap, 0.0), W=[sT])
            else:
                dma(sT.ap, o_ssd_p, R=[sT_dram], W=[sT])
            for q in range(N // 128):
                ssd_chunk(128, q * 128, sT, hmT, xbcT, yT, dtw)
            dma(o_ssd_p, sT.ap, R=[sT], W=[sT_dram])
        P.barrier()
        A.reset(m1)
        wz = WStream([i_ssdwin[j] for j in range(32)], (KC, 128), ceng=("act", "dve", "pool", "act", "dve"))
        sz = [A.f32(NB), A.f32(NB)]
        sq = [A.f32(NB), A.f32(NB)]
        pst = banks[7]
        for j in range(32):
            wb = wz.get()
            pb = banks[j % 4]
            for kc in range(KC):
                op("pe", lambda e, kc=kc: e.matmul(pb.ap[:, 0:N], wb.ap[:, kc, :], hmT.ap[:, kc, 0:N],
                                                    start=(kc == 0), stop=(kc == KC - 1)), R=[wb, hmT], W=[pb])
            s_, q_ = sz[j % 2], sq[j % 2]
            op("act", lambda e: e.activation(out=s_.ap[:, 0:N], in_=pb.ap[:, 0:N], func=AF.Silu), R=[pb], W=[s_])
            op("dve", lambda e: e.tensor_tensor(out=yT.ap[:, j, 0:N], in0=yT.ap[:, j, 0:N], in1=s_.ap[:, 0:N],
                                                op=ALU.mult), R=[yT, s_], W=[yT])
            op("act", lambda e: e.activation(out=q_.ap[:, 0:N], in_=yT.ap[:, j, 0:N], func=AF.Square),
               R=[yT], W=[q_])
            op("pe", lambda e: e.matmul(pst.ap[:, 0:N], ones, q_.ap[:, 0:N], start=(j == 0), stop=(j == 31)),
               R=[q_, cst], W=[pst])
        P.barrier()
        A.reset(m1)
        rstd = A.f32(NB)
        op("act", lambda e: e.activation(out=rstd.ap[:, 0:N], in_=pst.ap[:, 0:N], func=AF.Sqrt, scale=1.0 / 4096,
                                         bias=epsb.ap[:, 0:1]), R=[pst, epsb], W=[rstd])
        op("dve", lambda e: e.reciprocal(out=rstd.ap[:, 0:N], in_=rstd.ap[:, 0:N]), R=[rstd], W=[rstd])
        for j in range(32):
            op("dve", lambda e: e.scalar_tensor_tensor(out=yT.ap[:, j, 0:N], in0=yT.ap[:, j, 0:N],
                                                       scalar=ssd_ng[:, j:j + 1], in1=rstd.ap[:, 0:N],
                                                       op0=ALU.mult, op1=ALU.mult), R=[yT, vec, rstd], W=[yT])
        wo = WStream([i_ssdwout[j] for j in range(16)], (32, 128), ceng=("act", "dve", "pool", "act", "dve"), nstage=2, nbf=2)
        tmpb = [A.f32(NB), A.f32(NB)]
        for j in range(16):
            wb = wo.get()
            pb = banks[j % 4]
            for kc in range(32):
                op("pe", lambda e, kc=kc: e.matmul(pb.ap[:, 0:N], wb.ap[:, kc, :], yT.ap[:, kc, 0:N],
                                                    start=(kc == 0), stop=(kc == 31)), R=[wb, yT], W=[pb])
            resid_add(j, pb, N, None, modv(1, 2, j, samp), samp, tmpb[j % 2])
        P.barrier()
        A.reset(m)

    def peer(l, N, samp, hcT):
        m = A.mark()
        NG = N // 16
        s12 = A.f32(NB // 16, 256)
        thr = A.f32(NB // 16)
        negm = A.f32(NB // 16)
        Sel = A.bf16(NB // 16, 16)
        m1 = A.mark()
        qT = A.f32(2, NB, 8)
        wq = WStream([i_wq[l, j] for j in range(16)], (KC, 128), ceng=("act", "dve", "pool", "act", "dve"))
        for j in range(16):
            wb = wq.get()
            pb = banks[j % 4]
            for kc in range(KC):
                op("pe", lambda e, kc=kc: e.matmul(pb.ap[:, 0:N], wb.ap[:, kc, :], hcT.ap[:, kc, 0:N],
                                                    start=(kc == 0), stop=(kc == KC - 1)), R=[wb, hcT], W=[pb])
            op("act", lambda e: e.copy(out=qT.ap[:, j % 2, 0:N, j // 2], in_=pb.ap[:, 0:N]), R=[pb], W=[qT])
        vv = [A.f32(2, 16) for _ in range(2)]
        tmp1 = [A.f32(128) for _ in range(2)]
        cand = [A.f32(256) for _ in range(2)]
        cand2 = [A.f32(256) for _ in range(2)]
        c24 = [A.f32(24) for _ in range(2)]
        ez = [A.f32(16) for _ in range(2)]
        zz = [A.f32(2) for _ in range(2)]
        for gi in range(NG):
            pb = banks[4 + gi % 2]
            for half in range(2):
                lhsT = qT.ap[:, half, gi * 16:(gi + 1) * 16, :].rearrange("p t h -> p (t h)")
                op("pe", lambda e, half=half, lhsT=lhsT: e.matmul(pb.ap[:, half * 128:(half + 1) * 128], lhsT,
                                                                  kT.ap[:, l * 2 + half, :], start=True, stop=True),
                   R=[qT, kT], W=[pb])
            op("act", lambda e: e.copy(out=s12.ap[:, gi, :], in_=pb.ap[:, 0:256]), R=[pb], W=[s12])
            v_, t1, cd, cd2, c_, ez_, z_ = (vv[gi % 2], tmp1[gi % 2], cand[gi % 2], cand2[gi % 2], c24[gi % 2],
                                            ez[gi % 2], zz[gi % 2])
            for half in range(2):
                w = s12.ap[:, gi, half * 128:(half + 1) * 128]
                op("dve", lambda e: e.max(out=v_.ap[:, half, 0:8], in_=w), R=[s12], W=[v_])
                op("dve", lambda e: e.match_replace(out=t1.ap, in_to_replace=v_.ap[:, half, 0:8], in_values=w,
                                                    imm_value=-1e30), R=[s12, v_], W=[t1])
                op("dve", lambda e: e.max(out=v_.ap[:, half, 8:16], in_=t1.ap), R=[t1], W=[v_])
            cd3 = cd.ap.rearrange("p (a b) -> p a b", b=16)
            op("dve", lambda e: e.tensor_tensor(out=cd3, in0=bc(v_.ap[:, 0, :], 2, [128, 16, 16]),
                                                in1=bc(v_.ap[:, 1, :], 1, [128, 16, 16]), op=ALU.add),
               R=[v_], W=[cd])
            op("dve", lambda e: e.max(out=c_.ap[:, 0:8], in_=cd.ap), R=[cd], W=[c_])
            op("dve", lambda e: e.match_replace(out=cd2.ap, in_to_replace=c_.ap[:, 0:8], in_values=cd.ap,
                                                imm_value=-1e30), R=[cd, c_], W=[cd2])
            op("dve", lambda e: e.max(out=c_.ap[:, 8:16], in_=cd2.ap), R=[cd2], W=[c_])
            op("dve", lambda e: e.match_replace(out=cd.ap, in_to_replace=c_.ap[:, 8:16], in_values=cd2.ap,
                                                imm_value=-1e30), R=[cd2, c_], W=[cd])
            op("dve", lambda e: e.max(out=c_.ap[:, 16:24], in_=cd.ap), R=[cd], W=[c_])
            op("dve", lambda e: e.tensor_scalar(out=thr.ap[:, gi:gi + 1], in0=c_.ap[:, 15:16],
                                                scalar1=c_.ap[:, 16:17], scalar2=0.5, op0=ALU.add, op1=ALU.mult),
               R=[c_], W=[thr])
            op("dve", lambda e: e.tensor_scalar(out=negm.ap[:, gi:gi + 1], in0=c_.ap[:, 0:1], scalar1=-1.0,
                                                scalar2=None, op0=ALU.mult), R=[c_], W=[negm])
            op("act", lambda e: e.activation(out=ez_.ap, in_=c_.ap[:, 0:16], func=AF.Exp,
                                             bias=negm.ap[:, gi:gi + 1], scale=1.0, accum_out=z_.ap[:, 0:1]),
               R=[c_, negm], W=[ez_, z_])
            op("dve", lambda e: e.reciprocal(out=z_.ap[:, 1:2], in_=z_.ap[:, 0:1]), R=[z_], W=[z_])
            op("dve", lambda e: e.tensor_scalar(out=Sel.ap[:, gi, :], in0=selm, scalar1=z_.ap[:, 1:2], scalar2=None,
                                                op0=ALU.mult), R=[z_, cst], W=[Sel])
        P.barrier()
        A.reset(m1)
        usrc, vsrc = [], []
        for a in range(128):
            usrc += [i_uT[l, a][:, 0:8, :], i_uT[l, a][:, 8:16, :]]
            vsrc += [i_v[l, a][:, 0:1024], i_v[l, a][:, 1024:2048]]
        us = WStream(usrc, (8, 128), ceng=("act",), nstage=3, nbf=2 * GA + 1)
        vs = WStream(vsrc, (1024,), ceng=("act",), nstage=3, nbf=2 * GA + 1)
        FR = 16
        LAG = 3
        NGA = 128 // GA
        NGh = min(NG, 16)
        Dd = [A.f32(GA, 128) for _ in range(3)]
        ee = [A.bf16(GA, 128) for _ in range(3)]
        Fr = [A.bf16(GA, 128) for _ in range(FR)]
        gel = [A.bf16(NB) for _ in range(GA)]
        AT = [A.bf16(NB) for _ in range(GA)]
        stmp = [A.f32(NS), A.f32(NS)]
        st = {"f": 0, "pend": [], "fb": [], "u": {}, "v": {}}

        def fgen(ag, gi):
            k = st["f"]
            st["f"] += 1
            d_, e_, f_ = Dd[k % 3], ee[k % 3], Fr[k % FR]
            a0 = ag * GA
            op("dve", lambda e: e.tensor_tensor(out=d_.ap, in0=bc(s12.ap[:, gi, 128:256], 1, [128, GA, 128]),
                                                in1=bc(s12.ap[:, gi, a0:a0 + GA], 2, [128, GA, 128]),
                                                op=ALU.add), R=[s12], W=[d_])
            op("act", lambda e: e.activation(out=e_.ap, in_=d_.ap, func=AF.Exp, bias=negm.ap[:, gi:gi + 1],
                                             scale=1.0), R=[d_, negm], W=[e_])
            st["fb"].append((gi, d_, e_, f_))
            fgen_b(1)

        def fgen_b(lag):
            while len(st["fb"]) > lag:
                gi, d_, e_, f_ = st["fb"].pop(0)
                op("dve", lambda e: e.scalar_tensor_tensor(out=f_.ap, in0=d_.ap, scalar=thr.ap[:, gi:gi + 1],
                                                           in1=e_.ap, op0=ALU.is_ge, op1=ALU.mult),
                   R=[d_, e_, thr], W=[f_])
                st["pend"].append((gi, f_))

        def gmm_emit(lag):
            if lag == 0:
                fgen_b(0)
            while len(st["pend"]) > lag:
                gi, f_ = st["pend"].pop(0)
                for ai in range(GA):
                    pG = banks[2 + ai]
                    op("pe", lambda e, ai=ai, pG=pG: e.matmul(pG.ap[:, gi * 16:(gi + 1) * 16], f_.ap[:, ai, :],
                                                              Sel.ap[:, gi, :], start=True, stop=True),
                       R=[f_, Sel], W=[pG])

        def ucast(ag, k):
            st["u"][(ag, k)] = us.get()

        def vcast(ag, k):
            st["v"][(ag, k)] = vs.get()

        def sT_pe(ag, ai):
            pS = banks[ai % 2]
            for kc in range(KC):
                ub = st["u"][(ag, 2 * ai + kc // 8)]
                op("pe", lambda e, kc=kc, ub=ub: e.matmul(pS.ap[:, 0:N], ub.ap[:, kc % 8, :], hcT.ap[:, kc, 0:N],
                                                          start=(kc == 0), stop=(kc == KC - 1)),
                   R=[ub, hcT], W=[pS])

        def sT_gelu(ai):
            pS = banks[ai % 2]
            g_ = gel[ai]
            op("act", lambda e: e.activation(out=g_.ap[:, 0:N], in_=pS.ap[:, 0:N], func=AF.Gelu_apprx_tanh),
               R=[pS], W=[g_])

        def aTm():
            for ai in range(GA):
                pG = banks[2 + ai]
                g_, at_ = gel[ai], AT[ai]
                op("dve", lambda e: e.tensor_tensor(out=at_.ap[:, 0:N], in0=g_.ap[:, 0:N], in1=pG.ap[:, 0:N],
                                                    op=ALU.mult), R=[g_, pG], W=[at_])

        def outp(ag, dc):
            pO = banks[6 + dc % 2]
            for ai in range(GA):
                at_ = AT[ai]
                vb = st["v"][(ag, 2 * ai + dc // 8)]
                op("pe", lambda e, ai=ai, at_=at_, vb=vb: e.matmul(
                    pO.ap[:, 0:N], vb.ap[:, (dc % 8) * 128:(dc % 8 + 1) * 128], at_.ap[:, 0:N],
                    start=(ai == 0), stop=(ai == GA - 1)), R=[vb, at_], W=[pO])
            if samp:
                t_ = stmp[dc % 2]
                tv = t_.ap
                op("dve", lambda e: e.tensor_tensor(out=tv[:, 0:N], in0=pO.ap[:, 0:N], in1=modv(l, 5, dc, True),
                                                    op=ALU.mult), R=[pO, modT], W=[t_])
                op("dve", lambda e: e.tensor_tensor(out=xT.ap[:, dc, 0:N], in0=xT.ap[:, dc, 0:N], in1=tv[:, 0:N],
                                                    op=ALU.add), R=[t_, xT], W=[xT])
            else:
                op("dve", lambda e: e.scalar_tensor_tensor(out=xT.ap[:, dc, 0:N], in0=pO.ap[:, 0:N],
                                                           scalar=modv(l, 5, dc, False), in1=xT.ap[:, dc, 0:N],
                                                           op0=ALU.mult, op1=ALU.add), R=[pO, modT, xT], W=[xT])

        def phaseY(ag):
            for pr in range(GA // 2):
                sT_pe(ag, 2 * pr)
                sT_pe(ag, 2 * pr + 1)
                for k in range(8):
                    gi = NGh + 8 * pr + k
                    if k % 2 == 0:
                        vcast(ag, 4 * pr + k // 2)
                    if gi < NG:
                        fgen(ag, gi)
                        gmm_emit(LAG)
                sT_gelu(2 * pr)
                sT_gelu(2 * pr + 1)
            gmm_emit(0)
            aTm()

        for k in range(2 * GA):
            ucast(0, k)
        for gi in range(NGh):
            fgen(0, gi)
            gmm_emit(LAG)
        gmm_emit(0)
        phaseY(0)
        for ag in range(NGA):
            nxt = ag + 1 < NGA
            for dc in range(16):
                outp(ag, dc)
                if nxt:
                    if dc % 2 == 0:
                        ucast(ag + 1, dc // 2)
                    if dc < NGh:
                        fgen(ag + 1, dc)
                    gmm_emit(LAG)
            if nxt:
                gmm_emit(0)
                phaseY(ag + 1)
        P.barrier()
        A.reset(m)

    def run_block(N, samp, first, last, src, dst):
        dma(xT.ap[:, :, 0:N], src, W=[xT])
        for l in range(2):
            m = A.mark()
            hmT = A.bf16(KC, NB)
            norm_mod(l, 0, N, samp, hmT)
            if l == 0:
                rg_mixer(N, samp, last, hmT)
            else:
                ssd_mixer(N, samp, first, last, hmT)
            norm_mod(l, 1, N, samp, hmT)
            peer(l, N, samp, hmT)
            A.reset(m)
        m = A.mark()
        scr = [A.f32(NB), A.f32(NB)]
        rstd = A.f32(NB)
        yo = A.f32(KC, NB)
        rms_stats(lambda c: xT.ap[:, c, 0:N], [xT], N, KC, scr, banks[7], rstd)
        for c in range(KC):
            op("dve", lambda e: e.scalar_tensor_tensor(out=yo.ap[:, c, 0:N], in0=xT.ap[:, c, 0:N],
                                                       scalar=fing[:, c:c + 1], in1=rstd.ap[:, 0:N], op0=ALU.mult,
                                                       op1=ALU.mult), R=[xT, vec, rstd], W=[yo])
        dma(dst, yo.ap[:, :, 0:N], R=[yo])
        P.barrier()
        A.reset(m)

    for blk in range(nblk):
        run_block(NB, False, blk == 0, blk == nblk - 1,
                  i_xT[:, blk * NB:(blk + 1) * NB].rearrange("(c p) t -> p c t", p=128),
                  o_yT[:, blk * NB:(blk + 1) * NB].rearrange("(c p) t -> p c t", p=128))
    run_block(NS, True, True, False, i_xsT.rearrange("(c p) t -> p c t", p=128),
              o_ysT.rearrange("(c p) t -> p c t", p=128))
    P.finish()
    es.close()
    nc._arena_hw = A.hw
    return nc


def _wlay(w):
    K, N = w.shape
    return np.ascontiguousarray(w.reshape(K // 128, 128, N // 128, 128).transpose(2, 1, 0, 3))


def _vlay(v):
    return v.reshape(-1, 128).T


_CACHE = {}


def _prep_shared(inp):
    f = np.float32
    sh = {}
    cst = np.zeros((128, 4 * 128 + 16), f)
    cst[:, 0:128] = np.eye(128)
    cst[:, 128:256] = 1.0
    k = np.arange(128)
    cst[:, 256:384] = (k[:, None] <= k[None, :])
    cst[:, 384:512] = np.where(k[:, None] <= k[None, :], 0.0, NEG)
    cst[:, 512:528] = (k[:, None] // 8 == np.arange(16)[None, :])
    sh["cst"] = cst
    vec = np.zeros((128, 16 * 11 + 32 * 2 + 48 + 32), f)
    vec[:, 0:16] = _vlay(inp["norm1_g"][0]); vec[:, 16:32] = _vlay(inp["norm1_g"][1])
    vec[:, 32:48] = _vlay(inp["norm2_g"][0]); vec[:, 48:64] = _vlay(inp["norm2_g"][1])
    vec[:, 64:80] = _vlay(inp["final_g"])
    vec[:, 80:96] = _vlay(inp["rg_conv_b"][0])
    vec[:, 96:112] = _vlay(inp["rg_b_a"][0])
    vec[:, 112:128] = _vlay(inp["rg_b_i"][0])
    vec[:, 128:144] = _vlay(inp["rg_lambda"][0])
    vec[:, 144:160] = _vlay(inp["rg_b_out"][0])
    vec[:, 176:208] = _vlay(inp["rg_b_in"][0])
    vec[:, 208:240] = _vlay(inp["ssd_norm_g"][0])
    vec[:, 240:288] = _vlay(inp["ssd_conv_b"][0])
    vec[:, 288:320] = _vlay(np.repeat(inp["ssd_d"][0], 64))
    sh["vec"] = vec
    sh["bmod"] = np.ascontiguousarray(np.stack([_vlay(inp["b_mod"][l]) for l in range(2)], axis=1))
    sh["rgcw"] = np.ascontiguousarray(inp["rg_conv_w"][0].T.reshape(16, 128, 4).transpose(1, 0, 2))
    sh["ssdcw"] = np.ascontiguousarray(inp["ssd_conv_w"][0].T.reshape(48, 128, 4).transpose(1, 0, 2))
    sh["ssdh"] = np.ascontiguousarray(np.stack([inp["ssd_dt_bias"][0], inp["ssd_a_log"][0],
                                                inp["ssd_d"][0]]).astype(f))
    sh["wmod"] = np.stack([_wlay(inp["w_mod"][l]) for l in range(2)])
    sh["rgwin"] = _wlay(inp["rg_w_in"][0])

    def glay(w):
        return np.ascontiguousarray(w.reshape(8, 2, 128, 2, 128).transpose(0, 3, 2, 1, 4).reshape(16, 128, 2, 128))
    sh["rgwa"] = glay(inp["rg_w_a"][0])
    sh["rgwi"] = glay(inp["rg_w_i"][0])
    sh["rgwout"] = _wlay(inp["rg_w_out"][0])
    w = inp["ssd_w_in"][0]
    sh["ssdwin"] = _wlay(np.ascontiguousarray(w[:, :10240]))
    sh["ssdwdt"] = np.ascontiguousarray(w[:, 10240:].reshape(16, 128, 64).transpose(1, 0, 2))
    sh["ssdwout"] = _wlay(inp["ssd_w_out"][0])
    sh["wq"] = np.stack([_wlay(inp["peer_w_q"][l]) for l in range(2)])
    sh["kT"] = np.ascontiguousarray(np.stack([np.stack([inp["peer_k1"][l].T, inp["peer_k2"][l].T])
                                              for l in range(2)]))
    sh["uT"] = np.stack([np.ascontiguousarray(inp["peer_u"][l].reshape(128, 128, 16, 128).transpose(0, 3, 2, 1))
                         for l in range(2)])
    sh["v"] = np.ascontiguousarray(inp["peer_v"].reshape(2, 128, 128, 2048))
    return sh


def kernel(**inp):
    inp = {k: np.asarray(v) for k, v in inp.items()}
    TP = inp["x_prompt"].shape[1]
    nc = build(TP)
    sh = _prep_shared(inp)
    in_maps = []
    for c in range(8):
        s = c % 4
        sl = slice(c * NS, (c + 1) * NS)
        d = dict(sh)
        d["xT"] = np.ascontiguousarray(inp["x_prompt"][s].T)
        d["xsT"] = np.ascontiguousarray(inp["x_sample"][sl, 0, :].T)
        d["cT"] = np.ascontiguousarray(np.concatenate([inp["c_prompt"][s][None], inp["c_sample"][sl]], 0).T)
        d["rgconv"] = np.ascontiguousarray(inp["state_rg_conv"][0, sl].transpose(2, 1, 0))
        d["rgh"] = np.ascontiguousarray(inp["state_rg_h"][0, sl].T)
        d["ssdconv"] = np.ascontiguousarray(inp["state_ssd_conv"][0, sl].transpose(2, 1, 0))
        d["ssds"] = np.ascontiguousarray(inp["state_ssd"][0, sl].reshape(NS, 64, 64, 128).transpose(0, 3, 1, 2))
        in_maps.append(d)
    res = run_bass_kernel_spmd(nc, in_maps, core_ids=list(range(8))).results
    f = np.float32
    y_p = np.stack([res[s]["o_yT"].T for s in range(4)]).astype(f)
    y_s = np.concatenate([res[c]["o_ysT"].T for c in range(8)], 0)[:, None, :].astype(f)

    def unv(a):
        return a.transpose(1, 0, *range(2, a.ndim)).reshape(-1, *a.shape[2:])
    rg_conv_p = np.stack([unv(res[s]["o_rgconv_p"]).T for s in range(4)])[None].astype(f)
    rg_h_p = np.stack([unv(res[s]["o_rgh_p"]) for s in range(4)])[None].astype(f)
    ssd_conv_p = np.stack([unv(res[s]["o_ssdconv_p"]).T for s in range(4)])[None].astype(f)
    ssd_p = np.stack([res[s]["o_ssd_p"].transpose(1, 2, 0).reshape(8, 8, 64, 128) for s in range(4)])[None].astype(f)
    rg_conv_s = np.concatenate([unv(res[c]["o_rgconv_s"]).transpose(2, 1, 0) for c in range(8)], 0)[None].astype(f)
    rg_h_s = np.concatenate([unv(res[c]["o_rgh_s"]).T for c in range(8)], 0)[None].astype(f)
    ssd_conv_s = np.concatenate([unv(res[c]["o_ssdconv_s"]).transpose(2, 1, 0) for c in range(8)], 0)[None].astype(f)
    ssd_s = np.concatenate([res[c]["o_ssd_s"].transpose(0, 2, 3, 1).reshape(NS, 8, 8, 64, 128)
                            for c in range(8)], 0)[None].astype(f)
    return (y_p, y_s, rg_conv_p, rg_h_p, ssd_conv_p, ssd_p, rg_conv_s, rg_h_s, ssd_conv_s, ssd_s)
```

```python
import contextlib
import numpy as np
import concourse.bass as bass
import concourse.mybir as mybir
from concourse.bass_utils import run_bass_kernel_spmd

F32 = mybir.dt.float32
BF16 = mybir.dt.bfloat16
AF = mybir.ActivationFunctionType
ALU = mybir.AluOpType

D = 2048
KC = 16
NS = 16
NB = 512
EPS = 1e-6
NDS = 12
GA = 4
NEG = -30000.0


class Trk:
    __slots__ = ("w", "r")

    def __init__(self):
        self.w = None
        self.r = {}


class Buf:
    def __init__(self, ap):
        self.ap = ap
        self.t = Trk()


class Prog:
    def __init__(self, nc, es):
        self.nc = nc
        self.eng = {"pe": nc.tensor, "dve": nc.vector, "act": nc.scalar, "pool": nc.gpsimd, "sp": nc.sync}
        self.sem = {k: es.enter_context(nc.semaphore("s_" + k)) for k in ("pe", "dve", "act", "pool")}
        self.cnt = {k: 0 for k in self.sem}
        self.seen = {e: {k: 0 for k in self.sem} for e in self.eng}
        self.dsem = [es.enter_context(nc.semaphore("d%d" % i)) for i in range(NDS)]
        self.dcnt = [0] * NDS
        self.dn = 0
        self.dseen = {e: [0] * NDS for e in self.eng}

    def _need(self, e, tok):
        if tok is None:
            return
        if tok[0] == "c":
            _, f, n = tok
            if f == e and e == "pe":
                return
            if self.seen[e][f] < n:
                self.eng[e].wait_ge(self.sem[f], n)
                self.seen[e][f] = n
        else:
            _, i, v = tok
            if self.dseen[e][i] < v:
                self.eng[e].wait_ge(self.dsem[i], v)
                self.dseen[e][i] = v

    def _sync(self, e, R, W):
        for b in R:
            self._need(e, b.t.w)
        for b in W:
            self._need(e, b.t.w)
            for tok in b.t.r.values():
                self._need(e, tok)

    def op(self, e, fn, R=(), W=()):
        self._sync(e, R, W)
        ins = fn(self.eng[e])
        self.cnt[e] += 1
        ins.then_inc(self.sem[e], 1)
        tok = ("c", e, self.cnt[e])
        for b in R:
            b.t.r[e] = tok
        for b in W:
            b.t.w = tok
            b.t.r = {}
        return ins

    def dma(self, out, in_, R=(), W=(), q="sp"):
        i = self.dn % NDS
        self.dn += 1
        self._need(q, ("d", i, self.dcnt[i]))
        self._sync(q, R, W)
        ins = self.eng[q].dma_start(out=out, in_=in_)
        self.dcnt[i] += 16
        ins.then_inc(self.dsem[i], 16)
        tok = ("d", i, self.dcnt[i])
        for b in R:
            b.t.r["dma%d" % i] = tok
        for b in W:
            b.t.w = tok
            b.t.r = {}

    def barrier(self):
        for e in self.eng:
            for f in self.sem:
                self._need(e, ("c", f, self.cnt[f]))
            for i in range(NDS):
                self._need(e, ("d", i, self.dcnt[i]))

    def finish(self):
        for i in range(NDS):
            self._need("sp", ("d", i, self.dcnt[i]))
        for f in self.sem:
            self._need("sp", ("c", f, self.cnt[f]))


class Arena:
    def __init__(self, ap, width):
        self.ap = ap
        self.width = width
        self.off = 0
        self.hw = 0

    def mark(self):
        return self.off

    def reset(self, m):
        self.off = m

    def f32(self, *shape):
        n = int(np.prod(shape))
        assert self.off + n <= self.width, ("arena overflow", self.off, n, self.width)
        v = self.ap[:, self.off:self.off + n]
        self.off += n
        self.hw = max(self.hw, self.off)
        return Buf(_shape(v, shape))

    def bf16(self, *shape):
        n = int(np.prod(shape))
        w = (n + 1) // 2
        assert self.off + w <= self.width, ("arena overflow", self.off, w, self.width)
        v = self.ap[:, self.off:self.off + w].bitcast(BF16)[:, 0:n]
        self.off += w
        self.hw = max(self.hw, self.off)
        return Buf(_shape(v, shape))


def _shape(v, shape):
    if len(shape) == 1:
        return v
    if len(shape) == 2:
        return v.rearrange("p (a b) -> p a b", b=shape[1])
    if len(shape) == 3:
        return v.rearrange("p (a b c) -> p a b c", b=shape[1], c=shape[2])
    raise ValueError(shape)


def bc(ap, axis, shape):
    return ap.unsqueeze(axis).to_broadcast(list(shape))


def build(TP):
    nblk = TP // NB
    nc = bass.Bass("TRN2", target_bir_lowering=False)
    es = contextlib.ExitStack()

    def din(name, shape):
        return nc.dram_tensor(name, list(shape), F32, kind="ExternalInput").ap()

    def dout(name, shape):
        return nc.dram_tensor(name, list(shape), F32, kind="ExternalOutput").ap()

    i_xT = din("xT", [D, TP])
    i_xsT = din("xsT", [D, NS])
    i_cT = din("cT", [D, 1 + NS])
    i_rgconv = din("rgconv", [D, 3, NS])
    i_rgh = din("rgh", [D, NS])
    i_ssdconv = din("ssdconv", [6144, 3, NS])
    i_ssds = din("ssds", [NS, 128, 64, 64])
    i_cst = din("cst", [128, 4 * 128 + 16])
    i_vec = din("vec", [128, 16 * 11 + 32 * 2 + 48 + 32])
    i_bmod = din("bmod", [128, 2, 96])
    i_rgcw = din("rgcw", [128, 16, 4])
    i_ssdcw = din("ssdcw", [128, 48, 4])
    i_ssdh = din("ssdh", [3, 64])
    i_wmod = din("wmod", [2, 96, 128, 16, 128])
    i_rgwin = din("rgwin", [32, 128, 16, 128])
    i_rgwa = din("rgwa", [16, 128, 2, 128])
    i_rgwi = din("rgwi", [16, 128, 2, 128])
    i_rgwout = din("rgwout", [16, 128, 16, 128])
    i_ssdwin = din("ssdwin", [80, 128, 16, 128])
    i_ssdwdt = din("ssdwdt", [128, 16, 64])
    i_ssdwout = din("ssdwout", [16, 128, 32, 128])
    i_wq = din("wq", [2, 16, 128, 16, 128])
    i_kT = din("kT", [2, 2, 128, 128])
    i_uT = din("uT", [2, 128, 128, 16, 128])
    i_v = din("v", [2, 128, 128, 2048])
    o_yT = dout("o_yT", [D, TP])
    o_ysT = dout("o_ysT", [D, NS])
    o_rgconv_p = dout("o_rgconv_p", [128, 16, 3])
    o_rgh_p = dout("o_rgh_p", [128, 16])
    o_ssdconv_p = dout("o_ssdconv_p", [128, 48, 3])
    o_ssd_p = dout("o_ssd_p", [128, 64, 64])
    o_rgconv_s = dout("o_rgconv_s", [128, 16, 3, NS])
    o_rgh_s = dout("o_rgh_s", [128, 16, NS])
    o_ssdconv_s = dout("o_ssdconv_s", [128, 48, 3, NS])
    o_ssd_s = dout("o_ssd_s", [NS, 128, 64, 64])

    AW = 51800
    arena_t = es.enter_context(nc.sbuf_tensor("arena", [128, AW], F32))
    A = Arena(arena_t[:, :], AW)
    banks = [Buf(es.enter_context(nc.psum_tensor("pb%d" % i, [128, 512], F32))[:, :]) for i in range(8)]
    P = Prog(nc, es)
    op, dma = P.op, P.dma

    cst = A.f32(4 * 128 + 16)
    ident = cst.ap[:, 0:128]
    ones = cst.ap[:, 128:256]
    tri = cst.ap[:, 256:384]
    mneg = cst.ap[:, 384:512]
    selm = cst.ap[:, 512:528]
    dma(cst.ap, i_cst, W=[cst])
    identb = A.bf16(128)
    op("dve", lambda e: e.tensor_copy(out=identb.ap, in_=ident), R=[cst], W=[identb])
    vec = A.f32(16 * 11 + 32 * 2 + 48 + 32)
    dma(vec.ap, i_vec, W=[vec])

    def vsl(o, n):
        return vec.ap[:, o:o + n]
    n1g = [vsl(0, 16), vsl(16, 16)]
    n2g = [vsl(32, 16), vsl(48, 16)]
    fing = vsl(64, 16)
    rg_bconv = vsl(80, 16)
    rg_ba = vsl(96, 16)
    rg_bi = vsl(112, 16)
    rg_lam = vsl(128, 16)
    rg_bout = vsl(144, 16)
    rg_bin = vsl(176, 32)
    ssd_ng = vsl(208, 32)
    ssd_bconv = vsl(240, 48)
    ssd_dexp = vsl(288, 32)
    rgcw = A.f32(16, 4)
    dma(rgcw.ap, i_rgcw, W=[rgcw])
    ssdcw = A.f32(48, 4)
    dma(ssdcw.ap, i_ssdcw, W=[ssdcw])
    ssdh = A.f32(3, 64)
    dma(ssdh.ap, i_ssdh.partition_broadcast(128), W=[ssdh])
    Abc = A.f32(64)
    op("act", lambda e: e.activation(out=Abc.ap, in_=ssdh.ap[:, 1, :], func=AF.Exp), R=[ssdh], W=[Abc])
    op("dve", lambda e: e.tensor_scalar(out=Abc.ap, in0=Abc.ap, scalar1=-1.0, scalar2=None, op0=ALU.mult),
       R=[Abc], W=[Abc])
    c8 = A.f32(16)
    op("act", lambda e: e.activation(out=c8.ap, in_=rg_lam, func=AF.Exp, scale=-1.0), R=[vec], W=[c8])
    op("act", lambda e: e.activation(out=c8.ap, in_=c8.ap, func=AF.Ln, bias=1.0), R=[c8], W=[c8])
    op("dve", lambda e: e.tensor_scalar(out=c8.ap, in0=c8.ap, scalar1=-8.0, scalar2=None, op0=ALU.mult),
       R=[c8], W=[c8])
    epsb = A.f32(1)
    op("pool", lambda e: e.memset(epsb.ap, EPS), W=[epsb])
    kT = A.f32(4, 128)
    for l_ in range(2):
        for h_ in range(2):
            dma(kT.ap[:, l_ * 2 + h_, :], i_kT[l_, h_], W=[kT])

    modT = A.f32(2, 96, 1 + NS)
    csT = A.f32(KC, 1 + NS)
    for kc_ in range(KC):
        dma(csT.ap[:, kc_, :], i_cT[kc_ * 128:(kc_ + 1) * 128, :], W=[csT])
    op("act", lambda e: e.activation(out=csT.ap, in_=csT.ap, func=AF.Silu), R=[csT], W=[csT])
    bmod = A.f32(2, 96)
    dma(bmod.ap, i_bmod, W=[bmod])
    m0 = A.mark()
    NST = 4
    stg = [A.f32(KC, 128) for _ in range(NST)]
    jobs = [(l, j) for l in range(2) for j in range(96)]
    for idx in range(min(NST - 1, len(jobs))):
        l, j = jobs[idx]
        dma(stg[idx % NST].ap, i_wmod[l, j], W=[stg[idx % NST]])
    for idx, (l, j) in enumerate(jobs):
        nx = idx + NST - 1
        if nx < len(jobs):
            dma(stg[nx % NST].ap, i_wmod[jobs[nx][0], jobs[nx][1]], W=[stg[nx % NST]])
        s = stg[idx % NST]
        pb = banks[idx % 2]
        for kc in range(KC):
            op("pe", lambda e, kc=kc: e.matmul(pb.ap[:, 0:1 + NS], s.ap[:, kc, :], csT.ap[:, kc, :],
                                                start=(kc == 0), stop=(kc == KC - 1)), R=[s, csT], W=[pb])
        op("dve", lambda e: e.tensor_scalar(out=modT.ap[:, l, j, :], in0=pb.ap[:, 0:1 + NS],
                                            scalar1=bmod.ap[:, l, j:j + 1], scalar2=None, op0=ALU.add),
           R=[pb, bmod], W=[modT])
    A.reset(m0)
    P.barrier()
    gmp = A.f32(2, 2, 16)
    gms = A.f32(4, 16, NS)
    for l in range(2):
        for k, (sc0, ng) in enumerate(((16, n1g[l]), (64, n2g[l]))):
            op("dve", lambda e: e.scalar_tensor_tensor(out=gmp.ap[:, l, k, :], in0=modT.ap[:, l, sc0:sc0 + 16, 0],
                                                       scalar=1.0, in1=ng, op0=ALU.add, op1=ALU.mult),
               R=[modT, vec], W=[gmp])
            op("dve", lambda e: e.scalar_tensor_tensor(out=gms.ap[:, l * 2 + k, :, :],
                                                       in0=modT.ap[:, l, sc0:sc0 + 16, 1:1 + NS], scalar=1.0,
                                                       in1=bc(ng, 2, [128, 16, NS]), op0=ALU.add, op1=ALU.mult),
               R=[modT, vec], W=[gms])

    def modv(l, k, c, samp):
        if samp:
            return modT.ap[:, l, 16 * k + c, 1:1 + NS]
        return modT.ap[:, l, 16 * k + c, 0:1]

    xT = A.f32(KC, NB)
    rg_tail = A.f32(16, 3)
    rg_carry = A.f32(16)
    ssd_tail = A.f32(48, 3)
    for b_ in (rg_tail, rg_carry, ssd_tail):
        op("pool", lambda e, b_=b_: e.memset(b_.ap, 0.0), W=[b_])
    sT_dram = Buf(o_ssd_p)
    pmark = A.mark()

    class WStream:
        def __init__(self, srcs, shape, ceng=("pool",), nstage=3, nbf=3):
            self.srcs = srcs
            self.shape = shape
            self.stage = [A.f32(*shape) for _ in range(nstage)]
            self.bfs = [A.bf16(*shape) for _ in range(nbf)]
            self.ceng = ceng
            self.nl = 0
            self.ncast = 0
            self.pf = nstage - 1
            for _ in range(min(self.pf, len(srcs))):
                self._load()

        def _load(self):
            if self.nl < len(self.srcs):
                s = self.stage[self.nl % len(self.stage)]
                dma(s.ap, self.srcs[self.nl], W=[s])
                self.nl += 1

        def get(self):
            i = self.ncast
            self._load()
            s = self.stage[i % len(self.stage)]
            b = self.bfs[i % len(self.bfs)]
            e_ = self.ceng[i % len(self.ceng)]
            if e_ == "act":
                op("act", lambda e: e.copy(out=b.ap, in_=s.ap), R=[s], W=[b])
            else:
                op(e_, lambda e: e.tensor_copy(out=b.ap, in_=s.ap), R=[s], W=[b])
            self.ncast += 1
            return b

    def rms_stats(src_chunks, srcbufs, N, nch, scratch, pb, rstd, eng_sq="act"):
        for c in range(nch):
            sq = scratch[c % len(scratch)]
            op("act", lambda e, c=c, sq=sq: e.activation(out=sq.ap[:, 0:N], in_=src_chunks(c), func=AF.Square),
               R=srcbufs, W=[sq])
            op("pe", lambda e, c=c, sq=sq: e.matmul(pb.ap[:, 0:N], ones, sq.ap[:, 0:N], start=(c == 0),
                                                    stop=(c == nch - 1)), R=[sq, cst], W=[pb])
        op("act", lambda e: e.activation(out=rstd.ap[:, 0:N], in_=pb.ap[:, 0:N], func=AF.Sqrt,
                                         scale=1.0 / (nch * 128), bias=epsb.ap[:, 0:1]), R=[pb, epsb], W=[rstd])
        op("dve", lambda e: e.reciprocal(out=rstd.ap[:, 0:N], in_=rstd.ap[:, 0:N]), R=[rstd], W=[rstd])

    def norm_mod(l, which, N, samp, dst):
        m = A.mark()
        scr = [A.f32(NB), A.f32(NB)]
        rstd = A.f32(NB)
        tmp = [A.f32(NB), A.f32(NB)]
        rms_stats(lambda c: xT.ap[:, c, 0:N], [xT], N, KC, scr, banks[7], rstd)
        for c in range(KC):
            t = tmp[c % 2]
            if samp:
                gm = gms.ap[:, l * 2 + which, c, :]
                sh = modv(l, 3 * which, c, True)
                op("dve", lambda e: e.tensor_tensor(out=t.ap[:, 0:N], in0=xT.ap[:, c, 0:N], in1=rstd.ap[:, 0:N],
                                                    op=ALU.mult), R=[xT, rstd], W=[t])
                op("dve", lambda e: e.tensor_tensor(out=t.ap[:, 0:N], in0=t.ap[:, 0:N], in1=gm, op=ALU.mult),
                   R=[t, gms], W=[t])
                op("dve", lambda e: e.tensor_tensor(out=dst.ap[:, c, 0:N], in0=t.ap[:, 0:N], in1=sh, op=ALU.add),
                   R=[t, modT], W=[dst])
            else:
                gm = gmp.ap[:, l, which, c:c + 1]
                sh = modv(l, 3 * which, c, False)
                op("dve", lambda e: e.scalar_tensor_tensor(out=t.ap[:, 0:N], in0=xT.ap[:, c, 0:N], scalar=gm,
                                                           in1=rstd.ap[:, 0:N], op0=ALU.mult, op1=ALU.mult),
                   R=[xT, rstd, gmp], W=[t])
                op("act", lambda e: e.activation(out=dst.ap[:, c, 0:N], in_=t.ap[:, 0:N], func=AF.Identity,
                                                 bias=sh, scale=1.0), R=[t, modT], W=[dst])
        A.reset(m)

    def resid_add(c, pb, N, bias, gate, samp, tmpb):
        if samp:
            if bias is not None:
                op("dve", lambda e: e.scalar_tensor_tensor(out=tmpb.ap[:, 0:N], in0=pb.ap[:, 0:N], scalar=bias,
                                                           in1=gate, op0=ALU.add, op1=ALU.mult),
                   R=[pb, vec, modT], W=[tmpb])
            else:
                op("dve", lambda e: e.tensor_tensor(out=tmpb.ap[:, 0:N], in0=pb.ap[:, 0:N], in1=gate, op=ALU.mult),
                   R=[pb, modT], W=[tmpb])
            op("dve", lambda e: e.tensor_tensor(out=xT.ap[:, c, 0:N], in0=xT.ap[:, c, 0:N], in1=tmpb.ap[:, 0:N],
                                                op=ALU.add), R=[tmpb, xT], W=[xT])
        else:
            if bias is not None:
                op("dve", lambda e: e.tensor_scalar(out=tmpb.ap[:, 0:N], in0=pb.ap[:, 0:N], scalar1=bias,
                                                    scalar2=gate, op0=ALU.add, op1=ALU.mult),
                   R=[pb, vec, modT], W=[tmpb])
                op("dve", lambda e: e.tensor_tensor(out=xT.ap[:, c, 0:N], in0=xT.ap[:, c, 0:N],
                                                    in1=tmpb.ap[:, 0:N], op=ALU.add), R=[tmpb, xT], W=[xT])
            else:
                op("dve", lambda e: e.scalar_tensor_tensor(out=xT.ap[:, c, 0:N], in0=pb.ap[:, 0:N], scalar=gate,
                                                           in1=xT.ap[:, c, 0:N], op0=ALU.mult, op1=ALU.add),
                   R=[pb, modT, xT], W=[xT])

    def conv4(dst, taps, wcol, bcol, N, Rb, Wb):
        op("dve", lambda e: e.tensor_scalar(out=dst, in0=taps[0], scalar1=wcol(0), scalar2=bcol, op0=ALU.mult,
                                            op1=ALU.add), R=Rb, W=Wb)
        for k in range(1, 4):
            op("dve", lambda e, k=k: e.scalar_tensor_tensor(out=dst, in0=taps[k], scalar=wcol(k), in1=dst,
                                                            op0=ALU.mult, op1=ALU.add), R=Rb + Wb, W=Wb)

    def rg_mixer(N, samp, last, hmT):
        m = A.mark()
        gT = A.bf16(KC, NB)
        yT = A.bf16(KC, NB)
        if samp:
            nbuf = A.f32(KC, 3, NS)
            hout = A.f32(KC, NS)
        m_in = A.mark()
        win_srcs = []
        for p in range(8):
            win_srcs += [i_rgwin[2 * p], i_rgwin[2 * p + 1], i_rgwin[16 + 2 * p], i_rgwin[16 + 2 * p + 1]]
        ws = WStream(win_srcs, (KC, 128), ceng=("act", "dve", "pool", "act", "dve"), nstage=2, nbf=3)
        gsrcs = []
        for p in range(8):
            gsrcs += [i_rgwa[2 * p], i_rgwa[2 * p + 1], i_rgwi[2 * p], i_rgwi[2 * p + 1]]
        gs = WStream(gsrcs, (2, 128), ceng=("pool",), nstage=4, nbf=4)
        xe = [A.f32(2, NB + 3), A.f32(2, NB + 3)]
        xc = [A.f32(2, NB), A.f32(2, NB)]
        xcb = [A.bf16(2, NB), A.bf16(2, NB)]
        gate_r = [A.f32(NB), A.f32(NB)]
        gate_i = [A.f32(NB), A.f32(NB)]
        av = [A.f32(NB), A.f32(NB)]
        bv = [A.f32(NB), A.f32(NB)]
        hs = [A.f32(NB), A.f32(NB)]
        if samp:
            st_c = A.f32(KC, 3, NS)
            for c_ in range(KC):
                dma(st_c.ap[:, c_], i_rgconv[c_ * 128:(c_ + 1) * 128], W=[st_c])
            h0s = A.f32(KC, NS)
            for c_ in range(KC):
                dma(h0s.ap[:, c_, :], i_rgh[c_ * 128:(c_ + 1) * 128, :], W=[h0s])
        for p in range(8):
            xe_, xc_, xcb_ = xe[p % 2], xc[p % 2], xcb[p % 2]
            for q4 in range(4):
                wb = ws.get()
                pb = banks[q4 % 4]
                for kc in range(KC):
                    op("pe", lambda e, kc=kc: e.matmul(pb.ap[:, 0:N], wb.ap[:, kc, :], hmT.ap[:, kc, 0:N],
                                                        start=(kc == 0), stop=(kc == KC - 1)), R=[wb, hmT], W=[pb])
                if q4 < 2:
                    c = 2 * p + q4
                    op("act", lambda e: e.activation(out=gT.ap[:, c, 0:N], in_=pb.ap[:, 0:N],
                                                     func=AF.Gelu_apprx_tanh, bias=rg_bin[:, c:c + 1], scale=1.0),
                       R=[pb, vec], W=[gT])
                else:
                    q = q4 - 2
                    c = 2 * p + q
                    if not samp:
                        op("pool", lambda e: e.tensor_copy(out=xe_.ap[:, q, 0:3], in_=rg_tail.ap[:, c, :]),
                           R=[rg_tail], W=[xe_])
                    op("act", lambda e: e.activation(out=xe_.ap[:, q, 3:3 + N], in_=pb.ap[:, 0:N], func=AF.Identity,
                                                     bias=rg_bin[:, 16 + c:16 + c + 1], scale=1.0),
                       R=[pb, vec], W=[xe_])
            for q in range(2):
                c = 2 * p + q
                if samp:
                    taps = [st_c.ap[:, c, 0, :], st_c.ap[:, c, 1, :], st_c.ap[:, c, 2, :], xe_.ap[:, q, 3:3 + N]]
                    Rb = [xe_, st_c, rgcw, vec]
                else:
                    taps = [xe_.ap[:, q, k:k + N] for k in range(4)]
                    Rb = [xe_, rgcw, vec]
                conv4(xc_.ap[:, q, 0:N], taps, lambda k: rgcw.ap[:, c, k:k + 1], rg_bconv[:, c:c + 1], N, Rb, [xc_])
                op("act", lambda e: e.copy(out=xcb_.ap[:, q, 0:N], in_=xc_.ap[:, q, 0:N]), R=[xc_], W=[xcb_])
                if samp:
                    for k in range(2):
                        op("pool", lambda e, k=k: e.tensor_copy(out=nbuf.ap[:, c, k, :], in_=st_c.ap[:, c, k + 1, :]),
                           R=[st_c], W=[nbuf])
                    op("pool", lambda e: e.tensor_copy(out=nbuf.ap[:, c, 2, :], in_=xe_.ap[:, q, 3:3 + N]),
                       R=[xe_], W=[nbuf])
                else:
                    op("pool", lambda e: e.tensor_copy(out=rg_tail.ap[:, c, :], in_=xe_.ap[:, q, N:N + 3]),
                       R=[xe_], W=[rg_tail])
            gw = [gs.get() for _ in range(4)]
            for jh in range(2):
                c = 2 * p + jh
                r_, i_, a_, b_, h_ = gate_r[jh], gate_i[jh], av[jh], bv[jh], hs[jh]
                for gi_, (wt, dstb, bcol) in enumerate(((gw[jh], r_, rg_ba), (gw[2 + jh], i_, rg_bi))):
                    pb = banks[4 + (2 * jh + gi_) % 4]
                    for ih in range(2):
                        op("pe", lambda e, ih=ih: e.matmul(pb.ap[:, 0:N], wt.ap[:, ih, :], xcb_.ap[:, ih, 0:N],
                                                            start=(ih == 0), stop=(ih == 1)), R=[wt, xcb_], W=[pb])
                    op("act", lambda e: e.activation(out=dstb.ap[:, 0:N], in_=pb.ap[:, 0:N], func=AF.Sigmoid,
                                                     bias=bcol[:, c:c + 1], scale=1.0), R=[pb, vec], W=[dstb])
                op("act", lambda e: e.activation(out=a_.ap[:, 0:N], in_=r_.ap[:, 0:N], func=AF.Exp,
                                                 scale=c8.ap[:, c:c + 1]), R=[r_, c8], W=[a_])
                op("dve", lambda e: e.tensor_tensor(out=b_.ap[:, 0:N], in0=a_.ap[:, 0:N], in1=a_.ap[:, 0:N],
                                                    op=ALU.mult), R=[a_], W=[b_])
                op("dve", lambda e: e.tensor_scalar(out=b_.ap[:, 0:N], in0=b_.ap[:, 0:N], scalar1=-1.0, scalar2=1.0,
                                                    op0=ALU.mult, op1=ALU.add), R=[b_], W=[b_])
                op("dve", lambda e: e.tensor_scalar(out=b_.ap[:, 0:N], in0=b_.ap[:, 0:N], scalar1=1e-30,
                                                    scalar2=None, op0=ALU.max), R=[b_], W=[b_])
                op("act", lambda e: e.activation(out=b_.ap[:, 0:N], in_=b_.ap[:, 0:N], func=AF.Sqrt),
                   R=[b_], W=[b_])
                op("dve", lambda e: e.tensor_tensor(out=i_.ap[:, 0:N], in0=i_.ap[:, 0:N], in1=xc_.ap[:, jh, 0:N],
                                                    op=ALU.mult), R=[i_, xc_], W=[i_])
                op("dve", lambda e: e.tensor_tensor(out=b_.ap[:, 0:N], in0=b_.ap[:, 0:N], in1=i_.ap[:, 0:N],
                                                    op=ALU.mult), R=[b_, i_], W=[b_])
                if samp:
                    op("dve", lambda e: e.tensor_tensor(out=h_.ap[:, 0:N], in0=a_.ap[:, 0:N], in1=h0s.ap[:, c, :],
                                                        op=ALU.mult), R=[a_, h0s], W=[h_])
                    op("dve", lambda e: e.tensor_tensor(out=h_.ap[:, 0:N], in0=h_.ap[:, 0:N], in1=b_.ap[:, 0:N],
                                                        op=ALU.add), R=[h_, b_], W=[h_])
                    op("pool", lambda e: e.tensor_copy(out=hout.ap[:, c, :], in_=h_.ap[:, 0:N]), R=[h_], W=[hout])
                else:
                    op("dve", lambda e: e.tensor_tensor_scan(out=h_.ap[:, 0:N], data0=a_.ap[:, 0:N],
                                                             data1=b_.ap[:, 0:N], initial=rg_carry.ap[:, c:c + 1],
                                                             op0=ALU.mult, op1=ALU.add),
                       R=[a_, b_, rg_carry], W=[h_])
                    op("pool", lambda e: e.tensor_copy(out=rg_carry.ap[:, c:c + 1], in_=h_.ap[:, N - 1:N]),
                       R=[h_], W=[rg_carry])
                op("dve", lambda e: e.tensor_tensor(out=yT.ap[:, c, 0:N], in0=h_.ap[:, 0:N], in1=gT.ap[:, c, 0:N],
                                                    op=ALU.mult), R=[h_, gT], W=[yT])
        P.barrier()
        A.reset(m_in)
        wo = WStream([i_rgwout[j] for j in range(16)], (KC, 128), ceng=("act", "dve", "pool", "act", "dve"))
        tmpb = [A.f32(NB), A.f32(NB)]
        for j in range(16):
            wb = wo.get()
            pb = banks[j % 4]
            for kc in range(KC):
                op("pe", lambda e, kc=kc: e.matmul(pb.ap[:, 0:N], wb.ap[:, kc, :], yT.ap[:, kc, 0:N],
                                                    start=(kc == 0), stop=(kc == KC - 1)), R=[wb, yT], W=[pb])
            resid_add(j, pb, N, rg_bout[:, j:j + 1], modv(0, 2, j, samp), samp, tmpb[j % 2])
        if samp:
            dma(o_rgconv_s, nbuf.ap, R=[nbuf])
            dma(o_rgh_s, hout.ap, R=[hout])
        elif last:
            dma(o_rgconv_p, rg_tail.ap, R=[rg_tail])
            dma(o_rgh_p, rg_carry.ap, R=[rg_carry])
        P.barrier()
        A.reset(m)

    def ssd_temps():
        T = {}
        T["dt"] = A.f32(64)
        T["dtA"] = A.f32(64)
        T["ncum"] = A.f32(64)
        T["xtok"] = A.bf16(4096)
        T["btok"] = A.bf16(1024)
        T["cbt"] = A.f32(8, 128)
        T["sTb"] = A.bf16(64, 64)
        T["rhs4"] = [A.f32(4, 128), A.f32(4, 128)]
        T["arg4"] = [A.f32(4, 128), A.f32(4, 128)]
        T["ET4"] = [A.f32(4, 128), A.f32(4, 128)]
        T["ecr4"] = [A.f32(4, 128), A.f32(4, 128)]
        T["WT"] = [A.bf16(128) for _ in range(4)]
        T["CpT"] = [A.bf16(128) for _ in range(4)]
        T["xw"] = [A.bf16(64) for _ in range(4)]
        return T

    def ssd_chunk(Q, t0, sT, hmT, xbcT, yT, dtw, T):
        pdt = banks[0]
        for kc in range(KC):
            op("pe", lambda e, kc=kc: e.matmul(pdt.ap[0:Q, 0:64], hmT.ap[:, kc, t0:t0 + Q], dtw.ap[:, kc, :],
                                                start=(kc == 0), stop=(kc == KC - 1)), R=[hmT, dtw], W=[pdt])
        dt = T["dt"]
        dtA = T["dtA"]
        op("dve", lambda e: e.tensor_tensor(out=dt.ap[0:Q, :], in0=pdt.ap[0:Q, 0:64], in1=ssdh.ap[0:Q, 0, :],
                                            op=ALU.add), R=[pdt, ssdh], W=[dt])
        op("act", lambda e: e.activation(out=dt.ap[0:Q, :], in_=dt.ap[0:Q, :], func=AF.Exp), R=[dt], W=[dt])
        op("act", lambda e: e.activation(out=dt.ap[0:Q, :], in_=dt.ap[0:Q, :], func=AF.Ln, bias=1.0),
           R=[dt], W=[dt])
        op("dve", lambda e: e.tensor_tensor(out=dtA.ap[0:Q, :], in0=dt.ap[0:Q, :], in1=Abc.ap[0:Q, :],
                                            op=ALU.mult), R=[dt, Abc], W=[dtA])
        pc = banks[1]
        op("pe", lambda e: e.matmul(pc.ap[0:Q, 0:64], tri[0:Q, 0:Q], dtA.ap[0:Q, :], start=True, stop=True),
           R=[dtA, cst], W=[pc])
        ncum = T["ncum"]
        op("dve", lambda e: e.tensor_scalar(out=ncum.ap[0:Q, :], in0=pc.ap[0:Q, 0:64], scalar1=-1.0, scalar2=None,
                                            op0=ALU.mult), R=[pc], W=[ncum])
        xtok = T["xtok"]
        btok = T["btok"]
        for grp in range(5):
            pbt = banks[2 + grp % 2]
            pv = pbt.ap.bitcast(BF16)
            for k in range(8):
                j = grp * 8 + k
                op("pe", lambda e, j=j, k=k: e.transpose(pv[0:Q, k * 128:(k + 1) * 128], xbcT.ap[:, j, t0:t0 + Q],
                                                         identb.ap), R=[xbcT, identb], W=[pbt])
            if grp < 4:
                op("act", lambda e: e.copy(out=xtok.ap[0:Q, grp * 1024:(grp + 1) * 1024], in_=pv[0:Q, :]),
                   R=[pbt], W=[xtok])
            else:
                op("act", lambda e: e.copy(out=btok.ap[0:Q, :], in_=pv[0:Q, :]), R=[pbt], W=[btok])
        cbt = T["cbt"]
        for g in range(8):
            pcb = banks[4 + g % 2]
            op("pe", lambda e: e.matmul(pcb.ap[0:Q, 0:Q], xbcT.ap[:, 32 + g, t0:t0 + Q], xbcT.ap[:, 40 + g, t0:t0 + Q],
                                        start=True, stop=True), R=[xbcT], W=[pcb])
            op("act", lambda e: e.copy(out=cbt.ap[0:Q, g, 0:Q], in_=pcb.ap[0:Q, 0:Q]), R=[pcb], W=[cbt])
        sTb = T["sTb"]
        op("pool", lambda e: e.tensor_copy(out=sTb.ap, in_=sT.ap), R=[sT], W=[sTb])
        rhs4, arg4, ET4, ecr4 = T["rhs4"], T["arg4"], T["ET4"], T["ecr4"]
        WT, CpT, xw = T["WT"], T["CpT"], T["xw"]
        for h4 in range(16):
            r4, a4, E4, c4 = rhs4[h4 % 2], arg4[h4 % 2], ET4[h4 % 2], ecr4[h4 % 2]
            g = h4 // 2
            op("pool", lambda e: e.tensor_tensor(out=r4.ap[0:Q, :, 0:Q], in0=bc(tri[0:Q, 0:Q], 1, [Q, 4, Q]),
                                                 in1=bc(dtA.ap[0:Q, h4 * 4:h4 * 4 + 4], 2, [Q, 4, Q]), op=ALU.mult),
               R=[dtA, cst], W=[r4])
            pcr = banks[6 + h4 % 2]
            pcr_v = pcr.ap.rearrange("p (a b) -> p a b", b=128)
            for hh in range(4):
                op("pe", lambda e, hh=hh: e.matmul(pcr_v[:, hh, 0:Q], ones[0:Q, :], r4.ap[0:Q, hh, 0:Q],
                                                    start=True, stop=True), R=[r4, cst], W=[pcr])
            for hh in range(4):
                h = h4 * 4 + hh
                op("dve", lambda e, hh=hh, h=h: e.scalar_tensor_tensor(out=a4.ap[0:Q, hh, 0:Q], in0=pcr_v[0:Q, hh, 0:Q],
                                                                       scalar=ncum.ap[0:Q, h:h + 1], in1=mneg[0:Q, 0:Q],
                                                                       op0=ALU.add, op1=ALU.min),
                   R=[pcr, ncum, cst], W=[a4])
            op("act", lambda e: e.activation(out=E4.ap[0:Q, :, 0:Q], in_=a4.ap[0:Q, :, 0:Q], func=AF.Exp),
               R=[a4], W=[E4])
            op("act", lambda e: e.activation(out=c4.ap[:, :, 0:Q], in_=pcr_v[:, :, 0:Q], func=AF.Exp),
               R=[pcr], W=[c4])
            for hh in range(4):
                h = h4 * 4 + hh
                w_, cp_, xw_ = WT[hh], CpT[hh], xw[hh]
                op("dve", lambda e, hh=hh, h=h, w_=w_: e.scalar_tensor_tensor(
                    out=w_.ap[0:Q, 0:Q], in0=E4.ap[0:Q, hh, 0:Q], scalar=dt.ap[0:Q, h:h + 1], in1=cbt.ap[0:Q, g, 0:Q],
                    op0=ALU.mult, op1=ALU.mult), R=[E4, dt, cbt], W=[w_])
                op("pool", lambda e, hh=hh, cp_=cp_: e.tensor_tensor(
                    out=cp_.ap[:, 0:Q], in0=xbcT.ap[:, 40 + g, t0:t0 + Q], in1=c4.ap[:, hh, 0:Q], op=ALU.mult),
                   R=[xbcT, c4], W=[cp_])
                c = h // 2
                pby = banks[0 + (c // 4) % 2]
                po = (h % 2) * 64
                col = (c % 4) * 128
                op("pe", lambda e, h=h, w_=w_, pby=pby, po=po, col=col: e.matmul(
                    pby.ap[po:po + 64, col:col + Q], xtok.ap[0:Q, h * 64:(h + 1) * 64], w_.ap[0:Q, 0:Q],
                    start=True, stop=False), R=[xtok, w_], W=[pby])
                op("pe", lambda e, h=h, cp_=cp_, pby=pby, po=po, col=col: e.matmul(
                    pby.ap[po:po + 64, col:col + Q], sTb.ap[:, h, :], cp_.ap[:, 0:Q],
                    start=False, stop=True), R=[sTb, cp_], W=[pby])
                if h % 2 == 1:
                    op("dve", lambda e, c=c, pby=pby, col=col: e.scalar_tensor_tensor(
                        out=yT.ap[:, c, t0:t0 + Q], in0=xbcT.ap[:, c, t0:t0 + Q], scalar=ssd_dexp[:, c:c + 1],
                        in1=pby.ap[:, col:col + Q], op0=ALU.mult, op1=ALU.add), R=[xbcT, vec, pby], W=[yT])
                op("dve", lambda e, hh=hh, h=h, xw_=xw_: e.tensor_scalar(
                    out=xw_.ap[0:Q, :], in0=xtok.ap[0:Q, h * 64:(h + 1) * 64], scalar1=E4.ap[0:Q, hh, Q - 1:Q],
                    scalar2=dt.ap[0:Q, h:h + 1], op0=ALU.mult, op1=ALU.mult), R=[xtok, E4, dt], W=[xw_])
                pst = banks[2 + (h // 8) % 2]
                scol = (h % 8) * 64
                op("pe", lambda e, xw_=xw_, pst=pst, scol=scol: e.matmul(
                    pst.ap[:, scol:scol + 64], btok.ap[0:Q, g * 128:(g + 1) * 128], xw_.ap[0:Q, :],
                    start=True, stop=True), R=[btok, xw_], W=[pst])
                op("dve", lambda e, hh=hh, h=h, pst=pst, scol=scol: e.scalar_tensor_tensor(
                    out=sT.ap[:, h, :], in0=sT.ap[:, h, :], scalar=c4.ap[:, hh, Q - 1:Q], in1=pst.ap[:, scol:scol + 64],
                    op0=ALU.mult, op1=ALU.add), R=[sT, c4, pst], W=[sT])

    def ssd_mixer(N, samp, first, last, hmT):
        m = A.mark()
        xbcT = A.bf16(48, NB)
        yT = xbcT
        dtw = A.bf16(KC, 64)
        m1 = A.mark()
        dst_ = A.f32(KC, 64)
        dma(dst_.ap, i_ssdwdt, W=[dst_])
        op("pool", lambda e: e.tensor_copy(out=dtw.ap, in_=dst_.ap), R=[dst_], W=[dtw])
        ws = WStream([i_ssdwin[32 + j] for j in range(48)], (KC, 128), ceng=("act", "dve", "pool", "act", "dve"))
        xe = [A.f32(NB + 3), A.f32(NB + 3)]
        xcv = [A.f32(NB), A.f32(NB)]
        if samp:
            st_c = A.f32(48, 3, NS)
            for j in range(48):
                dma(st_c.ap[:, j], i_ssdconv[j * 128:(j + 1) * 128], W=[st_c])
            nbuf = A.f32(48, 3, NS)
        for j in range(48):
            wb = ws.get()
            pb = banks[j % 4]
            xe_, xc_ = xe[j % 2], xcv[j % 2]
            for kc in range(KC):
                op("pe", lambda e, kc=kc: e.matmul(pb.ap[:, 0:N], wb.ap[:, kc, :], hmT.ap[:, kc, 0:N],
                                                    start=(kc == 0), stop=(kc == KC - 1)), R=[wb, hmT], W=[pb])
            if not samp:
                op("pool", lambda e: e.tensor_copy(out=xe_.ap[:, 0:3], in_=ssd_tail.ap[:, j, :]),
                   R=[ssd_tail], W=[xe_])
            op("act", lambda e: e.copy(out=xe_.ap[:, 3:3 + N], in_=pb.ap[:, 0:N]), R=[pb], W=[xe_])
            if samp:
                taps = [st_c.ap[:, j, 0, :], st_c.ap[:, j, 1, :], st_c.ap[:, j, 2, :], xe_.ap[:, 3:3 + N]]
                Rb = [xe_, st_c, ssdcw, vec]
            else:
                taps = [xe_.ap[:, k:k + N] for k in range(4)]
                Rb = [xe_, ssdcw, vec]
            conv4(xc_.ap[:, 0:N], taps, lambda k: ssdcw.ap[:, j, k:k + 1], ssd_bconv[:, j:j + 1], N, Rb, [xc_])
            op("act", lambda e: e.activation(out=xbcT.ap[:, j, 0:N], in_=xc_.ap[:, 0:N], func=AF.Silu),
               R=[xc_], W=[xbcT])
            if samp:
                for k in range(2):
                    op("pool", lambda e, k=k: e.tensor_copy(out=nbuf.ap[:, j, k, :], in_=st_c.ap[:, j, k + 1, :]),
                       R=[st_c], W=[nbuf])
                op("pool", lambda e: e.tensor_copy(out=nbuf.ap[:, j, 2, :], in_=xe_.ap[:, 3:3 + N]),
                   R=[xe_], W=[nbuf])
            else:
                op("pool", lambda e: e.tensor_copy(out=ssd_tail.ap[:, j, :], in_=xe_.ap[:, N:N + 3]),
                   R=[xe_], W=[ssd_tail])
        if samp:
            dma(o_ssdconv_s, nbuf.ap, R=[nbuf])
        elif last:
            dma(o_ssdconv_p, ssd_tail.ap, R=[ssd_tail])
        P.barrier()
        A.reset(m1)
        T = ssd_temps()
        if samp:
            sTs = [A.f32(64, 64), A.f32(64, 64)]
            for b in range(NS):
                sT = sTs[b % 2]
                dma(sT.ap, i_ssds[b], W=[sT])
                ssd_chunk(1, b, sT, hmT, xbcT, yT, dtw, T)
                dma(o_ssd_s[b], sT.ap, R=[sT])
        else:
            sT = A.f32(64, 64)
            if first:
                op("pool", lambda e: e.memset(sT.ap, 0.0), W=[sT])
            else:
                dma(sT.ap, o_ssd_p, R=[sT_dram], W=[sT])
            for q in range(N // 128):
                ssd_chunk(128, q * 128, sT, hmT, xbcT, yT, dtw, T)
            dma(o_ssd_p, sT.ap, R=[sT], W=[sT_dram])
        P.barrier()
        A.reset(m1)
        wz = WStream([i_ssdwin[j] for j in range(32)], (KC, 128), ceng=("act", "dve", "pool", "act", "dve"))
        sz = [A.f32(NB), A.f32(NB)]
        sq = [A.f32(NB), A.f32(NB)]
        pst = banks[7]
        for j in range(32):
            wb = wz.get()
            pb = banks[j % 4]
            for kc in range(KC):
                op("pe", lambda e, kc=kc: e.matmul(pb.ap[:, 0:N], wb.ap[:, kc, :], hmT.ap[:, kc, 0:N],
                                                    start=(kc == 0), stop=(kc == KC - 1)), R=[wb, hmT], W=[pb])
            s_, q_ = sz[j % 2], sq[j % 2]
            op("act", lambda e: e.activation(out=s_.ap[:, 0:N], in_=pb.ap[:, 0:N], func=AF.Silu), R=[pb], W=[s_])
            op("dve", lambda e: e.tensor_tensor(out=yT.ap[:, j, 0:N], in0=yT.ap[:, j, 0:N], in1=s_.ap[:, 0:N],
                                                op=ALU.mult), R=[yT, s_], W=[yT])
            op("act", lambda e: e.activation(out=q_.ap[:, 0:N], in_=yT.ap[:, j, 0:N], func=AF.Square),
               R=[yT], W=[q_])
            op("pe", lambda e: e.matmul(pst.ap[:, 0:N], ones, q_.ap[:, 0:N], start=(j == 0), stop=(j == 31)),
               R=[q_, cst], W=[pst])
        P.barrier()
        A.reset(m1)
        rstd = A.f32(NB)
        op("act", lambda e: e.activation(out=rstd.ap[:, 0:N], in_=pst.ap[:, 0:N], func=AF.Sqrt, scale=1.0 / 4096,
                                         bias=epsb.ap[:, 0:1]), R=[pst, epsb], W=[rstd])
        op("dve", lambda e: e.reciprocal(out=rstd.ap[:, 0:N], in_=rstd.ap[:, 0:N]), R=[rstd], W=[rstd])
        for j in range(32):
            op("dve", lambda e: e.scalar_tensor_tensor(out=yT.ap[:, j, 0:N], in0=yT.ap[:, j, 0:N],
                                                       scalar=ssd_ng[:, j:j + 1], in1=rstd.ap[:, 0:N],
                                                       op0=ALU.mult, op1=ALU.mult), R=[yT, vec, rstd], W=[yT])
        wo = WStream([i_ssdwout[j] for j in range(16)], (32, 128), ceng=("act", "dve", "pool", "act", "dve"), nstage=2, nbf=2)
        tmpb = [A.f32(NB), A.f32(NB)]
        for j in range(16):
            wb = wo.get()
            pb = banks[j % 4]
            for kc in range(32):
                op("pe", lambda e, kc=kc: e.matmul(pb.ap[:, 0:N], wb.ap[:, kc, :], yT.ap[:, kc, 0:N],
                                                    start=(kc == 0), stop=(kc == 31)), R=[wb, yT], W=[pb])
            resid_add(j, pb, N, None, modv(1, 2, j, samp), samp, tmpb[j % 2])
        P.barrier()
        A.reset(m)

    def peer(l, N, samp, hcT):
        m = A.mark()
        NG = N // 16
        s12 = A.f32(NB // 16, 256)
        thr = A.f32(NB // 16)
        negm = A.f32(NB // 16)
        Sel = A.bf16(NB // 16, 16)
        m1 = A.mark()
        qT = A.f32(2, NB, 8)
        wq = WStream([i_wq[l, j] for j in range(16)], (KC, 128), ceng=("act", "dve", "pool", "act", "dve"))
        for j in range(16):
            wb = wq.get()
            pb = banks[j % 4]
            for kc in range(KC):
                op("pe", lambda e, kc=kc: e.matmul(pb.ap[:, 0:N], wb.ap[:, kc, :], hcT.ap[:, kc, 0:N],
                                                    start=(kc == 0), stop=(kc == KC - 1)), R=[wb, hcT], W=[pb])
            op("act", lambda e: e.copy(out=qT.ap[:, j % 2, 0:N, j // 2], in_=pb.ap[:, 0:N]), R=[pb], W=[qT])
        vv = [A.f32(2, 16) for _ in range(2)]
        tmp1 = [A.f32(128) for _ in range(2)]
        cand = [A.f32(256) for _ in range(2)]
        cand2 = [A.f32(256) for _ in range(2)]
        c24 = [A.f32(24) for _ in range(2)]
        ez = [A.f32(16) for _ in range(2)]
        zz = [A.f32(2) for _ in range(2)]
        for gi in range(NG):
            pb = banks[4 + gi % 2]
            for half in range(2):
                lhsT = qT.ap[:, half, gi * 16:(gi + 1) * 16, :].rearrange("p t h -> p (t h)")
                op("pe", lambda e, half=half, lhsT=lhsT: e.matmul(pb.ap[:, half * 128:(half + 1) * 128], lhsT,
                                                                  kT.ap[:, l * 2 + half, :], start=True, stop=True),
                   R=[qT, kT], W=[pb])
            op("act", lambda e: e.copy(out=s12.ap[:, gi, :], in_=pb.ap[:, 0:256]), R=[pb], W=[s12])
            v_, t1, cd, cd2, c_, ez_, z_ = (vv[gi % 2], tmp1[gi % 2], cand[gi % 2], cand2[gi % 2], c24[gi % 2],
                                            ez[gi % 2], zz[gi % 2])
            for half in range(2):
                w = s12.ap[:, gi, half * 128:(half + 1) * 128]
                op("dve", lambda e: e.max(out=v_.ap[:, half, 0:8], in_=w), R=[s12], W=[v_])
                op("dve", lambda e: e.match_replace(out=t1.ap, in_to_replace=v_.ap[:, half, 0:8], in_values=w,
                                                    imm_value=-1e30), R=[s12, v_], W=[t1])
                op("dve", lambda e: e.max(out=v_.ap[:, half, 8:16], in_=t1.ap), R=[t1], W=[v_])
            cd3 = cd.ap.rearrange("p (a b) -> p a b", b=16)
            op("dve", lambda e: e.tensor_tensor(out=cd3, in0=bc(v_.ap[:, 0, :], 2, [128, 16, 16]),
                                                in1=bc(v_.ap[:, 1, :], 1, [128, 16, 16]), op=ALU.add),
               R=[v_], W=[cd])
            op("dve", lambda e: e.max(out=c_.ap[:, 0:8], in_=cd.ap), R=[cd], W=[c_])
            op("dve", lambda e: e.match_replace(out=cd2.ap, in_to_replace=c_.ap[:, 0:8], in_values=cd.ap,
                                                imm_value=-1e30), R=[cd, c_], W=[cd2])
            op("dve", lambda e: e.max(out=c_.ap[:, 8:16], in_=cd2.ap), R=[cd2], W=[c_])
            op("dve", lambda e: e.match_replace(out=cd.ap, in_to_replace=c_.ap[:, 8:16], in_values=cd2.ap,
                                                imm_value=-1e30), R=[cd2, c_], W=[cd])
            op("dve", lambda e: e.max(out=c_.ap[:, 16:24], in_=cd.ap), R=[cd], W=[c_])
            op("dve", lambda e: e.tensor_scalar(out=thr.ap[:, gi:gi + 1], in0=c_.ap[:, 15:16],
                                                scalar1=c_.ap[:, 16:17], scalar2=0.5, op0=ALU.add, op1=ALU.mult),
               R=[c_], W=[thr])
            op("dve", lambda e: e.tensor_scalar(out=negm.ap[:, gi:gi + 1], in0=c_.ap[:, 0:1], scalar1=-1.0,
                                                scalar2=None, op0=ALU.mult), R=[c_], W=[negm])
            op("act", lambda e: e.activation(out=ez_.ap, in_=c_.ap[:, 0:16], func=AF.Exp,
                                             bias=negm.ap[:, gi:gi + 1], scale=1.0, accum_out=z_.ap[:, 0:1]),
               R=[c_, negm], W=[ez_, z_])
            op("dve", lambda e: e.reciprocal(out=z_.ap[:, 1:2], in_=z_.ap[:, 0:1]), R=[z_], W=[z_])
            op("dve", lambda e: e.tensor_scalar(out=Sel.ap[:, gi, :], in0=selm, scalar1=z_.ap[:, 1:2], scalar2=None,
                                                op0=ALU.mult), R=[z_, cst], W=[Sel])
        P.barrier()
        A.reset(m1)
        usrc, vsrc = [], []
        for a in range(128):
            usrc += [i_uT[l, a][:, 0:8, :], i_uT[l, a][:, 8:16, :]]
            vsrc += [i_v[l, a][:, 0:1024], i_v[l, a][:, 1024:2048]]
        us = WStream(usrc, (8, 128), ceng=("act",), nstage=3, nbf=2 * GA + 1)
        vs = WStream(vsrc, (1024,), ceng=("dve",), nstage=3, nbf=2 * GA + 1)
        FR = 12
        LAG = 3
        NGA = 128 // GA
        NGh = min(NG, 16)
        Dd = [A.f32(GA, 128) for _ in range(4)]
        ee = [A.bf16(GA, 128) for _ in range(4)]
        Fr = [A.bf16(GA, 128) for _ in range(FR)]
        gel = [A.bf16(NB) for _ in range(GA)]
        AT = [A.bf16(NB) for _ in range(GA)]
        stmp = [A.f32(NS), A.f32(NS)]
        st = {"f": 0, "pend": [], "fb": [], "u": {}, "v": {}}

        def fgen(ag, gi):
            k = st["f"]
            st["f"] += 1
            d_, e_, f_ = Dd[k % 4], ee[k % 4], Fr[k % FR]
            a0 = ag * GA
            op("pool", lambda e: e.tensor_tensor(out=d_.ap, in0=bc(s12.ap[:, gi, 128:256], 1, [128, GA, 128]),
                                                 in1=bc(s12.ap[:, gi, a0:a0 + GA], 2, [128, GA, 128]),
                                                 op=ALU.add), R=[s12], W=[d_])
            op("act", lambda e: e.activation(out=e_.ap, in_=d_.ap, func=AF.Exp, bias=negm.ap[:, gi:gi + 1],
                                             scale=1.0), R=[d_, negm], W=[e_])
            st["fb"].append((gi, d_, e_, f_))
            fgen_b(2)

        def fgen_b(lag):
            while len(st["fb"]) > lag:
                gi, d_, e_, f_ = st["fb"].pop(0)
                op("dve", lambda e: e.scalar_tensor_tensor(out=f_.ap, in0=d_.ap, scalar=thr.ap[:, gi:gi + 1],
                                                           in1=e_.ap, op0=ALU.is_ge, op1=ALU.mult),
                   R=[d_, e_, thr], W=[f_])
                st["pend"].append((gi, f_))

        def gmm_emit(lag):
            if lag == 0:
                fgen_b(0)
            while len(st["pend"]) > lag:
                gi, f_ = st["pend"].pop(0)
                for ai in range(GA):
                    pG = banks[2 + ai]
                    op("pe", lambda e, ai=ai, pG=pG: e.matmul(pG.ap[:, gi * 16:(gi + 1) * 16], f_.ap[:, ai, :],
                                                              Sel.ap[:, gi, :], start=True, stop=True),
                       R=[f_, Sel], W=[pG])

        def ucast(ag, k):
            st["u"][(ag, k)] = us.get()

        def vcast(ag, k):
            st["v"][(ag, k)] = vs.get()

        def sT_pe(ag, ai):
            pS = banks[ai % 2]
            for kc in range(KC):
                ub = st["u"][(ag, 2 * ai + kc // 8)]
                op("pe", lambda e, kc=kc, ub=ub: e.matmul(pS.ap[:, 0:N], ub.ap[:, kc % 8, :], hcT.ap[:, kc, 0:N],
                                                          start=(kc == 0), stop=(kc == KC - 1)),
                   R=[ub, hcT], W=[pS])

        def sT_gelu(ai):
            pS = banks[ai % 2]
            g_ = gel[ai]
            op("act", lambda e: e.activation(out=g_.ap[:, 0:N], in_=pS.ap[:, 0:N], func=AF.Gelu_apprx_tanh),
               R=[pS], W=[g_])

        def aTm():
            for ai in range(GA):
                pG = banks[2 + ai]
                g_, at_ = gel[ai], AT[ai]
                op("dve", lambda e: e.tensor_tensor(out=at_.ap[:, 0:N], in0=g_.ap[:, 0:N], in1=pG.ap[:, 0:N],
                                                    op=ALU.mult), R=[g_, pG], W=[at_])

        def outp(ag, dc):
            pO = banks[6 + dc % 2]
            for ai in range(GA):
                at_ = AT[ai]
                vb = st["v"][(ag, 2 * ai + dc // 8)]
                op("pe", lambda e, ai=ai, at_=at_, vb=vb: e.matmul(
                    pO.ap[:, 0:N], vb.ap[:, (dc % 8) * 128:(dc % 8 + 1) * 128], at_.ap[:, 0:N],
                    start=(ai == 0), stop=(ai == GA - 1)), R=[vb, at_], W=[pO])
            if samp:
                t_ = stmp[dc % 2]
                tv = t_.ap
                op("dve", lambda e: e.tensor_tensor(out=tv[:, 0:N], in0=pO.ap[:, 0:N], in1=modv(l, 5, dc, True),
                                                    op=ALU.mult), R=[pO, modT], W=[t_])
                op("dve", lambda e: e.tensor_tensor(out=xT.ap[:, dc, 0:N], in0=xT.ap[:, dc, 0:N], in1=tv[:, 0:N],
                                                    op=ALU.add), R=[t_, xT], W=[xT])
            else:
                op("dve", lambda e: e.scalar_tensor_tensor(out=xT.ap[:, dc, 0:N], in0=pO.ap[:, 0:N],
                                                           scalar=modv(l, 5, dc, False), in1=xT.ap[:, dc, 0:N],
                                                           op0=ALU.mult, op1=ALU.add), R=[pO, modT, xT], W=[xT])

        def phaseY(ag):
            for pr in range(GA // 2):
                sT_pe(ag, 2 * pr)
                sT_pe(ag, 2 * pr + 1)
                for k in range(8):
                    gi = NGh + 8 * pr + k
                    if k % 2 == 0:
                        vcast(ag, 4 * pr + k // 2)
                    if gi < NG:
                        fgen(ag, gi)
                        gmm_emit(LAG)
                sT_gelu(2 * pr)
                sT_gelu(2 * pr + 1)
            gmm_emit(0)
            aTm()

        for k in range(2 * GA):
            ucast(0, k)
        for gi in range(NGh):
            fgen(0, gi)
            gmm_emit(LAG)
        gmm_emit(0)
        phaseY(0)
        for ag in range(NGA):
            nxt = ag + 1 < NGA
            for dc in range(16):
                outp(ag, dc)
                if nxt:
                    if dc % 2 == 0:
                        ucast(ag + 1, dc // 2)
                    if dc < NGh:
                        fgen(ag + 1, dc)
                    gmm_emit(LAG)
            if nxt:
                gmm_emit(0)
                phaseY(ag + 1)
        P.barrier()
        A.reset(m)

    def run_block(N, samp, first, last, src, dst):
        dma(xT.ap[:, :, 0:N], src, W=[xT])
        for l in range(2):
            m = A.mark()
            hmT = A.bf16(KC, NB)
            norm_mod(l, 0, N, samp, hmT)
            if l == 0:
                rg_mixer(N, samp, last, hmT)
            else:
                ssd_mixer(N, samp, first, last, hmT)
            norm_mod(l, 1, N, samp, hmT)
            peer(l, N, samp, hmT)
            A.reset(m)
        m = A.mark()
        scr = [A.f32(NB), A.f32(NB)]
        rstd = A.f32(NB)
        yo = A.f32(KC, NB)
        rms_stats(lambda c: xT.ap[:, c, 0:N], [xT], N, KC, scr, banks[7], rstd)
        for c in range(KC):
            op("dve", lambda e: e.scalar_tensor_tensor(out=yo.ap[:, c, 0:N], in0=xT.ap[:, c, 0:N],
                                                       scalar=fing[:, c:c + 1], in1=rstd.ap[:, 0:N], op0=ALU.mult,
                                                       op1=ALU.mult), R=[xT, vec, rstd], W=[yo])
        dma(dst, yo.ap[:, :, 0:N], R=[yo])
        P.barrier()
        A.reset(m)

    for blk in range(nblk):
        run_block(NB, False, blk == 0, blk == nblk - 1,
                  i_xT[:, blk * NB:(blk + 1) * NB].rearrange("(c p) t -> p c t", p=128),
                  o_yT[:, blk * NB:(blk + 1) * NB].rearrange("(c p) t -> p c t", p=128))
    run_block(NS, True, True, False, i_xsT.rearrange("(c p) t -> p c t", p=128),
              o_ysT.rearrange("(c p) t -> p c t", p=128))
    P.finish()
    es.close()
    nc._arena_hw = A.hw
    return nc


def _wlay(w):
    K, N = w.shape
    return np.ascontiguousarray(w.reshape(K // 128, 128, N // 128, 128).transpose(2, 1, 0, 3))


def _vlay(v):
    return v.reshape(-1, 128).T


_CACHE = {}


def _prep_shared(inp):
    f = np.float32
    sh = {}
    cst = np.zeros((128, 4 * 128 + 16), f)
    cst[:, 0:128] = np.eye(128)
    cst[:, 128:256] = 1.0
    k = np.arange(128)
    cst[:, 256:384] = (k[:, None] <= k[None, :])
    cst[:, 384:512] = np.where(k[:, None] <= k[None, :], 0.0, NEG)
    cst[:, 512:528] = (k[:, None] // 8 == np.arange(16)[None, :])
    sh["cst"] = cst
    vec = np.zeros((128, 16 * 11 + 32 * 2 + 48 + 32), f)
    vec[:, 0:16] = _vlay(inp["norm1_g"][0]); vec[:, 16:32] = _vlay(inp["norm1_g"][1])
    vec[:, 32:48] = _vlay(inp["norm2_g"][0]); vec[:, 48:64] = _vlay(inp["norm2_g"][1])
    vec[:, 64:80] = _vlay(inp["final_g"])
    vec[:, 80:96] = _vlay(inp["rg_conv_b"][0])
    vec[:, 96:112] = _vlay(inp["rg_b_a"][0])
    vec[:, 112:128] = _vlay(inp["rg_b_i"][0])
    vec[:, 128:144] = _vlay(inp["rg_lambda"][0])
    vec[:, 144:160] = _vlay(inp["rg_b_out"][0])
    vec[:, 176:208] = _vlay(inp["rg_b_in"][0])
    vec[:, 208:240] = _vlay(inp["ssd_norm_g"][0])
    vec[:, 240:288] = _vlay(inp["ssd_conv_b"][0])
    vec[:, 288:320] = _vlay(np.repeat(inp["ssd_d"][0], 64))
    sh["vec"] = vec
    sh["bmod"] = np.ascontiguousarray(np.stack([_vlay(inp["b_mod"][l]) for l in range(2)], axis=1))
    sh["rgcw"] = np.ascontiguousarray(inp["rg_conv_w"][0].T.reshape(16, 128, 4).transpose(1, 0, 2))
    sh["ssdcw"] = np.ascontiguousarray(inp["ssd_conv_w"][0].T.reshape(48, 128, 4).transpose(1, 0, 2))
    sh["ssdh"] = np.ascontiguousarray(np.stack([inp["ssd_dt_bias"][0], inp["ssd_a_log"][0],
                                                inp["ssd_d"][0]]).astype(f))
    sh["wmod"] = np.stack([_wlay(inp["w_mod"][l]) for l in range(2)])
    sh["rgwin"] = _wlay(inp["rg_w_in"][0])

    def glay(w):
        return np.ascontiguousarray(w.reshape(8, 2, 128, 2, 128).transpose(0, 3, 2, 1, 4).reshape(16, 128, 2, 128))
    sh["rgwa"] = glay(inp["rg_w_a"][0])
    sh["rgwi"] = glay(inp["rg_w_i"][0])
    sh["rgwout"] = _wlay(inp["rg_w_out"][0])
    w = inp["ssd_w_in"][0]
    sh["ssdwin"] = _wlay(np.ascontiguousarray(w[:, :10240]))
    sh["ssdwdt"] = np.ascontiguousarray(w[:, 10240:].reshape(16, 128, 64).transpose(1, 0, 2))
    sh["ssdwout"] = _wlay(inp["ssd_w_out"][0])
    sh["wq"] = np.stack([_wlay(inp["peer_w_q"][l]) for l in range(2)])
    sh["kT"] = np.ascontiguousarray(np.stack([np.stack([inp["peer_k1"][l].T, inp["peer_k2"][l].T])
                                              for l in range(2)]))
    sh["uT"] = np.stack([np.ascontiguousarray(inp["peer_u"][l].reshape(128, 128, 16, 128).transpose(0, 3, 2, 1))
                         for l in range(2)])
    sh["v"] = np.ascontiguousarray(inp["peer_v"].reshape(2, 128, 128, 2048))
    return sh


def kernel(**inp):
    inp = {k: np.asarray(v) for k, v in inp.items()}
    TP = inp["x_prompt"].shape[1]
    nc = build(TP)
    sh = _prep_shared(inp)
    in_maps = []
    for c in range(8):
        s = c % 4
        sl = slice(c * NS, (c + 1) * NS)
        d = dict(sh)
        d["xT"] = np.ascontiguousarray(inp["x_prompt"][s].T)
        d["xsT"] = np.ascontiguousarray(inp["x_sample"][sl, 0, :].T)
        d["cT"] = np.ascontiguousarray(np.concatenate([inp["c_prompt"][s][None], inp["c_sample"][sl]], 0).T)
        d["rgconv"] = np.ascontiguousarray(inp["state_rg_conv"][0, sl].transpose(2, 1, 0))
        d["rgh"] = np.ascontiguousarray(inp["state_rg_h"][0, sl].T)
        d["ssdconv"] = np.ascontiguousarray(inp["state_ssd_conv"][0, sl].transpose(2, 1, 0))
        d["ssds"] = np.ascontiguousarray(inp["state_ssd"][0, sl].reshape(NS, 64, 64, 128).transpose(0, 3, 1, 2))
        in_maps.append(d)
    res = run_bass_kernel_spmd(nc, in_maps, core_ids=list(range(8))).results
    f = np.float32
    y_p = np.stack([res[s]["o_yT"].T for s in range(4)]).astype(f)
    y_s = np.concatenate([res[c]["o_ysT"].T for c in range(8)], 0)[:, None, :].astype(f)

    def unv(a):
        return a.transpose(1, 0, *range(2, a.ndim)).reshape(-1, *a.shape[2:])
    rg_conv_p = np.stack([unv(res[s]["o_rgconv_p"]).T for s in range(4)])[None].astype(f)
    rg_h_p = np.stack([unv(res[s]["o_rgh_p"]) for s in range(4)])[None].astype(f)
    ssd_conv_p = np.stack([unv(res[s]["o_ssdconv_p"]).T for s in range(4)])[None].astype(f)
    ssd_p = np.stack([res[s]["o_ssd_p"].transpose(1, 2, 0).reshape(8, 8, 64, 128) for s in range(4)])[None].astype(f)
    rg_conv_s = np.concatenate([unv(res[c]["o_rgconv_s"]).transpose(2, 1, 0) for c in range(8)], 0)[None].astype(f)
    rg_h_s = np.concatenate([unv(res[c]["o_rgh_s"]).T for c in range(8)], 0)[None].astype(f)
    ssd_conv_s = np.concatenate([unv(res[c]["o_ssdconv_s"]).transpose(2, 1, 0) for c in range(8)], 0)[None].astype(f)
    ssd_s = np.concatenate([res[c]["o_ssd_s"].transpose(0, 2, 3, 1).reshape(NS, 8, 8, 64, 128)
                            for c in range(8)], 0)[None].astype(f)
    return (y_p, y_s, rg_conv_p, rg_h_p, ssd_conv_p, ssd_p, rg_conv_s, rg_h_s, ssd_conv_s, ssd_s)
```

```python
import contextlib
import numpy as np
import concourse.bass as bass
import concourse.mybir as mybir
from concourse.bass_utils import run_bass_kernel_spmd

F32 = mybir.dt.float32
BF16 = mybir.dt.bfloat16
AF = mybir.ActivationFunctionType
ALU = mybir.AluOpType

D = 2048
KC = 16
NS = 16
NB = 512
EPS = 1e-6
NDS = 12
GA = 4
NEG = -30000.0


class Trk:
    __slots__ = ("w", "r")

    def __init__(self):
        self.w = None
        self.r = {}


class Buf:
    def __init__(self, ap):
        self.ap = ap
        self.t = Trk()


class Prog:
    def __init__(self, nc, es):
        self.nc = nc
        self.eng = {"pe": nc.tensor, "dve": nc.vector, "act": nc.scalar, "pool": nc.gpsimd, "sp": nc.sync}
        self.sem = {k: es.enter_context(nc.semaphore("s_" + k)) for k in ("pe", "dve", "act", "pool")}
        self.cnt = {k: 0 for k in self.sem}
        self.seen = {e: {k: 0 for k in self.sem} for e in self.eng}
        self.dsem = [es.enter_context(nc.semaphore("d%d" % i)) for i in range(NDS)]
        self.dcnt = [0] * NDS
        self.dn = 0
        self.dseen = {e: [0] * NDS for e in self.eng}

    def _need(self, e, tok):
        if tok is None:
            return
        if tok[0] == "c":
            _, f, n = tok
            if f == e and e == "pe":
                return
            if self.seen[e][f] < n:
                self.eng[e].wait_ge(self.sem[f], n)
                self.seen[e][f] = n
        else:
            _, i, v = tok
            if self.dseen[e][i] < v:
                self.eng[e].wait_ge(self.dsem[i], v)
                self.dseen[e][i] = v

    def _sync(self, e, R, W):
        for b in R:
            self._need(e, b.t.w)
        for b in W:
            self._need(e, b.t.w)
            for tok in b.t.r.values():
                self._need(e, tok)

    def op(self, e, fn, R=(), W=()):
        self._sync(e, R, W)
        ins = fn(self.eng[e])
        self.cnt[e] += 1
        ins.then_inc(self.sem[e], 1)
        tok = ("c", e, self.cnt[e])
        for b in R:
            b.t.r[e] = tok
        for b in W:
            b.t.w = tok
            b.t.r = {}
        return ins

    def dma(self, out, in_, R=(), W=(), q="sp"):
        i = self.dn % NDS
        self.dn += 1
        self._need(q, ("d", i, self.dcnt[i]))
        self._sync(q, R, W)
        ins = self.eng[q].dma_start(out=out, in_=in_)
        self.dcnt[i] += 16
        ins.then_inc(self.dsem[i], 16)
        tok = ("d", i, self.dcnt[i])
        for b in R:
            b.t.r["dma%d" % i] = tok
        for b in W:
            b.t.w = tok
            b.t.r = {}

    def barrier(self):
        for e in self.eng:
            for f in self.sem:
                self._need(e, ("c", f, self.cnt[f]))
            for i in range(NDS):
                self._need(e, ("d", i, self.dcnt[i]))

    def finish(self):
        for i in range(NDS):
            self._need("sp", ("d", i, self.dcnt[i]))
        for f in self.sem:
            self._need("sp", ("c", f, self.cnt[f]))


class Arena:
    def __init__(self, ap, width):
        self.ap = ap
        self.width = width
        self.off = 0
        self.hw = 0

    def mark(self):
        return self.off

    def reset(self, m):
        self.off = m

    def f32(self, *shape):
        n = int(np.prod(shape))
        assert self.off + n <= self.width, ("arena overflow", self.off, n, self.width)
        v = self.ap[:, self.off:self.off + n]
        self.off += n
        self.hw = max(self.hw, self.off)
        return Buf(_shape(v, shape))

    def bf16(self, *shape):
        n = int(np.prod(shape))
        w = (n + 1) // 2
        assert self.off + w <= self.width, ("arena overflow", self.off, w, self.width)
        v = self.ap[:, self.off:self.off + w].bitcast(BF16)[:, 0:n]
        self.off += w
        self.hw = max(self.hw, self.off)
        return Buf(_shape(v, shape))


def _shape(v, shape):
    if len(shape) == 1:
        return v
    if len(shape) == 2:
        return v.rearrange("p (a b) -> p a b", b=shape[1])
    if len(shape) == 3:
        return v.rearrange("p (a b c) -> p a b c", b=shape[1], c=shape[2])
    raise ValueError(shape)


def bc(ap, axis, shape):
    return ap.unsqueeze(axis).to_broadcast(list(shape))


def build(TP):
    nblk = TP // NB
    nc = bass.Bass("TRN2", target_bir_lowering=False)
    es = contextlib.ExitStack()

    def din(name, shape):
        return nc.dram_tensor(name, list(shape), F32, kind="ExternalInput").ap()

    def dout(name, shape):
        return nc.dram_tensor(name, list(shape), F32, kind="ExternalOutput").ap()

    i_xT = din("xT", [D, TP])
    i_xsT = din("xsT", [D, NS])
    i_cT = din("cT", [D, 1 + NS])
    i_rgconv = din("rgconv", [D, 3, NS])
    i_rgh = din("rgh", [D, NS])
    i_ssdconv = din("ssdconv", [6144, 3, NS])
    i_ssds = din("ssds", [NS, 128, 64, 64])
    i_cst = din("cst", [128, 4 * 128 + 16])
    i_vec = din("vec", [128, 16 * 11 + 32 * 2 + 48 + 32])
    i_bmod = din("bmod", [128, 2, 96])
    i_rgcw = din("rgcw", [128, 16, 4])
    i_ssdcw = din("ssdcw", [128, 48, 4])
    i_ssdh = din("ssdh", [3, 64])
    i_wmod = din("wmod", [2, 96, 128, 16, 128])
    i_rgwin = din("rgwin", [32, 128, 16, 128])
    i_rgwa = din("rgwa", [16, 128, 2, 128])
    i_rgwi = din("rgwi", [16, 128, 2, 128])
    i_rgwout = din("rgwout", [16, 128, 16, 128])
    i_ssdwin = din("ssdwin", [80, 128, 16, 128])
    i_ssdwdt = din("ssdwdt", [128, 16, 64])
    i_ssdwout = din("ssdwout", [16, 128, 32, 128])
    i_wq = din("wq", [2, 16, 128, 16, 128])
    i_kT = din("kT", [2, 2, 128, 128])
    i_uT = din("uT", [2, 128, 128, 16, 128])
    i_v = din("v", [2, 128, 128, 2048])
    o_yT = dout("o_yT", [D, TP])
    o_ysT = dout("o_ysT", [D, NS])
    o_rgconv_p = dout("o_rgconv_p", [128, 16, 3])
    o_rgh_p = dout("o_rgh_p", [128, 16])
    o_ssdconv_p = dout("o_ssdconv_p", [128, 48, 3])
    o_ssd_p = dout("o_ssd_p", [128, 64, 64])
    o_rgconv_s = dout("o_rgconv_s", [128, 16, 3, NS])
    o_rgh_s = dout("o_rgh_s", [128, 16, NS])
    o_ssdconv_s = dout("o_ssdconv_s", [128, 48, 3, NS])
    o_ssd_s = dout("o_ssd_s", [NS, 128, 64, 64])

    AW = 51800
    arena_t = es.enter_context(nc.sbuf_tensor("arena", [128, AW], F32))
    A = Arena(arena_t[:, :], AW)
    banks = [Buf(es.enter_context(nc.psum_tensor("pb%d" % i, [128, 512], F32))[:, :]) for i in range(8)]
    P = Prog(nc, es)
    op, dma = P.op, P.dma

    cst = A.f32(4 * 128 + 16)
    ident = cst.ap[:, 0:128]
    ones = cst.ap[:, 128:256]
    tri = cst.ap[:, 256:384]
    mneg = cst.ap[:, 384:512]
    selm = cst.ap[:, 512:528]
    dma(cst.ap, i_cst, W=[cst])
    identb = A.bf16(128)
    op("dve", lambda e: e.tensor_copy(out=identb.ap, in_=ident), R=[cst], W=[identb])
    vec = A.f32(16 * 11 + 32 * 2 + 48 + 32)
    dma(vec.ap, i_vec, W=[vec])

    def vsl(o, n):
        return vec.ap[:, o:o + n]
    n1g = [vsl(0, 16), vsl(16, 16)]
    n2g = [vsl(32, 16), vsl(48, 16)]
    fing = vsl(64, 16)
    rg_bconv = vsl(80, 16)
    rg_ba = vsl(96, 16)
    rg_bi = vsl(112, 16)
    rg_lam = vsl(128, 16)
    rg_bout = vsl(144, 16)
    rg_bin = vsl(176, 32)
    ssd_ng = vsl(208, 32)
    ssd_bconv = vsl(240, 48)
    ssd_dexp = vsl(288, 32)
    rgcw = A.f32(16, 4)
    dma(rgcw.ap, i_rgcw, W=[rgcw])
    ssdcw = A.f32(48, 4)
    dma(ssdcw.ap, i_ssdcw, W=[ssdcw])
    ssdh = A.f32(3, 64)
    dma(ssdh.ap, i_ssdh.partition_broadcast(128), W=[ssdh])
    Abc = A.f32(64)
    op("act", lambda e: e.activation(out=Abc.ap, in_=ssdh.ap[:, 1, :], func=AF.Exp), R=[ssdh], W=[Abc])
    op("dve", lambda e: e.tensor_scalar(out=Abc.ap, in0=Abc.ap, scalar1=-1.0, scalar2=None, op0=ALU.mult),
       R=[Abc], W=[Abc])
    c8 = A.f32(16)
    op("act", lambda e: e.activation(out=c8.ap, in_=rg_lam, func=AF.Exp, scale=-1.0), R=[vec], W=[c8])
    op("act", lambda e: e.activation(out=c8.ap, in_=c8.ap, func=AF.Ln, bias=1.0), R=[c8], W=[c8])
    op("dve", lambda e: e.tensor_scalar(out=c8.ap, in0=c8.ap, scalar1=-8.0, scalar2=None, op0=ALU.mult),
       R=[c8], W=[c8])
    epsb = A.f32(1)
    op("pool", lambda e: e.memset(epsb.ap, EPS), W=[epsb])
    kT = A.f32(4, 128)
    for l_ in range(2):
        for h_ in range(2):
            dma(kT.ap[:, l_ * 2 + h_, :], i_kT[l_, h_], W=[kT])

    modT = A.f32(2, 96, 1 + NS)
    csT = A.f32(KC, 1 + NS)
    for kc_ in range(KC):
        dma(csT.ap[:, kc_, :], i_cT[kc_ * 128:(kc_ + 1) * 128, :], W=[csT])
    op("act", lambda e: e.activation(out=csT.ap, in_=csT.ap, func=AF.Silu), R=[csT], W=[csT])
    bmod = A.f32(2, 96)
    dma(bmod.ap, i_bmod, W=[bmod])
    m0 = A.mark()
    NST = 4
    stg = [A.f32(KC, 128) for _ in range(NST)]
    jobs = [(l, j) for l in range(2) for j in range(96)]
    for idx in range(min(NST - 1, len(jobs))):
        l, j = jobs[idx]
        dma(stg[idx % NST].ap, i_wmod[l, j], W=[stg[idx % NST]])
    for idx, (l, j) in enumerate(jobs):
        nx = idx + NST - 1
        if nx < len(jobs):
            dma(stg[nx % NST].ap, i_wmod[jobs[nx][0], jobs[nx][1]], W=[stg[nx % NST]])
        s = stg[idx % NST]
        pb = banks[idx % 2]
        for kc in range(KC):
            op("pe", lambda e, kc=kc: e.matmul(pb.ap[:, 0:1 + NS], s.ap[:, kc, :], csT.ap[:, kc, :],
                                                start=(kc == 0), stop=(kc == KC - 1)), R=[s, csT], W=[pb])
        op("dve", lambda e: e.tensor_scalar(out=modT.ap[:, l, j, :], in0=pb.ap[:, 0:1 + NS],
                                            scalar1=bmod.ap[:, l, j:j + 1], scalar2=None, op0=ALU.add),
           R=[pb, bmod], W=[modT])
    A.reset(m0)
    P.barrier()
    gmp = A.f32(2, 2, 16)
    gms = A.f32(4, 16, NS)
    for l in range(2):
        for k, (sc0, ng) in enumerate(((16, n1g[l]), (64, n2g[l]))):
            op("dve", lambda e: e.scalar_tensor_tensor(out=gmp.ap[:, l, k, :], in0=modT.ap[:, l, sc0:sc0 + 16, 0],
                                                       scalar=1.0, in1=ng, op0=ALU.add, op1=ALU.mult),
               R=[modT, vec], W=[gmp])
            op("dve", lambda e: e.scalar_tensor_tensor(out=gms.ap[:, l * 2 + k, :, :],
                                                       in0=modT.ap[:, l, sc0:sc0 + 16, 1:1 + NS], scalar=1.0,
                                                       in1=bc(ng, 2, [128, 16, NS]), op0=ALU.add, op1=ALU.mult),
               R=[modT, vec], W=[gms])

    def modv(l, k, c, samp):
        if samp:
            return modT.ap[:, l, 16 * k + c, 1:1 + NS]
        return modT.ap[:, l, 16 * k + c, 0:1]

    xT = A.f32(KC, NB)
    rg_tail = A.f32(16, 3)
    rg_carry = A.f32(16)
    ssd_tail = A.f32(48, 3)
    for b_ in (rg_tail, rg_carry, ssd_tail):
        op("pool", lambda e, b_=b_: e.memset(b_.ap, 0.0), W=[b_])
    sT_dram = Buf(o_ssd_p)
    pmark = A.mark()

    class WStream:
        def __init__(self, srcs, shape, ceng=("pool",), nstage=3, nbf=3):
            self.srcs = srcs
            self.shape = shape
            self.stage = [A.f32(*shape) for _ in range(nstage)]
            self.bfs = [A.bf16(*shape) for _ in range(nbf)]
            self.ceng = ceng
            self.nl = 0
            self.ncast = 0
            self.pf = nstage - 1
            for _ in range(min(self.pf, len(srcs))):
                self._load()

        def _load(self):
            if self.nl < len(self.srcs):
                s = self.stage[self.nl % len(self.stage)]
                dma(s.ap, self.srcs[self.nl], W=[s])
                self.nl += 1

        def get(self):
            i = self.ncast
            self._load()
            s = self.stage[i % len(self.stage)]
            b = self.bfs[i % len(self.bfs)]
            e_ = self.ceng[i % len(self.ceng)]
            if e_ == "act":
                op("act", lambda e: e.copy(out=b.ap, in_=s.ap), R=[s], W=[b])
            else:
                op(e_, lambda e: e.tensor_copy(out=b.ap, in_=s.ap), R=[s], W=[b])
            self.ncast += 1
            return b

    def rms_stats(src_chunks, srcbufs, N, nch, scratch, pb, rstd, eng_sq="act"):
        for c in range(nch):
            sq = scratch[c % len(scratch)]
            op("act", lambda e, c=c, sq=sq: e.activation(out=sq.ap[:, 0:N], in_=src_chunks(c), func=AF.Square),
               R=srcbufs, W=[sq])
            op("pe", lambda e, c=c, sq=sq: e.matmul(pb.ap[:, 0:N], ones, sq.ap[:, 0:N], start=(c == 0),
                                                    stop=(c == nch - 1)), R=[sq, cst], W=[pb])
        op("act", lambda e: e.activation(out=rstd.ap[:, 0:N], in_=pb.ap[:, 0:N], func=AF.Sqrt,
                                         scale=1.0 / (nch * 128), bias=epsb.ap[:, 0:1]), R=[pb, epsb], W=[rstd])
        op("dve", lambda e: e.reciprocal(out=rstd.ap[:, 0:N], in_=rstd.ap[:, 0:N]), R=[rstd], W=[rstd])

    def norm_mod(l, which, N, samp, dst):
        m = A.mark()
        scr = [A.f32(NB), A.f32(NB)]
        rstd = A.f32(NB)
        tmp = [A.f32(NB), A.f32(NB)]
        rms_stats(lambda c: xT.ap[:, c, 0:N], [xT], N, KC, scr, banks[7], rstd)
        for c in range(KC):
            t = tmp[c % 2]
            if samp:
                gm = gms.ap[:, l * 2 + which, c, :]
                sh = modv(l, 3 * which, c, True)
                op("dve", lambda e: e.tensor_tensor(out=t.ap[:, 0:N], in0=xT.ap[:, c, 0:N], in1=rstd.ap[:, 0:N],
                                                    op=ALU.mult), R=[xT, rstd], W=[t])
                op("dve", lambda e: e.tensor_tensor(out=t.ap[:, 0:N], in0=t.ap[:, 0:N], in1=gm, op=ALU.mult),
                   R=[t, gms], W=[t])
                op("dve", lambda e: e.tensor_tensor(out=dst.ap[:, c, 0:N], in0=t.ap[:, 0:N], in1=sh, op=ALU.add),
                   R=[t, modT], W=[dst])
            else:
                gm = gmp.ap[:, l, which, c:c + 1]
                sh = modv(l, 3 * which, c, False)
                op("dve", lambda e: e.scalar_tensor_tensor(out=t.ap[:, 0:N], in0=xT.ap[:, c, 0:N], scalar=gm,
                                                           in1=rstd.ap[:, 0:N], op0=ALU.mult, op1=ALU.mult),
                   R=[xT, rstd, gmp], W=[t])
                op("act", lambda e: e.activation(out=dst.ap[:, c, 0:N], in_=t.ap[:, 0:N], func=AF.Identity,
                                                 bias=sh, scale=1.0), R=[t, modT], W=[dst])
        A.reset(m)

    def resid_add(c, pb, N, bias, gate, samp, tmpb):
        if samp:
            if bias is not None:
                op("dve", lambda e: e.scalar_tensor_tensor(out=tmpb.ap[:, 0:N], in0=pb.ap[:, 0:N], scalar=bias,
                                                           in1=gate, op0=ALU.add, op1=ALU.mult),
                   R=[pb, vec, modT], W=[tmpb])
            else:
                op("dve", lambda e: e.tensor_tensor(out=tmpb.ap[:, 0:N], in0=pb.ap[:, 0:N], in1=gate, op=ALU.mult),
                   R=[pb, modT], W=[tmpb])
            op("dve", lambda e: e.tensor_tensor(out=xT.ap[:, c, 0:N], in0=xT.ap[:, c, 0:N], in1=tmpb.ap[:, 0:N],
                                                op=ALU.add), R=[tmpb, xT], W=[xT])
        else:
            if bias is not None:
                op("dve", lambda e: e.tensor_scalar(out=tmpb.ap[:, 0:N], in0=pb.ap[:, 0:N], scalar1=bias,
                                                    scalar2=gate, op0=ALU.add, op1=ALU.mult),
                   R=[pb, vec, modT], W=[tmpb])
                op("dve", lambda e: e.tensor_tensor(out=xT.ap[:, c, 0:N], in0=xT.ap[:, c, 0:N],
                                                    in1=tmpb.ap[:, 0:N], op=ALU.add), R=[tmpb, xT], W=[xT])
            else:
                op("dve", lambda e: e.scalar_tensor_tensor(out=xT.ap[:, c, 0:N], in0=pb.ap[:, 0:N], scalar=gate,
                                                           in1=xT.ap[:, c, 0:N], op0=ALU.mult, op1=ALU.add),
                   R=[pb, modT, xT], W=[xT])

    def conv4(dst, taps, wcol, bcol, N, Rb, Wb):
        op("dve", lambda e: e.tensor_scalar(out=dst, in0=taps[0], scalar1=wcol(0), scalar2=bcol, op0=ALU.mult,
                                            op1=ALU.add), R=Rb, W=Wb)
        for k in range(1, 4):
            op("dve", lambda e, k=k: e.scalar_tensor_tensor(out=dst, in0=taps[k], scalar=wcol(k), in1=dst,
                                                            op0=ALU.mult, op1=ALU.add), R=Rb + Wb, W=Wb)

    def rg_mixer(N, samp, last, hmT):
        m = A.mark()
        gT = A.bf16(KC, NB)
        yT = A.bf16(KC, NB)
        if samp:
            nbuf = A.f32(KC, 3, NS)
            hout = A.f32(KC, NS)
        m_in = A.mark()
        win_srcs = []
        for p in range(8):
            win_srcs += [i_rgwin[2 * p], i_rgwin[2 * p + 1], i_rgwin[16 + 2 * p], i_rgwin[16 + 2 * p + 1]]
        ws = WStream(win_srcs, (KC, 128), ceng=("act", "dve", "pool", "act", "dve"), nstage=2, nbf=3)
        gsrcs = []
        for p in range(8):
            gsrcs += [i_rgwa[2 * p], i_rgwa[2 * p + 1], i_rgwi[2 * p], i_rgwi[2 * p + 1]]
        gs = WStream(gsrcs, (2, 128), ceng=("pool",), nstage=4, nbf=4)
        xe = [A.f32(2, NB + 3), A.f32(2, NB + 3)]
        xc = [A.f32(2, NB), A.f32(2, NB)]
        xcb = [A.bf16(2, NB), A.bf16(2, NB)]
        gate_r = [A.f32(NB), A.f32(NB)]
        gate_i = [A.f32(NB), A.f32(NB)]
        av = [A.f32(NB), A.f32(NB)]
        bv = [A.f32(NB), A.f32(NB)]
        hs = [A.f32(NB), A.f32(NB)]
        if samp:
            st_c = A.f32(KC, 3, NS)
            for c_ in range(KC):
                dma(st_c.ap[:, c_], i_rgconv[c_ * 128:(c_ + 1) * 128], W=[st_c])
            h0s = A.f32(KC, NS)
            for c_ in range(KC):
                dma(h0s.ap[:, c_, :], i_rgh[c_ * 128:(c_ + 1) * 128, :], W=[h0s])
        for p in range(8):
            xe_, xc_, xcb_ = xe[p % 2], xc[p % 2], xcb[p % 2]
            for q4 in range(4):
                wb = ws.get()
                pb = banks[q4 % 4]
                for kc in range(KC):
                    op("pe", lambda e, kc=kc: e.matmul(pb.ap[:, 0:N], wb.ap[:, kc, :], hmT.ap[:, kc, 0:N],
                                                        start=(kc == 0), stop=(kc == KC - 1)), R=[wb, hmT], W=[pb])
                if q4 < 2:
                    c = 2 * p + q4
                    op("act", lambda e: e.activation(out=gT.ap[:, c, 0:N], in_=pb.ap[:, 0:N],
                                                     func=AF.Gelu_apprx_tanh, bias=rg_bin[:, c:c + 1], scale=1.0),
                       R=[pb, vec], W=[gT])
                else:
                    q = q4 - 2
                    c = 2 * p + q
                    if not samp:
                        op("pool", lambda e: e.tensor_copy(out=xe_.ap[:, q, 0:3], in_=rg_tail.ap[:, c, :]),
                           R=[rg_tail], W=[xe_])
                    op("act", lambda e: e.activation(out=xe_.ap[:, q, 3:3 + N], in_=pb.ap[:, 0:N], func=AF.Identity,
                                                     bias=rg_bin[:, 16 + c:16 + c + 1], scale=1.0),
                       R=[pb, vec], W=[xe_])
            for q in range(2):
                c = 2 * p + q
                if samp:
                    taps = [st_c.ap[:, c, 0, :], st_c.ap[:, c, 1, :], st_c.ap[:, c, 2, :], xe_.ap[:, q, 3:3 + N]]
                    Rb = [xe_, st_c, rgcw, vec]
                else:
                    taps = [xe_.ap[:, q, k:k + N] for k in range(4)]
                    Rb = [xe_, rgcw, vec]
                conv4(xc_.ap[:, q, 0:N], taps, lambda k: rgcw.ap[:, c, k:k + 1], rg_bconv[:, c:c + 1], N, Rb, [xc_])
                op("act", lambda e: e.copy(out=xcb_.ap[:, q, 0:N], in_=xc_.ap[:, q, 0:N]), R=[xc_], W=[xcb_])
                if samp:
                    for k in range(2):
                        op("pool", lambda e, k=k: e.tensor_copy(out=nbuf.ap[:, c, k, :], in_=st_c.ap[:, c, k + 1, :]),
                           R=[st_c], W=[nbuf])
                    op("pool", lambda e: e.tensor_copy(out=nbuf.ap[:, c, 2, :], in_=xe_.ap[:, q, 3:3 + N]),
                       R=[xe_], W=[nbuf])
                else:
                    op("pool", lambda e: e.tensor_copy(out=rg_tail.ap[:, c, :], in_=xe_.ap[:, q, N:N + 3]),
                       R=[xe_], W=[rg_tail])
            gw = [gs.get() for _ in range(4)]
            for jh in range(2):
                c = 2 * p + jh
                r_, i_, a_, b_, h_ = gate_r[jh], gate_i[jh], av[jh], bv[jh], hs[jh]
                for gi_, (wt, dstb, bcol) in enumerate(((gw[jh], r_, rg_ba), (gw[2 + jh], i_, rg_bi))):
                    pb = banks[4 + (2 * jh + gi_) % 4]
                    for ih in range(2):
                        op("pe", lambda e, ih=ih: e.matmul(pb.ap[:, 0:N], wt.ap[:, ih, :], xcb_.ap[:, ih, 0:N],
                                                            start=(ih == 0), stop=(ih == 1)), R=[wt, xcb_], W=[pb])
                    op("act", lambda e: e.activation(out=dstb.ap[:, 0:N], in_=pb.ap[:, 0:N], func=AF.Sigmoid,
                                                     bias=bcol[:, c:c + 1], scale=1.0), R=[pb, vec], W=[dstb])
                op("act", lambda e: e.activation(out=a_.ap[:, 0:N], in_=r_.ap[:, 0:N], func=AF.Exp,
                                                 scale=c8.ap[:, c:c + 1]), R=[r_, c8], W=[a_])
                op("dve", lambda e: e.tensor_tensor(out=b_.ap[:, 0:N], in0=a_.ap[:, 0:N], in1=a_.ap[:, 0:N],
                                                    op=ALU.mult), R=[a_], W=[b_])
                op("dve", lambda e: e.tensor_scalar(out=b_.ap[:, 0:N], in0=b_.ap[:, 0:N], scalar1=-1.0, scalar2=1.0,
                                                    op0=ALU.mult, op1=ALU.add), R=[b_], W=[b_])
                op("dve", lambda e: e.tensor_scalar(out=b_.ap[:, 0:N], in0=b_.ap[:, 0:N], scalar1=1e-30,
                                                    scalar2=None, op0=ALU.max), R=[b_], W=[b_])
                op("act", lambda e: e.activation(out=b_.ap[:, 0:N], in_=b_.ap[:, 0:N], func=AF.Sqrt),
                   R=[b_], W=[b_])
                op("dve", lambda e: e.tensor_tensor(out=i_.ap[:, 0:N], in0=i_.ap[:, 0:N], in1=xc_.ap[:, jh, 0:N],
                                                    op=ALU.mult), R=[i_, xc_], W=[i_])
                op("dve", lambda e: e.tensor_tensor(out=b_.ap[:, 0:N], in0=b_.ap[:, 0:N], in1=i_.ap[:, 0:N],
                                                    op=ALU.mult), R=[b_, i_], W=[b_])
                if samp:
                    op("dve", lambda e: e.tensor_tensor(out=h_.ap[:, 0:N], in0=a_.ap[:, 0:N], in1=h0s.ap[:, c, :],
                                                        op=ALU.mult), R=[a_, h0s], W=[h_])
                    op("dve", lambda e: e.tensor_tensor(out=h_.ap[:, 0:N], in0=h_.ap[:, 0:N], in1=b_.ap[:, 0:N],
                                                        op=ALU.add), R=[h_, b_], W=[h_])
                    op("pool", lambda e: e.tensor_copy(out=hout.ap[:, c, :], in_=h_.ap[:, 0:N]), R=[h_], W=[hout])
                else:
                    op("dve", lambda e: e.tensor_tensor_scan(out=h_.ap[:, 0:N], data0=a_.ap[:, 0:N],
                                                             data1=b_.ap[:, 0:N], initial=rg_carry.ap[:, c:c + 1],
                                                             op0=ALU.mult, op1=ALU.add),
                       R=[a_, b_, rg_carry], W=[h_])
                    op("pool", lambda e: e.tensor_copy(out=rg_carry.ap[:, c:c + 1], in_=h_.ap[:, N - 1:N]),
                       R=[h_], W=[rg_carry])
                op("dve", lambda e: e.tensor_tensor(out=yT.ap[:, c, 0:N], in0=h_.ap[:, 0:N], in1=gT.ap[:, c, 0:N],
                                                    op=ALU.mult), R=[h_, gT], W=[yT])
        P.barrier()
        A.reset(m_in)
        wo = WStream([i_rgwout[j] for j in range(16)], (KC, 128), ceng=("act", "dve", "pool", "act", "dve"))
        tmpb = [A.f32(NB), A.f32(NB)]
        for j in range(16):
            wb = wo.get()
            pb = banks[j % 4]
            for kc in range(KC):
                op("pe", lambda e, kc=kc: e.matmul(pb.ap[:, 0:N], wb.ap[:, kc, :], yT.ap[:, kc, 0:N],
                                                    start=(kc == 0), stop=(kc == KC - 1)), R=[wb, yT], W=[pb])
            resid_add(j, pb, N, rg_bout[:, j:j + 1], modv(0, 2, j, samp), samp, tmpb[j % 2])
        if samp:
            dma(o_rgconv_s, nbuf.ap, R=[nbuf])
            dma(o_rgh_s, hout.ap, R=[hout])
        elif last:
            dma(o_rgconv_p, rg_tail.ap, R=[rg_tail])
            dma(o_rgh_p, rg_carry.ap, R=[rg_carry])
        P.barrier()
        A.reset(m)

    def ssd_temps():
        T = {}
        T["dt"] = A.f32(64)
        T["dtA"] = A.f32(64)
        T["ncum"] = A.f32(64)
        T["xtok"] = A.bf16(4096)
        T["btok"] = A.bf16(1024)
        T["cbt"] = A.f32(8, 128)
        T["sTb"] = A.bf16(64, 64)
        T["rhs4"] = [A.f32(4, 128), A.f32(4, 128)]
        T["arg4"] = [A.f32(4, 128), A.f32(4, 128)]
        T["ET4"] = [A.f32(4, 128), A.f32(4, 128)]
        T["ecr4"] = [A.f32(4, 128), A.f32(4, 128)]
        T["WT"] = [A.bf16(128) for _ in range(4)]
        T["CpT"] = [A.bf16(128) for _ in range(4)]
        T["xw"] = [A.bf16(64) for _ in range(4)]
        return T

    def ssd_chunk(Q, t0, sT, hmT, xbcT, yT, dtw, T):
        pdt = banks[0]
        for kc in range(KC):
            op("pe", lambda e, kc=kc: e.matmul(pdt.ap[0:Q, 0:64], hmT.ap[:, kc, t0:t0 + Q], dtw.ap[:, kc, :],
                                                start=(kc == 0), stop=(kc == KC - 1)), R=[hmT, dtw], W=[pdt])
        dt = T["dt"]
        dtA = T["dtA"]
        op("dve", lambda e: e.tensor_tensor(out=dt.ap[0:Q, :], in0=pdt.ap[0:Q, 0:64], in1=ssdh.ap[0:Q, 0, :],
                                            op=ALU.add), R=[pdt, ssdh], W=[dt])
        op("act", lambda e: e.activation(out=dt.ap[0:Q, :], in_=dt.ap[0:Q, :], func=AF.Exp), R=[dt], W=[dt])
        op("act", lambda e: e.activation(out=dt.ap[0:Q, :], in_=dt.ap[0:Q, :], func=AF.Ln, bias=1.0),
           R=[dt], W=[dt])
        op("dve", lambda e: e.tensor_tensor(out=dtA.ap[0:Q, :], in0=dt.ap[0:Q, :], in1=Abc.ap[0:Q, :],
                                            op=ALU.mult), R=[dt, Abc], W=[dtA])
        pc = banks[1]
        op("pe", lambda e: e.matmul(pc.ap[0:Q, 0:64], tri[0:Q, 0:Q], dtA.ap[0:Q, :], start=True, stop=True),
           R=[dtA, cst], W=[pc])
        ncum = T["ncum"]
        op("dve", lambda e: e.tensor_scalar(out=ncum.ap[0:Q, :], in0=pc.ap[0:Q, 0:64], scalar1=-1.0, scalar2=None,
                                            op0=ALU.mult), R=[pc], W=[ncum])
        xtok = T["xtok"]
        btok = T["btok"]
        for grp in range(5):
            pbt = banks[2 + grp % 2]
            pv = pbt.ap.bitcast(BF16)
            for k in range(8):
                j = grp * 8 + k
                op("pe", lambda e, j=j, k=k: e.transpose(pv[0:Q, k * 128:(k + 1) * 128], xbcT.ap[:, j, t0:t0 + Q],
                                                         identb.ap), R=[xbcT, identb], W=[pbt])
            if grp < 4:
                op("act", lambda e: e.copy(out=xtok.ap[0:Q, grp * 1024:(grp + 1) * 1024], in_=pv[0:Q, :]),
                   R=[pbt], W=[xtok])
            else:
                op("act", lambda e: e.copy(out=btok.ap[0:Q, :], in_=pv[0:Q, :]), R=[pbt], W=[btok])
        cbt = T["cbt"]
        for g in range(8):
            pcb = banks[4 + g % 2]
            op("pe", lambda e: e.matmul(pcb.ap[0:Q, 0:Q], xbcT.ap[:, 32 + g, t0:t0 + Q], xbcT.ap[:, 40 + g, t0:t0 + Q],
                                        start=True, stop=True), R=[xbcT], W=[pcb])
            op("act", lambda e: e.copy(out=cbt.ap[0:Q, g, 0:Q], in_=pcb.ap[0:Q, 0:Q]), R=[pcb], W=[cbt])
        sTb = T["sTb"]
        op("pool", lambda e: e.tensor_copy(out=sTb.ap, in_=sT.ap), R=[sT], W=[sTb])
        rhs4, arg4, ET4, ecr4 = T["rhs4"], T["arg4"], T["ET4"], T["ecr4"]
        WT, CpT, xw = T["WT"], T["CpT"], T["xw"]
        def partA(h4):
            r4, a4, E4, c4 = rhs4[h4 % 2], arg4[h4 % 2], ET4[h4 % 2], ecr4[h4 % 2]
            op("pool", lambda e: e.tensor_tensor(out=r4.ap[0:Q, :, 0:Q], in0=bc(tri[0:Q, 0:Q], 1, [Q, 4, Q]),
                                                 in1=bc(dtA.ap[0:Q, h4 * 4:h4 * 4 + 4], 2, [Q, 4, Q]), op=ALU.mult),
               R=[dtA, cst], W=[r4])
            pcr = banks[6 + h4 % 2]
            pcr_v = pcr.ap.rearrange("p (a b) -> p a b", b=128)
            for hh in range(4):
                op("pe", lambda e, hh=hh: e.matmul(pcr_v[:, hh, 0:Q], ones[0:Q, :], r4.ap[0:Q, hh, 0:Q],
                                                    start=True, stop=True), R=[r4, cst], W=[pcr])
            for hh in range(4):
                h = h4 * 4 + hh
                op("dve", lambda e, hh=hh, h=h: e.scalar_tensor_tensor(out=a4.ap[0:Q, hh, 0:Q], in0=pcr_v[0:Q, hh, 0:Q],
                                                                       scalar=ncum.ap[0:Q, h:h + 1], in1=mneg[0:Q, 0:Q],
                                                                       op0=ALU.add, op1=ALU.min),
                   R=[pcr, ncum, cst], W=[a4])
            op("act", lambda e: e.activation(out=E4.ap[0:Q, :, 0:Q], in_=a4.ap[0:Q, :, 0:Q], func=AF.Exp),
               R=[a4], W=[E4])
            op("act", lambda e: e.activation(out=c4.ap[:, :, 0:Q], in_=pcr_v[:, :, 0:Q], func=AF.Exp),
               R=[pcr], W=[c4])

        def partB(h4):
            E4, c4 = ET4[h4 % 2], ecr4[h4 % 2]
            g = h4 // 2
            hs_ = [h4 * 4 + hh for hh in range(4)]
            for hh, h in enumerate(hs_):
                w_ = WT[hh]
                op("dve", lambda e: e.scalar_tensor_tensor(
                    out=w_.ap[0:Q, 0:Q], in0=E4.ap[0:Q, hh, 0:Q], scalar=dt.ap[0:Q, h:h + 1], in1=cbt.ap[0:Q, g, 0:Q],
                    op0=ALU.mult, op1=ALU.mult), R=[E4, dt, cbt], W=[w_])
            for hh, h in enumerate(hs_):
                cp_ = CpT[hh]
                op("pool", lambda e: e.tensor_tensor(
                    out=cp_.ap[:, 0:Q], in0=xbcT.ap[:, 40 + g, t0:t0 + Q], in1=c4.ap[:, hh, 0:Q], op=ALU.mult),
                   R=[xbcT, c4], W=[cp_])
            for hh, h in enumerate(hs_):
                xw_ = xw[hh]
                op("dve", lambda e: e.tensor_scalar(
                    out=xw_.ap[0:Q, :], in0=xtok.ap[0:Q, h * 64:(h + 1) * 64], scalar1=E4.ap[0:Q, hh, Q - 1:Q],
                    scalar2=dt.ap[0:Q, h:h + 1], op0=ALU.mult, op1=ALU.mult), R=[xtok, E4, dt], W=[xw_])
            for hh, h in enumerate(hs_):
                w_, cp_ = WT[hh], CpT[hh]
                c = h // 2
                pby = banks[0 + (c // 4) % 2]
                po = (h % 2) * 64
                col = (c % 4) * 128
                op("pe", lambda e: e.matmul(
                    pby.ap[po:po + 64, col:col + Q], xtok.ap[0:Q, h * 64:(h + 1) * 64], w_.ap[0:Q, 0:Q],
                    start=True, stop=False), R=[xtok, w_], W=[pby])
                op("pe", lambda e: e.matmul(
                    pby.ap[po:po + 64, col:col + Q], sTb.ap[:, h, :], cp_.ap[:, 0:Q],
                    start=False, stop=True), R=[sTb, cp_], W=[pby])
            for hh, h in enumerate(hs_):
                xw_ = xw[hh]
                pst = banks[2 + (h // 8) % 2]
                scol = (h % 8) * 64
                op("pe", lambda e: e.matmul(
                    pst.ap[:, scol:scol + 64], btok.ap[0:Q, g * 128:(g + 1) * 128], xw_.ap[0:Q, :],
                    start=True, stop=True), R=[btok, xw_], W=[pst])
            for hh, h in enumerate(hs_):
                if h % 2 == 1:
                    c = h // 2
                    pby = banks[0 + (c // 4) % 2]
                    col = (c % 4) * 128
                    op("dve", lambda e: e.scalar_tensor_tensor(
                        out=yT.ap[:, c, t0:t0 + Q], in0=xbcT.ap[:, c, t0:t0 + Q], scalar=ssd_dexp[:, c:c + 1],
                        in1=pby.ap[:, col:col + Q], op0=ALU.mult, op1=ALU.add), R=[xbcT, vec, pby], W=[yT])
            for hh, h in enumerate(hs_):
                pst = banks[2 + (h // 8) % 2]
                scol = (h % 8) * 64
                op("dve", lambda e: e.scalar_tensor_tensor(
                    out=sT.ap[:, h, :], in0=sT.ap[:, h, :], scalar=c4.ap[:, hh, Q - 1:Q], in1=pst.ap[:, scol:scol + 64],
                    op0=ALU.mult, op1=ALU.add), R=[sT, c4, pst], W=[sT])

        partA(0)
        for h4 in range(16):
            if h4 + 1 < 16:
                partA(h4 + 1)
            partB(h4)

    def ssd_mixer(N, samp, first, last, hmT):
        m = A.mark()
        xbcT = A.bf16(48, NB)
        yT = xbcT
        dtw = A.bf16(KC, 64)
        m1 = A.mark()
        dst_ = A.f32(KC, 64)
        dma(dst_.ap, i_ssdwdt, W=[dst_])
        op("pool", lambda e: e.tensor_copy(out=dtw.ap, in_=dst_.ap), R=[dst_], W=[dtw])
        ws = WStream([i_ssdwin[32 + j] for j in range(48)], (KC, 128), ceng=("act", "dve", "pool", "act", "dve"))
        xe = [A.f32(NB + 3), A.f32(NB + 3)]
        xcv = [A.f32(NB), A.f32(NB)]
        if samp:
            st_c = A.f32(48, 3, NS)
            for j in range(48):
                dma(st_c.ap[:, j], i_ssdconv[j * 128:(j + 1) * 128], W=[st_c])
            nbuf = A.f32(48, 3, NS)
        for j in range(48):
            wb = ws.get()
            pb = banks[j % 4]
            xe_, xc_ = xe[j % 2], xcv[j % 2]
            for kc in range(KC):
                op("pe", lambda e, kc=kc: e.matmul(pb.ap[:, 0:N], wb.ap[:, kc, :], hmT.ap[:, kc, 0:N],
                                                    start=(kc == 0), stop=(kc == KC - 1)), R=[wb, hmT], W=[pb])
            if not samp:
                op("pool", lambda e: e.tensor_copy(out=xe_.ap[:, 0:3], in_=ssd_tail.ap[:, j, :]),
                   R=[ssd_tail], W=[xe_])
            op("act", lambda e: e.copy(out=xe_.ap[:, 3:3 + N], in_=pb.ap[:, 0:N]), R=[pb], W=[xe_])
            if samp:
                taps = [st_c.ap[:, j, 0, :], st_c.ap[:, j, 1, :], st_c.ap[:, j, 2, :], xe_.ap[:, 3:3 + N]]
                Rb = [xe_, st_c, ssdcw, vec]
            else:
                taps = [xe_.ap[:, k:k + N] for k in range(4)]
                Rb = [xe_, ssdcw, vec]
            conv4(xc_.ap[:, 0:N], taps, lambda k: ssdcw.ap[:, j, k:k + 1], ssd_bconv[:, j:j + 1], N, Rb, [xc_])
            op("act", lambda e: e.activation(out=xbcT.ap[:, j, 0:N], in_=xc_.ap[:, 0:N], func=AF.Silu),
               R=[xc_], W=[xbcT])
            if samp:
                for k in range(2):
                    op("pool", lambda e, k=k: e.tensor_copy(out=nbuf.ap[:, j, k, :], in_=st_c.ap[:, j, k + 1, :]),
                       R=[st_c], W=[nbuf])
                op("pool", lambda e: e.tensor_copy(out=nbuf.ap[:, j, 2, :], in_=xe_.ap[:, 3:3 + N]),
                   R=[xe_], W=[nbuf])
            else:
                op("pool", lambda e: e.tensor_copy(out=ssd_tail.ap[:, j, :], in_=xe_.ap[:, N:N + 3]),
                   R=[xe_], W=[ssd_tail])
        if samp:
            dma(o_ssdconv_s, nbuf.ap, R=[nbuf])
        elif last:
            dma(o_ssdconv_p, ssd_tail.ap, R=[ssd_tail])
        P.barrier()
        A.reset(m1)
        T = ssd_temps()
        if samp:
            sTs = [A.f32(64, 64), A.f32(64, 64)]
            for b in range(NS):
                sT = sTs[b % 2]
                dma(sT.ap, i_ssds[b], W=[sT])
                ssd_chunk(1, b, sT, hmT, xbcT, yT, dtw, T)
                dma(o_ssd_s[b], sT.ap, R=[sT])
        else:
            sT = A.f32(64, 64)
            if first:
                op("pool", lambda e: e.memset(sT.ap, 0.0), W=[sT])
            else:
                dma(sT.ap, o_ssd_p, R=[sT_dram], W=[sT])
            for q in range(N // 128):
                ssd_chunk(128, q * 128, sT, hmT, xbcT, yT, dtw, T)
            dma(o_ssd_p, sT.ap, R=[sT], W=[sT_dram])
        P.barrier()
        A.reset(m1)
        wz = WStream([i_ssdwin[j] for j in range(32)], (KC, 128), ceng=("act", "dve", "pool", "act", "dve"))
        sz = [A.f32(NB), A.f32(NB)]
        sq = [A.f32(NB), A.f32(NB)]
        pst = banks[7]
        for j in range(32):
            wb = wz.get()
            pb = banks[j % 4]
            for kc in range(KC):
                op("pe", lambda e, kc=kc: e.matmul(pb.ap[:, 0:N], wb.ap[:, kc, :], hmT.ap[:, kc, 0:N],
                                                    start=(kc == 0), stop=(kc == KC - 1)), R=[wb, hmT], W=[pb])
            s_, q_ = sz[j % 2], sq[j % 2]
            op("act", lambda e: e.activation(out=s_.ap[:, 0:N], in_=pb.ap[:, 0:N], func=AF.Silu), R=[pb], W=[s_])
            op("dve", lambda e: e.tensor_tensor(out=yT.ap[:, j, 0:N], in0=yT.ap[:, j, 0:N], in1=s_.ap[:, 0:N],
                                                op=ALU.mult), R=[yT, s_], W=[yT])
            op("act", lambda e: e.activation(out=q_.ap[:, 0:N], in_=yT.ap[:, j, 0:N], func=AF.Square),
               R=[yT], W=[q_])
            op("pe", lambda e: e.matmul(pst.ap[:, 0:N], ones, q_.ap[:, 0:N], start=(j == 0), stop=(j == 31)),
               R=[q_, cst], W=[pst])
        P.barrier()
        A.reset(m1)
        rstd = A.f32(NB)
        op("act", lambda e: e.activation(out=rstd.ap[:, 0:N], in_=pst.ap[:, 0:N], func=AF.Sqrt, scale=1.0 / 4096,
                                         bias=epsb.ap[:, 0:1]), R=[pst, epsb], W=[rstd])
        op("dve", lambda e: e.reciprocal(out=rstd.ap[:, 0:N], in_=rstd.ap[:, 0:N]), R=[rstd], W=[rstd])
        for j in range(32):
            op("dve", lambda e: e.scalar_tensor_tensor(out=yT.ap[:, j, 0:N], in0=yT.ap[:, j, 0:N],
                                                       scalar=ssd_ng[:, j:j + 1], in1=rstd.ap[:, 0:N],
                                                       op0=ALU.mult, op1=ALU.mult), R=[yT, vec, rstd], W=[yT])
        wo = WStream([i_ssdwout[j] for j in range(16)], (32, 128), ceng=("act", "dve", "pool", "act", "dve"), nstage=2, nbf=2)
        tmpb = [A.f32(NB), A.f32(NB)]
        for j in range(16):
            wb = wo.get()
            pb = banks[j % 4]
            for kc in range(32):
                op("pe", lambda e, kc=kc: e.matmul(pb.ap[:, 0:N], wb.ap[:, kc, :], yT.ap[:, kc, 0:N],
                                                    start=(kc == 0), stop=(kc == 31)), R=[wb, yT], W=[pb])
            resid_add(j, pb, N, None, modv(1, 2, j, samp), samp, tmpb[j % 2])
        P.barrier()
        A.reset(m)

    def peer(l, N, samp, hcT):
        m = A.mark()
        NG = N // 16
        s12 = A.f32(NB // 16, 256)
        thr = A.f32(NB // 16)
        negm = A.f32(NB // 16)
        Sel = A.bf16(NB // 16, 16)
        m1 = A.mark()
        qT = A.f32(2, NB, 8)
        wq = WStream([i_wq[l, j] for j in range(16)], (KC, 128), ceng=("act", "dve", "pool", "act", "dve"))
        for j in range(16):
            wb = wq.get()
            pb = banks[j % 4]
            for kc in range(KC):
                op("pe", lambda e, kc=kc: e.matmul(pb.ap[:, 0:N], wb.ap[:, kc, :], hcT.ap[:, kc, 0:N],
                                                    start=(kc == 0), stop=(kc == KC - 1)), R=[wb, hcT], W=[pb])
            op("act", lambda e: e.copy(out=qT.ap[:, j % 2, 0:N, j // 2], in_=pb.ap[:, 0:N]), R=[pb], W=[qT])
        vv = [A.f32(2, 16) for _ in range(2)]
        tmp1 = [A.f32(128) for _ in range(2)]
        cand = [A.f32(256) for _ in range(2)]
        cand2 = [A.f32(256) for _ in range(2)]
        c24 = [A.f32(24) for _ in range(2)]
        ez = [A.f32(16) for _ in range(2)]
        zz = [A.f32(2) for _ in range(2)]
        for gi in range(NG):
            pb = banks[4 + gi % 2]
            for half in range(2):
                lhsT = qT.ap[:, half, gi * 16:(gi + 1) * 16, :].rearrange("p t h -> p (t h)")
                op("pe", lambda e, half=half, lhsT=lhsT: e.matmul(pb.ap[:, half * 128:(half + 1) * 128], lhsT,
                                                                  kT.ap[:, l * 2 + half, :], start=True, stop=True),
                   R=[qT, kT], W=[pb])
            op("act", lambda e: e.copy(out=s12.ap[:, gi, :], in_=pb.ap[:, 0:256]), R=[pb], W=[s12])
            v_, t1, cd, cd2, c_, ez_, z_ = (vv[gi % 2], tmp1[gi % 2], cand[gi % 2], cand2[gi % 2], c24[gi % 2],
                                            ez[gi % 2], zz[gi % 2])
            for half in range(2):
                w = s12.ap[:, gi, half * 128:(half + 1) * 128]
                op("dve", lambda e: e.max(out=v_.ap[:, half, 0:8], in_=w), R=[s12], W=[v_])
                op("dve", lambda e: e.match_replace(out=t1.ap, in_to_replace=v_.ap[:, half, 0:8], in_values=w,
                                                    imm_value=-1e30), R=[s12, v_], W=[t1])
                op("dve", lambda e: e.max(out=v_.ap[:, half, 8:16], in_=t1.ap), R=[t1], W=[v_])
            cd3 = cd.ap.rearrange("p (a b) -> p a b", b=16)
            op("dve", lambda e: e.tensor_tensor(out=cd3, in0=bc(v_.ap[:, 0, :], 2, [128, 16, 16]),
                                                in1=bc(v_.ap[:, 1, :], 1, [128, 16, 16]), op=ALU.add),
               R=[v_], W=[cd])
            op("dve", lambda e: e.max(out=c_.ap[:, 0:8], in_=cd.ap), R=[cd], W=[c_])
            op("dve", lambda e: e.match_replace(out=cd2.ap, in_to_replace=c_.ap[:, 0:8], in_values=cd.ap,
                                                imm_value=-1e30), R=[cd, c_], W=[cd2])
            op("dve", lambda e: e.max(out=c_.ap[:, 8:16], in_=cd2.ap), R=[cd2], W=[c_])
            op("dve", lambda e: e.match_replace(out=cd.ap, in_to_replace=c_.ap[:, 8:16], in_values=cd2.ap,
                                                imm_value=-1e30), R=[cd2, c_], W=[cd])
            op("dve", lambda e: e.max(out=c_.ap[:, 16:24], in_=cd.ap), R=[cd], W=[c_])
            op("dve", lambda e: e.tensor_scalar(out=thr.ap[:, gi:gi + 1], in0=c_.ap[:, 15:16],
                                                scalar1=c_.ap[:, 16:17], scalar2=0.5, op0=ALU.add, op1=ALU.mult),
               R=[c_], W=[thr])
            op("dve", lambda e: e.tensor_scalar(out=negm.ap[:, gi:gi + 1], in0=c_.ap[:, 0:1], scalar1=-1.0,
                                                scalar2=None, op0=ALU.mult), R=[c_], W=[negm])
            op("act", lambda e: e.activation(out=ez_.ap, in_=c_.ap[:, 0:16], func=AF.Exp,
                                             bias=negm.ap[:, gi:gi + 1], scale=1.0, accum_out=z_.ap[:, 0:1]),
               R=[c_, negm], W=[ez_, z_])
            op("dve", lambda e: e.reciprocal(out=z_.ap[:, 1:2], in_=z_.ap[:, 0:1]), R=[z_], W=[z_])
            op("dve", lambda e: e.tensor_scalar(out=Sel.ap[:, gi, :], in0=selm, scalar1=z_.ap[:, 1:2], scalar2=None,
                                                op0=ALU.mult), R=[z_, cst], W=[Sel])
        P.barrier()
        A.reset(m1)
        usrc, vsrc = [], []
        for a in range(128):
            usrc += [i_uT[l, a][:, 0:8, :], i_uT[l, a][:, 8:16, :]]
            vsrc += [i_v[l, a][:, 0:1024], i_v[l, a][:, 1024:2048]]
        us = WStream(usrc, (8, 128), ceng=("act",), nstage=3, nbf=2 * GA + 1)
        vs = WStream(vsrc, (1024,), ceng=("act", "dve", "act", "act", "dve", "act", "dve", "act"), nstage=3, nbf=2 * GA + 1)
        FR = 12
        LAG = 3
        NGA = 128 // GA
        NGh = min(NG, 16)
        Dd = [A.f32(GA, 128) for _ in range(4)]
        ee = [A.bf16(GA, 128) for _ in range(4)]
        Fr = [A.bf16(GA, 128) for _ in range(FR)]
        gel = [A.bf16(NB) for _ in range(GA)]
        AT = [A.bf16(NB) for _ in range(GA)]
        stmp = [A.f32(NS), A.f32(NS)]
        st = {"f": 0, "pend": [], "fb": [], "u": {}, "v": {}}

        def fgen(ag, gi):
            k = st["f"]
            st["f"] += 1
            d_, e_, f_ = Dd[k % 4], ee[k % 4], Fr[k % FR]
            a0 = ag * GA
            op("pool", lambda e: e.tensor_tensor(out=d_.ap, in0=bc(s12.ap[:, gi, 128:256], 1, [128, GA, 128]),
                                                 in1=bc(s12.ap[:, gi, a0:a0 + GA], 2, [128, GA, 128]),
                                                 op=ALU.add), R=[s12], W=[d_])
            op("act", lambda e: e.activation(out=e_.ap, in_=d_.ap, func=AF.Exp, bias=negm.ap[:, gi:gi + 1],
                                             scale=1.0), R=[d_, negm], W=[e_])
            st["fb"].append((gi, d_, e_, f_))
            fgen_b(2)

        def fgen_b(lag):
            while len(st["fb"]) > lag:
                gi, d_, e_, f_ = st["fb"].pop(0)
                op("dve", lambda e: e.scalar_tensor_tensor(out=f_.ap, in0=d_.ap, scalar=thr.ap[:, gi:gi + 1],
                                                           in1=e_.ap, op0=ALU.is_ge, op1=ALU.mult),
                   R=[d_, e_, thr], W=[f_])
                st["pend"].append((gi, f_))

        def gmm_emit(lag):
            if lag == 0:
                fgen_b(0)
            while len(st["pend"]) > lag:
                gi, f_ = st["pend"].pop(0)
                for ai in range(GA):
                    pG = banks[2 + ai]
                    op("pe", lambda e, ai=ai, pG=pG: e.matmul(pG.ap[:, gi * 16:(gi + 1) * 16], f_.ap[:, ai, :],
                                                              Sel.ap[:, gi, :], start=True, stop=True),
                       R=[f_, Sel], W=[pG])

        def ucast(ag, k):
            st["u"][(ag, k)] = us.get()

        def vcast(ag, k):
            st["v"][(ag, k)] = vs.get()

        def sT_pe(ag, ai):
            pS = banks[ai % 2]
            for kc in range(KC):
                ub = st["u"][(ag, 2 * ai + kc // 8)]
                op("pe", lambda e, kc=kc, ub=ub: e.matmul(pS.ap[:, 0:N], ub.ap[:, kc % 8, :], hcT.ap[:, kc, 0:N],
                                                          start=(kc == 0), stop=(kc == KC - 1)),
                   R=[ub, hcT], W=[pS])

        def sT_gelu(ai):
            pS = banks[ai % 2]
            g_ = gel[ai]
            op("act", lambda e: e.activation(out=g_.ap[:, 0:N], in_=pS.ap[:, 0:N], func=AF.Gelu_apprx_tanh),
               R=[pS], W=[g_])

        def aTm():
            for ai in range(GA):
                pG = banks[2 + ai]
                g_, at_ = gel[ai], AT[ai]
                op("dve", lambda e: e.tensor_tensor(out=at_.ap[:, 0:N], in0=g_.ap[:, 0:N], in1=pG.ap[:, 0:N],
                                                    op=ALU.mult), R=[g_, pG], W=[at_])

        def outp(ag, dc):
            pO = banks[6 + dc % 2]
            for ai in range(GA):
                at_ = AT[ai]
                vb = st["v"][(ag, 2 * ai + dc // 8)]
                op("pe", lambda e, ai=ai, at_=at_, vb=vb: e.matmul(
                    pO.ap[:, 0:N], vb.ap[:, (dc % 8) * 128:(dc % 8 + 1) * 128], at_.ap[:, 0:N],
                    start=(ai == 0), stop=(ai == GA - 1)), R=[vb, at_], W=[pO])
            if samp:
                t_ = stmp[dc % 2]
                tv = t_.ap
                op("dve", lambda e: e.tensor_tensor(out=tv[:, 0:N], in0=pO.ap[:, 0:N], in1=modv(l, 5, dc, True),
                                                    op=ALU.mult), R=[pO, modT], W=[t_])
                op("dve", lambda e: e.tensor_tensor(out=xT.ap[:, dc, 0:N], in0=xT.ap[:, dc, 0:N], in1=tv[:, 0:N],
                                                    op=ALU.add), R=[t_, xT], W=[xT])
            else:
                op("dve", lambda e: e.scalar_tensor_tensor(out=xT.ap[:, dc, 0:N], in0=pO.ap[:, 0:N],
                                                           scalar=modv(l, 5, dc, False), in1=xT.ap[:, dc, 0:N],
                                                           op0=ALU.mult, op1=ALU.add), R=[pO, modT, xT], W=[xT])

        def phaseY(ag):
            for pr in range(GA // 2):
                sT_pe(ag, 2 * pr)
                sT_pe(ag, 2 * pr + 1)
                for k in range(8):
                    gi = NGh + 8 * pr + k
                    if k % 2 == 0:
                        vcast(ag, 4 * pr + k // 2)
                    if gi < NG:
                        fgen(ag, gi)
                        gmm_emit(LAG)
                sT_gelu(2 * pr)
                sT_gelu(2 * pr + 1)
            gmm_emit(0)
            aTm()

        for k in range(2 * GA):
            ucast(0, k)
        for gi in range(NGh):
            fgen(0, gi)
            gmm_emit(LAG)
        gmm_emit(0)
        phaseY(0)
        for ag in range(NGA):
            nxt = ag + 1 < NGA
            for dc in range(16):
                outp(ag, dc)
                if nxt:
                    if dc % 2 == 0:
                        ucast(ag + 1, dc // 2)
                    if dc < NGh:
                        fgen(ag + 1, dc)
                    gmm_emit(LAG)
            if nxt:
                gmm_emit(0)
                phaseY(ag + 1)
        P.barrier()
        A.reset(m)

    def run_block(N, samp, first, last, src, dst):
        dma(xT.ap[:, :, 0:N], src, W=[xT])
        for l in range(2):
            m = A.mark()
            hmT = A.bf16(KC, NB)
            norm_mod(l, 0, N, samp, hmT)
            if l == 0:
                rg_mixer(N, samp, last, hmT)
            else:
                ssd_mixer(N, samp, first, last, hmT)
            norm_mod(l, 1, N, samp, hmT)
            peer(l, N, samp, hmT)
            A.reset(m)
        m = A.mark()
        scr = [A.f32(NB), A.f32(NB)]
        rstd = A.f32(NB)
        yo = A.f32(KC, NB)
        rms_stats(lambda c: xT.ap[:, c, 0:N], [xT], N, KC, scr, banks[7], rstd)
        for c in range(KC):
            op("dve", lambda e: e.scalar_tensor_tensor(out=yo.ap[:, c, 0:N], in0=xT.ap[:, c, 0:N],
                                                       scalar=fing[:, c:c + 1], in1=rstd.ap[:, 0:N], op0=ALU.mult,
                                                       op1=ALU.mult), R=[xT, vec, rstd], W=[yo])
        dma(dst, yo.ap[:, :, 0:N], R=[yo])
        P.barrier()
        A.reset(m)

    for blk in range(nblk):
        run_block(NB, False, blk == 0, blk == nblk - 1,
                  i_xT[:, blk * NB:(blk + 1) * NB].rearrange("(c p) t -> p c t", p=128),
                  o_yT[:, blk * NB:(blk + 1) * NB].rearrange("(c p) t -> p c t", p=128))
    run_block(NS, True, True, False, i_xsT.rearrange("(c p) t -> p c t", p=128),
              o_ysT.rearrange("(c p) t -> p c t", p=128))
    P.finish()
    es.close()
    nc._arena_hw = A.hw
    return nc


def _wlay(w):
    K, N = w.shape
    return np.ascontiguousarray(w.reshape(K // 128, 128, N // 128, 128).transpose(2, 1, 0, 3))


def _vlay(v):
    return v.reshape(-1, 128).T


_CACHE = {}


def _prep_shared(inp):
    f = np.float32
    sh = {}
    cst = np.zeros((128, 4 * 128 + 16), f)
    cst[:, 0:128] = np.eye(128)
    cst[:, 128:256] = 1.0
    k = np.arange(128)
    cst[:, 256:384] = (k[:, None] <= k[None, :])
    cst[:, 384:512] = np.where(k[:, None] <= k[None, :], 0.0, NEG)
    cst[:, 512:528] = (k[:, None] // 8 == np.arange(16)[None, :])
    sh["cst"] = cst
    vec = np.zeros((128, 16 * 11 + 32 * 2 + 48 + 32), f)
    vec[:, 0:16] = _vlay(inp["norm1_g"][0]); vec[:, 16:32] = _vlay(inp["norm1_g"][1])
    vec[:, 32:48] = _vlay(inp["norm2_g"][0]); vec[:, 48:64] = _vlay(inp["norm2_g"][1])
    vec[:, 64:80] = _vlay(inp["final_g"])
    vec[:, 80:96] = _vlay(inp["rg_conv_b"][0])
    vec[:, 96:112] = _vlay(inp["rg_b_a"][0])
    vec[:, 112:128] = _vlay(inp["rg_b_i"][0])
    vec[:, 128:144] = _vlay(inp["rg_lambda"][0])
    vec[:, 144:160] = _vlay(inp["rg_b_out"][0])
    vec[:, 176:208] = _vlay(inp["rg_b_in"][0])
    vec[:, 208:240] = _vlay(inp["ssd_norm_g"][0])
    vec[:, 240:288] = _vlay(inp["ssd_conv_b"][0])
    vec[:, 288:320] = _vlay(np.repeat(inp["ssd_d"][0], 64))
    sh["vec"] = vec
    sh["bmod"] = np.ascontiguousarray(np.stack([_vlay(inp["b_mod"][l]) for l in range(2)], axis=1))
    sh["rgcw"] = np.ascontiguousarray(inp["rg_conv_w"][0].T.reshape(16, 128, 4).transpose(1, 0, 2))
    sh["ssdcw"] = np.ascontiguousarray(inp["ssd_conv_w"][0].T.reshape(48, 128, 4).transpose(1, 0, 2))
    sh["ssdh"] = np.ascontiguousarray(np.stack([inp["ssd_dt_bias"][0], inp["ssd_a_log"][0],
                                                inp["ssd_d"][0]]).astype(f))
    sh["wmod"] = np.stack([_wlay(inp["w_mod"][l]) for l in range(2)])
    sh["rgwin"] = _wlay(inp["rg_w_in"][0])

    def glay(w):
        return np.ascontiguousarray(w.reshape(8, 2, 128, 2, 128).transpose(0, 3, 2, 1, 4).reshape(16, 128, 2, 128))
    sh["rgwa"] = glay(inp["rg_w_a"][0])
    sh["rgwi"] = glay(inp["rg_w_i"][0])
    sh["rgwout"] = _wlay(inp["rg_w_out"][0])
    w = inp["ssd_w_in"][0]
    sh["ssdwin"] = _wlay(np.ascontiguousarray(w[:, :10240]))
    sh["ssdwdt"] = np.ascontiguousarray(w[:, 10240:].reshape(16, 128, 64).transpose(1, 0, 2))
    sh["ssdwout"] = _wlay(inp["ssd_w_out"][0])
    sh["wq"] = np.stack([_wlay(inp["peer_w_q"][l]) for l in range(2)])
    sh["kT"] = np.ascontiguousarray(np.stack([np.stack([inp["peer_k1"][l].T, inp["peer_k2"][l].T])
                                              for l in range(2)]))
    sh["uT"] = np.stack([np.ascontiguousarray(inp["peer_u"][l].reshape(128, 128, 16, 128).transpose(0, 3, 2, 1))
                         for l in range(2)])
    sh["v"] = np.ascontiguousarray(inp["peer_v"].reshape(2, 128, 128, 2048))
    return sh


def kernel(**inp):
    inp = {k: np.asarray(v) for k, v in inp.items()}
    TP = inp["x_prompt"].shape[1]
    nc = build(TP)
    sh = _prep_shared(inp)
    in_maps = []
    for c in range(8):
        s = c % 4
        sl = slice(c * NS, (c + 1) * NS)
        d = dict(sh)
        d["xT"] = np.ascontiguousarray(inp["x_prompt"][s].T)
        d["xsT"] = np.ascontiguousarray(inp["x_sample"][sl, 0, :].T)
        d["cT"] = np.ascontiguousarray(np.concatenate([inp["c_prompt"][s][None], inp["c_sample"][sl]], 0).T)
        d["rgconv"] = np.ascontiguousarray(inp["state_rg_conv"][0, sl].transpose(2, 1, 0))
        d["rgh"] = np.ascontiguousarray(inp["state_rg_h"][0, sl].T)
        d["ssdconv"] = np.ascontiguousarray(inp["state_ssd_conv"][0, sl].transpose(2, 1, 0))
        d["ssds"] = np.ascontiguousarray(inp["state_ssd"][0, sl].reshape(NS, 64, 64, 128).transpose(0, 3, 1, 2))
        in_maps.append(d)
    res = run_bass_kernel_spmd(nc, in_maps, core_ids=list(range(8))).results
    f = np.float32
    y_p = np.stack([res[s]["o_yT"].T for s in range(4)]).astype(f)
    y_s = np.concatenate([res[c]["o_ysT"].T for c in range(8)], 0)[:, None, :].astype(f)

    def unv(a):
        return a.transpose(1, 0, *range(2, a.ndim)).reshape(-1, *a.shape[2:])
    rg_conv_p = np.stack([unv(res[s]["o_rgconv_p"]).T for s in range(4)])[None].astype(f)
    rg_h_p = np.stack([unv(res[s]["o_rgh_p"]) for s in range(4)])[None].astype(f)
    ssd_conv_p = np.stack([unv(res[s]["o_ssdconv_p"]).T for s in range(4)])[None].astype(f)
    ssd_p = np.stack([res[s]["o_ssd_p"].transpose(1, 2, 0).reshape(8, 8, 64, 128) for s in range(4)])[None].astype(f)
    rg_conv_s = np.concatenate([unv(res[c]["o_rgconv_s"]).transpose(2, 1, 0) for c in range(8)], 0)[None].astype(f)
    rg_h_s = np.concatenate([unv(res[c]["o_rgh_s"]).T for c in range(8)], 0)[None].astype(f)
    ssd_conv_s = np.concatenate([unv(res[c]["o_ssdconv_s"]).transpose(2, 1, 0) for c in range(8)], 0)[None].astype(f)
    ssd_s = np.concatenate([res[c]["o_ssd_s"].transpose(0, 2, 3, 1).reshape(NS, 8, 8, 64, 128)
                            for c in range(8)], 0)[None].astype(f)
    return (y_p, y_s, rg_conv_p, rg_h_p, ssd_conv_p, ssd_p, rg_conv_s, rg_h_s, ssd_conv_s, ssd_s)
```

```python
import contextlib
import numpy as np
import concourse.bass as bass
import concourse.mybir as mybir
from concourse.bass_utils import run_bass_kernel_spmd

F32 = mybir.dt.float32
BF16 = mybir.dt.bfloat16
AF = mybir.ActivationFunctionType
ALU = mybir.AluOpType

D = 2048
KC = 16
NS = 16
NB = 512
EPS = 1e-6
NDS = 12
GA = 4
NEG = -30000.0


class Trk:
    __slots__ = ("w", "r")

    def __init__(self):
        self.w = None
        self.r = {}


class Buf:
    def __init__(self, ap):
        self.ap = ap
        self.t = Trk()


class Prog:
    def __init__(self, nc, es):
        self.nc = nc
        self.eng = {"pe": nc.tensor, "dve": nc.vector, "act": nc.scalar, "pool": nc.gpsimd, "sp": nc.sync}
        self.sem = {k: es.enter_context(nc.semaphore("s_" + k)) for k in ("pe", "dve", "act", "pool")}
        self.cnt = {k: 0 for k in self.sem}
        self.seen = {e: {k: 0 for k in self.sem} for e in self.eng}
        self.dsem = [es.enter_context(nc.semaphore("d%d" % i)) for i in range(NDS)]
        self.dcnt = [0] * NDS
        self.dn = 0
        self.dseen = {e: [0] * NDS for e in self.eng}

    def _need(self, e, tok):
        if tok is None:
            return
        if tok[0] == "c":
            _, f, n = tok
            if f == e and e == "pe":
                return
            if self.seen[e][f] < n:
                self.eng[e].wait_ge(self.sem[f], n)
                self.seen[e][f] = n
        else:
            _, i, v = tok
            if self.dseen[e][i] < v:
                self.eng[e].wait_ge(self.dsem[i], v)
                self.dseen[e][i] = v

    def _sync(self, e, R, W):
        for b in R:
            self._need(e, b.t.w)
        for b in W:
            self._need(e, b.t.w)
            for tok in b.t.r.values():
                self._need(e, tok)

    def op(self, e, fn, R=(), W=()):
        self._sync(e, R, W)
        ins = fn(self.eng[e])
        self.cnt[e] += 1
        ins.then_inc(self.sem[e], 1)
        tok = ("c", e, self.cnt[e])
        for b in R:
            b.t.r[e] = tok
        for b in W:
            b.t.w = tok
            b.t.r = {}
        return ins

    def dma(self, out, in_, R=(), W=(), q="sp"):
        i = self.dn % NDS
        self.dn += 1
        self._need(q, ("d", i, self.dcnt[i]))
        self._sync(q, R, W)
        ins = self.eng[q].dma_start(out=out, in_=in_)
        self.dcnt[i] += 16
        ins.then_inc(self.dsem[i], 16)
        tok = ("d", i, self.dcnt[i])
        for b in R:
            b.t.r["dma%d" % i] = tok
        for b in W:
            b.t.w = tok
            b.t.r = {}

    def barrier(self):
        for e in self.eng:
            for f in self.sem:
                self._need(e, ("c", f, self.cnt[f]))
            for i in range(NDS):
                self._need(e, ("d", i, self.dcnt[i]))

    def finish(self):
        for i in range(NDS):
            self._need("sp", ("d", i, self.dcnt[i]))
        for f in self.sem:
            self._need("sp", ("c", f, self.cnt[f]))


class Arena:
    def __init__(self, ap, width):
        self.ap = ap
        self.width = width
        self.off = 0
        self.hw = 0

    def mark(self):
        return self.off

    def reset(self, m):
        self.off = m

    def f32(self, *shape):
        n = int(np.prod(shape))
        assert self.off + n <= self.width, ("arena overflow", self.off, n, self.width)
        v = self.ap[:, self.off:self.off + n]
        self.off += n
        self.hw = max(self.hw, self.off)
        return Buf(_shape(v, shape))

    def bf16(self, *shape):
        n = int(np.prod(shape))
        w = (n + 1) // 2
        assert self.off + w <= self.width, ("arena overflow", self.off, w, self.width)
        v = self.ap[:, self.off:self.off + w].bitcast(BF16)[:, 0:n]
        self.off += w
        self.hw = max(self.hw, self.off)
        return Buf(_shape(v, shape))


def _shape(v, shape):
    if len(shape) == 1:
        return v
    if len(shape) == 2:
        return v.rearrange("p (a b) -> p a b", b=shape[1])
    if len(shape) == 3:
        return v.rearrange("p (a b c) -> p a b c", b=shape[1], c=shape[2])
    raise ValueError(shape)


def bc(ap, axis, shape):
    return ap.unsqueeze(axis).to_broadcast(list(shape))


def build(TP):
    nblk = TP // NB
    nc = bass.Bass("TRN2", target_bir_lowering=False)
    es = contextlib.ExitStack()

    def din(name, shape):
        return nc.dram_tensor(name, list(shape), F32, kind="ExternalInput").ap()

    def dout(name, shape):
        return nc.dram_tensor(name, list(shape), F32, kind="ExternalOutput").ap()

    i_xT = din("xT", [D, TP])
    i_xsT = din("xsT", [D, NS])
    i_cT = din("cT", [D, 1 + NS])
    i_rgconv = din("rgconv", [D, 3, NS])
    i_rgh = din("rgh", [D, NS])
    i_ssdconv = din("ssdconv", [6144, 3, NS])
    i_ssds = din("ssds", [NS, 128, 64, 64])
    i_cst = din("cst", [128, 4 * 128 + 16])
    i_vec = din("vec", [128, 16 * 11 + 32 * 2 + 48 + 32])
    i_bmod = din("bmod", [128, 2, 96])
    i_rgcw = din("rgcw", [128, 16, 4])
    i_ssdcw = din("ssdcw", [128, 48, 4])
    i_ssdh = din("ssdh", [3, 64])
    i_wmod = din("wmod", [2, 96, 128, 16, 128])
    i_rgwin = din("rgwin", [32, 128, 16, 128])
    i_rgwa = din("rgwa", [16, 128, 2, 128])
    i_rgwi = din("rgwi", [16, 128, 2, 128])
    i_rgwout = din("rgwout", [16, 128, 16, 128])
    i_ssdwin = din("ssdwin", [80, 128, 16, 128])
    i_ssdwdt = din("ssdwdt", [128, 16, 64])
    i_ssdwout = din("ssdwout", [16, 128, 32, 128])
    i_wq = din("wq", [2, 16, 128, 16, 128])
    i_kT = din("kT", [2, 2, 128, 128])
    i_uT = din("uT", [2, 128, 128, 16, 128])
    i_v = din("v", [2, 128, 128, 2048])
    o_yT = dout("o_yT", [D, TP])
    o_ysT = dout("o_ysT", [D, NS])
    o_rgconv_p = dout("o_rgconv_p", [128, 16, 3])
    o_rgh_p = dout("o_rgh_p", [128, 16])
    o_ssdconv_p = dout("o_ssdconv_p", [128, 48, 3])
    o_ssd_p = dout("o_ssd_p", [128, 64, 64])
    o_rgconv_s = dout("o_rgconv_s", [128, 16, 3, NS])
    o_rgh_s = dout("o_rgh_s", [128, 16, NS])
    o_ssdconv_s = dout("o_ssdconv_s", [128, 48, 3, NS])
    o_ssd_s = dout("o_ssd_s", [NS, 128, 64, 64])

    AW = 51800
    arena_t = es.enter_context(nc.sbuf_tensor("arena", [128, AW], F32))
    A = Arena(arena_t[:, :], AW)
    banks = [Buf(es.enter_context(nc.psum_tensor("pb%d" % i, [128, 512], F32))[:, :]) for i in range(8)]
    P = Prog(nc, es)
    op, dma = P.op, P.dma

    cst = A.f32(4 * 128 + 16)
    ident = cst.ap[:, 0:128]
    ones = cst.ap[:, 128:256]
    tri = cst.ap[:, 256:384]
    mneg = cst.ap[:, 384:512]
    selm = cst.ap[:, 512:528]
    dma(cst.ap, i_cst, W=[cst])
    identb = A.bf16(128)
    op("dve", lambda e: e.tensor_copy(out=identb.ap, in_=ident), R=[cst], W=[identb])
    vec = A.f32(16 * 11 + 32 * 2 + 48 + 32)
    dma(vec.ap, i_vec, W=[vec])

    def vsl(o, n):
        return vec.ap[:, o:o + n]
    n1g = [vsl(0, 16), vsl(16, 16)]
    n2g = [vsl(32, 16), vsl(48, 16)]
    fing = vsl(64, 16)
    rg_bconv = vsl(80, 16)
    rg_ba = vsl(96, 16)
    rg_bi = vsl(112, 16)
    rg_lam = vsl(128, 16)
    rg_bout = vsl(144, 16)
    rg_bin = vsl(176, 32)
    ssd_ng = vsl(208, 32)
    ssd_bconv = vsl(240, 48)
    ssd_dexp = vsl(288, 32)
    rgcw = A.f32(16, 4)
    dma(rgcw.ap, i_rgcw, W=[rgcw])
    ssdcw = A.f32(48, 4)
    dma(ssdcw.ap, i_ssdcw, W=[ssdcw])
    ssdh = A.f32(3, 64)
    dma(ssdh.ap, i_ssdh.partition_broadcast(128), W=[ssdh])
    Abc = A.f32(64)
    op("act", lambda e: e.activation(out=Abc.ap, in_=ssdh.ap[:, 1, :], func=AF.Exp), R=[ssdh], W=[Abc])
    op("dve", lambda e: e.tensor_scalar(out=Abc.ap, in0=Abc.ap, scalar1=-1.0, scalar2=None, op0=ALU.mult),
       R=[Abc], W=[Abc])
    c8 = A.f32(16)
    op("act", lambda e: e.activation(out=c8.ap, in_=rg_lam, func=AF.Exp, scale=-1.0), R=[vec], W=[c8])
    op("act", lambda e: e.activation(out=c8.ap, in_=c8.ap, func=AF.Ln, bias=1.0), R=[c8], W=[c8])
    op("dve", lambda e: e.tensor_scalar(out=c8.ap, in0=c8.ap, scalar1=-8.0, scalar2=None, op0=ALU.mult),
       R=[c8], W=[c8])
    epsb = A.f32(1)
    op("pool", lambda e: e.memset(epsb.ap, EPS), W=[epsb])
    kT = A.f32(4, 128)
    for l_ in range(2):
        for h_ in range(2):
            dma(kT.ap[:, l_ * 2 + h_, :], i_kT[l_, h_], W=[kT])

    class WStream:
        def __init__(self, srcs, shape, ceng=("pool",), nstage=3, nbf=3):
            self.srcs = srcs
            self.shape = shape
            self.stage = [A.f32(*shape) for _ in range(nstage)]
            self.bfs = [A.bf16(*shape) for _ in range(nbf)]
            self.ceng = ceng
            self.nl = 0
            self.ncast = 0
            self.pf = nstage - 1
            for _ in range(min(self.pf, len(srcs))):
                self._load()

        def _load(self):
            if self.nl < len(self.srcs):
                s = self.stage[self.nl % len(self.stage)]
                dma(s.ap, self.srcs[self.nl], W=[s])
                self.nl += 1

        def get(self):
            i = self.ncast
            self._load()
            s = self.stage[i % len(self.stage)]
            b = self.bfs[i % len(self.bfs)]
            e_ = self.ceng[i % len(self.ceng)]
            if e_ == "act":
                op("act", lambda e: e.copy(out=b.ap, in_=s.ap), R=[s], W=[b])
            else:
                op(e_, lambda e: e.tensor_copy(out=b.ap, in_=s.ap), R=[s], W=[b])
            self.ncast += 1
            return b

    modT = A.f32(2, 96, 1 + NS)
    csT = A.f32(KC, 1 + NS)
    for kc_ in range(KC):
        dma(csT.ap[:, kc_, :], i_cT[kc_ * 128:(kc_ + 1) * 128, :], W=[csT])
    op("act", lambda e: e.activation(out=csT.ap, in_=csT.ap, func=AF.Silu), R=[csT], W=[csT])
    bmod = A.f32(2, 96)
    dma(bmod.ap, i_bmod, W=[bmod])
    m0 = A.mark()
    csb = A.bf16(KC, 1 + NS)
    op("dve", lambda e: e.tensor_copy(out=csb.ap, in_=csT.ap), R=[csT], W=[csb])
    jobs = [(l, j) for l in range(2) for j in range(96)]
    wm = WStream([i_wmod[l, j] for (l, j) in jobs], (KC, 128), ceng=("act", "dve", "pool", "act", "dve"),
                 nstage=3, nbf=3)
    for idx, (l, j) in enumerate(jobs):
        s = wm.get()
        pb = banks[idx % 4]
        for kc in range(KC):
            op("pe", lambda e, kc=kc: e.matmul(pb.ap[:, 0:1 + NS], s.ap[:, kc, :], csb.ap[:, kc, :],
                                                start=(kc == 0), stop=(kc == KC - 1)), R=[s, csb], W=[pb])
        op("dve", lambda e: e.tensor_scalar(out=modT.ap[:, l, j, :], in0=pb.ap[:, 0:1 + NS],
                                            scalar1=bmod.ap[:, l, j:j + 1], scalar2=None, op0=ALU.add),
           R=[pb, bmod], W=[modT])
    A.reset(m0)
    P.barrier()
    gmp = A.f32(2, 2, 16)
    gms = A.f32(4, 16, NS)
    for l in range(2):
        for k, (sc0, ng) in enumerate(((16, n1g[l]), (64, n2g[l]))):
            op("dve", lambda e: e.scalar_tensor_tensor(out=gmp.ap[:, l, k, :], in0=modT.ap[:, l, sc0:sc0 + 16, 0],
                                                       scalar=1.0, in1=ng, op0=ALU.add, op1=ALU.mult),
               R=[modT, vec], W=[gmp])
            op("dve", lambda e: e.scalar_tensor_tensor(out=gms.ap[:, l * 2 + k, :, :],
                                                       in0=modT.ap[:, l, sc0:sc0 + 16, 1:1 + NS], scalar=1.0,
                                                       in1=bc(ng, 2, [128, 16, NS]), op0=ALU.add, op1=ALU.mult),
               R=[modT, vec], W=[gms])

    def modv(l, k, c, samp):
        if samp:
            return modT.ap[:, l, 16 * k + c, 1:1 + NS]
        return modT.ap[:, l, 16 * k + c, 0:1]

    xT = A.f32(KC, NB)
    rg_tail = A.f32(16, 3)
    rg_carry = A.f32(16)
    ssd_tail = A.f32(48, 3)
    for b_ in (rg_tail, rg_carry, ssd_tail):
        op("pool", lambda e, b_=b_: e.memset(b_.ap, 0.0), W=[b_])
    sT_dram = Buf(o_ssd_p)
    pmark = A.mark()

    def rms_stats(src_chunks, srcbufs, N, nch, scratch, pb, rstd, eng_sq="act"):
        for c in range(nch):
            sq = scratch[c % len(scratch)]
            op("act", lambda e, c=c, sq=sq: e.activation(out=sq.ap[:, 0:N], in_=src_chunks(c), func=AF.Square),
               R=srcbufs, W=[sq])
            op("pe", lambda e, c=c, sq=sq: e.matmul(pb.ap[:, 0:N], ones, sq.ap[:, 0:N], start=(c == 0),
                                                    stop=(c == nch - 1)), R=[sq, cst], W=[pb])
        op("act", lambda e: e.activation(out=rstd.ap[:, 0:N], in_=pb.ap[:, 0:N], func=AF.Sqrt,
                                         scale=1.0 / (nch * 128), bias=epsb.ap[:, 0:1]), R=[pb, epsb], W=[rstd])
        op("dve", lambda e: e.reciprocal(out=rstd.ap[:, 0:N], in_=rstd.ap[:, 0:N]), R=[rstd], W=[rstd])

    def norm_mod(l, which, N, samp, dst):
        m = A.mark()
        scr = [A.f32(NB), A.f32(NB)]
        rstd = A.f32(NB)
        tmp = [A.f32(NB), A.f32(NB)]
        rms_stats(lambda c: xT.ap[:, c, 0:N], [xT], N, KC, scr, banks[7], rstd)
        for c in range(KC):
            t = tmp[c % 2]
            if samp:
                gm = gms.ap[:, l * 2 + which, c, :]
                sh = modv(l, 3 * which, c, True)
                op("dve", lambda e: e.tensor_tensor(out=t.ap[:, 0:N], in0=xT.ap[:, c, 0:N], in1=rstd.ap[:, 0:N],
                                                    op=ALU.mult), R=[xT, rstd], W=[t])
                op("dve", lambda e: e.tensor_tensor(out=t.ap[:, 0:N], in0=t.ap[:, 0:N], in1=gm, op=ALU.mult),
                   R=[t, gms], W=[t])
                op("dve", lambda e: e.tensor_tensor(out=dst.ap[:, c, 0:N], in0=t.ap[:, 0:N], in1=sh, op=ALU.add),
                   R=[t, modT], W=[dst])
            else:
                gm = gmp.ap[:, l, which, c:c + 1]
                sh = modv(l, 3 * which, c, False)
                op("dve", lambda e: e.scalar_tensor_tensor(out=t.ap[:, 0:N], in0=xT.ap[:, c, 0:N], scalar=gm,
                                                           in1=rstd.ap[:, 0:N], op0=ALU.mult, op1=ALU.mult),
                   R=[xT, rstd, gmp], W=[t])
                op("act", lambda e: e.activation(out=dst.ap[:, c, 0:N], in_=t.ap[:, 0:N], func=AF.Identity,
                                                 bias=sh, scale=1.0), R=[t, modT], W=[dst])
        A.reset(m)

    def resid_add(c, pb, N, bias, gate, samp, tmpb):
        if samp:
            if bias is not None:
                op("dve", lambda e: e.scalar_tensor_tensor(out=tmpb.ap[:, 0:N], in0=pb.ap[:, 0:N], scalar=bias,
                                                           in1=gate, op0=ALU.add, op1=ALU.mult),
                   R=[pb, vec, modT], W=[tmpb])
            else:
                op("dve", lambda e: e.tensor_tensor(out=tmpb.ap[:, 0:N], in0=pb.ap[:, 0:N], in1=gate, op=ALU.mult),
                   R=[pb, modT], W=[tmpb])
            op("dve", lambda e: e.tensor_tensor(out=xT.ap[:, c, 0:N], in0=xT.ap[:, c, 0:N], in1=tmpb.ap[:, 0:N],
                                                op=ALU.add), R=[tmpb, xT], W=[xT])
        else:
            if bias is not None:
                op("dve", lambda e: e.tensor_scalar(out=tmpb.ap[:, 0:N], in0=pb.ap[:, 0:N], scalar1=bias,
                                                    scalar2=gate, op0=ALU.add, op1=ALU.mult),
                   R=[pb, vec, modT], W=[tmpb])
                op("dve", lambda e: e.tensor_tensor(out=xT.ap[:, c, 0:N], in0=xT.ap[:, c, 0:N],
                                                    in1=tmpb.ap[:, 0:N], op=ALU.add), R=[tmpb, xT], W=[xT])
            else:
                op("dve", lambda e: e.scalar_tensor_tensor(out=xT.ap[:, c, 0:N], in0=pb.ap[:, 0:N], scalar=gate,
                                                           in1=xT.ap[:, c, 0:N], op0=ALU.mult, op1=ALU.add),
                   R=[pb, modT, xT], W=[xT])

    def conv4(dst, taps, wcol, bcol, N, Rb, Wb):
        op("dve", lambda e: e.tensor_scalar(out=dst, in0=taps[0], scalar1=wcol(0), scalar2=bcol, op0=ALU.mult,
                                            op1=ALU.add), R=Rb, W=Wb)
        for k in range(1, 4):
            op("dve", lambda e, k=k: e.scalar_tensor_tensor(out=dst, in0=taps[k], scalar=wcol(k), in1=dst,
                                                            op0=ALU.mult, op1=ALU.add), R=Rb + Wb, W=Wb)

    def rg_mixer(N, samp, last, hmT):
        m = A.mark()
        gT = A.bf16(KC, NB)
        yT = A.bf16(KC, NB)
        if samp:
            nbuf = A.f32(KC, 3, NS)
            hout = A.f32(KC, NS)
        m_in = A.mark()
        win_srcs = []
        for p in range(8):
            win_srcs += [i_rgwin[2 * p], i_rgwin[2 * p + 1], i_rgwin[16 + 2 * p], i_rgwin[16 + 2 * p + 1]]
        ws = WStream(win_srcs, (KC, 128), ceng=("act", "dve", "pool", "act", "dve"), nstage=2, nbf=3)
        gsrcs = []
        for p in range(8):
            gsrcs += [i_rgwa[2 * p], i_rgwa[2 * p + 1], i_rgwi[2 * p], i_rgwi[2 * p + 1]]
        gs = WStream(gsrcs, (2, 128), ceng=("pool",), nstage=4, nbf=4)
        xe = [A.f32(2, NB + 3), A.f32(2, NB + 3)]
        xc = [A.f32(2, NB), A.f32(2, NB)]
        xcb = [A.bf16(2, NB), A.bf16(2, NB)]
        gate_r = [A.f32(NB), A.f32(NB)]
        gate_i = [A.f32(NB), A.f32(NB)]
        av = [A.f32(NB), A.f32(NB)]
        bv = [A.f32(NB), A.f32(NB)]
        hs = [A.f32(NB), A.f32(NB)]
        if samp:
            st_c = A.f32(KC, 3, NS)
            for c_ in range(KC):
                dma(st_c.ap[:, c_], i_rgconv[c_ * 128:(c_ + 1) * 128], W=[st_c])
            h0s = A.f32(KC, NS)
            for c_ in range(KC):
                dma(h0s.ap[:, c_, :], i_rgh[c_ * 128:(c_ + 1) * 128, :], W=[h0s])
        for p in range(8):
            xe_, xc_, xcb_ = xe[p % 2], xc[p % 2], xcb[p % 2]
            for q4 in range(4):
                wb = ws.get()
                pb = banks[q4 % 4]
                for kc in range(KC):
                    op("pe", lambda e, kc=kc: e.matmul(pb.ap[:, 0:N], wb.ap[:, kc, :], hmT.ap[:, kc, 0:N],
                                                        start=(kc == 0), stop=(kc == KC - 1)), R=[wb, hmT], W=[pb])
                if q4 < 2:
                    c = 2 * p + q4
                    op("act", lambda e: e.activation(out=gT.ap[:, c, 0:N], in_=pb.ap[:, 0:N],
                                                     func=AF.Gelu_apprx_tanh, bias=rg_bin[:, c:c + 1], scale=1.0),
                       R=[pb, vec], W=[gT])
                else:
                    q = q4 - 2
                    c = 2 * p + q
                    if not samp:
                        op("pool", lambda e: e.tensor_copy(out=xe_.ap[:, q, 0:3], in_=rg_tail.ap[:, c, :]),
                           R=[rg_tail], W=[xe_])
                    op("act", lambda e: e.activation(out=xe_.ap[:, q, 3:3 + N], in_=pb.ap[:, 0:N], func=AF.Identity,
                                                     bias=rg_bin[:, 16 + c:16 + c + 1], scale=1.0),
                       R=[pb, vec], W=[xe_])
            for q in range(2):
                c = 2 * p + q
                if samp:
                    taps = [st_c.ap[:, c, 0, :], st_c.ap[:, c, 1, :], st_c.ap[:, c, 2, :], xe_.ap[:, q, 3:3 + N]]
                    Rb = [xe_, st_c, rgcw, vec]
                else:
                    taps = [xe_.ap[:, q, k:k + N] for k in range(4)]
                    Rb = [xe_, rgcw, vec]
                conv4(xc_.ap[:, q, 0:N], taps, lambda k: rgcw.ap[:, c, k:k + 1], rg_bconv[:, c:c + 1], N, Rb, [xc_])
                op("act", lambda e: e.copy(out=xcb_.ap[:, q, 0:N], in_=xc_.ap[:, q, 0:N]), R=[xc_], W=[xcb_])
                if samp:
                    for k in range(2):
                        op("pool", lambda e, k=k: e.tensor_copy(out=nbuf.ap[:, c, k, :], in_=st_c.ap[:, c, k + 1, :]),
                           R=[st_c], W=[nbuf])
                    op("pool", lambda e: e.tensor_copy(out=nbuf.ap[:, c, 2, :], in_=xe_.ap[:, q, 3:3 + N]),
                       R=[xe_], W=[nbuf])
                else:
                    op("pool", lambda e: e.tensor_copy(out=rg_tail.ap[:, c, :], in_=xe_.ap[:, q, N:N + 3]),
                       R=[xe_], W=[rg_tail])
            gw = [gs.get() for _ in range(4)]
            for jh in range(2):
                c = 2 * p + jh
                r_, i_, a_, b_, h_ = gate_r[jh], gate_i[jh], av[jh], bv[jh], hs[jh]
                for gi_, (wt, dstb, bcol) in enumerate(((gw[jh], r_, rg_ba), (gw[2 + jh], i_, rg_bi))):
                    pb = banks[4 + (2 * jh + gi_) % 4]
                    for ih in range(2):
                        op("pe", lambda e, ih=ih: e.matmul(pb.ap[:, 0:N], wt.ap[:, ih, :], xcb_.ap[:, ih, 0:N],
                                                            start=(ih == 0), stop=(ih == 1)), R=[wt, xcb_], W=[pb])
                    op("act", lambda e: e.activation(out=dstb.ap[:, 0:N], in_=pb.ap[:, 0:N], func=AF.Sigmoid,
                                                     bias=bcol[:, c:c + 1], scale=1.0), R=[pb, vec], W=[dstb])
                op("act", lambda e: e.activation(out=a_.ap[:, 0:N], in_=r_.ap[:, 0:N], func=AF.Exp,
                                                 scale=c8.ap[:, c:c + 1]), R=[r_, c8], W=[a_])
                op("dve", lambda e: e.tensor_tensor(out=b_.ap[:, 0:N], in0=a_.ap[:, 0:N], in1=a_.ap[:, 0:N],
                                                    op=ALU.mult), R=[a_], W=[b_])
                op("dve", lambda e: e.tensor_scalar(out=b_.ap[:, 0:N], in0=b_.ap[:, 0:N], scalar1=-1.0, scalar2=1.0,
                                                    op0=ALU.mult, op1=ALU.add), R=[b_], W=[b_])
                op("dve", lambda e: e.tensor_scalar(out=b_.ap[:, 0:N], in0=b_.ap[:, 0:N], scalar1=1e-30,
                                                    scalar2=None, op0=ALU.max), R=[b_], W=[b_])
                op("act", lambda e: e.activation(out=b_.ap[:, 0:N], in_=b_.ap[:, 0:N], func=AF.Sqrt),
                   R=[b_], W=[b_])
                op("dve", lambda e: e.tensor_tensor(out=i_.ap[:, 0:N], in0=i_.ap[:, 0:N], in1=xc_.ap[:, jh, 0:N],
                                                    op=ALU.mult), R=[i_, xc_], W=[i_])
                op("dve", lambda e: e.tensor_tensor(out=b_.ap[:, 0:N], in0=b_.ap[:, 0:N], in1=i_.ap[:, 0:N],
                                                    op=ALU.mult), R=[b_, i_], W=[b_])
                if samp:
                    op("dve", lambda e: e.tensor_tensor(out=h_.ap[:, 0:N], in0=a_.ap[:, 0:N], in1=h0s.ap[:, c, :],
                                                        op=ALU.mult), R=[a_, h0s], W=[h_])
                    op("dve", lambda e: e.tensor_tensor(out=h_.ap[:, 0:N], in0=h_.ap[:, 0:N], in1=b_.ap[:, 0:N],
                                                        op=ALU.add), R=[h_, b_], W=[h_])
                    op("pool", lambda e: e.tensor_copy(out=hout.ap[:, c, :], in_=h_.ap[:, 0:N]), R=[h_], W=[hout])
                else:
                    op("dve", lambda e: e.tensor_tensor_scan(out=h_.ap[:, 0:N], data0=a_.ap[:, 0:N],
                                                             data1=b_.ap[:, 0:N], initial=rg_carry.ap[:, c:c + 1],
                                                             op0=ALU.mult, op1=ALU.add),
                       R=[a_, b_, rg_carry], W=[h_])
                    op("pool", lambda e: e.tensor_copy(out=rg_carry.ap[:, c:c + 1], in_=h_.ap[:, N - 1:N]),
                       R=[h_], W=[rg_carry])
                op("dve", lambda e: e.tensor_tensor(out=yT.ap[:, c, 0:N], in0=h_.ap[:, 0:N], in1=gT.ap[:, c, 0:N],
                                                    op=ALU.mult), R=[h_, gT], W=[yT])
        P.barrier()
        A.reset(m_in)
        wo = WStream([i_rgwout[j] for j in range(16)], (KC, 128), ceng=("act", "dve", "pool", "act", "dve"))
        tmpb = [A.f32(NB), A.f32(NB)]
        for j in range(16):
            wb = wo.get()
            pb = banks[j % 4]
            for kc in range(KC):
                op("pe", lambda e, kc=kc: e.matmul(pb.ap[:, 0:N], wb.ap[:, kc, :], yT.ap[:, kc, 0:N],
                                                    start=(kc == 0), stop=(kc == KC - 1)), R=[wb, yT], W=[pb])
            resid_add(j, pb, N, rg_bout[:, j:j + 1], modv(0, 2, j, samp), samp, tmpb[j % 2])
        if samp:
            dma(o_rgconv_s, nbuf.ap, R=[nbuf])
            dma(o_rgh_s, hout.ap, R=[hout])
        elif last:
            dma(o_rgconv_p, rg_tail.ap, R=[rg_tail])
            dma(o_rgh_p, rg_carry.ap, R=[rg_carry])
        P.barrier()
        A.reset(m)

    def ssd_temps():
        T = {}
        T["dt"] = A.f32(64)
        T["dtA"] = A.f32(64)
        T["ncum"] = A.f32(64)
        T["xtok"] = A.bf16(4096)
        T["btok"] = A.bf16(1024)
        T["cbt"] = A.f32(8, 128)
        T["sTb"] = A.bf16(64, 64)
        T["rhs4"] = [A.f32(4, 128), A.f32(4, 128)]
        T["arg4"] = [A.f32(4, 128), A.f32(4, 128)]
        T["ET4"] = [A.f32(4, 128), A.f32(4, 128)]
        T["ecr4"] = [A.f32(4, 128), A.f32(4, 128)]
        T["WT"] = [A.bf16(128) for _ in range(4)]
        T["CpT"] = [A.bf16(128) for _ in range(4)]
        T["xw"] = [A.bf16(64) for _ in range(4)]
        return T

    def ssd_chunk(Q, t0, sT, hmT, xbcT, yT, dtw, T):
        pdt = banks[0]
        for kc in range(KC):
            op("pe", lambda e, kc=kc: e.matmul(pdt.ap[0:Q, 0:64], hmT.ap[:, kc, t0:t0 + Q], dtw.ap[:, kc, :],
                                                start=(kc == 0), stop=(kc == KC - 1)), R=[hmT, dtw], W=[pdt])
        dt = T["dt"]
        dtA = T["dtA"]
        op("dve", lambda e: e.tensor_tensor(out=dt.ap[0:Q, :], in0=pdt.ap[0:Q, 0:64], in1=ssdh.ap[0:Q, 0, :],
                                            op=ALU.add), R=[pdt, ssdh], W=[dt])
        op("act", lambda e: e.activation(out=dt.ap[0:Q, :], in_=dt.ap[0:Q, :], func=AF.Exp), R=[dt], W=[dt])
        op("act", lambda e: e.activation(out=dt.ap[0:Q, :], in_=dt.ap[0:Q, :], func=AF.Ln, bias=1.0),
           R=[dt], W=[dt])
        op("dve", lambda e: e.tensor_tensor(out=dtA.ap[0:Q, :], in0=dt.ap[0:Q, :], in1=Abc.ap[0:Q, :],
                                            op=ALU.mult), R=[dt, Abc], W=[dtA])
        pc = banks[1]
        op("pe", lambda e: e.matmul(pc.ap[0:Q, 0:64], tri[0:Q, 0:Q], dtA.ap[0:Q, :], start=True, stop=True),
           R=[dtA, cst], W=[pc])
        ncum = T["ncum"]
        op("dve", lambda e: e.tensor_scalar(out=ncum.ap[0:Q, :], in0=pc.ap[0:Q, 0:64], scalar1=-1.0, scalar2=None,
                                            op0=ALU.mult), R=[pc], W=[ncum])
        xtok = T["xtok"]
        btok = T["btok"]
        for grp in range(5):
            pbt = banks[2 + grp % 2]
            pv = pbt.ap.bitcast(BF16)
            for k in range(8):
                j = grp * 8 + k
                op("pe", lambda e, j=j, k=k: e.transpose(pv[0:Q, k * 128:(k + 1) * 128], xbcT.ap[:, j, t0:t0 + Q],
                                                         identb.ap), R=[xbcT, identb], W=[pbt])
            if grp < 4:
                op("act", lambda e: e.copy(out=xtok.ap[0:Q, grp * 1024:(grp + 1) * 1024], in_=pv[0:Q, :]),
                   R=[pbt], W=[xtok])
            else:
                op("act", lambda e: e.copy(out=btok.ap[0:Q, :], in_=pv[0:Q, :]), R=[pbt], W=[btok])
        cbt = T["cbt"]
        for g in range(8):
            pcb = banks[4 + g % 2]
            op("pe", lambda e: e.matmul(pcb.ap[0:Q, 0:Q], xbcT.ap[:, 32 + g, t0:t0 + Q], xbcT.ap[:, 40 + g, t0:t0 + Q],
                                        start=True, stop=True), R=[xbcT], W=[pcb])
            op("act", lambda e: e.copy(out=cbt.ap[0:Q, g, 0:Q], in_=pcb.ap[0:Q, 0:Q]), R=[pcb], W=[cbt])
        sTb = T["sTb"]
        op("pool", lambda e: e.tensor_copy(out=sTb.ap, in_=sT.ap), R=[sT], W=[sTb])
        rhs4, arg4, ET4, ecr4 = T["rhs4"], T["arg4"], T["ET4"], T["ecr4"]
        WT, CpT, xw = T["WT"], T["CpT"], T["xw"]
        def partA(h4):
            r4, a4, E4, c4 = rhs4[h4 % 2], arg4[h4 % 2], ET4[h4 % 2], ecr4[h4 % 2]
            op("pool", lambda e: e.tensor_tensor(out=r4.ap[0:Q, :, 0:Q], in0=bc(tri[0:Q, 0:Q], 1, [Q, 4, Q]),
                                                 in1=bc(dtA.ap[0:Q, h4 * 4:h4 * 4 + 4], 2, [Q, 4, Q]), op=ALU.mult),
               R=[dtA, cst], W=[r4])
            pcr = banks[6 + h4 % 2]
            pcr_v = pcr.ap.rearrange("p (a b) -> p a b", b=128)
            for hh in range(4):
                op("pe", lambda e, hh=hh: e.matmul(pcr_v[:, hh, 0:Q], ones[0:Q, :], r4.ap[0:Q, hh, 0:Q],
                                                    start=True, stop=True), R=[r4, cst], W=[pcr])
            for hh in range(4):
                h = h4 * 4 + hh
                op("dve", lambda e, hh=hh, h=h: e.scalar_tensor_tensor(out=a4.ap[0:Q, hh, 0:Q], in0=pcr_v[0:Q, hh, 0:Q],
                                                                       scalar=ncum.ap[0:Q, h:h + 1], in1=mneg[0:Q, 0:Q],
                                                                       op0=ALU.add, op1=ALU.min),
                   R=[pcr, ncum, cst], W=[a4])
            op("act", lambda e: e.activation(out=E4.ap[0:Q, :, 0:Q], in_=a4.ap[0:Q, :, 0:Q], func=AF.Exp),
               R=[a4], W=[E4])
            op("act", lambda e: e.activation(out=c4.ap[:, :, 0:Q], in_=pcr_v[:, :, 0:Q], func=AF.Exp),
               R=[pcr], W=[c4])

        def partB(h4):
            E4, c4 = ET4[h4 % 2], ecr4[h4 % 2]
            g = h4 // 2
            hs_ = [h4 * 4 + hh for hh in range(4)]
            for hh, h in enumerate(hs_):
                w_ = WT[hh]
                op("dve", lambda e: e.scalar_tensor_tensor(
                    out=w_.ap[0:Q, 0:Q], in0=E4.ap[0:Q, hh, 0:Q], scalar=dt.ap[0:Q, h:h + 1], in1=cbt.ap[0:Q, g, 0:Q],
                    op0=ALU.mult, op1=ALU.mult), R=[E4, dt, cbt], W=[w_])
            for hh, h in enumerate(hs_):
                cp_ = CpT[hh]
                op("pool", lambda e: e.tensor_tensor(
                    out=cp_.ap[:, 0:Q], in0=xbcT.ap[:, 40 + g, t0:t0 + Q], in1=c4.ap[:, hh, 0:Q], op=ALU.mult),
                   R=[xbcT, c4], W=[cp_])
            for hh, h in enumerate(hs_):
                xw_ = xw[hh]
                op("dve", lambda e: e.tensor_scalar(
                    out=xw_.ap[0:Q, :], in0=xtok.ap[0:Q, h * 64:(h + 1) * 64], scalar1=E4.ap[0:Q, hh, Q - 1:Q],
                    scalar2=dt.ap[0:Q, h:h + 1], op0=ALU.mult, op1=ALU.mult), R=[xtok, E4, dt], W=[xw_])
            for hh, h in enumerate(hs_):
                w_, cp_ = WT[hh], CpT[hh]
                c = h // 2
                pby = banks[0 + (c // 4) % 2]
                po = (h % 2) * 64
                col = (c % 4) * 128
                op("pe", lambda e: e.matmul(
                    pby.ap[po:po + 64, col:col + Q], xtok.ap[0:Q, h * 64:(h + 1) * 64], w_.ap[0:Q, 0:Q],
                    start=True, stop=False), R=[xtok, w_], W=[pby])
                op("pe", lambda e: e.matmul(
                    pby.ap[po:po + 64, col:col + Q], sTb.ap[:, h, :], cp_.ap[:, 0:Q],
                    start=False, stop=True), R=[sTb, cp_], W=[pby])
            for hh, h in enumerate(hs_):
                xw_ = xw[hh]
                pst = banks[2 + (h // 8) % 2]
                scol = (h % 8) * 64
                op("pe", lambda e: e.matmul(
                    pst.ap[:, scol:scol + 64], btok.ap[0:Q, g * 128:(g + 1) * 128], xw_.ap[0:Q, :],
                    start=True, stop=True), R=[btok, xw_], W=[pst])
            for hh, h in enumerate(hs_):
                if h % 2 == 1:
                    c = h // 2
                    pby = banks[0 + (c // 4) % 2]
                    col = (c % 4) * 128
                    op("dve", lambda e: e.scalar_tensor_tensor(
                        out=yT.ap[:, c, t0:t0 + Q], in0=xbcT.ap[:, c, t0:t0 + Q], scalar=ssd_dexp[:, c:c + 1],
                        in1=pby.ap[:, col:col + Q], op0=ALU.mult, op1=ALU.add), R=[xbcT, vec, pby], W=[yT])
            for hh, h in enumerate(hs_):
                pst = banks[2 + (h // 8) % 2]
                scol = (h % 8) * 64
                op("dve", lambda e: e.scalar_tensor_tensor(
                    out=sT.ap[:, h, :], in0=sT.ap[:, h, :], scalar=c4.ap[:, hh, Q - 1:Q], in1=pst.ap[:, scol:scol + 64],
                    op0=ALU.mult, op1=ALU.add), R=[sT, c4, pst], W=[sT])

        partA(0)
        for h4 in range(16):
            if h4 + 1 < 16:
                partA(h4 + 1)
            partB(h4)

    def ssd_mixer(N, samp, first, last, hmT):
        m = A.mark()
        xbcT = A.bf16(48, NB)
        yT = xbcT
        dtw = A.bf16(KC, 64)
        m1 = A.mark()
        dst_ = A.f32(KC, 64)
        dma(dst_.ap, i_ssdwdt, W=[dst_])
        op("pool", lambda e: e.tensor_copy(out=dtw.ap, in_=dst_.ap), R=[dst_], W=[dtw])
        ws = WStream([i_ssdwin[32 + j] for j in range(48)], (KC, 128), ceng=("act", "dve", "pool", "act", "dve"))
        xe = [A.f32(NB + 3), A.f32(NB + 3)]
        xcv = [A.f32(NB), A.f32(NB)]
        if samp:
            st_c = A.f32(48, 3, NS)
            for j in range(48):
                dma(st_c.ap[:, j], i_ssdconv[j * 128:(j + 1) * 128], W=[st_c])
            nbuf = A.f32(48, 3, NS)
        for j in range(48):
            wb = ws.get()
            pb = banks[j % 4]
            xe_, xc_ = xe[j % 2], xcv[j % 2]
            for kc in range(KC):
                op("pe", lambda e, kc=kc: e.matmul(pb.ap[:, 0:N], wb.ap[:, kc, :], hmT.ap[:, kc, 0:N],
                                                    start=(kc == 0), stop=(kc == KC - 1)), R=[wb, hmT], W=[pb])
            if not samp:
                op("pool", lambda e: e.tensor_copy(out=xe_.ap[:, 0:3], in_=ssd_tail.ap[:, j, :]),
                   R=[ssd_tail], W=[xe_])
            op("act", lambda e: e.copy(out=xe_.ap[:, 3:3 + N], in_=pb.ap[:, 0:N]), R=[pb], W=[xe_])
            if samp:
                taps = [st_c.ap[:, j, 0, :], st_c.ap[:, j, 1, :], st_c.ap[:, j, 2, :], xe_.ap[:, 3:3 + N]]
                Rb = [xe_, st_c, ssdcw, vec]
            else:
                taps = [xe_.ap[:, k:k + N] for k in range(4)]
                Rb = [xe_, ssdcw, vec]
            conv4(xc_.ap[:, 0:N], taps, lambda k: ssdcw.ap[:, j, k:k + 1], ssd_bconv[:, j:j + 1], N, Rb, [xc_])
            op("act", lambda e: e.activation(out=xbcT.ap[:, j, 0:N], in_=xc_.ap[:, 0:N], func=AF.Silu),
               R=[xc_], W=[xbcT])
            if samp:
                for k in range(2):
                    op("pool", lambda e, k=k: e.tensor_copy(out=nbuf.ap[:, j, k, :], in_=st_c.ap[:, j, k + 1, :]),
                       R=[st_c], W=[nbuf])
                op("pool", lambda e: e.tensor_copy(out=nbuf.ap[:, j, 2, :], in_=xe_.ap[:, 3:3 + N]),
                   R=[xe_], W=[nbuf])
            else:
                op("pool", lambda e: e.tensor_copy(out=ssd_tail.ap[:, j, :], in_=xe_.ap[:, N:N + 3]),
                   R=[xe_], W=[ssd_tail])
        if samp:
            dma(o_ssdconv_s, nbuf.ap, R=[nbuf])
        elif last:
            dma(o_ssdconv_p, ssd_tail.ap, R=[ssd_tail])
        P.barrier()
        A.reset(m1)
        T = ssd_temps()
        if samp:
            sTs = [A.f32(64, 64), A.f32(64, 64)]
            for b in range(NS):
                sT = sTs[b % 2]
                dma(sT.ap, i_ssds[b], W=[sT])
                ssd_chunk(1, b, sT, hmT, xbcT, yT, dtw, T)
                dma(o_ssd_s[b], sT.ap, R=[sT])
        else:
            sT = A.f32(64, 64)
            if first:
                op("pool", lambda e: e.memset(sT.ap, 0.0), W=[sT])
            else:
                dma(sT.ap, o_ssd_p, R=[sT_dram], W=[sT])
            for q in range(N // 128):
                ssd_chunk(128, q * 128, sT, hmT, xbcT, yT, dtw, T)
            dma(o_ssd_p, sT.ap, R=[sT], W=[sT_dram])
        P.barrier()
        A.reset(m1)
        wz = WStream([i_ssdwin[j] for j in range(32)], (KC, 128), ceng=("act", "dve", "pool", "act", "dve"))
        sz = [A.f32(NB), A.f32(NB)]
        sq = [A.f32(NB), A.f32(NB)]
        pst = banks[7]
        for j in range(32):
            wb = wz.get()
            pb = banks[j % 4]
            for kc in range(KC):
                op("pe", lambda e, kc=kc: e.matmul(pb.ap[:, 0:N], wb.ap[:, kc, :], hmT.ap[:, kc, 0:N],
                                                    start=(kc == 0), stop=(kc == KC - 1)), R=[wb, hmT], W=[pb])
            s_, q_ = sz[j % 2], sq[j % 2]
            op("act", lambda e: e.activation(out=s_.ap[:, 0:N], in_=pb.ap[:, 0:N], func=AF.Silu), R=[pb], W=[s_])
            op("dve", lambda e: e.tensor_tensor(out=yT.ap[:, j, 0:N], in0=yT.ap[:, j, 0:N], in1=s_.ap[:, 0:N],
                                                op=ALU.mult), R=[yT, s_], W=[yT])
            op("act", lambda e: e.activation(out=q_.ap[:, 0:N], in_=yT.ap[:, j, 0:N], func=AF.Square),
               R=[yT], W=[q_])
            op("pe", lambda e: e.matmul(pst.ap[:, 0:N], ones, q_.ap[:, 0:N], start=(j == 0), stop=(j == 31)),
               R=[q_, cst], W=[pst])
        P.barrier()
        A.reset(m1)
        rstd = A.f32(NB)
        op("act", lambda e: e.activation(out=rstd.ap[:, 0:N], in_=pst.ap[:, 0:N], func=AF.Sqrt, scale=1.0 / 4096,
                                         bias=epsb.ap[:, 0:1]), R=[pst, epsb], W=[rstd])
        op("dve", lambda e: e.reciprocal(out=rstd.ap[:, 0:N], in_=rstd.ap[:, 0:N]), R=[rstd], W=[rstd])
        for j in range(32):
            op("dve", lambda e: e.scalar_tensor_tensor(out=yT.ap[:, j, 0:N], in0=yT.ap[:, j, 0:N],
                                                       scalar=ssd_ng[:, j:j + 1], in1=rstd.ap[:, 0:N],
                                                       op0=ALU.mult, op1=ALU.mult), R=[yT, vec, rstd], W=[yT])
        wo = WStream([i_ssdwout[j] for j in range(16)], (32, 128), ceng=("act", "dve", "pool", "act", "dve"), nstage=2, nbf=2)
        tmpb = [A.f32(NB), A.f32(NB)]
        for j in range(16):
            wb = wo.get()
            pb = banks[j % 4]
            for kc in range(32):
                op("pe", lambda e, kc=kc: e.matmul(pb.ap[:, 0:N], wb.ap[:, kc, :], yT.ap[:, kc, 0:N],
                                                    start=(kc == 0), stop=(kc == 31)), R=[wb, yT], W=[pb])
            resid_add(j, pb, N, None, modv(1, 2, j, samp), samp, tmpb[j % 2])
        P.barrier()
        A.reset(m)

    def peer(l, N, samp, hcT):
        m = A.mark()
        NG = N // 16
        s12 = A.f32(NB // 16, 256)
        thr = A.f32(NB // 16)
        negm = A.f32(NB // 16)
        Sel = A.bf16(NB // 16, 16)
        m1 = A.mark()
        qT = A.f32(2, NB, 8)
        wq = WStream([i_wq[l, j] for j in range(16)], (KC, 128), ceng=("act", "dve", "pool", "act", "dve"))
        for j in range(16):
            wb = wq.get()
            pb = banks[j % 4]
            for kc in range(KC):
                op("pe", lambda e, kc=kc: e.matmul(pb.ap[:, 0:N], wb.ap[:, kc, :], hcT.ap[:, kc, 0:N],
                                                    start=(kc == 0), stop=(kc == KC - 1)), R=[wb, hcT], W=[pb])
            op("act", lambda e: e.copy(out=qT.ap[:, j % 2, 0:N, j // 2], in_=pb.ap[:, 0:N]), R=[pb], W=[qT])
        vv = [A.f32(2, 16) for _ in range(2)]
        tmp1 = [A.f32(128) for _ in range(2)]
        cand = [A.f32(256) for _ in range(2)]
        cand2 = [A.f32(256) for _ in range(2)]
        c24 = [A.f32(24) for _ in range(2)]
        ez = [A.f32(16) for _ in range(2)]
        zz = [A.f32(2) for _ in range(2)]
        for gi in range(NG):
            pb = banks[4 + gi % 2]
            for half in range(2):
                lhsT = qT.ap[:, half, gi * 16:(gi + 1) * 16, :].rearrange("p t h -> p (t h)")
                op("pe", lambda e, half=half, lhsT=lhsT: e.matmul(pb.ap[:, half * 128:(half + 1) * 128], lhsT,
                                                                  kT.ap[:, l * 2 + half, :], start=True, stop=True),
                   R=[qT, kT], W=[pb])
            op("act", lambda e: e.copy(out=s12.ap[:, gi, :], in_=pb.ap[:, 0:256]), R=[pb], W=[s12])
            v_, t1, cd, cd2, c_, ez_, z_ = (vv[gi % 2], tmp1[gi % 2], cand[gi % 2], cand2[gi % 2], c24[gi % 2],
                                            ez[gi % 2], zz[gi % 2])
            for half in range(2):
                w = s12.ap[:, gi, half * 128:(half + 1) * 128]
                op("dve", lambda e: e.max(out=v_.ap[:, half, 0:8], in_=w), R=[s12], W=[v_])
                op("dve", lambda e: e.match_replace(out=t1.ap, in_to_replace=v_.ap[:, half, 0:8], in_values=w,
                                                    imm_value=-1e30), R=[s12, v_], W=[t1])
                op("dve", lambda e: e.max(out=v_.ap[:, half, 8:16], in_=t1.ap), R=[t1], W=[v_])
            cd3 = cd.ap.rearrange("p (a b) -> p a b", b=16)
            op("dve", lambda e: e.tensor_tensor(out=cd3, in0=bc(v_.ap[:, 0, :], 2, [128, 16, 16]),
                                                in1=bc(v_.ap[:, 1, :], 1, [128, 16, 16]), op=ALU.add),
               R=[v_], W=[cd])
            op("dve", lambda e: e.max(out=c_.ap[:, 0:8], in_=cd.ap), R=[cd], W=[c_])
            op("dve", lambda e: e.match_replace(out=cd2.ap, in_to_replace=c_.ap[:, 0:8], in_values=cd.ap,
                                                imm_value=-1e30), R=[cd, c_], W=[cd2])
            op("dve", lambda e: e.max(out=c_.ap[:, 8:16], in_=cd2.ap), R=[cd2], W=[c_])
            op("dve", lambda e: e.match_replace(out=cd.ap, in_to_replace=c_.ap[:, 8:16], in_values=cd2.ap,
                                                imm_value=-1e30), R=[cd2, c_], W=[cd])
            op("dve", lambda e: e.max(out=c_.ap[:, 16:24], in_=cd.ap), R=[cd], W=[c_])
            op("dve", lambda e: e.tensor_scalar(out=thr.ap[:, gi:gi + 1], in0=c_.ap[:, 15:16],
                                                scalar1=c_.ap[:, 16:17], scalar2=0.5, op0=ALU.add, op1=ALU.mult),
               R=[c_], W=[thr])
            op("dve", lambda e: e.tensor_scalar(out=negm.ap[:, gi:gi + 1], in0=c_.ap[:, 0:1], scalar1=-1.0,
                                                scalar2=None, op0=ALU.mult), R=[c_], W=[negm])
            op("act", lambda e: e.activation(out=ez_.ap, in_=c_.ap[:, 0:16], func=AF.Exp,
                                             bias=negm.ap[:, gi:gi + 1], scale=1.0, accum_out=z_.ap[:, 0:1]),
               R=[c_, negm], W=[ez_, z_])
            op("dve", lambda e: e.reciprocal(out=z_.ap[:, 1:2], in_=z_.ap[:, 0:1]), R=[z_], W=[z_])
            op("dve", lambda e: e.tensor_scalar(out=Sel.ap[:, gi, :], in0=selm, scalar1=z_.ap[:, 1:2], scalar2=None,
                                                op0=ALU.mult), R=[z_, cst], W=[Sel])
        P.barrier()
        A.reset(m1)
        usrc, vsrc = [], []
        for a in range(128):
            usrc += [i_uT[l, a][:, 0:8, :], i_uT[l, a][:, 8:16, :]]
            vsrc += [i_v[l, a][:, 0:1024], i_v[l, a][:, 1024:2048]]
        us = WStream(usrc, (8, 128), ceng=("act",), nstage=3, nbf=2 * GA + 1)
        vs = WStream(vsrc, (1024,), ceng=("act", "dve", "act", "act", "dve", "act", "dve", "act"), nstage=3, nbf=2 * GA + 1)
        FR = 12
        LAG = 3
        NGA = 128 // GA
        NGh = min(NG, 16)
        Dd = [A.f32(GA, 128) for _ in range(4)]
        ee = [A.bf16(GA, 128) for _ in range(4)]
        Fr = [A.bf16(GA, 128) for _ in range(FR)]
        gel = [A.bf16(NB) for _ in range(GA)]
        AT = [A.bf16(NB) for _ in range(GA)]
        stmp = [A.f32(NS), A.f32(NS)]
        st = {"f": 0, "pend": [], "fb": [], "u": {}, "v": {}}

        def fgen(ag, gi):
            k = st["f"]
            st["f"] += 1
            d_, e_, f_ = Dd[k % 4], ee[k % 4], Fr[k % FR]
            a0 = ag * GA
            op("pool", lambda e: e.tensor_tensor(out=d_.ap, in0=bc(s12.ap[:, gi, 128:256], 1, [128, GA, 128]),
                                                 in1=bc(s12.ap[:, gi, a0:a0 + GA], 2, [128, GA, 128]),
                                                 op=ALU.add), R=[s12], W=[d_])
            op("act", lambda e: e.activation(out=e_.ap, in_=d_.ap, func=AF.Exp, bias=negm.ap[:, gi:gi + 1],
                                             scale=1.0), R=[d_, negm], W=[e_])
            st["fb"].append((gi, d_, e_, f_))
            fgen_b(2)

        def fgen_b(lag):
            while len(st["fb"]) > lag:
                gi, d_, e_, f_ = st["fb"].pop(0)
                op("dve", lambda e: e.scalar_tensor_tensor(out=f_.ap, in0=d_.ap, scalar=thr.ap[:, gi:gi + 1],
                                                           in1=e_.ap, op0=ALU.is_ge, op1=ALU.mult),
                   R=[d_, e_, thr], W=[f_])
                st["pend"].append((gi, f_))

        def gmm_emit(lag):
            if lag == 0:
                fgen_b(0)
            while len(st["pend"]) > lag:
                gi, f_ = st["pend"].pop(0)
                for ai in range(GA):
                    pG = banks[2 + ai]
                    op("pe", lambda e, ai=ai, pG=pG: e.matmul(pG.ap[:, gi * 16:(gi + 1) * 16], f_.ap[:, ai, :],
                                                              Sel.ap[:, gi, :], start=True, stop=True),
                       R=[f_, Sel], W=[pG])

        def ucast(ag, k):
            st["u"][(ag, k)] = us.get()

        def vcast(ag, k):
            st["v"][(ag, k)] = vs.get()

        def sT_pe(ag, ai):
            pS = banks[ai % 2]
            for kc in range(KC):
                ub = st["u"][(ag, 2 * ai + kc // 8)]
                op("pe", lambda e, kc=kc, ub=ub: e.matmul(pS.ap[:, 0:N], ub.ap[:, kc % 8, :], hcT.ap[:, kc, 0:N],
                                                          start=(kc == 0), stop=(kc == KC - 1)),
                   R=[ub, hcT], W=[pS])

        def sT_gelu(ai):
            pS = banks[ai % 2]
            g_ = gel[ai]
            op("act", lambda e: e.activation(out=g_.ap[:, 0:N], in_=pS.ap[:, 0:N], func=AF.Gelu_apprx_tanh),
               R=[pS], W=[g_])

        def aTm():
            for ai in range(GA):
                pG = banks[2 + ai]
                g_, at_ = gel[ai], AT[ai]
                op("dve", lambda e: e.tensor_tensor(out=at_.ap[:, 0:N], in0=g_.ap[:, 0:N], in1=pG.ap[:, 0:N],
                                                    op=ALU.mult), R=[g_, pG], W=[at_])

        def outp(ag, dc):
            pO = banks[6 + dc % 2]
            for ai in range(GA):
                at_ = AT[ai]
                vb = st["v"][(ag, 2 * ai + dc // 8)]
                op("pe", lambda e, ai=ai, at_=at_, vb=vb: e.matmul(
                    pO.ap[:, 0:N], vb.ap[:, (dc % 8) * 128:(dc % 8 + 1) * 128], at_.ap[:, 0:N],
                    start=(ai == 0), stop=(ai == GA - 1)), R=[vb, at_], W=[pO])
            if samp:
                t_ = stmp[dc % 2]
                tv = t_.ap
                op("dve", lambda e: e.tensor_tensor(out=tv[:, 0:N], in0=pO.ap[:, 0:N], in1=modv(l, 5, dc, True),
                                                    op=ALU.mult), R=[pO, modT], W=[t_])
                op("dve", lambda e: e.tensor_tensor(out=xT.ap[:, dc, 0:N], in0=xT.ap[:, dc, 0:N], in1=tv[:, 0:N],
                                                    op=ALU.add), R=[t_, xT], W=[xT])
            else:
                op("dve", lambda e: e.scalar_tensor_tensor(out=xT.ap[:, dc, 0:N], in0=pO.ap[:, 0:N],
                                                           scalar=modv(l, 5, dc, False), in1=xT.ap[:, dc, 0:N],
                                                           op0=ALU.mult, op1=ALU.add), R=[pO, modT, xT], W=[xT])

        def phaseY(ag):
            for pr in range(GA // 2):
                sT_pe(ag, 2 * pr)
                sT_pe(ag, 2 * pr + 1)
                for k in range(8):
                    gi = NGh + 8 * pr + k
                    if k % 2 == 0:
                        vcast(ag, 4 * pr + k // 2)
                    if gi < NG:
                        fgen(ag, gi)
                        gmm_emit(LAG)
                sT_gelu(2 * pr)
                sT_gelu(2 * pr + 1)
            gmm_emit(0)
            aTm()

        for k in range(2 * GA):
            ucast(0, k)
        for gi in range(NGh):
            fgen(0, gi)
            gmm_emit(LAG)
        gmm_emit(0)
        phaseY(0)
        for ag in range(NGA):
            nxt = ag + 1 < NGA
            for dc in range(16):
                outp(ag, dc)
                if nxt:
                    if dc % 2 == 0:
                        ucast(ag + 1, dc // 2)
                    if dc < NGh:
                        fgen(ag + 1, dc)
                    gmm_emit(LAG)
            if nxt:
                gmm_emit(0)
                phaseY(ag + 1)
        P.barrier()
        A.reset(m)

    def run_block(N, samp, first, last, src, dst):
        dma(xT.ap[:, :, 0:N], src, W=[xT])
        for l in range(2):
            m = A.mark()
            hmT = A.bf16(KC, NB)
            norm_mod(l, 0, N, samp, hmT)
            if l == 0:
                rg_mixer(N, samp, last, hmT)
            else:
                ssd_mixer(N, samp, first, last, hmT)
            norm_mod(l, 1, N, samp, hmT)
            peer(l, N, samp, hmT)
            A.reset(m)
        m = A.mark()
        scr = [A.f32(NB), A.f32(NB)]
        rstd = A.f32(NB)
        yo = A.f32(KC, NB)
        rms_stats(lambda c: xT.ap[:, c, 0:N], [xT], N, KC, scr, banks[7], rstd)
        for c in range(KC):
            op("dve", lambda e: e.scalar_tensor_tensor(out=yo.ap[:, c, 0:N], in0=xT.ap[:, c, 0:N],
                                                       scalar=fing[:, c:c + 1], in1=rstd.ap[:, 0:N], op0=ALU.mult,
                                                       op1=ALU.mult), R=[xT, vec, rstd], W=[yo])
        dma(dst, yo.ap[:, :, 0:N], R=[yo])
        P.barrier()
        A.reset(m)

    for blk in range(nblk):
        run_block(NB, False, blk == 0, blk == nblk - 1,
                  i_xT[:, blk * NB:(blk + 1) * NB].rearrange("(c p) t -> p c t", p=128),
                  o_yT[:, blk * NB:(blk + 1) * NB].rearrange("(c p) t -> p c t", p=128))
    run_block(NS, True, True, False, i_xsT.rearrange("(c p) t -> p c t", p=128),
              o_ysT.rearrange("(c p) t -> p c t", p=128))
    P.finish()
    es.close()
    nc._arena_hw = A.hw
    return nc


def _wlay(w):
    K, N = w.shape
    return np.ascontiguousarray(w.reshape(K // 128, 128, N // 128, 128).transpose(2, 1, 0, 3))


def _vlay(v):
    return v.reshape(-1, 128).T


_CACHE = {}


def _prep_shared(inp):
    f = np.float32
    sh = {}
    cst = np.zeros((128, 4 * 128 + 16), f)
    cst[:, 0:128] = np.eye(128)
    cst[:, 128:256] = 1.0
    k = np.arange(128)
    cst[:, 256:384] = (k[:, None] <= k[None, :])
    cst[:, 384:512] = np.where(k[:, None] <= k[None, :], 0.0, NEG)
    cst[:, 512:528] = (k[:, None] // 8 == np.arange(16)[None, :])
    sh["cst"] = cst
    vec = np.zeros((128, 16 * 11 + 32 * 2 + 48 + 32), f)
    vec[:, 0:16] = _vlay(inp["norm1_g"][0]); vec[:, 16:32] = _vlay(inp["norm1_g"][1])
    vec[:, 32:48] = _vlay(inp["norm2_g"][0]); vec[:, 48:64] = _vlay(inp["norm2_g"][1])
    vec[:, 64:80] = _vlay(inp["final_g"])
    vec[:, 80:96] = _vlay(inp["rg_conv_b"][0])
    vec[:, 96:112] = _vlay(inp["rg_b_a"][0])
    vec[:, 112:128] = _vlay(inp["rg_b_i"][0])
    vec[:, 128:144] = _vlay(inp["rg_lambda"][0])
    vec[:, 144:160] = _vlay(inp["rg_b_out"][0])
    vec[:, 176:208] = _vlay(inp["rg_b_in"][0])
    vec[:, 208:240] = _vlay(inp["ssd_norm_g"][0])
    vec[:, 240:288] = _vlay(inp["ssd_conv_b"][0])
    vec[:, 288:320] = _vlay(np.repeat(inp["ssd_d"][0], 64))
    sh["vec"] = vec
    sh["bmod"] = np.ascontiguousarray(np.stack([_vlay(inp["b_mod"][l]) for l in range(2)], axis=1))
    sh["rgcw"] = np.ascontiguousarray(inp["rg_conv_w"][0].T.reshape(16, 128, 4).transpose(1, 0, 2))
    sh["ssdcw"] = np.ascontiguousarray(inp["ssd_conv_w"][0].T.reshape(48, 128, 4).transpose(1, 0, 2))
    sh["ssdh"] = np.ascontiguousarray(np.stack([inp["ssd_dt_bias"][0], inp["ssd_a_log"][0],
                                                inp["ssd_d"][0]]).astype(f))
    sh["wmod"] = np.stack([_wlay(inp["w_mod"][l]) for l in range(2)])
    sh["rgwin"] = _wlay(inp["rg_w_in"][0])

    def glay(w):
        return np.ascontiguousarray(w.reshape(8, 2, 128, 2, 128).transpose(0, 3, 2, 1, 4).reshape(16, 128, 2, 128))
    sh["rgwa"] = glay(inp["rg_w_a"][0])
    sh["rgwi"] = glay(inp["rg_w_i"][0])
    sh["rgwout"] = _wlay(inp["rg_w_out"][0])
    w = inp["ssd_w_in"][0]
    sh["ssdwin"] = _wlay(np.ascontiguousarray(w[:, :10240]))
    sh["ssdwdt"] = np.ascontiguousarray(w[:, 10240:].reshape(16, 128, 64).transpose(1, 0, 2))
    sh["ssdwout"] = _wlay(inp["ssd_w_out"][0])
    sh["wq"] = np.stack([_wlay(inp["peer_w_q"][l]) for l in range(2)])
    sh["kT"] = np.ascontiguousarray(np.stack([np.stack([inp["peer_k1"][l].T, inp["peer_k2"][l].T])
                                              for l in range(2)]))
    sh["uT"] = np.stack([np.ascontiguousarray(inp["peer_u"][l].reshape(128, 128, 16, 128).transpose(0, 3, 2, 1))
                         for l in range(2)])
    sh["v"] = np.ascontiguousarray(inp["peer_v"].reshape(2, 128, 128, 2048))
    return sh


def kernel(**inp):
    inp = {k: np.asarray(v) for k, v in inp.items()}
    TP = inp["x_prompt"].shape[1]
    nc = build(TP)
    sh = _prep_shared(inp)
    in_maps = []
    for c in range(8):
        s = c % 4
        sl = slice(c * NS, (c + 1) * NS)
        d = dict(sh)
        d["xT"] = np.ascontiguousarray(inp["x_prompt"][s].T)
        d["xsT"] = np.ascontiguousarray(inp["x_sample"][sl, 0, :].T)
        d["cT"] = np.ascontiguousarray(np.concatenate([inp["c_prompt"][s][None], inp["c_sample"][sl]], 0).T)
        d["rgconv"] = np.ascontiguousarray(inp["state_rg_conv"][0, sl].transpose(2, 1, 0))
        d["rgh"] = np.ascontiguousarray(inp["state_rg_h"][0, sl].T)
        d["ssdconv"] = np.ascontiguousarray(inp["state_ssd_conv"][0, sl].transpose(2, 1, 0))
        d["ssds"] = np.ascontiguousarray(inp["state_ssd"][0, sl].reshape(NS, 64, 64, 128).transpose(0, 3, 1, 2))
        in_maps.append(d)
    res = run_bass_kernel_spmd(nc, in_maps, core_ids=list(range(8))).results
    f = np.float32
    y_p = np.stack([res[s]["o_yT"].T for s in range(4)]).astype(f)
    y_s = np.concatenate([res[c]["o_ysT"].T for c in range(8)], 0)[:, None, :].astype(f)

    def unv(a):
        return a.transpose(1, 0, *range(2, a.ndim)).reshape(-1, *a.shape[2:])
    rg_conv_p = np.stack([unv(res[s]["o_rgconv_p"]).T for s in range(4)])[None].astype(f)
    rg_h_p = np.stack([unv(res[s]["o_rgh_p"]) for s in range(4)])[None].astype(f)
    ssd_conv_p = np.stack([unv(res[s]["o_ssdconv_p"]).T for s in range(4)])[None].astype(f)
    ssd_p = np.stack([res[s]["o_ssd_p"].transpose(1, 2, 0).reshape(8, 8, 64, 128) for s in range(4)])[None].astype(f)
    rg_conv_s = np.concatenate([unv(res[c]["o_rgconv_s"]).transpose(2, 1, 0) for c in range(8)], 0)[None].astype(f)
    rg_h_s = np.concatenate([unv(res[c]["o_rgh_s"]).T for c in range(8)], 0)[None].astype(f)
    ssd_conv_s = np.concatenate([unv(res[c]["o_ssdconv_s"]).transpose(2, 1, 0) for c in range(8)], 0)[None].astype(f)
    ssd_s = np.concatenate([res[c]["o_ssd_s"].transpose(0, 2, 3, 1).reshape(NS, 8, 8, 64, 128)
                            for c in range(8)], 0)[None].astype(f)
    return (y_p, y_s, rg_conv_p, rg_h_p, ssd_conv_p, ssd_p, rg_conv_s, rg_h_s, ssd_conv_s, ssd_s)
```
